# Optimizing a Trainium2 kernel written in Bass

```python
import jax
import jax.numpy as jnp
from jax import lax
import numpy as np

D_MODEL = 1024
BATCH = 8
SEQ = 4096
DEPTH = 2

CTX_LEN = 256
GRID_W = 64
HEAD_DIM = 64
MIX_WIDTH = D_MODEL
POOL_GROUPS = 4
POOL_WIDTH = MIX_WIDTH // 4
POOL_CH = POOL_WIDTH // POOL_GROUPS
POOL_WINDOWS = (2, 4, 8, 16)
GQA_HEADS = (MIX_WIDTH - POOL_WIDTH) // HEAD_DIM
GQA_KV_HEADS = 4
GQA_GROUP = GQA_HEADS // GQA_KV_HEADS
GQA_Q = GQA_HEADS * HEAD_DIM
GQA_KV = GQA_KV_HEADS * HEAD_DIM
EVEN_IN = POOL_WIDTH + GQA_Q + 2 * GQA_KV
FOURIER_GROUPS = 4
FOURIER_WIDTH = MIX_WIDTH // 4
FOURIER_CH = FOURIER_WIDTH // FOURIER_GROUPS
NA_HEADS = (MIX_WIDTH - FOURIER_WIDTH) // HEAD_DIM
NA_WIDTH = NA_HEADS * HEAD_DIM
ODD_IN = FOURIER_WIDTH + 3 * NA_WIDTH
MAX_WIN_R = 8
WIN_C = 16
Q_BLOCK = 128
ROPE_THETA = 10000.0
ROPE_FREQS = HEAD_DIM // 4
FFN_DIM = 2816
N_EXPERTS = 8
TOP_K = 2
N_EVEN = (DEPTH + 1) // 2
N_ODD = DEPTH // 2
DEEPNORM_ALPHA = (2 * DEPTH) ** 0.25
DEEPNORM_BETA = (8 * DEPTH) ** -0.25
LN_EPS = 1e-6
RMS_EPS = 1e-6

kernel_name = 'hybrid_pool_gqa_fourier_natten_moe_dit'


def _layer_norm(x, g, b):
    xf = x.astype(jnp.float32)
    mu = jnp.mean(xf, axis=-1, keepdims=True)
    var = jnp.mean(jnp.square(xf - mu), axis=-1, keepdims=True)
    return ((xf - mu) * lax.rsqrt(var + LN_EPS) * g.astype(jnp.float32) + b.astype(jnp.float32)).astype(x.dtype)


def _rms_norm(x, g):
    xf = x.astype(jnp.float32)
    inv = lax.rsqrt(jnp.mean(jnp.square(xf), axis=-1, keepdims=True) + RMS_EPS)
    return (xf * inv * g.astype(jnp.float32)).astype(x.dtype)


def _modulate(u, shift, scale):
    return u * (1 + scale) + shift


def _swiglu(u, w1, w3, w2):
    return (jax.nn.silu(u @ w1) * (u @ w3)) @ w2


def _axial_rope_tables(n):
    t = jnp.arange(n, dtype=jnp.int32)
    row = (t // GRID_W).astype(jnp.float32)
    col = (t % GRID_W).astype(jnp.float32)
    inv_freq = jnp.power(ROPE_THETA, -jnp.arange(ROPE_FREQS, dtype=jnp.float32) / ROPE_FREQS)
    ang = jnp.stack([row[:, None] * inv_freq, col[:, None] * inv_freq], axis=1)
    return jnp.cos(ang), jnp.sin(ang)


def _apply_axial_rope(u, cos, sin):
    b, n, h, d = u.shape
    uf = u.astype(jnp.float32).reshape(b, n, h, 2, 2, ROPE_FREQS)
    u1, u2 = uf[..., 0, :], uf[..., 1, :]
    cs, sn = cos[None, :, None], sin[None, :, None]
    out = jnp.stack([u1 * cs - u2 * sn, u2 * cs + u1 * sn], axis=-2)
    return out.reshape(b, n, h, d).astype(u.dtype)


def _softmax_attend(q, k, v):
    s = jnp.einsum('bqkgd,bskd->bkgqs', q, k, preferred_element_type=jnp.float32) * (q.shape[-1] ** -0.5)
    p = jax.nn.softmax(s, axis=-1).astype(v.dtype)
    return jnp.einsum('bkgqs,bskd->bqkgd', p, v)


def _blocked_attention(q, k, v):
    b, n, kvh, g, d = q.shape
    nblk = n // Q_BLOCK
    qb = jnp.moveaxis(q.reshape(b, nblk, Q_BLOCK, kvh, g, d), 1, 0)
    ob = lax.map(lambda qi: _softmax_attend(qi, k, v), qb)
    return jnp.moveaxis(ob, 0, 1).reshape(b, n, kvh, g, d)


def _multiscale_pool(u):
    b, n, g, ch = u.shape
    s = jnp.cumsum(u.astype(jnp.float32), axis=1)
    s = jnp.concatenate([jnp.zeros((b, 1, g, ch), jnp.float32), s], axis=1)
    t = jnp.arange(n, dtype=jnp.int32)[:, None]
    win = jnp.asarray(POOL_WINDOWS, dtype=jnp.int32)[None, :]
    lo = jnp.clip(t - win // 2, 0, n)
    hi = jnp.clip(t - win // 2 + win, 0, n)
    gidx = jnp.arange(g, dtype=jnp.int32)[None, :]
    total = s[:, hi, gidx] - s[:, lo, gidx]
    mean = total / (hi - lo).astype(jnp.float32)[None, :, :, None]
    return (mean - u.astype(jnp.float32)).astype(u.dtype)


def _neighbourhood_attention(q, k, v, k_ctx, v_ctx, rpb):
    nb, n, nh, d = q.shape
    rows = n // GRID_W
    win_r = min(MAX_WIN_R, rows)
    kg = k.reshape(nb, rows, GRID_W, nh, d)
    vg = v.reshape(nb, rows, GRID_W, nh, d)
    qg = jnp.moveaxis(q.reshape(nb, rows, GRID_W, nh, d), 1, 0)
    col = jnp.arange(GRID_W, dtype=jnp.int32)
    col_start = jnp.clip(col - WIN_C // 2, 0, GRID_W - WIN_C)
    col_idx = col_start[:, None] + jnp.arange(WIN_C, dtype=jnp.int32)[None, :]
    col_off = col_idx - col[:, None] + (WIN_C - 1)
    scale = d ** -0.5
    n_loc = win_r * WIN_C

    def one_row(args):
        r, q_row = args
        r_start = jnp.clip(r - win_r // 2, 0, rows - win_r)
        k_rows = lax.dynamic_slice_in_dim(kg, r_start, win_r, axis=1)
        v_rows = lax.dynamic_slice_in_dim(vg, r_start, win_r, axis=1)
        k_win = k_rows[:, :, col_idx]
        v_win = v_rows[:, :, col_idx]
        row_off = r_start + jnp.arange(win_r, dtype=jnp.int32) - r + (MAX_WIN_R - 1)
        bias = jnp.transpose(rpb[:, row_off][:, :, col_off], (0, 2, 1, 3)).astype(jnp.float32)
        s_loc = jnp.einsum('bqhd,brqchd->bhqrc', q_row, k_win, preferred_element_type=jnp.float32) * scale + bias[None]
        s_ctx = jnp.einsum('bqhd,bkhd->bhqk', q_row, k_ctx, preferred_element_type=jnp.float32) * scale
        s = jnp.concatenate([s_loc.reshape(nb, nh, GRID_W, n_loc), s_ctx], axis=-1)
        p = jax.nn.softmax(s, axis=-1).astype(v.dtype)
        p_loc = p[..., :n_loc].reshape(nb, nh, GRID_W, win_r, WIN_C)
        p_ctx = p[..., n_loc:]
        return (jnp.einsum('bhqrc,brqchd->bqhd', p_loc, v_win)
                + jnp.einsum('bhqk,bkhd->bqhd', p_ctx, v_ctx))

    o = lax.map(one_row, (jnp.arange(rows, dtype=jnp.int32), qg))
    return jnp.moveaxis(o, 0, 1).reshape(nb, n, nh * d)


def _moe_swiglu(u, w_router, w1, w3, w2):
    b, n, d = u.shape
    t = u.reshape(b * n, d)
    logits = (t @ w_router).astype(jnp.float32)
    top_val, top_idx = lax.top_k(logits, TOP_K)
    top_w = jax.nn.softmax(top_val, axis=-1)
    gates = jnp.sum(jax.nn.one_hot(top_idx, N_EXPERTS, dtype=jnp.float32) * top_w[..., None], axis=1)
    out = jnp.zeros_like(t)
    for e in range(N_EXPERTS):
        out = out + gates[:, e:e + 1].astype(t.dtype) * _swiglu(t, w1[e], w3[e], w2[e])
    return out.reshape(b, n, d)


def _mixer_pool_gqa(h, hc, w_in, pool_w, pool_scale, q_gain, k_gain, w_out, with_ctx):
    def project(u):
        b, n, _ = u.shape
        p = u @ w_in
        a, q, k, v = jnp.split(p, [POOL_WIDTH, POOL_WIDTH + GQA_Q, POOL_WIDTH + GQA_Q + GQA_KV], axis=-1)
        q = _rms_norm(q.reshape(b, n, GQA_HEADS, HEAD_DIM), q_gain)
        k = _rms_norm(k.reshape(b, n, GQA_KV_HEADS, HEAD_DIM), k_gain)
        return a, q, k, v.reshape(b, n, GQA_KV_HEADS, HEAD_DIM)

    def pool_branch(a):
        b, n, _ = a.shape
        m = _multiscale_pool(a.reshape(b, n, POOL_GROUPS, POOL_CH))
        m = jnp.einsum('bngc,gcd->bngd', m, pool_w)
        return m.reshape(b, n, POOL_WIDTH) * pool_scale

    b, n, _ = h.shape
    a, q, k, v = project(h)
    a_c, q_c, k_c, v_c = project(hc)
    cos, sin = _axial_rope_tables(n)
    q = _apply_axial_rope(q, cos, sin)
    k = _apply_axial_rope(k, cos, sin)
    k_all = jnp.concatenate([k, k_c], axis=1)
    v_all = jnp.concatenate([v, v_c], axis=1)
    o = _blocked_attention(q.reshape(b, n, GQA_KV_HEADS, GQA_GROUP, HEAD_DIM), k_all, v_all)
    y = jnp.concatenate([pool_branch(a), o.reshape(b, n, GQA_Q)], axis=-1) @ w_out
    yc = None
    if with_ctx:
        lc = hc.shape[1]
        oc = _softmax_attend(q_c.reshape(b, lc, GQA_KV_HEADS, GQA_GROUP, HEAD_DIM), k_c, v_c)
        yc = jnp.concatenate([pool_branch(a_c), oc.reshape(b, lc, GQA_Q)], axis=-1) @ w_out
    return y, yc


def _mixer_fourier_na(h, hc, w_in, fourier_gain, rpb, w_out, with_ctx):
    def project(u):
        b, n, _ = u.shape
        p = u @ w_in
        f, q, k, v = jnp.split(p, [FOURIER_WIDTH, FOURIER_WIDTH + NA_WIDTH, FOURIER_WIDTH + 2 * NA_WIDTH], axis=-1)
        heads = lambda z: z.reshape(b, n, NA_HEADS, HEAD_DIM)
        return f, heads(q), heads(k), heads(v)

    def fourier_branch(f):
        b, n, _ = f.shape
        u = _rms_norm(f.reshape(b, n, FOURIER_GROUPS, FOURIER_CH), fourier_gain)
        spec = jnp.fft.fftn(u.astype(jnp.float32), axes=(1, 3), norm='ortho')
        return jnp.real(spec).astype(f.dtype).reshape(b, n, FOURIER_WIDTH)

    f, q, k, v = project(h)
    f_c, q_c, k_c, v_c = project(hc)
    o = _neighbourhood_attention(q, k, v, k_c, v_c, rpb)
    y = jnp.concatenate([fourier_branch(f), o], axis=-1) @ w_out
    yc = None
    if with_ctx:
        b, lc = hc.shape[:2]
        oc = _softmax_attend(q_c[:, :, :, None], k_c, v_c).reshape(b, lc, NA_WIDTH)
        yc = jnp.concatenate([fourier_branch(f_c), oc], axis=-1) @ w_out
    return y, yc


def setup_inputs(seed: int = 0) -> dict:
    key = jax.random.key(seed)
    ks = iter(jax.random.split(key, 32))
    D = D_MODEL

    def nrm(shape, scale):
        return jax.random.normal(next(ks), shape, jnp.float32) * scale

    def gain(shape):
        return 1.0 + nrm(shape, 0.02)

    return {
        'x': nrm((BATCH, SEQ, D), 1.0),
        'c': nrm((BATCH, D), 1.0),
        'ctx': nrm((BATCH, CTX_LEN, D), 1.0),
        'c_ctx': nrm((D,), 1.0),
        'ada_w': nrm((DEPTH, D, 6 * D), 0.5 * D ** -0.5),
        'ada_b': nrm((DEPTH, 6 * D), 0.01),
        'ln_mix_g': gain((DEPTH, D)),
        'ln_mix_b': nrm((DEPTH, D), 0.01),
        'ln_ffn_g': gain((DEPTH, D)),
        'ln_ffn_b': nrm((DEPTH, D), 0.01),
        'w_out': nrm((DEPTH, MIX_WIDTH, D), MIX_WIDTH ** -0.5 * DEEPNORM_BETA),
        'ev_w_in': nrm((N_EVEN, D, EVEN_IN), D ** -0.5),
        'ev_pool_w': nrm((N_EVEN, POOL_GROUPS, POOL_CH, POOL_CH), POOL_CH ** -0.5),
        'ev_pool_scale': gain((N_EVEN, POOL_WIDTH)),
        'ev_q_gain': gain((N_EVEN, HEAD_DIM)),
        'ev_k_gain': gain((N_EVEN, HEAD_DIM)),
        'ev_ffn_w1': nrm((N_EVEN, D, FFN_DIM), D ** -0.5),
        'ev_ffn_w3': nrm((N_EVEN, D, FFN_DIM), D ** -0.5),
        'ev_ffn_w2': nrm((N_EVEN, FFN_DIM, D), FFN_DIM ** -0.5 * DEEPNORM_BETA),
        'od_w_in': nrm((N_ODD, D, ODD_IN), D ** -0.5),
        'od_fourier_gain': gain((N_ODD, FOURIER_GROUPS, FOURIER_CH)),
        'od_rpb': nrm((N_ODD, NA_HEADS, 2 * MAX_WIN_R - 1, 2 * WIN_C - 1), 0.1),
        'od_router': nrm((N_ODD, D, N_EXPERTS), D ** -0.5),
        'od_exp_w1': nrm((N_ODD, N_EXPERTS, D, FFN_DIM), D ** -0.5),
        'od_exp_w3': nrm((N_ODD, N_EXPERTS, D, FFN_DIM), D ** -0.5),
        'od_exp_w2': nrm((N_ODD, N_EXPERTS, FFN_DIM, D), FFN_DIM ** -0.5 * DEEPNORM_BETA),
    }


def reference(x, c, ctx, c_ctx, ada_w, ada_b, ln_mix_g, ln_mix_b, ln_ffn_g, ln_ffn_b, w_out,
              ev_w_in, ev_pool_w, ev_pool_scale, ev_q_gain, ev_k_gain, ev_ffn_w1, ev_ffn_w3, ev_ffn_w2,
              od_w_in, od_fourier_gain, od_rpb, od_router, od_exp_w1, od_exp_w3, od_exp_w2):
    alpha = DEEPNORM_ALPHA
    for layer in range(DEPTH):
        with_ctx = layer < DEPTH - 1
        i = layer // 2
        mod = jax.nn.silu(c) @ ada_w[layer] + ada_b[layer]
        mod_c = jax.nn.silu(c_ctx) @ ada_w[layer] + ada_b[layer]
        sh1, sc1, g1, sh2, sc2, g2 = jnp.split(mod[:, None, :], 6, axis=-1)
        csh1, csc1, cg1, csh2, csc2, cg2 = jnp.split(mod_c, 6, axis=-1)
        h = _modulate(x, sh1, sc1)
        hc = _modulate(ctx, csh1, csc1)
        if layer % 2 == 0:
            y, yc = _mixer_pool_gqa(h, hc, ev_w_in[i], ev_pool_w[i], ev_pool_scale[i],
                                    ev_q_gain[i], ev_k_gain[i], w_out[layer], with_ctx)
            ffn = lambda u: _swiglu(u, ev_ffn_w1[i], ev_ffn_w3[i], ev_ffn_w2[i])
        else:
            y, yc = _mixer_fourier_na(h, hc, od_w_in[i], od_fourier_gain[i], od_rpb[i],
                                      w_out[layer], with_ctx)
            ffn = lambda u: _moe_swiglu(u, od_router[i], od_exp_w1[i], od_exp_w3[i], od_exp_w2[i])
        x = _layer_norm(alpha * x + g1 * y, ln_mix_g[layer], ln_mix_b[layer])
        x = _layer_norm(alpha * x + g2 * ffn(_modulate(x, sh2, sc2)), ln_ffn_g[layer], ln_ffn_b[layer])
        if with_ctx:
            ctx = _layer_norm(alpha * ctx + cg1 * yc, ln_mix_g[layer], ln_mix_b[layer])
            ctx = _layer_norm(alpha * ctx + cg2 * ffn(_modulate(ctx, csh2, csc2)), ln_ffn_g[layer], ln_ffn_b[layer])
    return x
```

```python
import contextlib
import math
import numpy as np
import ml_dtypes
import concourse.bass as bass
import concourse.mybir as mybir
from concourse.bass_utils import run_bass_kernel_spmd

F32 = mybir.dt.float32
BF16 = mybir.dt.bfloat16
AF = mybir.ActivationFunctionType
ALU = mybir.AluOpType
AX = mybir.AxisListType

D = 1024
SEQ = 4096
CTXL = 256
NT = 34
NTL = 32
FFN = 2816
NF = 22
NEXP = 8
ALPHA = 4.0 ** 0.25
LN_EPS = 1e-6
RMS_EPS = 1e-6
NEG = -30000.0
N_CORES = 8


class Buf:
    __slots__ = ("name", "w", "r")

    def __init__(self, name=""):
        self.name = name
        self.w = None
        self.r = {}


class SemC:
    __slots__ = ("sem", "cnt")

    def __init__(self, sem):
        self.sem = sem
        self.cnt = 0


class Tracker:
    def __init__(self, nc, es):
        self.nc = nc
        self.es = es
        self.E = {"pe": nc.tensor, "act": nc.scalar, "dve": nc.vector, "pool": nc.gpsimd, "sp": nc.sync}
        self.esem = {}
        self.ecnt = {}
        self.own = {k: set() for k in self.E}
        self.seen = {k: {} for k in self.E}
        self.nsem = 0
        self.dsem = {}
        for k in self.E:
            self._new_esem(k)
        self.out_events = []

    def new_sem(self, name):
        self.nsem += 1
        return self.es.enter_context(self.nc.semaphore(f"{name}_{self.nsem}"))

    def semc(self, name="d"):
        sc = SemC(self.new_sem(name))
        self.dsem[sc.sem.num] = sc
        return sc

    def _new_esem(self, k):
        s = self.new_sem("e" + k)
        self.esem[k] = s
        self.ecnt[k] = 0
        self.own[k].add(s.num)

    def _wait_all(self, eng, evs, allow_own=False):
        best = {}
        for ev in evs:
            if ev is None:
                continue
            s, v = ev
            if s.num in self.own[eng] and not allow_own:
                continue
            if s.num not in best or best[s.num][1] < v:
                best[s.num] = (s, v)
        for num, (s, v) in best.items():
            if num in self.dsem:
                v = self.dsem[num].cnt
            if self.seen[eng].get(num, 0) < v:
                self.E[eng].wait_ge(s, v)
                self.seen[eng][num] = v

    def _collect(self, reads, writes, skip_num=None):
        evs = []
        for b in reads:
            evs.append(b.w)
        for b in writes:
            if b.w is not None and (skip_num is None or b.w[0].num != skip_num):
                evs.append(b.w)
            evs.extend(b.r.values())
        return evs

    def _update(self, ev, reads, writes):
        for b in reads:
            old = b.r.get(ev[0].num)
            if old is None or old[1] < ev[1]:
                b.r[ev[0].num] = ev
        for b in writes:
            b.w = ev
            b.r = {}

    def op(self, eng, fn, reads=(), writes=(), hard=False):
        self._wait_all(eng, self._collect(reads, writes), allow_own=hard)
        inst = fn()
        own = self.esem[eng]
        self.ecnt[eng] += 1
        inst.then_inc(own, 1)
        ev = (own, self.ecnt[eng])
        self._update(ev, reads, writes)
        if self.ecnt[eng] >= 12000:
            self._new_esem(eng)
        return ev

    def dma(self, q, sc, out, in_, reads=(), writes=(), is_output=False, **kw):
        self._wait_all(q, self._collect(reads, writes, skip_num=sc.sem.num))
        inst = self.E[q].dma_start(out=out, in_=in_, **kw)
        sc.cnt += 16
        inst.then_inc(sc.sem, 16)
        ev = (sc.sem, sc.cnt)
        self._update(ev, reads, writes)
        if is_output:
            self.out_events.append(ev)
        return ev

    def barrier(self):
        evs = []
        for k in self.E:
            if self.ecnt[k] > 0:
                evs.append((self.esem[k], self.ecnt[k]))
        for sc in self.dsem.values():
            if sc.cnt > 0:
                evs.append((sc.sem, sc.cnt))
        for k in self.E:
            self._wait_all(k, evs)

    def finish(self):
        self._wait_all("sp", self.out_events)


class Ring:
    def __init__(self, T, es, nc, name, n, shape, dtype):
        self.n = n
        self.t = [es.enter_context(nc.sbuf_tensor(f"r_{name}{i}", shape, dtype)) for i in range(n)]
        self.b = [Buf(f"{name}{i}") for i in range(n)]
        self.s = [T.semc(name) for i in range(n)]
        self.i = 0

    def next(self):
        k = self.i % self.n
        self.i += 1
        return self.t[k], self.b[k], self.s[k]


def _rope_tables():
    t = np.arange(SEQ)
    row = (t // 64).astype(np.float32)
    col = (t % 64).astype(np.float32)
    inv = np.power(np.float32(10000.0), -np.arange(16, dtype=np.float32) / np.float32(16)).astype(np.float32)
    ang = np.stack([row[:, None] * inv, col[:, None] * inv], axis=1).astype(np.float32)
    cs, sn = np.cos(ang).astype(np.float32), np.sin(ang).astype(np.float32)
    C = np.zeros((SEQ, 2, 2, 16), np.float32)
    S = np.zeros((SEQ, 2, 2, 16), np.float32)
    C[:, :, 0, :] = cs
    C[:, :, 1, :] = cs
    S[:, :, 0, :] = -sn
    S[:, :, 1, :] = sn
    C = C.reshape(NTL, 128, 64).transpose(1, 0, 2)
    S = S.reshape(NTL, 128, 64).transpose(1, 0, 2)
    return np.ascontiguousarray(C), np.ascontiguousarray(S)


def _band_mats():
    out = np.zeros((4, 5, 128, 128), np.float32)
    n = 1024
    for g, w in enumerate((2, 4, 8, 16)):
        B = np.zeros((n, n), np.float64)
        for t in range(n):
            lo = min(max(t - w // 2, 0), n)
            hi = min(max(t - w // 2 + w, 0), n)
            B[lo:hi, t] += 1.0 / (hi - lo)
            B[t, t] -= 1.0
        i = 3
        out[g, 0] = B[(i - 1) * 128:i * 128, i * 128:(i + 1) * 128]
        out[g, 1] = B[i * 128:(i + 1) * 128, i * 128:(i + 1) * 128]
        out[g, 2] = B[(i + 1) * 128:(i + 2) * 128, i * 128:(i + 1) * 128]
        out[g, 3] = B[0:128, 0:128]
        out[g, 4] = B[n - 128:n, n - 128:n]
    return np.ascontiguousarray(out.transpose(2, 0, 1, 3)).astype(ml_dtypes.bfloat16)


def _dft_consts():
    c = np.arange(64)
    ang = 2.0 * np.pi * ((c[:, None] * c[None, :]) % 64) / 64.0
    C64 = np.zeros((128, 128), np.float64)
    S64 = np.zeros((128, 128), np.float64)
    for a in range(2):
        C64[a * 64:(a + 1) * 64, a * 64:(a + 1) * 64] = np.cos(ang)
        S64[a * 64:(a + 1) * 64, a * 64:(a + 1) * 64] = np.sin(ang)
    s = np.arange(SEQ, dtype=np.int64)
    m = (s[:, None] * s[None, :]) % SEQ
    angn = (2.0 * np.pi / SEQ) * m
    CN = (np.cos(angn) / 512.0).astype(ml_dtypes.bfloat16)
    SN = (-np.sin(angn) / 512.0).astype(ml_dtypes.bfloat16)
    return C64.astype(ml_dtypes.bfloat16), S64.astype(ml_dtypes.bfloat16), CN, SN


def _na_index():
    MASK = 15 * 31
    idx = np.full((3, 8, 128, 512), MASK, np.int64)
    for bt, qb in enumerate((0, 3, 7)):
        for slot in range(8):
            kt = 4 * qb - 2 + slot
            if kt < 0 or kt >= 32:
                continue
            for krl in range(2):
                kr = 2 * kt + krl
                for qrl in range(8):
                    qr = 8 * qb + qrl
                    rs = min(max(qr - 4, 0), 56)
                    if not (rs <= kr < rs + 8):
                        continue
                    dr = kr - qr + 7
                    qc = np.arange(64)
                    cs = np.clip(qc - 8, 0, 48)
                    kc = np.arange(64)
                    valid = (kc[:, None] >= cs[None, :]) & (kc[:, None] < cs[None, :] + 16)
                    dc = kc[:, None] - qc[None, :] + 15
                    blk = np.where(valid, dr * 31 + dc, MASK)
                    idx[bt, slot, krl * 64:(krl + 1) * 64, qrl * 64:(qrl + 1) * 64] = blk
    return idx


_CONST_CACHE = {}


def _consts():
    if not _CONST_CACHE:
        C, S = _rope_tables()
        C64, S64, CN, SN = _dft_consts()
        _CONST_CACHE.update(dict(
            ropeC=C, ropeS=S, band=_band_mats(), C64=C64, S64=S64, CN=CN, SN=SN,
            identf=np.eye(128, dtype=np.float32), naidx=_na_index()))
    return _CONST_CACHE


def build_program(n_layers=2, debug=False, stop_after=None):
    global _LAST_NC
    nc = bass.Bass("TRN2", target_bir_lowering=False)
    _LAST_NC = nc
    es = contextlib.ExitStack()

    def din(name, shape, dt=F32):
        return nc.dram_tensor(name, list(shape), dt, kind="ExternalInput").ap()

    def dscr(name, shape, dt=F32):
        return nc.dram_tensor(name, list(shape), dt, kind="Internal").ap()

    x_in = din("x", [SEQ, D])
    ctx_in = din("ctx", [CTXL, D])
    cvec = din("cvec", [128, 8, 2])
    ada_w = din("ada_w", [2, D, 6 * D])
    ada_b = din("ada_b", [2, 6 * D])
    adab_col = din("adab_col", [2, 128, 48])
    ln_mix_g = din("ln_mix_g", [2, D]); ln_mix_b = din("ln_mix_b", [2, D])
    ln_ffn_g = din("ln_ffn_g", [2, D]); ln_ffn_b = din("ln_ffn_b", [2, D])
    w_out = din("w_out", [2, D, D])
    ev_w_in = din("ev_w_in", [D, 1536])
    ev_pool_w = din("ev_pool_w", [4, 64, 64])
    ev_pool_scale = din("ev_pool_scale", [256])
    ev_qk_gain = din("ev_qk_gain", [1024])
    ev_ffn_w1 = din("ev_ffn_w1", [D, FFN]); ev_ffn_w3 = din("ev_ffn_w3", [D, FFN]); ev_ffn_w2 = din("ev_ffn_w2", [FFN, D])
    if n_layers == 2:
        od_w_in = din("od_w_in", [D, 2560])
        od_fgain = din("od_fgain", [256])
        od_bias = din("od_bias", [12, 3, 8, 128, 512])
        od_router = din("od_router", [D, 8])
        od_w1 = din("od_w1", [NEXP, D, FFN]); od_w3 = din("od_w3", [NEXP, D, FFN]); od_w2 = din("od_w2", [NEXP, FFN, D])
    ropeC = din("ropeC", [128, NTL, 64]); ropeS = din("ropeS", [128, NTL, 64])
    band = din("band", [128, 4, 5, 128], BF16)
    if n_layers == 2:
        C64 = din("C64", [128, 128], BF16); S64 = din("S64", [128, 128], BF16)
        CN = din("CN", [SEQ, SEQ], BF16); SN = din("SN", [SEQ, SEQ], BF16)
    identf_in = din("identf", [128, 128])

    out_ap = nc.dram_tensor("out", [SEQ, D], F32, kind="ExternalOutput").ap()
    dbg = {}
    if debug:
        for nm, shp in (("dbg_x1", [NT * 128, D]), ("dbg_x2", [NT * 128, D]), ("dbg_x3", [SEQ, D])):
            dbg[nm] = nc.dram_tensor(nm, shp, F32, kind="ExternalOutput").ap()

    X1 = dscr("X1", [NT * 128, D]) if not debug else dbg["dbg_x1"]
    X2 = dscr("X2", [NT * 128, D]) if not debug else dbg["dbg_x2"]
    X3 = dscr("X3", [SEQ, D]) if not debug else dbg["dbg_x3"]
    X1b = [Buf(f"X1_{i}") for i in range(NT)]
    X2b = [Buf(f"X2_{i}") for i in range(NT)]
    X3b = [Buf(f"X3_{i}") for i in range(NTL)]

    T = Tracker(nc, es)

    def dump(name, ap, shape, dt, reads):
        if not debug:
            return
        d = nc.dram_tensor("dbg_" + name, list(shape), dt, kind="ExternalOutput").ap()
        T.dma("sp", T.semc("dbg"), d, ap, reads=reads, is_output=True)
    pe, act, dve, pool, sp = nc.tensor, nc.scalar, nc.vector, nc.gpsimd, nc.sync

    def sb(name, shape, dt, stack=None):
        return (stack or es).enter_context(nc.sbuf_tensor("s_" + name, list(shape), dt))

    def ps(name, shape, dt, stack):
        return stack.enter_context(nc.psum_tensor("p_" + name, list(shape), dt))

    def x_src(t):
        return x_in[t * 128:(t + 1) * 128, :] if t < NTL else ctx_in[(t - NTL) * 128:(t - NTL + 1) * 128, :]

    identf = sb("identf", [128, 128], F32); identf_b = Buf()
    identb = sb("identb", [128, 128], BF16); identb_b = Buf()
    onesf = sb("onesf", [128, 64], F32); ones_b = Buf()
    cst_s = T.semc("cst")
    T.dma("sp", cst_s, identf[:], identf_in[:, :], writes=[identf_b])
    T.dma("pool", cst_s, identb[:], identf_in[:, :], writes=[identb_b])
    T.op("dve", lambda: dve.memset(onesf[:], 1.0), writes=[ones_b])

    modcol = [sb(f"modcol{l}", [128, 4, 8, 2], F32) for l in range(2)]
    modcol_b = [Buf() for l in range(2)]
    epsc = sb("epsc", [128, 2], F32); eps_b = Buf()

    def _eps():
        dve.memset(epsc[:, 0:1], LN_EPS)
        return dve.memset(epsc[:, 1:2], RMS_EPS)
    T.op("dve", _eps, writes=[eps_b])
    AD = dscr("AD", [NT * 128, 256], BF16)
    AD_b = [Buf() for _ in range(NT)]

    def load_lnp(l, k, stack, sc):
        src = (ln_mix_g, ln_mix_b, ln_ffn_g, ln_ffn_b)[k]
        t = sb(f"lnp{l}{k}", [128, D], F32, stack)
        b = Buf()
        T.dma("sp", sc, t[:], src[l:l + 1, :].to_broadcast([128, D]), writes=[b])
        return t, b

    def compute_mod(l, lstack, mstack):
        nstream = 2 if l == 0 else 1
        gate = {}
        gate_b = {}
        for g in (1, 0):
            for s in range(nstream):
                gate[(s, g)] = sb(f"gate{l}{s}{g}", [128, D], F32, mstack if g == 0 else lstack)
                gate_b[(s, g)] = Buf()
        with contextlib.ExitStack() as st:
            cc = sb(f"cc{l}", [128, 8, 2], F32, st); cc_b = Buf()
            ccrep = sb(f"ccrep{l}", [128, 8, 2, 128], F32, st); ccrep_b = Buf()
            abcol = sb(f"abcol{l}", [128, 48], F32, st); abcol_b = Buf()
            abrow = sb(f"abrow{l}", [128, 2048], F32, st); abrow_b = Buf()
            ms_ = T.semc("mod")
            T.dma("sp", ms_, cc[:], cvec[:, :, :], writes=[cc_b])
            T.dma("sp", ms_, abcol[:], adab_col[l, :, :], writes=[abcol_b])
            for gi, seg in enumerate((2, 5)):
                T.dma("sp", ms_, abrow[:, gi * 1024:(gi + 1) * 1024],
                      ada_b[l:l + 1, seg * 1024:(seg + 1) * 1024].to_broadcast([128, 1024]), writes=[abrow_b])
            T.op("act", lambda: act.activation(out=cc[:], in_=cc[:], func=AF.Silu), reads=[cc_b], writes=[cc_b])

            def _rep():
                i = None
                for k in range(8):
                    for s in range(2):
                        i = dve.tensor_copy(out=ccrep[:, k, s, :], in_=cc[:, k, s:s + 1].to_broadcast([128, 128]))
                return i
            T.op("dve", _rep, reads=[cc_b], writes=[ccrep_b])
            wring = Ring(T, st, nc, f"adaw{l}", 2, [128, 8, 512], F32)
            ps_c = ps(f"ps_c{l}", [128, 512], F32, st); ps_c_b = Buf()
            ps_g = [ps(f"ps_g{l}{i}", [128, 512], F32, st) for i in range(2)]; ps_g_b = [Buf(), Buf()]
            for j in range(12):
                seg, hf = j // 2, j % 2
                wt, wb, ws = wring.next()
                T.dma("sp", ws, wt[:], ada_w[l, :, j * 512:(j + 1) * 512].rearrange("(k p) n -> p k n", p=128),
                      writes=[wb])
                if seg in (2, 5):
                    gi = 0 if seg == 2 else 1
                    for s in range(nstream):
                        def _mm(s=s, wt=wt):
                            i = None
                            for k in range(8):
                                i = pe.matmul(ps_g[s][:, :], lhsT=ccrep[:, k, s, :], rhs=wt[:, k, :],
                                              start=(k == 0), stop=(k == 7))
                            return i
                        T.op("pe", _mm, reads=[ccrep_b, wb], writes=[ps_g_b[s]])
                        T.op("dve", lambda s=s, gi=gi, hf=hf: dve.tensor_tensor(
                            out=gate[(s, gi)][:, hf * 512:(hf + 1) * 512], in0=ps_g[s][:, :],
                            in1=abrow[:, gi * 1024 + hf * 512: gi * 1024 + (hf + 1) * 512], op=ALU.add),
                            reads=[ps_g_b[s], abrow_b], writes=[gate_b[(s, gi)]])
                else:
                    vi = {0: 0, 1: 1, 3: 2, 4: 3}[seg]
                    for c4 in range(4):
                        def _mm(c4=c4, wt=wt):
                            i = None
                            for k in range(8):
                                i = pe.matmul(ps_c[:, 0:2], lhsT=wt[:, k, c4 * 128:(c4 + 1) * 128], rhs=cc[:, k, :],
                                              start=(k == 0), stop=(k == 7))
                            return i
                        T.op("pe", _mm, reads=[cc_b, wb], writes=[ps_c_b])
                        chunk = hf * 4 + c4
                        colidx = seg * 8 + chunk
                        T.op("dve", lambda vi=vi, chunk=chunk, colidx=colidx: dve.tensor_scalar(
                            out=modcol[l][:, vi, chunk, :], in0=ps_c[:, 0:2],
                            scalar1=abcol[:, colidx:colidx + 1], scalar2=(1.0 if vi in (1, 3) else 0.0),
                            op0=ALU.add, op1=ALU.add),
                            reads=[ps_c_b, abcol_b], writes=[modcol_b[l]])
        T.barrier()
        if l == 0:
            dump("modcol", modcol[0][:].rearrange("p a b c -> p (a b c)"), [128, 64], F32, [modcol_b[0]])
            dump("gate00", gate[(0, 0)][:], [128, D], F32, [gate_b[(0, 0)]])
        return gate, gate_b

    def make_hT(xt, xb, l, vsh, vsc, stream, ps_tr, ps_tr_b, hT_ap, hT_b, hTf_ap=None, hTf_b=None):
        def _tr():
            i = None
            for k in range(8):
                i = pe.transpose(ps_tr[:, k * 128:(k + 1) * 128], xt[:, k * 128:(k + 1) * 128], identf[:])
            return i
        T.op("pe", _tr, reads=[xb, identf_b], writes=[ps_tr_b])

        def _ev():
            i = None
            for k in range(8):
                i = dve.tensor_scalar(out=hT_ap(k), in0=ps_tr[:, k * 128:(k + 1) * 128],
                                      scalar1=modcol[l][:, vsc, k, stream:stream + 1],
                                      scalar2=modcol[l][:, vsh, k, stream:stream + 1], op0=ALU.mult, op1=ALU.add)
            return i
        T.op("dve", _ev, reads=[ps_tr_b, modcol_b[l]], writes=[hT_b])
        if hTf_ap is not None:
            def _ev2():
                i = None
                for k in range(8):
                    i = act.activation(out=hTf_ap(k), in_=ps_tr[:, k * 128:(k + 1) * 128], func=AF.Identity,
                                       scale=modcol[l][:, vsc, k, stream:stream + 1],
                                       bias=modcol[l][:, vsh, k, stream:stream + 1])
                return i
            T.op("act", _ev2, reads=[ps_tr_b, modcol_b[l], hT_b], writes=[hTf_b])

    lnw = {}
    lnw["st"] = sb("ln_st", [128, 2, 6], F32); lnw["mv"] = sb("ln_mv", [128, 2], F32)
    lnw["sd"] = sb("ln_sd", [128, 1], F32); lnw["rs"] = sb("ln_rs", [128, 1], F32)
    lnw["b0"] = Buf(); lnw["b1"] = Buf(); lnw["b2"] = Buf(); lnw["b3"] = Buf()

    def resid_ln(ps_halves, ps_bufs, zt, zb, gate_t, gate_bf, lg, lgb, lb, lbb, tmp, tmp_b):
        stt, mv, sd, rs = lnw["st"], lnw["mv"], lnw["sd"], lnw["rs"]

        def _a():
            for h in range(2):
                dve.tensor_tensor(out=tmp[:, h * 512:(h + 1) * 512], in0=ps_halves[h],
                                  in1=gate_t[:, h * 512:(h + 1) * 512], op=ALU.mult)
            dve.scalar_tensor_tensor(out=zt[:], in0=zt[:], scalar=ALPHA, in1=tmp[:], op0=ALU.mult, op1=ALU.add)
            i = None
            for h in range(2):
                i = dve.bn_stats(out=stt[:, h, :], in_=zt[:, h * 512:(h + 1) * 512])
            return i
        T.op("dve", _a, reads=list(ps_bufs) + [gate_bf], writes=[zb, tmp_b, lnw["b0"]])
        T.op("dve", lambda: dve.bn_aggr(out=mv[:], in_=stt[:].rearrange("p a b -> p (a b)")),
             reads=[lnw["b0"]], writes=[lnw["b1"]], hard=True)
        T.op("act", lambda: act.activation(out=sd[:], in_=mv[:, 1:2], func=AF.Sqrt, bias=epsc[:, 0:1], scale=1.0),
             reads=[lnw["b1"], eps_b], writes=[lnw["b2"]])
        T.op("dve", lambda: dve.reciprocal(out=rs[:], in_=sd[:]), reads=[lnw["b2"]], writes=[lnw["b3"]])

        def _b():
            dve.tensor_scalar(out=zt[:], in0=zt[:], scalar1=mv[:, 0:1], scalar2=rs[:, 0:1],
                              op0=ALU.subtract, op1=ALU.mult)
            dve.tensor_tensor(out=zt[:], in0=zt[:], in1=lg[:], op=ALU.mult)
            return dve.tensor_tensor(out=zt[:], in0=zt[:], in1=lb[:], op=ALU.add)
        T.op("dve", _b, reads=[lnw["b3"], lnw["b1"], lgb, lbb], writes=[zb], hard=True)

    def rms_heads(src_ap, nh, sq, ms, rt, dst_ap, wbuf, reads, gain_t=None, gain_b=None):
        T.op("act", lambda: act.activation(out=sq[:, 0:nh * 64], in_=src_ap, func=AF.Square),
             reads=reads, writes=[wbuf["sq"]])
        T.op("dve", lambda: dve.tensor_reduce(out=ms[:, 0:nh], in_=sq[:, 0:nh * 64].rearrange("p (h d) -> p h d", d=64),
                                              axis=AX.X, op=ALU.add),
             reads=[wbuf["sq"]], writes=[wbuf["ms"]])
        T.op("act", lambda: act.activation(out=rt[:, 0:nh], in_=ms[:, 0:nh], func=AF.Sqrt, bias=epsc[:, 1:2],
                                           scale=1.0 / 64.0),
             reads=[wbuf["ms"], eps_b], writes=[wbuf["rt"]])

        T.op("dve", lambda: dve.reciprocal(out=rt[:, 0:nh], in_=rt[:, 0:nh]), reads=[wbuf["rt"]], writes=[wbuf["rt"]])

        def _n():
            i = dve.tensor_tensor(out=dst_ap, in0=src_ap.rearrange("p (h d) -> p h d", d=64),
                                  in1=rt[:, 0:nh].unsqueeze(2).to_broadcast([128, nh, 64]), op=ALU.mult)
            if gain_t is not None:
                i = dve.tensor_tensor(out=dst_ap, in0=dst_ap, in1=gain_t, op=ALU.mult)
            return i
        T.op("dve", _n, reads=list(reads) + [wbuf["rt"]] + ([gain_b] if gain_b else []), writes=[wbuf["dst"]], hard=True)

    acnt = [0]

    def attn_head(qT_ap, kT_fn, v_fn, key_list, ps_s, ps_s_b, pT_ring, ps_o, ps_o_b, nq, reads_q, bias_fn=None):
        nk = len(key_list)
        for i, kt in enumerate(key_list):
            kap, kb = kT_fn(kt)
            bi = bias_fn(kt) if bias_fn is not None else None
            sidx = acnt[0] % 2
            acnt[0] += 1

            def _s(kap=kap, bi=bi, sidx=sidx):
                ins = pe.matmul(ps_s[sidx][:, 0:nq], lhsT=kap, rhs=qT_ap, start=True, stop=(bi is None))
                if bi is not None:
                    ins = pe.matmul(ps_s[sidx][:, 0:nq], lhsT=identb[:], rhs=bi[0], start=False, stop=True)
                return ins
            T.op("pe", _s, reads=[kb] + list(reads_q) + ([bi[1], identb_b] if bi else []), writes=[ps_s_b[sidx]])
            pt, pb, _ = pT_ring.next()
            T.op("act", lambda pt=pt, sidx=sidx: act.activation(out=pt[:, 0:nq], in_=ps_s[sidx][:, 0:nq], func=AF.Exp),
                 reads=[ps_s_b[sidx]], writes=[pb])
            vap, vb = v_fn(kt)
            T.op("pe", lambda vap=vap, pt=pt, i=i: pe.matmul(ps_o[0:65, 0:nq], lhsT=vap, rhs=pt[:, 0:nq],
                                                            start=(i == 0), stop=(i == nk - 1)),
                 reads=[vb, pb], writes=[ps_o_b])

    def attn_norm(ps_o, ps_o_b, osb, osb_b, ps_bc, ps_bc_b, dst_ap, dst_b, nq):
        def _c():
            dve.tensor_copy(out=osb[0:65, 0:nq], in_=ps_o[0:65, 0:nq])
            return dve.reciprocal(out=osb[64:65, 0:nq], in_=osb[64:65, 0:nq])
        T.op("dve", _c, reads=[ps_o_b], writes=[osb_b])
        T.op("pe", lambda: pe.matmul(ps_bc[0:64, 0:nq], lhsT=onesf[64:65, 0:64], rhs=osb[64:65, 0:nq],
                                     start=True, stop=True),
             reads=[osb_b, ones_b], writes=[ps_bc_b])
        T.op("dve", lambda: dve.tensor_tensor(out=dst_ap, in0=osb[0:64, 0:nq], in1=ps_bc[0:64, 0:nq], op=ALU.mult),
             reads=[osb_b, ps_bc_b], writes=[dst_b])

    def x_src(t):
        return x_in[t * 128:(t + 1) * 128, :] if t < NTL else ctx_in[(t - NTL) * 128:(t - NTL + 1) * 128, :]

    def qhead_pos(h):
        return (h % 3) + 3 * (h // 6), (h // 3) % 2

    L = 0
    with contextlib.ExitStack() as stL0:
        stR = contextlib.ExitStack()
        gate, gate_b = compute_mod(0, stL0, stR)
        QT = sb("QT", [128, 6, SEQ + CTXL], BF16, stR); QT_b = [Buf() for _ in range(NT)]
        KT = sb("KT", [128, 2, SEQ + CTXL], BF16, stR); KT_b = [Buf() for _ in range(NT)]
        VA = sb("VA", [128, NT, 4, 66], BF16, stR); VA_b = [Buf() for _ in range(NT)]
        bandt = sb("bandt", [128, 4, 5, 128], BF16, stR); band_b = Buf()
        poolw = sb("poolw", [64, 4, 64], BF16, stR); poolw_b = Buf()
        w0_s = T.semc("w0")
        T.op("dve", lambda: dve.memset(VA[:].rearrange("p t k d -> p (t k) d")[:, :, 64:65], 1.0), writes=VA_b)
        T.dma("sp", w0_s, bandt[:], band[:, :, :, :], writes=[band_b])

        with contextlib.ExitStack() as stA:
            win = sb("win0", [128, 8, 1536], BF16, stA); win_b = Buf()
            T.dma("pool", w0_s, win[:], ev_w_in.rearrange("(k p) n -> p k n", p=128), writes=[win_b])
            rring = Ring(T, stA, nc, "rope", 2, [128, 2, 64], F32)
            gaint = sb("gaint", [128, 16, 64], F32, stA); gain_b = Buf()
            T.dma("sp", w0_s, gaint[:].rearrange("p h d -> p (h d)"),
                  ev_qk_gain.rearrange("(o n) -> o n", o=1).to_broadcast([128, 1024]), writes=[gain_b])
            T.op("dve", lambda: dve.tensor_scalar(out=gaint[:, 0:12, :], in0=gaint[:, 0:12, :], scalar1=0.125,
                                                  scalar2=None, op0=ALU.mult), reads=[gain_b], writes=[gain_b])
            pwf = sb("pwf", [64, 4, 64], F32, stA); pws = sb("pws", [64, 4, 64], F32, stA); pw_b = Buf()
            T.dma("sp", w0_s, pwf[:], ev_pool_w.rearrange("g c d -> c g d"), writes=[pw_b])
            T.dma("sp", w0_s, pws[:].rearrange("p g d -> p (g d)"),
                  ev_pool_scale.rearrange("(o n) -> o n", o=1).to_broadcast([64, 256]), writes=[pw_b])
            T.op("dve", lambda: dve.tensor_tensor(out=poolw[:], in0=pwf[:], in1=pws[:], op=ALU.mult),
                 reads=[pw_b], writes=[poolw_b])

            xring = Ring(T, stA, nc, "xa", 2, [128, D], F32)
            aring = Ring(T, stA, nc, "aa", 2, [128, 256], BF16)
            hT = sb("hTa", [128, 8, 128], BF16, stA); hT_b = Buf()
            ms = sb("msa", [128, 16], F32, stA); rt = sb("rta", [128, 16], F32, stA)
            qn = sb("qna", [128, 16, 64], F32, stA)
            t1 = sb("t1a", [128, 16, 64], F32, stA); t2 = sb("t2a", [128, 16, 64], F32, stA)
            sq = t2[:].rearrange("p h d -> p (h d)")
            stg = sb("stga", [128, 1024], BF16, stA)
            t12_b = Buf(); stg_b = Buf()
            wb_ = {"sq": t12_b, "ms": Buf(), "rt": Buf(), "dst": Buf()}
            ps_tr = ps("ps_trA", [128, 1024], F32, stA); ps_tr_b = Buf()
            ps_pj = ps("ps_pjA", [128, 1536], F32, stA); ps_pj_b = Buf()
            ps_t2 = ps("ps_t2A", [128, 1024], BF16, stA); ps_t2_b = Buf()
            for t in range(NT):
                stream = 0 if t < NTL else 1
                xt, xb, xs = xring.next()
                T.dma("sp", xs, xt[:], x_src(t), writes=[xb])
                make_hT(xt, xb, L, 0, 1, stream, ps_tr, ps_tr_b, lambda k: hT[:, k, :], hT_b)

                def _pj():
                    i = None
                    for (c0, n, w0) in ((0, 512, 256), (512, 512, 768), (1024, 256, 0), (1280, 256, 1280)):
                        for k in range(8):
                            i = pe.matmul(ps_pj[:, c0:c0 + n], lhsT=hT[:, k, :],
                                          rhs=win[:, k, w0:w0 + n], start=(k == 0), stop=(k == 7))
                    return i
                T.op("pe", _pj, reads=[hT_b, win_b], writes=[ps_pj_b])
                if t == 0:
                    dump("hT", hT[:].rearrange("p a b -> p (a b)"), [128, 1024], BF16, [hT_b])
                at_, ab_, as_ = aring.next()

                def _av(t=t, at_=at_):
                    act.copy(out=at_[:], in_=ps_pj[:, 1024:1280])
                    return act.copy(out=VA[:, t, :, 0:64], in_=ps_pj[:, 1280:1536].rearrange("p (k d) -> p k d", d=64))
                T.op("act", _av, reads=[ps_pj_b], writes=[ab_, VA_b[t]])
                T.dma("sp", as_, AD[t * 128:(t + 1) * 128, :], at_[:], reads=[ab_], writes=[AD_b[t]])
                rms_heads(ps_pj[:, 0:1024], 16, sq, ms, rt, qn[:], wb_, [ps_pj_b], gaint[:], gain_b)
                if t == 0:
                    dump("ms", ms[:], [128, 16], F32, [wb_["ms"]])
                    dump("qn", qn[:].rearrange("p a b -> p (a b)"), [128, 1024], F32, [wb_["dst"]])
                stq = stg[:, 0:768].rearrange("p (jj r hf d) -> p jj r hf d", jj=2, r=3, hf=2)
                stk = stg[:, 768:1024].rearrange("p (k d) -> p k d", d=64)

                def qperm(tt):
                    return tt[:, 0:12, :].rearrange("p (jj hf r) d -> p jj r hf d", jj=2, hf=2, r=3)
                if t < NTL:
                    rt_, rb_, rs_ = rring.next()
                    T.dma("sp", rs_, rt_[:, 0, :], ropeC[:, t, :], writes=[rb_])
                    T.dma("sp", rs_, rt_[:, 1, :], ropeS[:, t, :], writes=[rb_])

                    def _rope(rt_=rt_):
                        cb = rt_[:, 0, :].unsqueeze(1).to_broadcast([128, 16, 64])
                        dve.tensor_tensor(out=t1[:], in0=qn[:], in1=cb, op=ALU.mult)
                        qv = qn[:].rearrange("p h (a f) -> p h a f", a=4)
                        tv = t2[:].rearrange("p h (a f) -> p h a f", a=4)
                        sv = rt_[:, 1, :].rearrange("p (a f) -> p a f", a=4)
                        i = None
                        for ax in range(2):
                            for hf in range(2):
                                a_dst = ax * 2 + hf
                                a_src = ax * 2 + (1 - hf)
                                i = dve.tensor_tensor(out=tv[:, :, a_dst, :], in0=qv[:, :, a_src, :],
                                                      in1=sv[:, a_dst, :].unsqueeze(1).to_broadcast([128, 16, 16]),
                                                      op=ALU.mult)
                        return i
                    T.op("dve", _rope, reads=[wb_["dst"], rb_], writes=[t12_b])

                    def _st():
                        for jj in range(2):
                            dve.tensor_tensor(out=stq[:, jj], in0=qperm(t1)[:, jj], in1=qperm(t2)[:, jj], op=ALU.add)
                        return dve.tensor_tensor(out=stk, in0=t1[:, 12:16, :], in1=t2[:, 12:16, :], op=ALU.add)
                    T.op("dve", _st, reads=[t12_b], writes=[stg_b])
                else:
                    def _st():
                        for jj in range(2):
                            dve.tensor_copy(out=stq[:, jj], in_=qperm(qn)[:, jj])
                        return dve.tensor_copy(out=stk, in_=qn[:, 12:16, :])
                    T.op("dve", _st, reads=[wb_["dst"]], writes=[stg_b])

                def _t2():
                    i = None
                    for j in range(8):
                        i = pe.transpose(ps_t2[:, j * 128:(j + 1) * 128], stg[:, j * 128:(j + 1) * 128], identb[:])
                    return i
                T.op("pe", _t2, reads=[stg_b, identb_b], writes=[ps_t2_b])

                def _e2(t=t):
                    act.copy(out=QT[:, :, t * 128:(t + 1) * 128],
                             in_=ps_t2[:, 0:768].rearrange("p (j n) -> p j n", n=128))
                    return act.copy(out=KT[:, :, t * 128:(t + 1) * 128],
                                    in_=ps_t2[:, 768:1024].rearrange("p (j n) -> p j n", n=128))
                T.op("act", _e2, reads=[ps_t2_b], writes=[QT_b[t], KT_b[t]])
                if t == 0:
                    dump("stg", stg[:], [128, 1024], BF16, [stg_b])
                    dump("QT0", QT[:, :, 0:128], [128, 6, 128], BF16, [QT_b[0]])
                    dump("KT0", KT[:, :, 0:128], [128, 2, 128], BF16, [KT_b[0]])
                    dump("VA0", VA[:, 0, :, :], [128, 4, 66], BF16, [VA_b[0]])

        T.barrier()
        with contextlib.ExitStack() as stB:
            wout = sb("wout0", [64, 16, D], BF16, stB); wout_b = Buf()
            T.dma("pool", w0_s, wout[:], w_out[0].rearrange("(c p) n -> p c n", p=64), writes=[wout_b])
            lg, lgb = load_lnp(0, 0, stB, w0_s)
            lb, lbb = load_lnp(0, 1, stB, w0_s)
            ps_s = [ps(f"ps_s{i}", [128, 512], F32, stB) for i in range(2)]; ps_s_b = [Buf(), Buf()]
            ps_o = [ps(f"ps_o{i}", [128, 512], F32, stB) for i in range(2)]; ps_o_b = [Buf(), Buf()]
            ps_bc = ps("ps_bc", [128, 512], F32, stB); ps_bc_b = Buf()
            ps_y = ps("ps_y", [128, 1024], F32, stB); ps_y_b = Buf()
            ps_pl = ps("ps_pl", [128, 512], F32, stB); ps_pl_b = Buf()
            pT_ring = Ring(T, stB, nc, "pT", 3, [128, 512], BF16)
            osb = [sb(f"osb{i}", [128, 512], F32, stB) for i in range(2)]; osb_b = [Buf(), Buf()]
            OT = sb("OT0", [64, 12, 512], BF16, stB); OT_b = [Buf() for _ in range(12)]
            mT = sb("mT", [64, 4, 128], BF16, stB); mT_b = Buf()
            PTt = sb("PTt", [64, 4, 128], BF16, stB); PT_b = Buf()
            a3ring = Ring(T, stB, nc, "a3", 2, [128, 3, 256], BF16)
            zring = Ring(T, stB, nc, "zb", 2, [128, D], F32)
            tmp = sb("tmpb", [128, D], F32, stB); tmp_b = Buf()
            hcount = 0
            chunks = [(qc * 4, 4, list(range(NT))) for qc in range(8)] + [(NTL, 2, [NTL, NTL + 1])]
            for (t0, ntl, keys) in chunks:
                nq = ntl * 128
                stream = 0 if t0 < NTL else 1
                for h in range(12):
                    ch, half = qhead_pos(h)
                    kv = h // 3
                    rows = slice(half * 64, (half + 1) * 64)
                    oi = hcount % 2
                    hcount += 1
                    attn_head(QT[rows, ch, t0 * 128:t0 * 128 + nq],
                              lambda kt, rows=rows, kv=kv: (KT[rows, kv // 2, kt * 128:(kt + 1) * 128], KT_b[kt]),
                              lambda kt, kv=kv: (VA[:, kt, kv, 0:65], VA_b[kt]),
                              keys, ps_s, ps_s_b, pT_ring, ps_o[oi], ps_o_b[oi], nq,
                              [QT_b[t0 + i] for i in range(ntl)])
                    attn_norm(ps_o[oi], ps_o_b[oi], osb[oi], osb_b[oi], ps_bc, ps_bc_b, OT[:, h, 0:nq], OT_b[h], nq)
                for il in range(ntl):
                    t = t0 + il
                    first = (t == 0) or (t == NTL)
                    last = (t == NTL - 1) or (t == NT - 1)
                    tlo = t if first else t - 1
                    thi = t if last else t + 1
                    a3, a3b, a3s = a3ring.next()
                    nsrc = thi - tlo + 1
                    T.dma("sp", a3s, a3[:, 0:nsrc, :], AD[tlo * 128:(thi + 1) * 128, :].rearrange("(j p) c -> p j c", p=128),
                          reads=[AD_b[i] for i in range(tlo, thi + 1)], writes=[a3b])
                    srcs = []
                    if not first:
                        srcs.append((t - 1 - tlo, 0))
                    srcs.append((t - tlo, 3 if first else (4 if last else 1)))
                    if not last:
                        srcs.append((t + 1 - tlo, 2))

                    def _pm(srcs=srcs, a3=a3):
                        i = None
                        for g in range(4):
                            for j, (sl, var) in enumerate(srcs):
                                i = pe.matmul(ps_pl[0:64, g * 128:(g + 1) * 128], lhsT=a3[:, sl, g * 64:(g + 1) * 64],
                                              rhs=bandt[:, g, var, :], start=(j == 0), stop=(j == len(srcs) - 1))
                        return i
                    T.op("pe", _pm, reads=[a3b, band_b], writes=[ps_pl_b])
                    T.op("act", lambda: act.copy(out=mT[:].rearrange("p g n -> p (g n)"), in_=ps_pl[0:64, :]),
                         reads=[ps_pl_b], writes=[mT_b])

                    def _pp():
                        i = None
                        for g in range(4):
                            i = pe.matmul(ps_pl[0:64, g * 128:(g + 1) * 128], lhsT=poolw[:, g, :], rhs=mT[:, g, :],
                                          start=True, stop=True)
                        return i
                    T.op("pe", _pp, reads=[mT_b, poolw_b], writes=[ps_pl_b])
                    T.op("act", lambda: act.copy(out=PTt[:].rearrange("p g n -> p (g n)"), in_=ps_pl[0:64, :]),
                         reads=[ps_pl_b], writes=[PT_b])

                    def _y(il=il):
                        i = None
                        for n in range(2):
                            for c in range(16):
                                lhs = PTt[:, c, :] if c < 4 else OT[:, c - 4, il * 128:(il + 1) * 128]
                                i = pe.matmul(ps_y[:, n * 512:(n + 1) * 512], lhsT=lhs, rhs=wout[:, c, n * 512:(n + 1) * 512],
                                              start=(c == 0), stop=(c == 15))
                        return i
                    T.op("pe", _y, reads=[PT_b, wout_b] + OT_b, writes=[ps_y_b])
                    if t == 0:
                        dump("OT", OT[:], [64, 12, 512], BF16, OT_b)
                        dump("PT", PTt[:], [64, 4, 128], BF16, [PT_b])
                    zt, zb, zs = zring.next()
                    T.dma("sp", zs, zt[:], x_src(t), writes=[zb])
                    resid_ln([ps_y[:, 0:512], ps_y[:, 512:1024]], [ps_y_b], zt, zb,
                             gate[(stream, 0)], gate_b[(stream, 0)], lg, lgb, lb, lbb, tmp, tmp_b)
                    T.dma("sp", zs, X1[t * 128:(t + 1) * 128, :], zt[:], reads=[zb], writes=[X1b[t]], is_output=debug)

        T.barrier()
        stR.close()
        with contextlib.ExitStack() as stF:
            w1 = sb("w1f", [128, 8, FFN], BF16, stF); w3 = sb("w3f", [128, 8, FFN], BF16, stF)
            w2 = sb("w2f", [128, NF, D], BF16, stF); wf_b = Buf(); wf_s = T.semc("wf")
            for k in range(8):
                T.dma("pool", wf_s, w1[:, k, :], ev_ffn_w1[k * 128:(k + 1) * 128, :], writes=[wf_b], max_dma_last_dim=4096)
                T.dma("pool", wf_s, w3[:, k, :], ev_ffn_w3[k * 128:(k + 1) * 128, :], writes=[wf_b], max_dma_last_dim=4096)
            for f in range(NF):
                T.dma("pool", wf_s, w2[:, f, :], ev_ffn_w2[f * 128:(f + 1) * 128, :], writes=[wf_b], max_dma_last_dim=4096)
            lg, lgb = load_lnp(0, 2, stF, wf_s)
            lb, lbb = load_lnp(0, 3, stF, wf_s)
            x1c = [sb(f"x1c{i}", [128, D], F32, stF) for i in range(4)]; x1c_b = [Buf() for _ in range(4)]
            x1c_s = [T.semc("x1c") for _ in range(4)]
            h2T = sb("h2T", [128, 8, 512], BF16, stF); h2T_b = [Buf() for _ in range(4)]
            AT = sb("ATf", [128, NF, 512], BF16, stF); AT_b = [Buf() for _ in range(NF)]
            sg = [sb(f"sgf{i}", [128, 512], BF16, stF) for i in range(2)]; sg_b = [Buf(), Buf()]
            tmp = sb("tmpf", [128, D], F32, stF); tmp_b = Buf()
            ps_tr = ps("ps_trF", [128, 1024], F32, stF); ps_tr_b = Buf()
            ps_g = [ps(f"ps_gF{i}", [128, 512], F32, stF) for i in range(2)]; ps_g_b = [Buf(), Buf()]
            ps_u = [ps(f"ps_uF{i}", [128, 512], F32, stF) for i in range(2)]; ps_u_b = [Buf(), Buf()]
            ps_o2 = [ps(f"ps_oF{i}", [128, 512], F32, stF) for i in range(2)]; ps_o2_b = [Buf(), Buf()]
            oc = 0
            chunks = [(qc * 4, 4) for qc in range(8)] + [(NTL, 2)]
            for (t0, ntl) in chunks:
                nq = ntl * 128
                stream = 0 if t0 < NTL else 1
                for il in range(ntl):
                    t = t0 + il
                    T.dma("sp", x1c_s[il], x1c[il][:], X1[t * 128:(t + 1) * 128, :], reads=[X1b[t]], writes=[x1c_b[il]])
                    make_hT(x1c[il], x1c_b[il], L, 2, 3, stream, ps_tr, ps_tr_b,
                            lambda k, il=il: h2T[:, k, il * 128:(il + 1) * 128], h2T_b[il])
                for f in range(NF):
                    gi = f % 2

                    def _gu(f=f, gi=gi, nq=nq):
                        i = None
                        for k in range(8):
                            i = pe.matmul(ps_g[gi][:, 0:nq], lhsT=w1[:, k, f * 128:(f + 1) * 128], rhs=h2T[:, k, 0:nq],
                                          start=(k == 0), stop=(k == 7))
                        for k in range(8):
                            i = pe.matmul(ps_u[gi][:, 0:nq], lhsT=w3[:, k, f * 128:(f + 1) * 128], rhs=h2T[:, k, 0:nq],
                                          start=(k == 0), stop=(k == 7))
                        return i
                    T.op("pe", _gu, reads=[wf_b] + h2T_b[0:ntl], writes=[ps_g_b[gi], ps_u_b[gi]])
                    T.op("act", lambda gi=gi, nq=nq: act.activation(out=sg[gi][:, 0:nq], in_=ps_g[gi][:, 0:nq], func=AF.Silu),
                         reads=[ps_g_b[gi]], writes=[sg_b[gi]])
                    T.op("dve", lambda gi=gi, f=f, nq=nq: dve.tensor_tensor(out=AT[:, f, 0:nq], in0=sg[gi][:, 0:nq],
                                                                           in1=ps_u[gi][:, 0:nq], op=ALU.mult),
                         reads=[sg_b[gi], ps_u_b[gi]], writes=[AT_b[f]])
                for il in range(ntl):
                    t = t0 + il
                    ois = []
                    for n in range(2):
                        oi = oc % 2
                        oc += 1
                        ois.append(oi)

                        def _o(il=il, n=n, oi=oi):
                            i = None
                            for f in range(NF):
                                i = pe.matmul(ps_o2[oi][:, :], lhsT=AT[:, f, il * 128:(il + 1) * 128],
                                              rhs=w2[:, f, n * 512:(n + 1) * 512], start=(f == 0), stop=(f == NF - 1))
                            return i
                        T.op("pe", _o, reads=AT_b + [wf_b], writes=[ps_o2_b[oi]])
                    resid_ln([ps_o2[ois[0]][:, :], ps_o2[ois[1]][:, :]], [ps_o2_b[ois[0]], ps_o2_b[ois[1]]],
                             x1c[il], x1c_b[il], gate[(stream, 1)], gate_b[(stream, 1)], lg, lgb, lb, lbb, tmp, tmp_b)
                    if n_layers == 1 and t < NTL:
                        T.dma("sp", x1c_s[il], out_ap[t * 128:(t + 1) * 128, :], x1c[il][:], reads=[x1c_b[il]], is_output=True)
                    T.dma("sp", x1c_s[il], X2[t * 128:(t + 1) * 128, :], x1c[il][:], reads=[x1c_b[il]], writes=[X2b[t]],
                          is_output=debug)

    T.barrier()
    if n_layers == 1:
        T.finish()
        es.close()
        return nc

    L = 1
    QD = dscr("QD", [128, 6, SEQ], BF16); QD_b = [Buf() for _ in range(NTL)]
    KD = dscr("KD", [128, 6, SEQ + CTXL], BF16); KD_b = [Buf() for _ in range(NT)]
    VD = dscr("VD", [NT, 128, 12 * 66], BF16); VD_b = [Buf() for _ in range(NT)]
    W1s = dscr("W1s", [NEXP, 11, 128, 8, 256], BF16); W3s = dscr("W3s", [NEXP, 11, 128, 8, 256], BF16)
    W2s = dscr("W2s", [NEXP, 2, 128, NF, 512], BF16)
    W1s_b = [Buf() for _ in range(NEXP)]; W3s_b = [Buf() for _ in range(NEXP)]; W2s_b = [Buf() for _ in range(NEXP)]
    wc_s = T.semc("wcast")
    for e in range(NEXP):
        for j in range(11):
            T.dma("pool", wc_s, W1s[e, j], od_w1[e][:, j * 256:(j + 1) * 256].rearrange("(k p) n -> p k n", p=128),
                  writes=[W1s_b[e]])
            T.dma("pool", wc_s, W3s[e, j], od_w3[e][:, j * 256:(j + 1) * 256].rearrange("(k p) n -> p k n", p=128),
                  writes=[W3s_b[e]])
        for hh in range(2):
            T.dma("pool", wc_s, W2s[e, hh], od_w2[e][:, hh * 512:(hh + 1) * 512].rearrange("(f p) n -> p f n", p=128),
                  writes=[W2s_b[e]])

    with contextlib.ExitStack() as stL1:
        stR = contextlib.ExitStack()
        gate, gate_b = compute_mod(1, stL1, stR)
        YT = sb("YT", [128, 2, SEQ], BF16, stR); YT_b = [Buf() for _ in range(8)]
        w1_s = T.semc("w1")
        stU = contextlib.ExitStack()
        UCS = sb("UCS", [128, NTL, 512], BF16, stU); UCS_b = [Buf() for _ in range(NTL)]
        with contextlib.ExitStack() as stA:
            win = sb("win1", [128, 8, 2560], BF16, stA); win_b = Buf()
            for k in range(8):
                T.dma("pool", w1_s, win[:, k, :], od_w_in[k * 128:(k + 1) * 128, :], writes=[win_b], max_dma_last_dim=4096)
            fg = sb("fg1", [128, 4, 64], F32, stA); fg_b = Buf()
            T.dma("sp", w1_s, fg[:].rearrange("p g d -> p (g d)"),
                  od_fgain.rearrange("(o n) -> o n", o=1).to_broadcast([128, 256]), writes=[fg_b])
            c64 = sb("c64", [128, 2, 128], BF16, stA); c64_b = Buf()
            T.dma("sp", w1_s, c64[:, 0, :], C64[:, :], writes=[c64_b])
            T.dma("sp", w1_s, c64[:, 1, :], S64[:, :], writes=[c64_b])
            xring = Ring(T, stA, nc, "xa1", 2, [128, D], F32)
            hT = sb("hTa1", [128, 8, 128], BF16, stA); hT_b = Buf()
            sq = sb("sqa1", [128, 256], F32, stA); ms = sb("msa1", [128, 4], F32, stA); rt = sb("rta1", [128, 4], F32, stA)
            un = sb("una1", [128, 4, 64], F32, stA)
            wb_ = {"sq": Buf(), "ms": Buf(), "rt": Buf(), "dst": Buf()}
            stg = sb("stga1", [128, 14 * 128], BF16, stA); stg_b = Buf()
            qkT = Ring(T, stA, nc, "qkT1", 2, [128, 12, 128], BF16)
            uT = sb("uT1", [128, 2, 128], BF16, stA); uT_b = Buf()
            vring = Ring(T, stA, nc, "va1", 2, [128, 12, 66], BF16)
            for i_ in range(2):
                T.op("dve", lambda i_=i_: dve.memset(vring.t[i_][:, :, 64:66], 1.0), writes=[vring.b[i_]])
            ps_tr = ps("ps_trA1", [128, 1024], F32, stA); ps_tr_b = Buf()
            ps_pj = ps("ps_pjA1", [128, 2048], F32, stA); ps_pj_b = Buf()
            ps_t2 = ps("ps_t2A1", [128, 2048], BF16, stA); ps_t2_b = Buf()
            for t in range(NT):
                lat = t < NTL
                stream = 0 if lat else 1
                xt, xb, xs = xring.next()
                T.dma("sp", xs, xt[:], X2[t * 128:(t + 1) * 128, :], reads=[X2b[t]], writes=[xb])
                make_hT(xt, xb, L, 0, 1, stream, ps_tr, ps_tr_b, lambda k: hT[:, k, :], hT_b)

                def _pj(lat=lat):
                    i = None
                    groups = [(1024, 512, 1024), (1536, 256, 1536)]
                    if lat:
                        groups = [(0, 512, 256), (512, 256, 768), (768, 256, 0)] + groups
                    for (c0, n, w0) in groups:
                        for k in range(8):
                            i = pe.matmul(ps_pj[:, c0:c0 + n], lhsT=hT[:, k, :], rhs=win[:, k, w0:w0 + n],
                                          start=(k == 0), stop=(k == 7))
                    return i
                T.op("pe", _pj, reads=[hT_b, win_b], writes=[ps_pj_b])
                if lat:
                    rms_heads(ps_pj[:, 768:1024], 4, sq, ms, rt, un[:], wb_, [ps_pj_b], fg[:], fg_b)

                def _stq(lat=lat):
                    i = act.copy(out=stg[:, 768:1536], in_=ps_pj[:, 1024:1792])
                    if lat:
                        i = act.mul(out=stg[:, 0:768], in_=ps_pj[:, 0:768], mul=0.125)
                    return i
                T.op("act", _stq, reads=[ps_pj_b] + ([wb_["dst"]] if lat else []), writes=[stg_b])
                if lat:
                    T.op("dve", lambda: dve.tensor_copy(out=stg[:, 1536:1792], in_=un[:].rearrange("p g d -> p (g d)")),
                         reads=[wb_["dst"]], writes=[stg_b])

                def _t2(lat=lat):
                    i = None
                    for j in (range(14) if lat else range(6, 12)):
                        i = pe.transpose(ps_t2[:, j * 128:(j + 1) * 128], stg[:, j * 128:(j + 1) * 128], identb[:])
                    return i
                T.op("pe", _t2, reads=[stg_b, identb_b], writes=[ps_t2_b])
                qk, qkb, qks = qkT.next()

                def _e2(lat=lat, qk=qk):
                    i = act.copy(out=qk[:, 6:12, :], in_=ps_t2[:, 768:1536].rearrange("p (j n) -> p j n", n=128))
                    if lat:
                        i = act.copy(out=qk[:, 0:6, :], in_=ps_t2[:, 0:768].rearrange("p (j n) -> p j n", n=128))
                    return i
                T.op("act", _e2, reads=[ps_t2_b], writes=[qkb])
                T.dma("sp", qks, KD[:, :, t * 128:(t + 1) * 128], qk[:, 6:12, :], reads=[qkb], writes=[KD_b[t]])
                if lat:
                    T.dma("sp", qks, QD[:, :, t * 128:(t + 1) * 128], qk[:, 0:6, :], reads=[qkb], writes=[QD_b[t]])
                    T.op("dve", lambda: dve.tensor_copy(out=uT[:].rearrange("p a n -> p (a n)"), in_=ps_t2[:, 1536:1792]),
                         reads=[ps_t2_b], writes=[uT_b])

                    def _uc():
                        i = None
                        for cs_ in range(2):
                            for c2 in range(2):
                                i = pe.matmul(ps_tr[:, (cs_ * 2 + c2) * 128:(cs_ * 2 + c2 + 1) * 128], lhsT=uT[:, c2, :],
                                              rhs=c64[:, cs_, :], start=True, stop=True)
                        return i
                    T.op("pe", _uc, reads=[uT_b, c64_b], writes=[ps_tr_b])
                    T.op("dve", lambda t=t: dve.tensor_copy(out=UCS[:, t, :], in_=ps_tr[:, 0:512]),
                         reads=[ps_tr_b], writes=[UCS_b[t]])
                def _pv():
                    i = None
                    for (c0, n, w0) in ((0, 512, 1792), (512, 256, 2304)):
                        for k in range(8):
                            i = pe.matmul(ps_pj[:, c0:c0 + n], lhsT=hT[:, k, :], rhs=win[:, k, w0:w0 + n],
                                          start=(k == 0), stop=(k == 7))
                    return i
                T.op("pe", _pv, reads=[hT_b, win_b], writes=[ps_pj_b])
                vt, vb, vs = vring.next()
                T.op("act", lambda vt=vt: act.copy(out=vt[:, :, 0:64], in_=ps_pj[:, 0:768].rearrange("p (h d) -> p h d", d=64)),
                     reads=[ps_pj_b], writes=[vb])
                T.dma("sp", vs, VD[t], vt[:].rearrange("p h d -> p (h d)"), reads=[vb], writes=[VD_b[t]])
        T.barrier()
        if stop_after == "A1":
            T.finish(); stU.close(); stR.close(); return nc
        with contextlib.ExitStack() as stFo:
            tring = Ring(T, stFo, nc, "dft", 3, [128, 2, 8, 512], BF16)
            ps_f = [ps(f"ps_f{i}", [128, 512], F32, stFo) for i in range(4)]; ps_f_b = [Buf() for _ in range(4)]
            for tc in range(8):
                pb_ = (tc % 2) * 2
                for sp_ in range(4):
                    tt, tb, ts_ = tring.next()
                    for ci, src in enumerate((CN, SN)):
                        T.dma("sp", ts_, tt[:, ci, :, :],
                              src[sp_ * 1024:(sp_ + 1) * 1024, tc * 512:(tc + 1) * 512].rearrange("(j p) n -> p j n", p=128),
                              writes=[tb])

                    def _f(tt=tt, sp_=sp_, pb_=pb_):
                        i = None
                        for c2 in range(2):
                            for j in range(8):
                                s_ = sp_ * 8 + j
                                for ci in range(2):
                                    i = pe.matmul(ps_f[pb_ + c2][:, :], lhsT=UCS[:, s_, (ci * 2 + c2) * 128:(ci * 2 + c2 + 1) * 128],
                                                  rhs=tt[:, ci, j, :], start=(s_ == 0 and ci == 0), stop=(s_ == 31 and ci == 1))
                        return i
                    T.op("pe", _f, reads=[tb] + UCS_b[sp_ * 8:(sp_ + 1) * 8], writes=[ps_f_b[pb_], ps_f_b[pb_ + 1]])

                def _fe(tc=tc, pb_=pb_):
                    i = None
                    for c2 in range(2):
                        i = act.copy(out=YT[:, c2, tc * 512:(tc + 1) * 512], in_=ps_f[pb_ + c2][:, :])
                    return i
                T.op("act", _fe, reads=[ps_f_b[pb_], ps_f_b[pb_ + 1]], writes=[YT_b[tc]])
        T.barrier()
        if stop_after == "FO":
            T.finish(); stU.close(); stR.close(); return nc
        stU.close()

        with contextlib.ExitStack() as stB:
            wout = sb("wout1", [64, 12, D], BF16, stB); woutF = sb("woutF1", [128, 2, D], BF16, stB); wout_b = Buf()
            T.dma("pool", w1_s, wout[:], w_out[1, 256:1024, :].rearrange("(c p) n -> p c n", p=64), writes=[wout_b])
            T.dma("pool", w1_s, woutF[:], w_out[1, 0:256, :].rearrange("(c p) n -> p c n", p=128), writes=[wout_b])
            lg, lgb = load_lnp(1, 0, stB, w1_s)
            lb, lbb = load_lnp(1, 1, stB, w1_s)
            KTc = sb("KTc", [128, 6, 256], BF16, stB); Vc = sb("Vc", [128, 2, 12 * 66], BF16, stB); kvc_b = Buf()
            T.dma("sp", w1_s, KTc[:], KD[:, :, SEQ:SEQ + CTXL], reads=[KD_b[32], KD_b[33]], writes=[kvc_b])
            T.dma("sp", w1_s, Vc[:], VD[NTL:NT].rearrange("j p n -> p j n"), reads=[VD_b[32], VD_b[33]], writes=[kvc_b])
            qring = Ring(T, stB, nc, "qb1", 2, [128, 6, 512], BF16)
            kring = Ring(T, stB, nc, "kb1", 2, [128, 6, 1024], BF16)
            vwring = Ring(T, stB, nc, "vb1", 2, [128, 8, 12 * 66], BF16)
            bring = Ring(T, stB, nc, "bias1", 3, [128, 8, 512], BF16)
            ps_s = [ps(f"ps_s1{i}", [128, 512], F32, stB) for i in range(2)]; ps_s_b = [Buf(), Buf()]
            ps_o = [ps(f"ps_o1{i}", [128, 512], F32, stB) for i in range(2)]; ps_o_b = [Buf(), Buf()]
            ps_bc = ps("ps_bc1", [128, 512], F32, stB); ps_bc_b = Buf()
            ps_y = ps("ps_y1", [128, 1024], F32, stB); ps_y_b = Buf()
            pT_ring = Ring(T, stB, nc, "pT1", 3, [128, 512], BF16)
            osb = [sb(f"osb1{i}", [128, 512], F32, stB) for i in range(2)]; osb_b = [Buf(), Buf()]
            OT = sb("OT1", [64, 12, 512], BF16, stB); OT_b = [Buf() for _ in range(12)]
            zring = Ring(T, stB, nc, "zb1", 2, [128, D], F32)
            tmp = sb("tmpb1", [128, D], F32, stB); tmp_b = Buf()
            hcount = 0
            for qb in range(8):
                t0 = 4 * qb
                bt = 0 if qb == 0 else (2 if qb == 7 else 1)
                klo = max(0, t0 - 2)
                khi = min(NTL, t0 + 6)
                nk = khi - klo
                slot0 = klo - (t0 - 2)
                qt_, qb_, qs_ = qring.next()
                T.dma("sp", qs_, qt_[:], QD[:, :, t0 * 128:(t0 + 4) * 128], reads=QD_b[t0:t0 + 4], writes=[qb_])
                kt_, kb_, ks_ = kring.next()
                T.dma("sp", ks_, kt_[:, :, 0:nk * 128], KD[:, :, klo * 128:khi * 128], reads=KD_b[klo:khi], writes=[kb_])
                vt_, vb_, vs_ = vwring.next()
                T.dma("sp", vs_, vt_[:, 0:nk, :], VD[klo:khi].rearrange("j p n -> p j n"), reads=VD_b[klo:khi], writes=[vb_])
                keys = list(range(klo, khi)) + [NTL, NTL + 1]
                for h in range(12):
                    ch, half = h // 2, h % 2
                    rows = slice(half * 64, (half + 1) * 64)
                    oi = hcount % 2
                    hcount += 1
                    bt_, bb_, bs_ = bring.next()
                    T.dma("pool", bs_, bt_[:, 0:nk, :], od_bias[h, bt, slot0:slot0 + nk].rearrange("j p n -> p j n"),
                          writes=[bb_])

                    def kfn(kt, rows=rows, ch=ch, kt_=kt_, kb_=kb_, klo=klo):
                        if kt >= NTL:
                            return KTc[rows, ch, (kt - NTL) * 128:(kt - NTL + 1) * 128], kvc_b
                        return kt_[rows, ch, (kt - klo) * 128:(kt - klo + 1) * 128], kb_

                    def vfn(kt, h=h, vt_=vt_, vb_=vb_, klo=klo):
                        if kt >= NTL:
                            return Vc[:, kt - NTL, h * 66:h * 66 + 65], kvc_b
                        return vt_[:, kt - klo, h * 66:h * 66 + 65], vb_

                    def bfn(kt, bt_=bt_, bb_=bb_, klo=klo):
                        if kt >= NTL:
                            return None
                        return bt_[:, kt - klo, :], bb_
                    attn_head(qt_[rows, ch, :], kfn, vfn, keys, ps_s, ps_s_b, pT_ring, ps_o[oi], ps_o_b[oi], 512,
                              [qb_], bias_fn=bfn)
                    attn_norm(ps_o[oi], ps_o_b[oi], osb[oi], osb_b[oi], ps_bc, ps_bc_b, OT[:, h, :], OT_b[h], 512)
                for il in range(4):
                    t = t0 + il

                    def _y(il=il, t=t):
                        i = None
                        for n in range(2):
                            for c in range(14):
                                if c < 2:
                                    lhs, rhs = YT[:, c, t * 128:(t + 1) * 128], woutF[:, c, n * 512:(n + 1) * 512]
                                else:
                                    lhs, rhs = OT[:, c - 2, il * 128:(il + 1) * 128], wout[:, c - 2, n * 512:(n + 1) * 512]
                                i = pe.matmul(ps_y[:, n * 512:(n + 1) * 512], lhsT=lhs, rhs=rhs, start=(c == 0), stop=(c == 13))
                        return i
                    T.op("pe", _y, reads=[YT_b[t // 4], wout_b] + OT_b, writes=[ps_y_b])
                    zt, zb, zs = zring.next()
                    T.dma("sp", zs, zt[:], X2[t * 128:(t + 1) * 128, :], reads=[X2b[t]], writes=[zb])
                    resid_ln([ps_y[:, 0:512], ps_y[:, 512:1024]], [ps_y_b], zt, zb,
                             gate[(0, 0)], gate_b[(0, 0)], lg, lgb, lb, lbb, tmp, tmp_b)
                    T.dma("sp", zs, X3[t * 128:(t + 1) * 128, :], zt[:], reads=[zb], writes=[X3b[t]], is_output=debug)
        T.barrier()
        if stop_after == "NA":
            T.finish(); stR.close(); return nc
        stR.close()

        with contextlib.ExitStack() as stM:
            lg, lgb = load_lnp(1, 2, stM, w1_s)
            lb, lbb = load_lnp(1, 3, stM, w1_s)
            wr = sb("wr", [128, 8, 8], F32, stM); wr_b = Buf()
            T.dma("sp", w1_s, wr[:], od_router.rearrange("(k p) e -> p k e", p=128), writes=[wr_b])
            x3c = [sb(f"x3c{i}", [128, D], F32, stM) for i in range(4)]; x3c_b = [Buf() for _ in range(4)]
            x3c_s = [T.semc("x3c") for _ in range(4)]
            acc = [sb(f"acc{i}", [128, D], F32, stM) for i in range(4)]; acc_b = [Buf() for _ in range(4)]
            h2T = sb("h2Tm", [128, 8, 512], BF16, stM); h2T_b = [Buf() for _ in range(4)]
            h2Tf = sb("h2Tf", [128, 8, 128], F32, stM); h2Tf_b = Buf()
            AT = sb("ATm", [128, NF, 512], BF16, stM); AT_b = [Buf() for _ in range(NF)]
            sg = [sb(f"sgm{i}", [128, 512], BF16, stM) for i in range(2)]; sg_b = [Buf(), Buf()]
            tmp = sb("tmpm", [128, D], F32, stM); tmp_b = Buf()
            lgt = sb("lgt", [128, 8], F32, stM); mx = sb("mxm", [128, 8], F32, stM); dd = sb("ddm", [128, 2], F32, stM)
            eq = sb("eqm", [128, 2, 8], F32, stM)
            gts = sb("gts", [128, 4, 8], F32, stM); gts_b = [Buf() for _ in range(4)]
            gb = [Buf() for _ in range(5)]
            w13 = Ring(T, stM, nc, "w13", 3, [128, 2, 8, 256], BF16)
            w2r = Ring(T, stM, nc, "w2r", 3, [128, NF, 512], BF16)
            ps_tr = ps("ps_trM", [128, 1024], F32, stM); ps_tr_b = Buf()
            ps_g = [ps(f"ps_gM{i}", [128, 512], F32, stM) for i in range(2)]; ps_g_b = [Buf(), Buf()]
            ps_u = [ps(f"ps_uM{i}", [128, 512], F32, stM) for i in range(2)]; ps_u_b = [Buf(), Buf()]
            ps_o2 = [ps(f"ps_oM{i}", [128, 512], F32, stM) for i in range(2)]; ps_o2_b = [Buf(), Buf()]
            oc = 0
            if stop_after == "M0":
                T.barrier(); T.finish(); stM.close(); return nc
            for tcn in range(8):
                t0 = tcn * 4
                for il in range(4):
                    t = t0 + il
                    if stop_after == "M1" and il == 1:
                        dump("h2Tf", h2Tf[:].rearrange("p a b -> p (a b)"), [128, 1024], F32, [h2Tf_b])
                        dump("lgt", lgt[:], [128, 8], F32, [gb[0]])
                        T.barrier(); T.finish(); stM.close(); return nc
                    T.dma("sp", x3c_s[il], x3c[il][:], X3[t * 128:(t + 1) * 128, :], reads=[X3b[t]], writes=[x3c_b[il]])
                    make_hT(x3c[il], x3c_b[il], L, 2, 3, 0, ps_tr, ps_tr_b,
                            lambda k, il=il: h2T[:, k, il * 128:(il + 1) * 128], h2T_b[il],
                            lambda k: h2Tf[:, k, :], h2Tf_b)
                    def chk(stage):
                        if stop_after == "S" + str(il * 10 + stage):
                            T.barrier(); T.finish(); stM.close()
                            raise StopIteration
                    chk(1)
                    oi = oc % 2
                    oc += 1

                    def _r(oi=oi):
                        i = None
                        for k in range(8):
                            i = pe.matmul(ps_o2[oi][:, 0:8], lhsT=h2Tf[:, k, :], rhs=wr[:, k, :], start=(k == 0), stop=(k == 7))
                        return i
                    T.op("pe", _r, reads=[h2Tf_b, wr_b], writes=[ps_o2_b[oi]])
                    chk(2)
                    T.op("dve", lambda oi=oi: dve.tensor_copy(out=lgt[:], in_=ps_o2[oi][:, 0:8]), reads=[ps_o2_b[oi]], writes=[gb[0]])
                    chk(3)
                    T.op("dve", lambda: dve.max(out=mx[:], in_=lgt[:]), reads=[gb[0]], writes=[gb[1]], hard=True)
                    chk(4)
                    T.op("dve", lambda: dve.tensor_tensor(out=dd[:, 0:1], in0=mx[:, 0:1], in1=mx[:, 1:2], op=ALU.subtract),
                         reads=[gb[1]], writes=[gb[2]], hard=True)
                    T.op("act", lambda: act.activation(out=dd[:, 0:1], in_=dd[:, 0:1], func=AF.Sigmoid), reads=[gb[2]], writes=[gb[2]])

                    chk(5)

                    def _g1():
                        dve.tensor_scalar(out=dd[:, 1:2], in0=dd[:, 0:1], scalar1=-1.0, scalar2=1.0, op0=ALU.mult, op1=ALU.add)
                        dve.tensor_scalar(out=eq[:, 0, :], in0=lgt[:], scalar1=mx[:, 0:1], scalar2=None, op0=ALU.is_equal)
                        return dve.tensor_scalar(out=eq[:, 1, :], in0=lgt[:], scalar1=mx[:, 1:2], scalar2=None, op0=ALU.is_equal)
                    T.op("dve", _g1, reads=[gb[2], gb[1], gb[0]], writes=[gb[3]], hard=True)

                    chk(6)

                    def _g2(il=il):
                        dve.tensor_scalar(out=eq[:, 0, :], in0=eq[:, 0, :], scalar1=dd[:, 0:1], scalar2=None, op0=ALU.mult)
                        return dve.scalar_tensor_tensor(out=gts[:, il, :], in0=eq[:, 1, :], scalar=dd[:, 1:2], in1=eq[:, 0, :],
                                                        op0=ALU.mult, op1=ALU.add)
                    T.op("dve", _g2, reads=[gb[3]], writes=[gts_b[il], gb[4]], hard=True)
                    chk(7)
                if stop_after == "MG":
                    dump("gts", gts[:].rearrange("p a b -> p (a b)"), [128, 32], F32, gts_b)
                    T.barrier(); T.finish(); stM.close(); return nc
                for e in range(NEXP):
                    if stop_after == "ME" and e == 1:
                        dump("acc0", acc[0][:], [128, D], F32, [acc_b[0]])
                        T.barrier(); T.finish(); stM.close(); return nc
                    for fp in range(11):
                        wt_, wb2_, ws2_ = w13.next()
                        T.dma("sp", ws2_, wt_[:, 0, :, :], W1s[e, fp], reads=[W1s_b[e]], writes=[wb2_])
                        T.dma("sp", ws2_, wt_[:, 1, :, :], W3s[e, fp], reads=[W3s_b[e]], writes=[wb2_])
                        for f2 in range(2):
                            f = fp * 2 + f2
                            gi = f % 2

                            def _gu(f2=f2, gi=gi, wt_=wt_):
                                i = None
                                for k in range(8):
                                    i = pe.matmul(ps_g[gi][:, :], lhsT=wt_[:, 0, k, f2 * 128:(f2 + 1) * 128], rhs=h2T[:, k, :],
                                                  start=(k == 0), stop=(k == 7))
                                for k in range(8):
                                    i = pe.matmul(ps_u[gi][:, :], lhsT=wt_[:, 1, k, f2 * 128:(f2 + 1) * 128], rhs=h2T[:, k, :],
                                                  start=(k == 0), stop=(k == 7))
                                return i
                            T.op("pe", _gu, reads=[wb2_] + h2T_b, writes=[ps_g_b[gi], ps_u_b[gi]])
                            T.op("act", lambda gi=gi: act.activation(out=sg[gi][:, :], in_=ps_g[gi][:, :], func=AF.Silu),
                                 reads=[ps_g_b[gi]], writes=[sg_b[gi]])
                            T.op("dve", lambda gi=gi, f=f: dve.tensor_tensor(out=AT[:, f, :], in0=sg[gi][:, :], in1=ps_u[gi][:, :],
                                                                             op=ALU.mult),
                                 reads=[sg_b[gi], ps_u_b[gi]], writes=[AT_b[f]])
                    for n in range(2):
                        w2t, w2b, w2s = w2r.next()
                        T.dma("sp", w2s, w2t[:].rearrange("p f n -> p (f n)"), W2s[e, n].rearrange("p f n -> p (f n)"),
                              reads=[W2s_b[e]], writes=[w2b])
                        for il in range(4):
                            oi = oc % 2
                            oc += 1

                            def _o(il=il, oi=oi, w2t=w2t):
                                i = None
                                for f in range(NF):
                                    i = pe.matmul(ps_o2[oi][:, :], lhsT=AT[:, f, il * 128:(il + 1) * 128], rhs=w2t[:, f, :],
                                                  start=(f == 0), stop=(f == NF - 1))
                                return i
                            T.op("pe", _o, reads=AT_b + [w2b], writes=[ps_o2_b[oi]])
                            dst = acc[il][:, n * 512:(n + 1) * 512]
                            if e == 0:
                                T.op("dve", lambda oi=oi, il=il, dst=dst: dve.tensor_scalar(
                                    out=dst, in0=ps_o2[oi][:, :], scalar1=gts[:, il, 0:1], scalar2=None, op0=ALU.mult),
                                    reads=[ps_o2_b[oi], gts_b[il]], writes=[acc_b[il]])
                            else:
                                T.op("dve", lambda oi=oi, il=il, dst=dst, e=e: dve.scalar_tensor_tensor(
                                    out=dst, in0=ps_o2[oi][:, :], scalar=gts[:, il, e:e + 1], in1=dst, op0=ALU.mult, op1=ALU.add),
                                    reads=[ps_o2_b[oi], gts_b[il]], writes=[acc_b[il]])
                for il in range(4):
                    t = t0 + il
                    resid_ln([acc[il][:, 0:512], acc[il][:, 512:1024]], [acc_b[il]], x3c[il], x3c_b[il],
                             gate[(0, 1)], gate_b[(0, 1)], lg, lgb, lb, lbb, tmp, tmp_b)
                    T.dma("sp", x3c_s[il], out_ap[t * 128:(t + 1) * 128, :], x3c[il][:], reads=[x3c_b[il]], is_output=True)
    T.barrier()
    T.finish()
    es.close()
    return nc


_PROG = {}


def _get_prog(n_layers=2, debug=False):
    key = (n_layers, debug)
    if key not in _PROG:
        _PROG[key] = build_program(n_layers, debug)
    return _PROG[key]


L1_KEYS = ("od_w_in", "od_fgain", "od_bias", "od_router", "od_w1", "od_w3", "od_w2", "C64", "S64", "CN", "SN")


def make_in_maps(inp, n_cores=N_CORES, n_layers=2):
    cs = _consts()
    f32 = lambda a: np.ascontiguousarray(np.asarray(a, dtype=np.float32))
    x = f32(inp["x"]); c = f32(inp["c"]); ctx = f32(inp["ctx"]); c_ctx = f32(inp["c_ctx"])
    ada_b = f32(inp["ada_b"])
    adab_col = np.ascontiguousarray(ada_b.reshape(2, 48, 128).transpose(0, 2, 1))
    qk_gain = np.concatenate([np.tile(f32(inp["ev_q_gain"])[0], 12), np.tile(f32(inp["ev_k_gain"])[0], 4)])
    rpb = f32(inp["od_rpb"])[0]
    rpb_ext = np.concatenate([rpb.reshape(12, -1), np.full((12, 1), NEG, np.float32)], axis=1)
    od_bias = np.ascontiguousarray(rpb_ext[:, cs["naidx"]])
    shared = dict(
        ada_w=f32(inp["ada_w"]), ada_b=ada_b, adab_col=adab_col,
        ln_mix_g=f32(inp["ln_mix_g"]), ln_mix_b=f32(inp["ln_mix_b"]),
        ln_ffn_g=f32(inp["ln_ffn_g"]), ln_ffn_b=f32(inp["ln_ffn_b"]),
        w_out=f32(inp["w_out"]), ev_w_in=f32(inp["ev_w_in"])[0], ev_pool_w=f32(inp["ev_pool_w"])[0],
        ev_pool_scale=f32(inp["ev_pool_scale"])[0], ev_qk_gain=f32(qk_gain),
        ev_ffn_w1=f32(inp["ev_ffn_w1"])[0], ev_ffn_w3=f32(inp["ev_ffn_w3"])[0], ev_ffn_w2=f32(inp["ev_ffn_w2"])[0],
        od_w_in=f32(inp["od_w_in"])[0], od_fgain=f32(inp["od_fourier_gain"])[0].reshape(256),
        od_bias=od_bias, od_router=f32(inp["od_router"])[0],
        od_w1=f32(inp["od_exp_w1"])[0], od_w3=f32(inp["od_exp_w3"])[0], od_w2=f32(inp["od_exp_w2"])[0],
        ropeC=cs["ropeC"], ropeS=cs["ropeS"], band=cs["band"], C64=cs["C64"], S64=cs["S64"],
        CN=cs["CN"], SN=cs["SN"], identf=cs["identf"],
    )
    if n_layers < 2:
        for k in L1_KEYS:
            shared.pop(k)
    maps = []
    for b in range(n_cores):
        cv = np.stack([c[b].reshape(8, 128).T, c_ctx.reshape(8, 128).T], axis=-1)
        m = dict(shared)
        m.update(x=x[b], ctx=ctx[b], cvec=np.ascontiguousarray(cv.astype(np.float32)))
        maps.append(m)
    return maps


def kernel(**inputs):
    nc = _get_prog(2, False)
    maps = make_in_maps(inputs)
    res = run_bass_kernel_spmd(nc, maps, core_ids=list(range(N_CORES)))
    return np.stack([np.asarray(r["out"], dtype=np.float32) for r in res.results], axis=0)
```

```python
import contextlib
import math
import numpy as np
import ml_dtypes
import concourse.bass as bass
import concourse.mybir as mybir
from concourse.bass_utils import run_bass_kernel_spmd

F32 = mybir.dt.float32
BF16 = mybir.dt.bfloat16
AF = mybir.ActivationFunctionType
ALU = mybir.AluOpType
AX = mybir.AxisListType

D = 1024
SEQ = 4096
CTXL = 256
NT = 34
NTL = 32
FFN = 2816
NF = 22
NEXP = 8
ALPHA = 4.0 ** 0.25
LN_EPS = 1e-6
RMS_EPS = 1e-6
NEG = -30000.0
N_CORES = 8


class Buf:
    __slots__ = ("name", "w", "r")

    def __init__(self, name=""):
        self.name = name
        self.w = None
        self.r = {}


class SemC:
    __slots__ = ("sem", "cnt")

    def __init__(self, sem):
        self.sem = sem
        self.cnt = 0


class Tracker:
    def __init__(self, nc, es):
        self.nc = nc
        self.es = es
        self.E = {"pe": nc.tensor, "act": nc.scalar, "dve": nc.vector, "pool": nc.gpsimd, "sp": nc.sync}
        self.esem = {}
        self.ecnt = {}
        self.own = {k: set() for k in self.E}
        self.seen = {k: {} for k in self.E}
        self.nsem = 0
        self.dsem = {}
        for k in self.E:
            self._new_esem(k)
        self.out_events = []

    def new_sem(self, name):
        self.nsem += 1
        return self.es.enter_context(self.nc.semaphore(f"{name}_{self.nsem}"))

    def semc(self, name="d"):
        sc = SemC(self.new_sem(name))
        self.dsem[sc.sem.num] = sc
        return sc

    def _new_esem(self, k):
        s = self.new_sem("e" + k)
        self.esem[k] = s
        self.ecnt[k] = 0
        self.own[k].add(s.num)

    def _wait_all(self, eng, evs, allow_own=False):
        best = {}
        for ev in evs:
            if ev is None:
                continue
            s, v = ev
            if s.num in self.own[eng] and not allow_own:
                continue
            if s.num not in best or best[s.num][1] < v:
                best[s.num] = (s, v)
        for num, (s, v) in best.items():
            if num in self.dsem:
                v = self.dsem[num].cnt
            if self.seen[eng].get(num, 0) < v:
                self.E[eng].wait_ge(s, v)
                self.seen[eng][num] = v

    def _collect(self, reads, writes, skip_num=None):
        evs = []
        for b in reads:
            evs.append(b.w)
        for b in writes:
            if b.w is not None and (skip_num is None or b.w[0].num != skip_num):
                evs.append(b.w)
            evs.extend(b.r.values())
        return evs

    def _update(self, ev, reads, writes):
        for b in reads:
            old = b.r.get(ev[0].num)
            if old is None or old[1] < ev[1]:
                b.r[ev[0].num] = ev
        for b in writes:
            b.w = ev
            b.r = {}

    def op(self, eng, fn, reads=(), writes=(), hard=False):
        self._wait_all(eng, self._collect(reads, writes), allow_own=hard)
        inst = fn()
        own = self.esem[eng]
        self.ecnt[eng] += 1
        inst.then_inc(own, 1)
        ev = (own, self.ecnt[eng])
        self._update(ev, reads, writes)
        if self.ecnt[eng] >= 12000:
            self._new_esem(eng)
        return ev

    def dma(self, q, sc, out, in_, reads=(), writes=(), is_output=False, **kw):
        self._wait_all(q, self._collect(reads, writes, skip_num=sc.sem.num))
        inst = self.E[q].dma_start(out=out, in_=in_, **kw)
        sc.cnt += 16
        inst.then_inc(sc.sem, 16)
        ev = (sc.sem, sc.cnt)
        self._update(ev, reads, writes)
        if is_output:
            self.out_events.append(ev)
        return ev

    def barrier(self):
        evs = []
        for k in self.E:
            if self.ecnt[k] > 0:
                evs.append((self.esem[k], self.ecnt[k]))
        for sc in self.dsem.values():
            if sc.cnt > 0:
                evs.append((sc.sem, sc.cnt))
        for k in self.E:
            self._wait_all(k, evs)

    def finish(self):
        self._wait_all("sp", self.out_events)


class Ring:
    def __init__(self, T, es, nc, name, n, shape, dtype):
        self.n = n
        self.t = [es.enter_context(nc.sbuf_tensor(f"r_{name}{i}", shape, dtype)) for i in range(n)]
        self.b = [Buf(f"{name}{i}") for i in range(n)]
        self.s = [T.semc(name) for i in range(n)]
        self.i = 0

    def next(self):
        k = self.i % self.n
        self.i += 1
        return self.t[k], self.b[k], self.s[k]


def _rope_tables():
    t = np.arange(SEQ)
    row = (t // 64).astype(np.float32)
    col = (t % 64).astype(np.float32)
    inv = np.power(np.float32(10000.0), -np.arange(16, dtype=np.float32) / np.float32(16)).astype(np.float32)
    ang = np.stack([row[:, None] * inv, col[:, None] * inv], axis=1).astype(np.float32)
    cs, sn = np.cos(ang).astype(np.float32), np.sin(ang).astype(np.float32)
    C = np.zeros((SEQ, 2, 2, 16), np.float32)
    S = np.zeros((SEQ, 2, 2, 16), np.float32)
    C[:, :, 0, :] = cs
    C[:, :, 1, :] = cs
    S[:, :, 0, :] = -sn
    S[:, :, 1, :] = sn
    C = C.reshape(NTL, 128, 64).transpose(1, 0, 2)
    S = S.reshape(NTL, 128, 64).transpose(1, 0, 2)
    return np.ascontiguousarray(C), np.ascontiguousarray(S)


def _band_mats():
    out = np.zeros((4, 5, 128, 128), np.float32)
    n = 1024
    for g, w in enumerate((2, 4, 8, 16)):
        B = np.zeros((n, n), np.float64)
        for t in range(n):
            lo = min(max(t - w // 2, 0), n)
            hi = min(max(t - w // 2 + w, 0), n)
            B[lo:hi, t] += 1.0 / (hi - lo)
            B[t, t] -= 1.0
        i = 3
        out[g, 0] = B[(i - 1) * 128:i * 128, i * 128:(i + 1) * 128]
        out[g, 1] = B[i * 128:(i + 1) * 128, i * 128:(i + 1) * 128]
        out[g, 2] = B[(i + 1) * 128:(i + 2) * 128, i * 128:(i + 1) * 128]
        out[g, 3] = B[0:128, 0:128]
        out[g, 4] = B[n - 128:n, n - 128:n]
    return np.ascontiguousarray(out.transpose(2, 0, 1, 3)).astype(ml_dtypes.bfloat16)


def _dft_consts():
    c = np.arange(64)
    ang = 2.0 * np.pi * ((c[:, None] * c[None, :]) % 64) / 64.0
    C64 = np.zeros((128, 128), np.float64)
    S64 = np.zeros((128, 128), np.float64)
    for a in range(2):
        C64[a * 64:(a + 1) * 64, a * 64:(a + 1) * 64] = np.cos(ang)
        S64[a * 64:(a + 1) * 64, a * 64:(a + 1) * 64] = np.sin(ang)
    s = np.arange(SEQ, dtype=np.int64)
    m = (s[:, None] * s[None, :]) % SEQ
    angn = (2.0 * np.pi / SEQ) * m
    CN = (np.cos(angn) / 512.0).astype(ml_dtypes.bfloat16)
    SN = (-np.sin(angn) / 512.0).astype(ml_dtypes.bfloat16)
    return C64.astype(ml_dtypes.bfloat16), S64.astype(ml_dtypes.bfloat16), CN, SN


def _na_index():
    MASK = 15 * 31
    idx = np.full((3, 8, 128, 512), MASK, np.int64)
    for bt, qb in enumerate((0, 3, 7)):
        for slot in range(8):
            kt = 4 * qb - 2 + slot
            if kt < 0 or kt >= 32:
                continue
            for krl in range(2):
                kr = 2 * kt + krl
                for qrl in range(8):
                    qr = 8 * qb + qrl
                    rs = min(max(qr - 4, 0), 56)
                    if not (rs <= kr < rs + 8):
                        continue
                    dr = kr - qr + 7
                    qc = np.arange(64)
                    cs = np.clip(qc - 8, 0, 48)
                    kc = np.arange(64)
                    valid = (kc[:, None] >= cs[None, :]) & (kc[:, None] < cs[None, :] + 16)
                    dc = kc[:, None] - qc[None, :] + 15
                    blk = np.where(valid, dr * 31 + dc, MASK)
                    idx[bt, slot, krl * 64:(krl + 1) * 64, qrl * 64:(qrl + 1) * 64] = blk
    return idx


_CONST_CACHE = {}


def _consts():
    if not _CONST_CACHE:
        C, S = _rope_tables()
        C64, S64, CN, SN = _dft_consts()
        _CONST_CACHE.update(dict(
            ropeC=C, ropeS=S, band=_band_mats(), C64=C64, S64=S64, CN=CN, SN=SN,
            identf=np.eye(128, dtype=np.float32), naidx=_na_index()))
    return _CONST_CACHE


def build_program(n_layers=2, debug=False, stop_after=None):
    global _LAST_NC
    nc = bass.Bass("TRN2", target_bir_lowering=False)
    _LAST_NC = nc
    es = contextlib.ExitStack()

    def din(name, shape, dt=F32):
        return nc.dram_tensor(name, list(shape), dt, kind="ExternalInput").ap()

    def dscr(name, shape, dt=F32):
        return nc.dram_tensor(name, list(shape), dt, kind="Internal").ap()

    x_in = din("x", [SEQ, D])
    ctx_in = din("ctx", [CTXL, D])
    cvec = din("cvec", [128, 8, 2])
    ada_w = din("ada_w", [2, D, 6 * D])
    ada_b = din("ada_b", [2, 6 * D])
    adab_col = din("adab_col", [2, 128, 48])
    ln_mix_g = din("ln_mix_g", [2, D]); ln_mix_b = din("ln_mix_b", [2, D])
    ln_ffn_g = din("ln_ffn_g", [2, D]); ln_ffn_b = din("ln_ffn_b", [2, D])
    w_out = din("w_out", [2, D, D])
    ev_w_in = din("ev_w_in", [D, 1536])
    ev_pool_w = din("ev_pool_w", [4, 64, 64])
    ev_pool_scale = din("ev_pool_scale", [256])
    ev_qk_gain = din("ev_qk_gain", [1024])
    ev_ffn_w1 = din("ev_ffn_w1", [D, FFN]); ev_ffn_w3 = din("ev_ffn_w3", [D, FFN]); ev_ffn_w2 = din("ev_ffn_w2", [FFN, D])
    if n_layers == 2:
        od_w_in = din("od_w_in", [D, 2560])
        od_fgain = din("od_fgain", [256])
        od_bias = din("od_bias", [12, 3, 8, 128, 512])
        od_router = din("od_router", [D, 8])
        od_w1 = din("od_w1", [NEXP, D, FFN]); od_w3 = din("od_w3", [NEXP, D, FFN]); od_w2 = din("od_w2", [NEXP, FFN, D])
    ropeC = din("ropeC", [128, NTL, 64]); ropeS = din("ropeS", [128, NTL, 64])
    band = din("band", [128, 4, 5, 128], BF16)
    if n_layers == 2:
        C64 = din("C64", [128, 128], BF16); S64 = din("S64", [128, 128], BF16)
        CN = din("CN", [SEQ, SEQ], BF16); SN = din("SN", [SEQ, SEQ], BF16)
    identf_in = din("identf", [128, 128])

    out_ap = nc.dram_tensor("out", [SEQ, D], F32, kind="ExternalOutput").ap()
    dbg = {}
    if debug:
        for nm, shp in (("dbg_x1", [NT * 128, D]), ("dbg_x2", [NT * 128, D]), ("dbg_x3", [SEQ, D])):
            dbg[nm] = nc.dram_tensor(nm, shp, F32, kind="ExternalOutput").ap()

    X1 = dscr("X1", [NT * 128, D]) if not debug else dbg["dbg_x1"]
    X2 = dscr("X2", [NT * 128, D]) if not debug else dbg["dbg_x2"]
    X3 = dscr("X3", [SEQ, D]) if not debug else dbg["dbg_x3"]
    X1b = [Buf(f"X1_{i}") for i in range(NT)]
    X2b = [Buf(f"X2_{i}") for i in range(NT)]
    X3b = [Buf(f"X3_{i}") for i in range(NTL)]

    T = Tracker(nc, es)

    def dump(name, ap, shape, dt, reads):
        if not debug:
            return
        d = nc.dram_tensor("dbg_" + name, list(shape), dt, kind="ExternalOutput").ap()
        T.dma("sp", T.semc("dbg"), d, ap, reads=reads, is_output=True)
    pe, act, dve, pool, sp = nc.tensor, nc.scalar, nc.vector, nc.gpsimd, nc.sync

    def sb(name, shape, dt, stack=None):
        return (stack or es).enter_context(nc.sbuf_tensor("s_" + name, list(shape), dt))

    def ps(name, shape, dt, stack):
        return stack.enter_context(nc.psum_tensor("p_" + name, list(shape), dt))

    def x_src(t):
        return x_in[t * 128:(t + 1) * 128, :] if t < NTL else ctx_in[(t - NTL) * 128:(t - NTL + 1) * 128, :]

    identf = sb("identf", [128, 128], F32); identf_b = Buf()
    identb = sb("identb", [128, 128], BF16); identb_b = Buf()
    onesf = sb("onesf", [128, 64], F32); ones_b = Buf()
    cst_s = T.semc("cst")
    T.dma("sp", cst_s, identf[:], identf_in[:, :], writes=[identf_b])
    T.dma("pool", cst_s, identb[:], identf_in[:, :], writes=[identb_b])
    T.op("dve", lambda: dve.memset(onesf[:], 1.0), writes=[ones_b])

    modcol = [sb(f"modcol{l}", [128, 4, 8, 2], F32) for l in range(2)]
    modcol_b = [Buf() for l in range(2)]
    epsc = sb("epsc", [128, 2], F32); eps_b = Buf()

    def _eps():
        dve.memset(epsc[:, 0:1], LN_EPS)
        return dve.memset(epsc[:, 1:2], RMS_EPS)
    T.op("dve", _eps, writes=[eps_b])
    AD = dscr("AD", [NT * 128, 256], BF16)
    AD_b = [Buf() for _ in range(NT)]

    def load_lnp(l, k, stack, sc):
        src = (ln_mix_g, ln_mix_b, ln_ffn_g, ln_ffn_b)[k]
        t = sb(f"lnp{l}{k}", [128, D], F32, stack)
        b = Buf()
        T.dma("sp", sc, t[:], src[l:l + 1, :].to_broadcast([128, D]), writes=[b])
        return t, b

    def compute_mod(l, lstack, mstack):
        nstream = 2 if l == 0 else 1
        gate = {}
        gate_b = {}
        for g in (1, 0):
            for s in range(nstream):
                gate[(s, g)] = sb(f"gate{l}{s}{g}", [128, D], F32, mstack if g == 0 else lstack)
                gate_b[(s, g)] = Buf()
        with contextlib.ExitStack() as st:
            cc = sb(f"cc{l}", [128, 8, 2], F32, st); cc_b = Buf()
            ccrep = sb(f"ccrep{l}", [128, 8, 2, 128], F32, st); ccrep_b = Buf()
            abcol = sb(f"abcol{l}", [128, 48], F32, st); abcol_b = Buf()
            abrow = sb(f"abrow{l}", [128, 2048], F32, st); abrow_b = Buf()
            ms_ = T.semc("mod")
            T.dma("sp", ms_, cc[:], cvec[:, :, :], writes=[cc_b])
            T.dma("sp", ms_, abcol[:], adab_col[l, :, :], writes=[abcol_b])
            for gi, seg in enumerate((2, 5)):
                T.dma("sp", ms_, abrow[:, gi * 1024:(gi + 1) * 1024],
                      ada_b[l:l + 1, seg * 1024:(seg + 1) * 1024].to_broadcast([128, 1024]), writes=[abrow_b])
            T.op("act", lambda: act.activation(out=cc[:], in_=cc[:], func=AF.Silu), reads=[cc_b], writes=[cc_b])

            def _rep():
                i = None
                for k in range(8):
                    for s in range(2):
                        i = dve.tensor_copy(out=ccrep[:, k, s, :], in_=cc[:, k, s:s + 1].to_broadcast([128, 128]))
                return i
            T.op("dve", _rep, reads=[cc_b], writes=[ccrep_b])
            wring = Ring(T, st, nc, f"adaw{l}", 2, [128, 8, 512], F32)
            ps_c = ps(f"ps_c{l}", [128, 512], F32, st); ps_c_b = Buf()
            ps_g = [ps(f"ps_g{l}{i}", [128, 512], F32, st) for i in range(2)]; ps_g_b = [Buf(), Buf()]
            for j in range(12):
                seg, hf = j // 2, j % 2
                wt, wb, ws = wring.next()
                T.dma("sp", ws, wt[:], ada_w[l, :, j * 512:(j + 1) * 512].rearrange("(k p) n -> p k n", p=128),
                      writes=[wb])
                if seg in (2, 5):
                    gi = 0 if seg == 2 else 1
                    for s in range(nstream):
                        def _mm(s=s, wt=wt):
                            i = None
                            for k in range(8):
                                i = pe.matmul(ps_g[s][:, :], lhsT=ccrep[:, k, s, :], rhs=wt[:, k, :],
                                              start=(k == 0), stop=(k == 7))
                            return i
                        T.op("pe", _mm, reads=[ccrep_b, wb], writes=[ps_g_b[s]])
                        T.op("dve", lambda s=s, gi=gi, hf=hf: dve.tensor_tensor(
                            out=gate[(s, gi)][:, hf * 512:(hf + 1) * 512], in0=ps_g[s][:, :],
                            in1=abrow[:, gi * 1024 + hf * 512: gi * 1024 + (hf + 1) * 512], op=ALU.add),
                            reads=[ps_g_b[s], abrow_b], writes=[gate_b[(s, gi)]])
                else:
                    vi = {0: 0, 1: 1, 3: 2, 4: 3}[seg]
                    for c4 in range(4):
                        def _mm(c4=c4, wt=wt):
                            i = None
                            for k in range(8):
                                i = pe.matmul(ps_c[:, 0:2], lhsT=wt[:, k, c4 * 128:(c4 + 1) * 128], rhs=cc[:, k, :],
                                              start=(k == 0), stop=(k == 7))
                            return i
                        T.op("pe", _mm, reads=[cc_b, wb], writes=[ps_c_b])
                        chunk = hf * 4 + c4
                        colidx = seg * 8 + chunk
                        T.op("dve", lambda vi=vi, chunk=chunk, colidx=colidx: dve.tensor_scalar(
                            out=modcol[l][:, vi, chunk, :], in0=ps_c[:, 0:2],
                            scalar1=abcol[:, colidx:colidx + 1], scalar2=(1.0 if vi in (1, 3) else 0.0),
                            op0=ALU.add, op1=ALU.add),
                            reads=[ps_c_b, abcol_b], writes=[modcol_b[l]])
        T.barrier()
        if l == 0:
            dump("modcol", modcol[0][:].rearrange("p a b c -> p (a b c)"), [128, 64], F32, [modcol_b[0]])
            dump("gate00", gate[(0, 0)][:], [128, D], F32, [gate_b[(0, 0)]])
        return gate, gate_b

    def make_hT(xt, xb, l, vsh, vsc, stream, ps_tr, ps_tr_b, hT_ap, hT_b, hTf_ap=None, hTf_b=None):
        def _tr():
            i = None
            for k in range(8):
                i = pe.transpose(ps_tr[:, k * 128:(k + 1) * 128], xt[:, k * 128:(k + 1) * 128], identf[:])
            return i
        T.op("pe", _tr, reads=[xb, identf_b], writes=[ps_tr_b])

        def _ev():
            i = None
            for k in range(8):
                i = dve.tensor_scalar(out=hT_ap(k), in0=ps_tr[:, k * 128:(k + 1) * 128],
                                      scalar1=modcol[l][:, vsc, k, stream:stream + 1],
                                      scalar2=modcol[l][:, vsh, k, stream:stream + 1], op0=ALU.mult, op1=ALU.add)
            return i
        T.op("dve", _ev, reads=[ps_tr_b, modcol_b[l]], writes=[hT_b])
        if hTf_ap is not None:
            def _ev2():
                i = None
                for k in range(8):
                    i = dve.tensor_scalar(out=hTf_ap(k), in0=ps_tr[:, k * 128:(k + 1) * 128],
                                          scalar1=modcol[l][:, vsc, k, stream:stream + 1],
                                          scalar2=modcol[l][:, vsh, k, stream:stream + 1], op0=ALU.mult, op1=ALU.add)
                return i
            T.op("dve", _ev2, reads=[ps_tr_b, modcol_b[l]], writes=[hTf_b])

    lnw = {}
    lnw["st"] = sb("ln_st", [128, 2, 6], F32); lnw["mv"] = sb("ln_mv", [128, 2], F32)
    lnw["sd"] = sb("ln_sd", [128, 1], F32); lnw["rs"] = sb("ln_rs", [128, 1], F32)
    lnw["b0"] = Buf(); lnw["b1"] = Buf(); lnw["b2"] = Buf(); lnw["b3"] = Buf()

    def resid_ln(ps_halves, ps_bufs, zt, zb, gate_t, gate_bf, lg, lgb, lb, lbb, tmp, tmp_b):
        stt, mv, sd, rs = lnw["st"], lnw["mv"], lnw["sd"], lnw["rs"]

        def _a():
            for h in range(2):
                dve.tensor_tensor(out=tmp[:, h * 512:(h + 1) * 512], in0=ps_halves[h],
                                  in1=gate_t[:, h * 512:(h + 1) * 512], op=ALU.mult)
            dve.scalar_tensor_tensor(out=zt[:], in0=zt[:], scalar=ALPHA, in1=tmp[:], op0=ALU.mult, op1=ALU.add)
            i = None
            for h in range(2):
                i = dve.bn_stats(out=stt[:, h, :], in_=zt[:, h * 512:(h + 1) * 512])
            return i
        T.op("dve", _a, reads=list(ps_bufs) + [gate_bf], writes=[zb, tmp_b, lnw["b0"]])
        T.op("dve", lambda: dve.bn_aggr(out=mv[:], in_=stt[:].rearrange("p a b -> p (a b)")),
             reads=[lnw["b0"]], writes=[lnw["b1"]], hard=True)
        T.op("act", lambda: act.activation(out=sd[:], in_=mv[:, 1:2], func=AF.Sqrt, bias=epsc[:, 0:1], scale=1.0),
             reads=[lnw["b1"], eps_b], writes=[lnw["b2"]])
        T.op("dve", lambda: dve.reciprocal(out=rs[:], in_=sd[:]), reads=[lnw["b2"]], writes=[lnw["b3"]])

        def _b():
            dve.tensor_scalar(out=zt[:], in0=zt[:], scalar1=mv[:, 0:1], scalar2=rs[:, 0:1],
                              op0=ALU.subtract, op1=ALU.mult)
            dve.tensor_tensor(out=zt[:], in0=zt[:], in1=lg[:], op=ALU.mult)
            return dve.tensor_tensor(out=zt[:], in0=zt[:], in1=lb[:], op=ALU.add)
        T.op("dve", _b, reads=[lnw["b3"], lnw["b1"], lgb, lbb], writes=[zb], hard=True)

    def rms_heads(src_ap, nh, sq, ms, rt, dst_ap, wbuf, reads, gain_t=None, gain_b=None):
        T.op("act", lambda: act.activation(out=sq[:, 0:nh * 64], in_=src_ap, func=AF.Square),
             reads=reads, writes=[wbuf["sq"]])
        T.op("dve", lambda: dve.tensor_reduce(out=ms[:, 0:nh], in_=sq[:, 0:nh * 64].rearrange("p (h d) -> p h d", d=64),
                                              axis=AX.X, op=ALU.add),
             reads=[wbuf["sq"]], writes=[wbuf["ms"]])
        T.op("act", lambda: act.activation(out=rt[:, 0:nh], in_=ms[:, 0:nh], func=AF.Sqrt, bias=epsc[:, 1:2],
                                           scale=1.0 / 64.0),
             reads=[wbuf["ms"], eps_b], writes=[wbuf["rt"]])

        T.op("dve", lambda: dve.reciprocal(out=rt[:, 0:nh], in_=rt[:, 0:nh]), reads=[wbuf["rt"]], writes=[wbuf["rt"]])

        def _n():
            i = dve.tensor_tensor(out=dst_ap, in0=src_ap.rearrange("p (h d) -> p h d", d=64),
                                  in1=rt[:, 0:nh].unsqueeze(2).to_broadcast([128, nh, 64]), op=ALU.mult)
            if gain_t is not None:
                i = dve.tensor_tensor(out=dst_ap, in0=dst_ap, in1=gain_t, op=ALU.mult)
            return i
        T.op("dve", _n, reads=list(reads) + [wbuf["rt"]] + ([gain_b] if gain_b else []), writes=[wbuf["dst"]], hard=True)

    acnt = [0]

    asteps = []
    apre = []

    def attn_head(qT_ap, kT_fn, v_fn, key_list, ps_s, ps_s_b, pT_ring, ps_o, ps_o_b, nq, reads_q, bias_fn=None,
                  pre=None, post=None):
        nk = len(key_list)
        hseq = len(apre)
        apre.append(pre)
        for i, kt in enumerate(key_list):
            sidx = acnt[0] % 2
            acnt[0] += 1

            def s_fn(kt=kt, sidx=sidx):
                kap, kb = kT_fn(kt)
                bi = bias_fn(kt) if bias_fn is not None else None

                def _s():
                    ins = pe.matmul(ps_s[sidx][:, 0:nq], lhsT=kap, rhs=qT_ap, start=True, stop=(bi is None))
                    if bi is not None:
                        ins = pe.matmul(ps_s[sidx][:, 0:nq], lhsT=identb[:], rhs=bi[0], start=False, stop=True)
                    return ins
                T.op("pe", _s, reads=[kb] + list(reads_q) + ([bi[1], identb_b] if bi else []), writes=[ps_s_b[sidx]])

            def epv_fn(kt=kt, sidx=sidx, i=i):
                pt, pb, _ = pT_ring.next()
                T.op("act", lambda: act.activation(out=pt[:, 0:nq], in_=ps_s[sidx][:, 0:nq], func=AF.Exp),
                     reads=[ps_s_b[sidx]], writes=[pb])
                vap, vb = v_fn(kt)
                T.op("pe", lambda: pe.matmul(ps_o[0:65, 0:nq], lhsT=vap, rhs=pt[:, 0:nq],
                                             start=(i == 0), stop=(i == nk - 1)),
                     reads=[vb, pb], writes=[ps_o_b])
            asteps.append((s_fn, epv_fn, post if i == nk - 1 else None, hseq))

    def attn_flush(lookahead=2):
        n = len(asteps)
        done_pre = [0]

        def run_pre(upto):
            while done_pre[0] <= min(upto, len(apre) - 1):
                f = apre[done_pre[0]]
                if f is not None:
                    f()
                done_pre[0] += 1
        pend = []
        for j in range(n):
            run_pre(asteps[j][3] + lookahead)
            if j == 0:
                asteps[0][0]()
            if j + 1 < n:
                run_pre(asteps[j + 1][3])
                asteps[j + 1][0]()
            asteps[j][1]()
            while pend and pend[0][0] <= j:
                pend.pop(0)[1]()
            if asteps[j][2] is not None:
                pend.append((j + 3, asteps[j][2]))
        for _, f in pend:
            f()
        asteps.clear()
        apre.clear()

    def attn_norm(ps_o, ps_o_b, osb, osb_b, ps_bc, ps_bc_b, dst_ap, dst_b, nq):
        def _c():
            dve.tensor_copy(out=osb[0:65, 0:nq], in_=ps_o[0:65, 0:nq])
            return dve.reciprocal(out=osb[64:65, 0:nq], in_=osb[64:65, 0:nq])
        T.op("dve", _c, reads=[ps_o_b], writes=[osb_b])
        T.op("pe", lambda: pe.matmul(ps_bc[0:64, 0:nq], lhsT=onesf[64:65, 0:64], rhs=osb[64:65, 0:nq],
                                     start=True, stop=True),
             reads=[osb_b, ones_b], writes=[ps_bc_b])
        T.op("dve", lambda: dve.tensor_tensor(out=dst_ap, in0=osb[0:64, 0:nq], in1=ps_bc[0:64, 0:nq], op=ALU.mult),
             reads=[osb_b, ps_bc_b], writes=[dst_b])

    def x_src(t):
        return x_in[t * 128:(t + 1) * 128, :] if t < NTL else ctx_in[(t - NTL) * 128:(t - NTL + 1) * 128, :]

    def qhead_pos(h):
        return (h % 3) + 3 * (h // 6), (h // 3) % 2

    L = 0
    with contextlib.ExitStack() as stL0:
        stR = contextlib.ExitStack()
        gate, gate_b = compute_mod(0, stL0, stR)
        QT = sb("QT", [128, 6, SEQ + CTXL], BF16, stR); QT_b = [Buf() for _ in range(NT)]
        KT = sb("KT", [128, 2, SEQ + CTXL], BF16, stR); KT_b = [Buf() for _ in range(NT)]
        VA = sb("VA", [128, NT, 4, 66], BF16, stR); VA_b = [Buf() for _ in range(NT)]
        bandt = sb("bandt", [128, 4, 5, 128], BF16, stR); band_b = Buf()
        poolw = sb("poolw", [64, 4, 64], BF16, stR); poolw_b = Buf()
        w0_s = T.semc("w0")
        T.op("dve", lambda: dve.memset(VA[:].rearrange("p t k d -> p (t k) d")[:, :, 64:65], 1.0), writes=VA_b)
        T.dma("sp", w0_s, bandt[:], band[:, :, :, :], writes=[band_b])

        with contextlib.ExitStack() as stA:
            win = sb("win0", [128, 8, 1536], BF16, stA); win_b = Buf()
            T.dma("pool", w0_s, win[:], ev_w_in.rearrange("(k p) n -> p k n", p=128), writes=[win_b])
            rring = Ring(T, stA, nc, "rope", 2, [128, 2, 64], F32)
            gaint = sb("gaint", [128, 16, 64], F32, stA); gain_b = Buf()
            T.dma("sp", w0_s, gaint[:].rearrange("p h d -> p (h d)"),
                  ev_qk_gain.rearrange("(o n) -> o n", o=1).to_broadcast([128, 1024]), writes=[gain_b])
            T.op("dve", lambda: dve.tensor_scalar(out=gaint[:, 0:12, :], in0=gaint[:, 0:12, :], scalar1=0.125,
                                                  scalar2=None, op0=ALU.mult), reads=[gain_b], writes=[gain_b])
            pwf = sb("pwf", [64, 4, 64], F32, stA); pws = sb("pws", [64, 4, 64], F32, stA); pw_b = Buf()
            T.dma("sp", w0_s, pwf[:], ev_pool_w.rearrange("g c d -> c g d"), writes=[pw_b])
            T.dma("sp", w0_s, pws[:].rearrange("p g d -> p (g d)"),
                  ev_pool_scale.rearrange("(o n) -> o n", o=1).to_broadcast([64, 256]), writes=[pw_b])
            T.op("dve", lambda: dve.tensor_tensor(out=poolw[:], in0=pwf[:], in1=pws[:], op=ALU.mult),
                 reads=[pw_b], writes=[poolw_b])

            xring = Ring(T, stA, nc, "xa", 2, [128, D], F32)
            aring = Ring(T, stA, nc, "aa", 2, [128, 256], BF16)
            hT = sb("hTa", [128, 8, 128], BF16, stA); hT_b = Buf()
            ms = sb("msa", [128, 16], F32, stA); rt = sb("rta", [128, 16], F32, stA)
            qn = sb("qna", [128, 16, 64], F32, stA)
            t1 = sb("t1a", [128, 16, 64], F32, stA); t2 = sb("t2a", [128, 16, 64], F32, stA)
            sq = t2[:].rearrange("p h d -> p (h d)")
            stg = sb("stga", [128, 1024], BF16, stA)
            t12_b = Buf(); stg_b = Buf()
            wb_ = {"sq": t12_b, "ms": Buf(), "rt": Buf(), "dst": Buf()}
            ps_tr = ps("ps_trA", [128, 1024], F32, stA); ps_tr_b = Buf()
            ps_pj = ps("ps_pjA", [128, 1536], F32, stA); ps_pj_b = Buf()
            ps_t2 = ps("ps_t2A", [128, 1024], BF16, stA); ps_t2_b = Buf()
            for t in range(NT):
                stream = 0 if t < NTL else 1
                xt, xb, xs = xring.next()
                T.dma("sp", xs, xt[:], x_src(t), writes=[xb])
                make_hT(xt, xb, L, 0, 1, stream, ps_tr, ps_tr_b, lambda k: hT[:, k, :], hT_b)

                def _pj():
                    i = None
                    for (c0, n, w0) in ((0, 512, 256), (512, 512, 768), (1024, 256, 0), (1280, 256, 1280)):
                        for k in range(8):
                            i = pe.matmul(ps_pj[:, c0:c0 + n], lhsT=hT[:, k, :],
                                          rhs=win[:, k, w0:w0 + n], start=(k == 0), stop=(k == 7))
                    return i
                T.op("pe", _pj, reads=[hT_b, win_b], writes=[ps_pj_b])
                if t == 0:
                    dump("hT", hT[:].rearrange("p a b -> p (a b)"), [128, 1024], BF16, [hT_b])
                at_, ab_, as_ = aring.next()

                def _av(t=t, at_=at_):
                    act.copy(out=at_[:], in_=ps_pj[:, 1024:1280])
                    return act.copy(out=VA[:, t, :, 0:64], in_=ps_pj[:, 1280:1536].rearrange("p (k d) -> p k d", d=64))
                T.op("act", _av, reads=[ps_pj_b], writes=[ab_, VA_b[t]])
                T.dma("sp", as_, AD[t * 128:(t + 1) * 128, :], at_[:], reads=[ab_], writes=[AD_b[t]])
                rms_heads(ps_pj[:, 0:1024], 16, sq, ms, rt, qn[:], wb_, [ps_pj_b], gaint[:], gain_b)
                if t == 0:
                    dump("ms", ms[:], [128, 16], F32, [wb_["ms"]])
                    dump("qn", qn[:].rearrange("p a b -> p (a b)"), [128, 1024], F32, [wb_["dst"]])
                stq = stg[:, 0:768].rearrange("p (jj r hf d) -> p jj r hf d", jj=2, r=3, hf=2)
                stk = stg[:, 768:1024].rearrange("p (k d) -> p k d", d=64)

                def qperm(tt):
                    return tt[:, 0:12, :].rearrange("p (jj hf r) d -> p jj r hf d", jj=2, hf=2, r=3)
                if t < NTL:
                    rt_, rb_, rs_ = rring.next()
                    T.dma("sp", rs_, rt_[:, 0, :], ropeC[:, t, :], writes=[rb_])
                    T.dma("sp", rs_, rt_[:, 1, :], ropeS[:, t, :], writes=[rb_])

                    def _rope(rt_=rt_):
                        cb = rt_[:, 0, :].unsqueeze(1).to_broadcast([128, 16, 64])
                        dve.tensor_tensor(out=t1[:], in0=qn[:], in1=cb, op=ALU.mult)
                        qv = qn[:].rearrange("p h (a f) -> p h a f", a=4)
                        tv = t2[:].rearrange("p h (a f) -> p h a f", a=4)
                        sv = rt_[:, 1, :].rearrange("p (a f) -> p a f", a=4)
                        i = None
                        for ax in range(2):
                            for hf in range(2):
                                a_dst = ax * 2 + hf
                                a_src = ax * 2 + (1 - hf)
                                i = dve.tensor_tensor(out=tv[:, :, a_dst, :], in0=qv[:, :, a_src, :],
                                                      in1=sv[:, a_dst, :].unsqueeze(1).to_broadcast([128, 16, 16]),
                                                      op=ALU.mult)
                        return i
                    T.op("dve", _rope, reads=[wb_["dst"], rb_], writes=[t12_b])

                    def _st():
                        for jj in range(2):
                            dve.tensor_tensor(out=stq[:, jj], in0=qperm(t1)[:, jj], in1=qperm(t2)[:, jj], op=ALU.add)
                        return dve.tensor_tensor(out=stk, in0=t1[:, 12:16, :], in1=t2[:, 12:16, :], op=ALU.add)
                    T.op("dve", _st, reads=[t12_b], writes=[stg_b])
                else:
                    def _st():
                        for jj in range(2):
                            dve.tensor_copy(out=stq[:, jj], in_=qperm(qn)[:, jj])
                        return dve.tensor_copy(out=stk, in_=qn[:, 12:16, :])
                    T.op("dve", _st, reads=[wb_["dst"]], writes=[stg_b])

                def _t2():
                    i = None
                    for j in range(8):
                        i = pe.transpose(ps_t2[:, j * 128:(j + 1) * 128], stg[:, j * 128:(j + 1) * 128], identb[:])
                    return i
                T.op("pe", _t2, reads=[stg_b, identb_b], writes=[ps_t2_b])

                def _e2(t=t):
                    act.copy(out=QT[:, :, t * 128:(t + 1) * 128],
                             in_=ps_t2[:, 0:768].rearrange("p (j n) -> p j n", n=128))
                    return act.copy(out=KT[:, :, t * 128:(t + 1) * 128],
                                    in_=ps_t2[:, 768:1024].rearrange("p (j n) -> p j n", n=128))
                T.op("act", _e2, reads=[ps_t2_b], writes=[QT_b[t], KT_b[t]])
                if t == 0:
                    dump("stg", stg[:], [128, 1024], BF16, [stg_b])
                    dump("QT0", QT[:, :, 0:128], [128, 6, 128], BF16, [QT_b[0]])
                    dump("KT0", KT[:, :, 0:128], [128, 2, 128], BF16, [KT_b[0]])
                    dump("VA0", VA[:, 0, :, :], [128, 4, 66], BF16, [VA_b[0]])

        T.barrier()
        with contextlib.ExitStack() as stB:
            wout = sb("wout0", [64, 16, D], BF16, stB); wout_b = Buf()
            T.dma("pool", w0_s, wout[:], w_out[0].rearrange("(c p) n -> p c n", p=64), writes=[wout_b])
            lg, lgb = load_lnp(0, 0, stB, w0_s)
            lb, lbb = load_lnp(0, 1, stB, w0_s)
            ps_s = [ps(f"ps_s{i}", [128, 512], F32, stB) for i in range(2)]; ps_s_b = [Buf(), Buf()]
            ps_o = [ps(f"ps_o{i}", [128, 512], F32, stB) for i in range(2)]; ps_o_b = [Buf(), Buf()]
            ps_bc = ps("ps_bc", [128, 512], F32, stB); ps_bc_b = Buf()
            ps_y = ps("ps_y", [128, 1024], F32, stB); ps_y_b = Buf()
            ps_pl = ps("ps_pl", [128, 512], F32, stB); ps_pl_b = Buf()
            pT_ring = Ring(T, stB, nc, "pT", 3, [128, 512], BF16)
            osb = [sb(f"osb{i}", [128, 512], F32, stB) for i in range(2)]; osb_b = [Buf(), Buf()]
            OT = sb("OT0", [64, 12, 512], BF16, stB); OT_b = [Buf() for _ in range(12)]
            mT = sb("mT", [64, 4, 128], BF16, stB); mT_b = Buf()
            PTt = sb("PTt", [64, 4, 128], BF16, stB); PT_b = Buf()
            a3ring = Ring(T, stB, nc, "a3", 2, [128, 3, 256], BF16)
            zring = Ring(T, stB, nc, "zb", 2, [128, D], F32)
            tmp = sb("tmpb", [128, D], F32, stB); tmp_b = Buf()
            hcount = 0
            chunks = [(qc * 4, 4, list(range(NT))) for qc in range(8)] + [(NTL, 2, [NTL, NTL + 1])]
            for (t0, ntl, keys) in chunks:
                nq = ntl * 128
                stream = 0 if t0 < NTL else 1
                for h in range(12):
                    ch, half = qhead_pos(h)
                    kv = h // 3
                    rows = slice(half * 64, (half + 1) * 64)
                    oi = hcount % 2
                    hcount += 1
                    attn_head(QT[rows, ch, t0 * 128:t0 * 128 + nq],
                              lambda kt, rows=rows, kv=kv: (KT[rows, kv // 2, kt * 128:(kt + 1) * 128], KT_b[kt]),
                              lambda kt, kv=kv: (VA[:, kt, kv, 0:65], VA_b[kt]),
                              keys, ps_s, ps_s_b, pT_ring, ps_o[oi], ps_o_b[oi], nq,
                              [QT_b[t0 + i] for i in range(ntl)],
                              post=lambda oi=oi, h=h, nq=nq: attn_norm(ps_o[oi], ps_o_b[oi], osb[oi], osb_b[oi], ps_bc, ps_bc_b,
                                                                       OT[:, h, 0:nq], OT_b[h], nq))
                attn_flush()
                for il in range(ntl):
                    t = t0 + il
                    first = (t == 0) or (t == NTL)
                    last = (t == NTL - 1) or (t == NT - 1)
                    tlo = t if first else t - 1
                    thi = t if last else t + 1
                    a3, a3b, a3s = a3ring.next()
                    nsrc = thi - tlo + 1
                    T.dma("sp", a3s, a3[:, 0:nsrc, :], AD[tlo * 128:(thi + 1) * 128, :].rearrange("(j p) c -> p j c", p=128),
                          reads=[AD_b[i] for i in range(tlo, thi + 1)], writes=[a3b])
                    srcs = []
                    if not first:
                        srcs.append((t - 1 - tlo, 0))
                    srcs.append((t - tlo, 3 if first else (4 if last else 1)))
                    if not last:
                        srcs.append((t + 1 - tlo, 2))

                    def _pm(srcs=srcs, a3=a3):
                        i = None
                        for g in range(4):
                            for j, (sl, var) in enumerate(srcs):
                                i = pe.matmul(ps_pl[0:64, g * 128:(g + 1) * 128], lhsT=a3[:, sl, g * 64:(g + 1) * 64],
                                              rhs=bandt[:, g, var, :], start=(j == 0), stop=(j == len(srcs) - 1))
                        return i
                    T.op("pe", _pm, reads=[a3b, band_b], writes=[ps_pl_b])
                    T.op("act", lambda: act.copy(out=mT[:].rearrange("p g n -> p (g n)"), in_=ps_pl[0:64, :]),
                         reads=[ps_pl_b], writes=[mT_b])

                    def _pp():
                        i = None
                        for g in range(4):
                            i = pe.matmul(ps_pl[0:64, g * 128:(g + 1) * 128], lhsT=poolw[:, g, :], rhs=mT[:, g, :],
                                          start=True, stop=True)
                        return i
                    T.op("pe", _pp, reads=[mT_b, poolw_b], writes=[ps_pl_b])
                    T.op("act", lambda: act.copy(out=PTt[:].rearrange("p g n -> p (g n)"), in_=ps_pl[0:64, :]),
                         reads=[ps_pl_b], writes=[PT_b])

                    def _y(il=il):
                        i = None
                        for n in range(2):
                            for c in range(16):
                                lhs = PTt[:, c, :] if c < 4 else OT[:, c - 4, il * 128:(il + 1) * 128]
                                i = pe.matmul(ps_y[:, n * 512:(n + 1) * 512], lhsT=lhs, rhs=wout[:, c, n * 512:(n + 1) * 512],
                                              start=(c == 0), stop=(c == 15))
                        return i
                    T.op("pe", _y, reads=[PT_b, wout_b] + OT_b, writes=[ps_y_b])
                    if t == 0:
                        dump("OT", OT[:], [64, 12, 512], BF16, OT_b)
                        dump("PT", PTt[:], [64, 4, 128], BF16, [PT_b])
                    zt, zb, zs = zring.next()
                    T.dma("sp", zs, zt[:], x_src(t), writes=[zb])
                    resid_ln([ps_y[:, 0:512], ps_y[:, 512:1024]], [ps_y_b], zt, zb,
                             gate[(stream, 0)], gate_b[(stream, 0)], lg, lgb, lb, lbb, tmp, tmp_b)
                    T.dma("sp", zs, X1[t * 128:(t + 1) * 128, :], zt[:], reads=[zb], writes=[X1b[t]], is_output=debug)

        T.barrier()
        stR.close()
        with contextlib.ExitStack() as stF:
            w1 = sb("w1f", [128, 8, FFN], BF16, stF); w3 = sb("w3f", [128, 8, FFN], BF16, stF)
            w2 = sb("w2f", [128, NF, D], BF16, stF); wf_b = Buf(); wf_s = T.semc("wf")
            for k in range(8):
                T.dma("pool", wf_s, w1[:, k, :], ev_ffn_w1[k * 128:(k + 1) * 128, :], writes=[wf_b], max_dma_last_dim=4096)
                T.dma("pool", wf_s, w3[:, k, :], ev_ffn_w3[k * 128:(k + 1) * 128, :], writes=[wf_b], max_dma_last_dim=4096)
            for f in range(NF):
                T.dma("pool", wf_s, w2[:, f, :], ev_ffn_w2[f * 128:(f + 1) * 128, :], writes=[wf_b], max_dma_last_dim=4096)
            lg, lgb = load_lnp(0, 2, stF, wf_s)
            lb, lbb = load_lnp(0, 3, stF, wf_s)
            x1c = [sb(f"x1c{i}", [128, D], F32, stF) for i in range(4)]; x1c_b = [Buf() for _ in range(4)]
            x1c_s = [T.semc("x1c") for _ in range(4)]
            h2T = sb("h2T", [128, 8, 512], BF16, stF); h2T_b = [Buf() for _ in range(4)]
            AT = sb("ATf", [128, NF, 512], BF16, stF); AT_b = [Buf() for _ in range(NF)]
            sg = [sb(f"sgf{i}", [128, 512], BF16, stF) for i in range(2)]; sg_b = [Buf(), Buf()]
            tmp = sb("tmpf", [128, D], F32, stF); tmp_b = Buf()
            ps_tr = ps("ps_trF", [128, 1024], F32, stF); ps_tr_b = Buf()
            ps_g = [ps(f"ps_gF{i}", [128, 512], F32, stF) for i in range(2)]; ps_g_b = [Buf(), Buf()]
            ps_u = [ps(f"ps_uF{i}", [128, 512], F32, stF) for i in range(2)]; ps_u_b = [Buf(), Buf()]
            ps_o2 = [ps(f"ps_oF{i}", [128, 512], F32, stF) for i in range(2)]; ps_o2_b = [Buf(), Buf()]
            oc = 0
            chunks = [(qc * 4, 4) for qc in range(8)] + [(NTL, 2)]
            for (t0, ntl) in chunks:
                nq = ntl * 128
                stream = 0 if t0 < NTL else 1
                for il in range(ntl):
                    t = t0 + il
                    T.dma("sp", x1c_s[il], x1c[il][:], X1[t * 128:(t + 1) * 128, :], reads=[X1b[t]], writes=[x1c_b[il]])
                    make_hT(x1c[il], x1c_b[il], L, 2, 3, stream, ps_tr, ps_tr_b,
                            lambda k, il=il: h2T[:, k, il * 128:(il + 1) * 128], h2T_b[il])
                for f in range(NF):
                    gi = f % 2

                    def _gu(f=f, gi=gi, nq=nq):
                        i = None
                        for k in range(8):
                            i = pe.matmul(ps_g[gi][:, 0:nq], lhsT=w1[:, k, f * 128:(f + 1) * 128], rhs=h2T[:, k, 0:nq],
                                          start=(k == 0), stop=(k == 7))
                        for k in range(8):
                            i = pe.matmul(ps_u[gi][:, 0:nq], lhsT=w3[:, k, f * 128:(f + 1) * 128], rhs=h2T[:, k, 0:nq],
                                          start=(k == 0), stop=(k == 7))
                        return i
                    T.op("pe", _gu, reads=[wf_b] + h2T_b[0:ntl], writes=[ps_g_b[gi], ps_u_b[gi]])
                    T.op("act", lambda gi=gi, nq=nq: act.activation(out=sg[gi][:, 0:nq], in_=ps_g[gi][:, 0:nq], func=AF.Silu),
                         reads=[ps_g_b[gi]], writes=[sg_b[gi]])
                    T.op("dve", lambda gi=gi, f=f, nq=nq: dve.tensor_tensor(out=AT[:, f, 0:nq], in0=sg[gi][:, 0:nq],
                                                                           in1=ps_u[gi][:, 0:nq], op=ALU.mult),
                         reads=[sg_b[gi], ps_u_b[gi]], writes=[AT_b[f]])
                for il in range(ntl):
                    t = t0 + il
                    ois = []
                    for n in range(2):
                        oi = oc % 2
                        oc += 1
                        ois.append(oi)

                        def _o(il=il, n=n, oi=oi):
                            i = None
                            for f in range(NF):
                                i = pe.matmul(ps_o2[oi][:, :], lhsT=AT[:, f, il * 128:(il + 1) * 128],
                                              rhs=w2[:, f, n * 512:(n + 1) * 512], start=(f == 0), stop=(f == NF - 1))
                            return i
                        T.op("pe", _o, reads=AT_b + [wf_b], writes=[ps_o2_b[oi]])
                    resid_ln([ps_o2[ois[0]][:, :], ps_o2[ois[1]][:, :]], [ps_o2_b[ois[0]], ps_o2_b[ois[1]]],
                             x1c[il], x1c_b[il], gate[(stream, 1)], gate_b[(stream, 1)], lg, lgb, lb, lbb, tmp, tmp_b)
                    if n_layers == 1 and t < NTL:
                        T.dma("sp", x1c_s[il], out_ap[t * 128:(t + 1) * 128, :], x1c[il][:], reads=[x1c_b[il]], is_output=True)
                    T.dma("sp", x1c_s[il], X2[t * 128:(t + 1) * 128, :], x1c[il][:], reads=[x1c_b[il]], writes=[X2b[t]],
                          is_output=debug)

    T.barrier()
    if n_layers == 1:
        T.finish()
        es.close()
        return nc

    L = 1
    QD = dscr("QD", [128, 6, SEQ], BF16); QD_b = [Buf() for _ in range(NTL)]
    KD = dscr("KD", [128, 6, SEQ + CTXL], BF16); KD_b = [Buf() for _ in range(NT)]
    VD = dscr("VD", [NT, 128, 12 * 66], BF16); VD_b = [Buf() for _ in range(NT)]
    W1s = dscr("W1s", [NEXP, 11, 128, 8, 256], BF16); W3s = dscr("W3s", [NEXP, 11, 128, 8, 256], BF16)
    W2s = dscr("W2s", [NEXP, 2, 128, NF, 512], BF16)
    W1s_b = [Buf() for _ in range(NEXP)]; W3s_b = [Buf() for _ in range(NEXP)]; W2s_b = [Buf() for _ in range(NEXP)]
    wc_s = T.semc("wcast")
    for e in range(NEXP):
        for j in range(11):
            T.dma("pool", wc_s, W1s[e, j], od_w1[e][:, j * 256:(j + 1) * 256].rearrange("(k p) n -> p k n", p=128),
                  writes=[W1s_b[e]])
            T.dma("pool", wc_s, W3s[e, j], od_w3[e][:, j * 256:(j + 1) * 256].rearrange("(k p) n -> p k n", p=128),
                  writes=[W3s_b[e]])
        for hh in range(2):
            T.dma("pool", wc_s, W2s[e, hh], od_w2[e][:, hh * 512:(hh + 1) * 512].rearrange("(f p) n -> p f n", p=128),
                  writes=[W2s_b[e]])

    with contextlib.ExitStack() as stL1:
        stR = contextlib.ExitStack()
        gate, gate_b = compute_mod(1, stL1, stR)
        YT = sb("YT", [128, 2, SEQ], BF16, stR); YT_b = [Buf() for _ in range(8)]
        w1_s = T.semc("w1")
        stU = contextlib.ExitStack()
        UCS = sb("UCS", [128, NTL, 512], BF16, stU); UCS_b = [Buf() for _ in range(NTL)]
        with contextlib.ExitStack() as stA:
            win = sb("win1", [128, 8, 2560], BF16, stA); win_b = Buf()
            for k in range(8):
                T.dma("pool", w1_s, win[:, k, :], od_w_in[k * 128:(k + 1) * 128, :], writes=[win_b], max_dma_last_dim=4096)
            fg = sb("fg1", [128, 4, 64], F32, stA); fg_b = Buf()
            T.dma("sp", w1_s, fg[:].rearrange("p g d -> p (g d)"),
                  od_fgain.rearrange("(o n) -> o n", o=1).to_broadcast([128, 256]), writes=[fg_b])
            c64 = sb("c64", [128, 2, 128], BF16, stA); c64_b = Buf()
            T.dma("sp", w1_s, c64[:, 0, :], C64[:, :], writes=[c64_b])
            T.dma("sp", w1_s, c64[:, 1, :], S64[:, :], writes=[c64_b])
            xring = Ring(T, stA, nc, "xa1", 2, [128, D], F32)
            hT = sb("hTa1", [128, 8, 128], BF16, stA); hT_b = Buf()
            sq = sb("sqa1", [128, 256], F32, stA); ms = sb("msa1", [128, 4], F32, stA); rt = sb("rta1", [128, 4], F32, stA)
            un = sb("una1", [128, 4, 64], F32, stA)
            wb_ = {"sq": Buf(), "ms": Buf(), "rt": Buf(), "dst": Buf()}
            stg = sb("stga1", [128, 14 * 128], BF16, stA); stg_b = Buf()
            qkT = Ring(T, stA, nc, "qkT1", 2, [128, 12, 128], BF16)
            uT = sb("uT1", [128, 2, 128], BF16, stA); uT_b = Buf()
            vring = Ring(T, stA, nc, "va1", 2, [128, 12, 66], BF16)
            for i_ in range(2):
                T.op("dve", lambda i_=i_: dve.memset(vring.t[i_][:, :, 64:66], 1.0), writes=[vring.b[i_]])
            ps_tr = ps("ps_trA1", [128, 1024], F32, stA); ps_tr_b = Buf()
            ps_pj = ps("ps_pjA1", [128, 2048], F32, stA); ps_pj_b = Buf()
            ps_t2 = ps("ps_t2A1", [128, 2048], BF16, stA); ps_t2_b = Buf()
            for t in range(NT):
                lat = t < NTL
                stream = 0 if lat else 1
                xt, xb, xs = xring.next()
                T.dma("sp", xs, xt[:], X2[t * 128:(t + 1) * 128, :], reads=[X2b[t]], writes=[xb])
                make_hT(xt, xb, L, 0, 1, stream, ps_tr, ps_tr_b, lambda k: hT[:, k, :], hT_b)

                def _pj(lat=lat):
                    i = None
                    groups = [(1024, 512, 1024), (1536, 256, 1536)]
                    if lat:
                        groups = [(0, 512, 256), (512, 256, 768), (768, 256, 0)] + groups
                    for (c0, n, w0) in groups:
                        for k in range(8):
                            i = pe.matmul(ps_pj[:, c0:c0 + n], lhsT=hT[:, k, :], rhs=win[:, k, w0:w0 + n],
                                          start=(k == 0), stop=(k == 7))
                    return i
                T.op("pe", _pj, reads=[hT_b, win_b], writes=[ps_pj_b])
                if lat:
                    rms_heads(ps_pj[:, 768:1024], 4, sq, ms, rt, un[:], wb_, [ps_pj_b], fg[:], fg_b)

                def _stq(lat=lat):
                    i = act.copy(out=stg[:, 768:1536], in_=ps_pj[:, 1024:1792])
                    if lat:
                        i = act.mul(out=stg[:, 0:768], in_=ps_pj[:, 0:768], mul=0.125)
                    return i
                T.op("act", _stq, reads=[ps_pj_b] + ([wb_["dst"]] if lat else []), writes=[stg_b])
                if lat:
                    T.op("dve", lambda: dve.tensor_copy(out=stg[:, 1536:1792], in_=un[:].rearrange("p g d -> p (g d)")),
                         reads=[wb_["dst"]], writes=[stg_b])

                def _t2(lat=lat):
                    i = None
                    for j in (range(14) if lat else range(6, 12)):
                        i = pe.transpose(ps_t2[:, j * 128:(j + 1) * 128], stg[:, j * 128:(j + 1) * 128], identb[:])
                    return i
                T.op("pe", _t2, reads=[stg_b, identb_b], writes=[ps_t2_b])
                qk, qkb, qks = qkT.next()

                def _e2(lat=lat, qk=qk):
                    i = act.copy(out=qk[:, 6:12, :], in_=ps_t2[:, 768:1536].rearrange("p (j n) -> p j n", n=128))
                    if lat:
                        i = act.copy(out=qk[:, 0:6, :], in_=ps_t2[:, 0:768].rearrange("p (j n) -> p j n", n=128))
                    return i
                T.op("act", _e2, reads=[ps_t2_b], writes=[qkb])
                T.dma("sp", qks, KD[:, :, t * 128:(t + 1) * 128], qk[:, 6:12, :], reads=[qkb], writes=[KD_b[t]])
                if lat:
                    T.dma("sp", qks, QD[:, :, t * 128:(t + 1) * 128], qk[:, 0:6, :], reads=[qkb], writes=[QD_b[t]])
                    T.op("dve", lambda: dve.tensor_copy(out=uT[:].rearrange("p a n -> p (a n)"), in_=ps_t2[:, 1536:1792]),
                         reads=[ps_t2_b], writes=[uT_b])

                    def _uc():
                        i = None
                        for cs_ in range(2):
                            for c2 in range(2):
                                i = pe.matmul(ps_tr[:, (cs_ * 2 + c2) * 128:(cs_ * 2 + c2 + 1) * 128], lhsT=uT[:, c2, :],
                                              rhs=c64[:, cs_, :], start=True, stop=True)
                        return i
                    T.op("pe", _uc, reads=[uT_b, c64_b], writes=[ps_tr_b])
                    T.op("dve", lambda t=t: dve.tensor_copy(out=UCS[:, t, :], in_=ps_tr[:, 0:512]),
                         reads=[ps_tr_b], writes=[UCS_b[t]])
                def _pv():
                    i = None
                    for (c0, n, w0) in ((0, 512, 1792), (512, 256, 2304)):
                        for k in range(8):
                            i = pe.matmul(ps_pj[:, c0:c0 + n], lhsT=hT[:, k, :], rhs=win[:, k, w0:w0 + n],
                                          start=(k == 0), stop=(k == 7))
                    return i
                T.op("pe", _pv, reads=[hT_b, win_b], writes=[ps_pj_b])
                vt, vb, vs = vring.next()
                T.op("act", lambda vt=vt: act.copy(out=vt[:, :, 0:64], in_=ps_pj[:, 0:768].rearrange("p (h d) -> p h d", d=64)),
                     reads=[ps_pj_b], writes=[vb])
                T.dma("sp", vs, VD[t], vt[:].rearrange("p h d -> p (h d)"), reads=[vb], writes=[VD_b[t]])
        T.barrier()
        if stop_after == "A1":
            T.finish(); stU.close(); stR.close(); return nc
        with contextlib.ExitStack() as stFo:
            tring = Ring(T, stFo, nc, "dft", 3, [128, 2, 8, 512], BF16)
            ps_f = [ps(f"ps_f{i}", [128, 512], F32, stFo) for i in range(4)]; ps_f_b = [Buf() for _ in range(4)]
            for tc in range(8):
                pb_ = (tc % 2) * 2
                for sp_ in range(4):
                    tt, tb, ts_ = tring.next()
                    for ci, src in enumerate((CN, SN)):
                        T.dma("sp", ts_, tt[:, ci, :, :],
                              src[sp_ * 1024:(sp_ + 1) * 1024, tc * 512:(tc + 1) * 512].rearrange("(j p) n -> p j n", p=128),
                              writes=[tb])

                    def _f(tt=tt, sp_=sp_, pb_=pb_):
                        i = None
                        for c2 in range(2):
                            for j in range(8):
                                s_ = sp_ * 8 + j
                                for ci in range(2):
                                    i = pe.matmul(ps_f[pb_ + c2][:, :], lhsT=UCS[:, s_, (ci * 2 + c2) * 128:(ci * 2 + c2 + 1) * 128],
                                                  rhs=tt[:, ci, j, :], start=(s_ == 0 and ci == 0), stop=(s_ == 31 and ci == 1))
                        return i
                    T.op("pe", _f, reads=[tb] + UCS_b[sp_ * 8:(sp_ + 1) * 8], writes=[ps_f_b[pb_], ps_f_b[pb_ + 1]])

                def _fe(tc=tc, pb_=pb_):
                    i = None
                    for c2 in range(2):
                        i = act.copy(out=YT[:, c2, tc * 512:(tc + 1) * 512], in_=ps_f[pb_ + c2][:, :])
                    return i
                T.op("act", _fe, reads=[ps_f_b[pb_], ps_f_b[pb_ + 1]], writes=[YT_b[tc]])
        T.barrier()
        if stop_after == "FO":
            T.finish(); stU.close(); stR.close(); return nc
        stU.close()

        with contextlib.ExitStack() as stB:
            wout = sb("wout1", [64, 12, D], BF16, stB); woutF = sb("woutF1", [128, 2, D], BF16, stB); wout_b = Buf()
            T.dma("pool", w1_s, wout[:], w_out[1, 256:1024, :].rearrange("(c p) n -> p c n", p=64), writes=[wout_b])
            T.dma("pool", w1_s, woutF[:], w_out[1, 0:256, :].rearrange("(c p) n -> p c n", p=128), writes=[wout_b])
            lg, lgb = load_lnp(1, 0, stB, w1_s)
            lb, lbb = load_lnp(1, 1, stB, w1_s)
            KTc = sb("KTc", [128, 6, 256], BF16, stB); Vc = sb("Vc", [128, 2, 12 * 66], BF16, stB); kvc_b = Buf()
            T.dma("sp", w1_s, KTc[:], KD[:, :, SEQ:SEQ + CTXL], reads=[KD_b[32], KD_b[33]], writes=[kvc_b])
            T.dma("sp", w1_s, Vc[:], VD[NTL:NT].rearrange("j p n -> p j n"), reads=[VD_b[32], VD_b[33]], writes=[kvc_b])
            qring = Ring(T, stB, nc, "qb1", 2, [128, 6, 512], BF16)
            kring = Ring(T, stB, nc, "kb1", 2, [128, 6, 1024], BF16)
            vwring = Ring(T, stB, nc, "vb1", 2, [128, 8, 12 * 66], BF16)
            bring = Ring(T, stB, nc, "bias1", 3, [128, 8, 512], BF16)
            ps_s = [ps(f"ps_s1{i}", [128, 512], F32, stB) for i in range(2)]; ps_s_b = [Buf(), Buf()]
            ps_o = [ps(f"ps_o1{i}", [128, 512], F32, stB) for i in range(2)]; ps_o_b = [Buf(), Buf()]
            ps_bc = ps("ps_bc1", [128, 512], F32, stB); ps_bc_b = Buf()
            ps_y = ps("ps_y1", [128, 1024], F32, stB); ps_y_b = Buf()
            pT_ring = Ring(T, stB, nc, "pT1", 3, [128, 512], BF16)
            osb = [sb(f"osb1{i}", [128, 512], F32, stB) for i in range(2)]; osb_b = [Buf(), Buf()]
            OT = sb("OT1", [64, 12, 512], BF16, stB); OT_b = [Buf() for _ in range(12)]
            zring = Ring(T, stB, nc, "zb1", 2, [128, D], F32)
            tmp = sb("tmpb1", [128, D], F32, stB); tmp_b = Buf()
            hcount = 0
            for qb in range(8):
                t0 = 4 * qb
                bt = 0 if qb == 0 else (2 if qb == 7 else 1)
                klo = max(0, t0 - 2)
                khi = min(NTL, t0 + 6)
                nk = khi - klo
                slot0 = klo - (t0 - 2)
                qt_, qb_, qs_ = qring.next()
                T.dma("sp", qs_, qt_[:], QD[:, :, t0 * 128:(t0 + 4) * 128], reads=QD_b[t0:t0 + 4], writes=[qb_])
                kt_, kb_, ks_ = kring.next()
                T.dma("sp", ks_, kt_[:, :, 0:nk * 128], KD[:, :, klo * 128:khi * 128], reads=KD_b[klo:khi], writes=[kb_])
                vt_, vb_, vs_ = vwring.next()
                T.dma("sp", vs_, vt_[:, 0:nk, :], VD[klo:khi].rearrange("j p n -> p j n"), reads=VD_b[klo:khi], writes=[vb_])
                keys = list(range(klo, khi)) + [NTL, NTL + 1]
                for h in range(12):
                    ch, half = h // 2, h % 2
                    rows = slice(half * 64, (half + 1) * 64)
                    oi = hcount % 2
                    hcount += 1
                    bt_, bb_, bs_ = bring.next()

                    def bpre(bt_=bt_, bb_=bb_, bs_=bs_, h=h, bt=bt, slot0=slot0, nk=nk):
                        T.dma("pool", bs_, bt_[:, 0:nk, :], od_bias[h, bt, slot0:slot0 + nk].rearrange("j p n -> p j n"),
                              writes=[bb_])

                    def kfn(kt, rows=rows, ch=ch, kt_=kt_, kb_=kb_, klo=klo):
                        if kt >= NTL:
                            return KTc[rows, ch, (kt - NTL) * 128:(kt - NTL + 1) * 128], kvc_b
                        return kt_[rows, ch, (kt - klo) * 128:(kt - klo + 1) * 128], kb_

                    def vfn(kt, h=h, vt_=vt_, vb_=vb_, klo=klo):
                        if kt >= NTL:
                            return Vc[:, kt - NTL, h * 66:h * 66 + 65], kvc_b
                        return vt_[:, kt - klo, h * 66:h * 66 + 65], vb_

                    def bfn(kt, bt_=bt_, bb_=bb_, klo=klo):
                        if kt >= NTL:
                            return None
                        return bt_[:, kt - klo, :], bb_
                    attn_head(qt_[rows, ch, :], kfn, vfn, keys, ps_s, ps_s_b, pT_ring, ps_o[oi], ps_o_b[oi], 512,
                              [qb_], bias_fn=bfn, pre=bpre,
                              post=lambda oi=oi, h=h: attn_norm(ps_o[oi], ps_o_b[oi], osb[oi], osb_b[oi], ps_bc, ps_bc_b,
                                                                OT[:, h, :], OT_b[h], 512))
                attn_flush()
                for il in range(4):
                    t = t0 + il

                    def _y(il=il, t=t):
                        i = None
                        for n in range(2):
                            for c in range(14):
                                if c < 2:
                                    lhs, rhs = YT[:, c, t * 128:(t + 1) * 128], woutF[:, c, n * 512:(n + 1) * 512]
                                else:
                                    lhs, rhs = OT[:, c - 2, il * 128:(il + 1) * 128], wout[:, c - 2, n * 512:(n + 1) * 512]
                                i = pe.matmul(ps_y[:, n * 512:(n + 1) * 512], lhsT=lhs, rhs=rhs, start=(c == 0), stop=(c == 13))
                        return i
                    T.op("pe", _y, reads=[YT_b[t // 4], wout_b] + OT_b, writes=[ps_y_b])
                    zt, zb, zs = zring.next()
                    T.dma("sp", zs, zt[:], X2[t * 128:(t + 1) * 128, :], reads=[X2b[t]], writes=[zb])
                    resid_ln([ps_y[:, 0:512], ps_y[:, 512:1024]], [ps_y_b], zt, zb,
                             gate[(0, 0)], gate_b[(0, 0)], lg, lgb, lb, lbb, tmp, tmp_b)
                    T.dma("sp", zs, X3[t * 128:(t + 1) * 128, :], zt[:], reads=[zb], writes=[X3b[t]], is_output=debug)
        T.barrier()
        if stop_after == "NA":
            T.finish(); stR.close(); return nc
        stR.close()

        with contextlib.ExitStack() as stM:
            lg, lgb = load_lnp(1, 2, stM, w1_s)
            lb, lbb = load_lnp(1, 3, stM, w1_s)
            wr = sb("wr", [128, 8, 8], F32, stM); wr_b = Buf()
            T.dma("sp", w1_s, wr[:], od_router.rearrange("(k p) e -> p k e", p=128), writes=[wr_b])
            x3c = [sb(f"x3c{i}", [128, D], F32, stM) for i in range(4)]; x3c_b = [Buf() for _ in range(4)]
            x3c_s = [T.semc("x3c") for _ in range(4)]
            acc = [sb(f"acc{i}", [128, D], F32, stM) for i in range(4)]; acc_b = [Buf() for _ in range(4)]
            h2T = sb("h2Tm", [128, 8, 512], BF16, stM); h2T_b = [Buf() for _ in range(4)]
            h2Tf = sb("h2Tf", [128, 8, 128], F32, stM); h2Tf_b = Buf()
            AT = sb("ATm", [128, NF, 512], BF16, stM); AT_b = [Buf() for _ in range(NF)]
            sg = [sb(f"sgm{i}", [128, 512], BF16, stM) for i in range(2)]; sg_b = [Buf(), Buf()]
            tmp = sb("tmpm", [128, D], F32, stM); tmp_b = Buf()
            lgt = sb("lgt", [128, 8], F32, stM); mx = sb("mxm", [128, 8], F32, stM); dd = sb("ddm", [128, 2], F32, stM)
            eq = sb("eqm", [128, 2, 8], F32, stM)
            gts = sb("gts", [128, 4, 8], F32, stM); gts_b = [Buf() for _ in range(4)]
            gb = [Buf() for _ in range(5)]
            w13 = Ring(T, stM, nc, "w13", 3, [128, 2, 8, 256], BF16)
            w2r = Ring(T, stM, nc, "w2r", 3, [128, NF, 512], BF16)
            ps_tr = ps("ps_trM", [128, 1024], F32, stM); ps_tr_b = Buf()
            ps_g = [ps(f"ps_gM{i}", [128, 512], F32, stM) for i in range(2)]; ps_g_b = [Buf(), Buf()]
            ps_u = [ps(f"ps_uM{i}", [128, 512], F32, stM) for i in range(2)]; ps_u_b = [Buf(), Buf()]
            ps_o2 = [ps(f"ps_oM{i}", [128, 512], F32, stM) for i in range(2)]; ps_o2_b = [Buf(), Buf()]
            oc = 0
            if stop_after == "M0":
                T.barrier(); T.finish(); stM.close(); return nc
            for tcn in range(8):
                t0 = tcn * 4
                for il in range(4):
                    t = t0 + il
                    if stop_after == "M1" and il == 1:
                        dump("h2Tf", h2Tf[:].rearrange("p a b -> p (a b)"), [128, 1024], F32, [h2Tf_b])
                        dump("lgt", lgt[:], [128, 8], F32, [gb[0]])
                        T.barrier(); T.finish(); stM.close(); return nc
                    T.dma("sp", x3c_s[il], x3c[il][:], X3[t * 128:(t + 1) * 128, :], reads=[X3b[t]], writes=[x3c_b[il]])
                    make_hT(x3c[il], x3c_b[il], L, 2, 3, 0, ps_tr, ps_tr_b,
                            lambda k, il=il: h2T[:, k, il * 128:(il + 1) * 128], h2T_b[il],
                            lambda k: h2Tf[:, k, :], h2Tf_b)
                    def chk(stage):
                        if stop_after == "S" + str(il * 10 + stage):
                            T.barrier(); T.finish(); stM.close()
                            raise StopIteration
                    chk(1)
                    oi = oc % 2
                    oc += 1

                    def _r(oi=oi):
                        i = None
                        for k in range(8):
                            i = pe.matmul(ps_o2[oi][:, 0:8], lhsT=h2Tf[:, k, :], rhs=wr[:, k, :], start=(k == 0), stop=(k == 7))
                        return i
                    T.op("pe", _r, reads=[h2Tf_b, wr_b], writes=[ps_o2_b[oi]])
                    chk(2)
                    T.op("dve", lambda oi=oi: dve.tensor_copy(out=lgt[:], in_=ps_o2[oi][:, 0:8]), reads=[ps_o2_b[oi]], writes=[gb[0]])
                    chk(3)
                    T.op("dve", lambda: dve.max(out=mx[:], in_=lgt[:]), reads=[gb[0]], writes=[gb[1]], hard=True)
                    chk(4)
                    T.op("dve", lambda: dve.tensor_tensor(out=dd[:, 0:1], in0=mx[:, 0:1], in1=mx[:, 1:2], op=ALU.subtract),
                         reads=[gb[1]], writes=[gb[2]], hard=True)
                    T.op("act", lambda: act.activation(out=dd[:, 0:1], in_=dd[:, 0:1], func=AF.Sigmoid), reads=[gb[2]], writes=[gb[2]])

                    chk(5)

                    def _g1():
                        dve.tensor_scalar(out=dd[:, 1:2], in0=dd[:, 0:1], scalar1=-1.0, scalar2=1.0, op0=ALU.mult, op1=ALU.add)
                        dve.tensor_scalar(out=eq[:, 0, :], in0=lgt[:], scalar1=mx[:, 0:1], scalar2=None, op0=ALU.is_equal)
                        return dve.tensor_scalar(out=eq[:, 1, :], in0=lgt[:], scalar1=mx[:, 1:2], scalar2=None, op0=ALU.is_equal)
                    T.op("dve", _g1, reads=[gb[2], gb[1], gb[0]], writes=[gb[3]], hard=True)

                    chk(6)

                    def _g2(il=il):
                        dve.tensor_scalar(out=eq[:, 0, :], in0=eq[:, 0, :], scalar1=dd[:, 0:1], scalar2=None, op0=ALU.mult)
                        return dve.scalar_tensor_tensor(out=gts[:, il, :], in0=eq[:, 1, :], scalar=dd[:, 1:2], in1=eq[:, 0, :],
                                                        op0=ALU.mult, op1=ALU.add)
                    T.op("dve", _g2, reads=[gb[3]], writes=[gts_b[il], gb[4]], hard=True)
                    chk(7)
                if stop_after == "MG":
                    dump("gts", gts[:].rearrange("p a b -> p (a b)"), [128, 32], F32, gts_b)
                    T.barrier(); T.finish(); stM.close(); return nc
                for e in range(NEXP):
                    if stop_after == "ME" and e == 1:
                        dump("acc0", acc[0][:], [128, D], F32, [acc_b[0]])
                        T.barrier(); T.finish(); stM.close(); return nc
                    for fp in range(11):
                        wt_, wb2_, ws2_ = w13.next()
                        T.dma("sp", ws2_, wt_[:, 0, :, :], W1s[e, fp], reads=[W1s_b[e]], writes=[wb2_])
                        T.dma("sp", ws2_, wt_[:, 1, :, :], W3s[e, fp], reads=[W3s_b[e]], writes=[wb2_])
                        for f2 in range(2):
                            f = fp * 2 + f2
                            gi = f % 2

                            def _gu(f2=f2, gi=gi, wt_=wt_):
                                i = None
                                for k in range(8):
                                    i = pe.matmul(ps_g[gi][:, :], lhsT=wt_[:, 0, k, f2 * 128:(f2 + 1) * 128], rhs=h2T[:, k, :],
                                                  start=(k == 0), stop=(k == 7))
                                for k in range(8):
                                    i = pe.matmul(ps_u[gi][:, :], lhsT=wt_[:, 1, k, f2 * 128:(f2 + 1) * 128], rhs=h2T[:, k, :],
                                                  start=(k == 0), stop=(k == 7))
                                return i
                            T.op("pe", _gu, reads=[wb2_] + h2T_b, writes=[ps_g_b[gi], ps_u_b[gi]])
                            T.op("act", lambda gi=gi: act.activation(out=sg[gi][:, :], in_=ps_g[gi][:, :], func=AF.Silu),
                                 reads=[ps_g_b[gi]], writes=[sg_b[gi]])
                            T.op("dve", lambda gi=gi, f=f: dve.tensor_tensor(out=AT[:, f, :], in0=sg[gi][:, :], in1=ps_u[gi][:, :],
                                                                             op=ALU.mult),
                                 reads=[sg_b[gi], ps_u_b[gi]], writes=[AT_b[f]])
                    for n in range(2):
                        w2t, w2b, w2s = w2r.next()
                        T.dma("sp", w2s, w2t[:].rearrange("p f n -> p (f n)"), W2s[e, n].rearrange("p f n -> p (f n)"),
                              reads=[W2s_b[e]], writes=[w2b])
                        for il in range(4):
                            oi = oc % 2
                            oc += 1

                            def _o(il=il, oi=oi, w2t=w2t):
                                i = None
                                for f in range(NF):
                                    i = pe.matmul(ps_o2[oi][:, :], lhsT=AT[:, f, il * 128:(il + 1) * 128], rhs=w2t[:, f, :],
                                                  start=(f == 0), stop=(f == NF - 1))
                                return i
                            T.op("pe", _o, reads=AT_b + [w2b], writes=[ps_o2_b[oi]])
                            dst = acc[il][:, n * 512:(n + 1) * 512]
                            if e == 0:
                                T.op("dve", lambda oi=oi, il=il, dst=dst: dve.tensor_scalar(
                                    out=dst, in0=ps_o2[oi][:, :], scalar1=gts[:, il, 0:1], scalar2=None, op0=ALU.mult),
                                    reads=[ps_o2_b[oi], gts_b[il]], writes=[acc_b[il]])
                            else:
                                T.op("dve", lambda oi=oi, il=il, dst=dst, e=e: dve.scalar_tensor_tensor(
                                    out=dst, in0=ps_o2[oi][:, :], scalar=gts[:, il, e:e + 1], in1=dst, op0=ALU.mult, op1=ALU.add),
                                    reads=[ps_o2_b[oi], gts_b[il]], writes=[acc_b[il]])
                for il in range(4):
                    t = t0 + il
                    resid_ln([acc[il][:, 0:512], acc[il][:, 512:1024]], [acc_b[il]], x3c[il], x3c_b[il],
                             gate[(0, 1)], gate_b[(0, 1)], lg, lgb, lb, lbb, tmp, tmp_b)
                    T.dma("sp", x3c_s[il], out_ap[t * 128:(t + 1) * 128, :], x3c[il][:], reads=[x3c_b[il]], is_output=True)
    T.barrier()
    T.finish()
    es.close()
    return nc


_PROG = {}


def _get_prog(n_layers=2, debug=False):
    key = (n_layers, debug)
    if key not in _PROG:
        _PROG[key] = build_program(n_layers, debug)
    return _PROG[key]


L1_KEYS = ("od_w_in", "od_fgain", "od_bias", "od_router", "od_w1", "od_w3", "od_w2", "C64", "S64", "CN", "SN")


def make_in_maps(inp, n_cores=N_CORES, n_layers=2):
    cs = _consts()
    f32 = lambda a: np.ascontiguousarray(np.asarray(a, dtype=np.float32))
    x = f32(inp["x"]); c = f32(inp["c"]); ctx = f32(inp["ctx"]); c_ctx = f32(inp["c_ctx"])
    ada_b = f32(inp["ada_b"])
    adab_col = np.ascontiguousarray(ada_b.reshape(2, 48, 128).transpose(0, 2, 1))
    qk_gain = np.concatenate([np.tile(f32(inp["ev_q_gain"])[0], 12), np.tile(f32(inp["ev_k_gain"])[0], 4)])
    rpb = f32(inp["od_rpb"])[0]
    rpb_ext = np.concatenate([rpb.reshape(12, -1), np.full((12, 1), NEG, np.float32)], axis=1)
    od_bias = np.ascontiguousarray(rpb_ext[:, cs["naidx"]])
    shared = dict(
        ada_w=f32(inp["ada_w"]), ada_b=ada_b, adab_col=adab_col,
        ln_mix_g=f32(inp["ln_mix_g"]), ln_mix_b=f32(inp["ln_mix_b"]),
        ln_ffn_g=f32(inp["ln_ffn_g"]), ln_ffn_b=f32(inp["ln_ffn_b"]),
        w_out=f32(inp["w_out"]), ev_w_in=f32(inp["ev_w_in"])[0], ev_pool_w=f32(inp["ev_pool_w"])[0],
        ev_pool_scale=f32(inp["ev_pool_scale"])[0], ev_qk_gain=f32(qk_gain),
        ev_ffn_w1=f32(inp["ev_ffn_w1"])[0], ev_ffn_w3=f32(inp["ev_ffn_w3"])[0], ev_ffn_w2=f32(inp["ev_ffn_w2"])[0],
        od_w_in=f32(inp["od_w_in"])[0], od_fgain=f32(inp["od_fourier_gain"])[0].reshape(256),
        od_bias=od_bias, od_router=f32(inp["od_router"])[0],
        od_w1=f32(inp["od_exp_w1"])[0], od_w3=f32(inp["od_exp_w3"])[0], od_w2=f32(inp["od_exp_w2"])[0],
        ropeC=cs["ropeC"], ropeS=cs["ropeS"], band=cs["band"], C64=cs["C64"], S64=cs["S64"],
        CN=cs["CN"], SN=cs["SN"], identf=cs["identf"],
    )
    if n_layers < 2:
        for k in L1_KEYS:
            shared.pop(k)
    maps = []
    for b in range(n_cores):
        cv = np.stack([c[b].reshape(8, 128).T, c_ctx.reshape(8, 128).T], axis=-1)
        m = dict(shared)
        m.update(x=x[b], ctx=ctx[b], cvec=np.ascontiguousarray(cv.astype(np.float32)))
        maps.append(m)
    return maps


def kernel(**inputs):
    nc = _get_prog(2, False)
    maps = make_in_maps(inputs)
    res = run_bass_kernel_spmd(nc, maps, core_ids=list(range(N_CORES)))
    return np.stack([np.asarray(r["out"], dtype=np.float32) for r in res.results], axis=0)
```

```python
import contextlib
import math
import numpy as np
import ml_dtypes
import concourse.bass as bass
import concourse.mybir as mybir
from concourse.bass_utils import run_bass_kernel_spmd

F32 = mybir.dt.float32
BF16 = mybir.dt.bfloat16
AF = mybir.ActivationFunctionType
ALU = mybir.AluOpType
AX = mybir.AxisListType

D = 1024
SEQ = 4096
CTXL = 256
NT = 34
NTL = 32
FFN = 2816
NF = 22
NEXP = 8
ALPHA = 4.0 ** 0.25
LN_EPS = 1e-6
RMS_EPS = 1e-6
NEG = -30000.0
N_CORES = 8


class Buf:
    __slots__ = ("name", "w", "r")

    def __init__(self, name=""):
        self.name = name
        self.w = None
        self.r = {}


class SemC:
    __slots__ = ("sem", "cnt")

    def __init__(self, sem):
        self.sem = sem
        self.cnt = 0


class Tracker:
    def __init__(self, nc, es):
        self.nc = nc
        self.es = es
        self.E = {"pe": nc.tensor, "act": nc.scalar, "dve": nc.vector, "pool": nc.gpsimd, "sp": nc.sync}
        self.esem = {}
        self.ecnt = {}
        self.own = {k: set() for k in self.E}
        self.seen = {k: {} for k in self.E}
        self.nsem = 0
        self.dsem = {}
        for k in self.E:
            self._new_esem(k)
        self.out_events = []

    def new_sem(self, name):
        self.nsem += 1
        return self.es.enter_context(self.nc.semaphore(f"{name}_{self.nsem}"))

    def semc(self, name="d"):
        sc = SemC(self.new_sem(name))
        self.dsem[sc.sem.num] = sc
        return sc

    def _new_esem(self, k):
        s = self.new_sem("e" + k)
        self.esem[k] = s
        self.ecnt[k] = 0
        self.own[k].add(s.num)

    def _wait_all(self, eng, evs, allow_own=False):
        best = {}
        for ev in evs:
            if ev is None:
                continue
            s, v = ev
            if s.num in self.own[eng] and not allow_own:
                continue
            if s.num not in best or best[s.num][1] < v:
                best[s.num] = (s, v)
        for num, (s, v) in best.items():
            if num in self.dsem:
                v = self.dsem[num].cnt
            if self.seen[eng].get(num, 0) < v:
                self.E[eng].wait_ge(s, v)
                self.seen[eng][num] = v

    def _collect(self, reads, writes, skip_num=None):
        evs = []
        for b in reads:
            evs.append(b.w)
        for b in writes:
            if b.w is not None and (skip_num is None or b.w[0].num != skip_num):
                evs.append(b.w)
            evs.extend(b.r.values())
        return evs

    def _update(self, ev, reads, writes):
        for b in reads:
            old = b.r.get(ev[0].num)
            if old is None or old[1] < ev[1]:
                b.r[ev[0].num] = ev
        for b in writes:
            b.w = ev
            b.r = {}

    def op(self, eng, fn, reads=(), writes=(), hard=False):
        self._wait_all(eng, self._collect(reads, writes), allow_own=hard)
        inst = fn()
        own = self.esem[eng]
        self.ecnt[eng] += 1
        inst.then_inc(own, 1)
        ev = (own, self.ecnt[eng])
        self._update(ev, reads, writes)
        if self.ecnt[eng] >= 12000:
            self._new_esem(eng)
        return ev

    def dma(self, q, sc, out, in_, reads=(), writes=(), is_output=False, **kw):
        self._wait_all(q, self._collect(reads, writes, skip_num=sc.sem.num))
        inst = self.E[q].dma_start(out=out, in_=in_, **kw)
        sc.cnt += 16
        inst.then_inc(sc.sem, 16)
        ev = (sc.sem, sc.cnt)
        self._update(ev, reads, writes)
        if is_output:
            self.out_events.append(ev)
        return ev

    def barrier(self):
        evs = []
        for k in self.E:
            if self.ecnt[k] > 0:
                evs.append((self.esem[k], self.ecnt[k]))
        for sc in self.dsem.values():
            if sc.cnt > 0:
                evs.append((sc.sem, sc.cnt))
        for k in self.E:
            self._wait_all(k, evs)

    def finish(self):
        self._wait_all("sp", self.out_events)


class Ring:
    def __init__(self, T, es, nc, name, n, shape, dtype):
        self.n = n
        self.t = [es.enter_context(nc.sbuf_tensor(f"r_{name}{i}", shape, dtype)) for i in range(n)]
        self.b = [Buf(f"{name}{i}") for i in range(n)]
        self.s = [T.semc(name) for i in range(n)]
        self.i = 0

    def next(self):
        k = self.i % self.n
        self.i += 1
        return self.t[k], self.b[k], self.s[k]


def _rope_tables():
    t = np.arange(SEQ)
    row = (t // 64).astype(np.float32)
    col = (t % 64).astype(np.float32)
    inv = np.power(np.float32(10000.0), -np.arange(16, dtype=np.float32) / np.float32(16)).astype(np.float32)
    ang = np.stack([row[:, None] * inv, col[:, None] * inv], axis=1).astype(np.float32)
    cs, sn = np.cos(ang).astype(np.float32), np.sin(ang).astype(np.float32)
    C = np.zeros((SEQ, 2, 2, 16), np.float32)
    S = np.zeros((SEQ, 2, 2, 16), np.float32)
    C[:, :, 0, :] = cs
    C[:, :, 1, :] = cs
    S[:, :, 0, :] = -sn
    S[:, :, 1, :] = sn
    C = C.reshape(NTL, 128, 64).transpose(1, 0, 2)
    S = S.reshape(NTL, 128, 64).transpose(1, 0, 2)
    return np.ascontiguousarray(C), np.ascontiguousarray(S)


def _band_mats():
    out = np.zeros((4, 5, 128, 128), np.float32)
    n = 1024
    for g, w in enumerate((2, 4, 8, 16)):
        B = np.zeros((n, n), np.float64)
        for t in range(n):
            lo = min(max(t - w // 2, 0), n)
            hi = min(max(t - w // 2 + w, 0), n)
            B[lo:hi, t] += 1.0 / (hi - lo)
            B[t, t] -= 1.0
        i = 3
        out[g, 0] = B[(i - 1) * 128:i * 128, i * 128:(i + 1) * 128]
        out[g, 1] = B[i * 128:(i + 1) * 128, i * 128:(i + 1) * 128]
        out[g, 2] = B[(i + 1) * 128:(i + 2) * 128, i * 128:(i + 1) * 128]
        out[g, 3] = B[0:128, 0:128]
        out[g, 4] = B[n - 128:n, n - 128:n]
    return np.ascontiguousarray(out.transpose(2, 0, 1, 3)).astype(ml_dtypes.bfloat16)


def _dft_consts():
    c = np.arange(64)
    ang = 2.0 * np.pi * ((c[:, None] * c[None, :]) % 64) / 64.0
    C64 = np.zeros((128, 128), np.float64)
    S64 = np.zeros((128, 128), np.float64)
    for a in range(2):
        C64[a * 64:(a + 1) * 64, a * 64:(a + 1) * 64] = np.cos(ang)
        S64[a * 64:(a + 1) * 64, a * 64:(a + 1) * 64] = np.sin(ang)
    s = np.arange(SEQ, dtype=np.int64)
    m = (s[:, None] * s[None, :]) % SEQ
    angn = (2.0 * np.pi / SEQ) * m
    CN = (np.cos(angn) / 512.0).astype(ml_dtypes.bfloat16)
    SN = (-np.sin(angn) / 512.0).astype(ml_dtypes.bfloat16)
    return C64.astype(ml_dtypes.bfloat16), S64.astype(ml_dtypes.bfloat16), CN, SN


def _na_index():
    MASK = 15 * 31
    idx = np.full((3, 8, 128, 512), MASK, np.int64)
    for bt, qb in enumerate((0, 3, 7)):
        for slot in range(8):
            kt = 4 * qb - 2 + slot
            if kt < 0 or kt >= 32:
                continue
            for krl in range(2):
                kr = 2 * kt + krl
                for qrl in range(8):
                    qr = 8 * qb + qrl
                    rs = min(max(qr - 4, 0), 56)
                    if not (rs <= kr < rs + 8):
                        continue
                    dr = kr - qr + 7
                    qc = np.arange(64)
                    cs = np.clip(qc - 8, 0, 48)
                    kc = np.arange(64)
                    valid = (kc[:, None] >= cs[None, :]) & (kc[:, None] < cs[None, :] + 16)
                    dc = kc[:, None] - qc[None, :] + 15
                    blk = np.where(valid, dr * 31 + dc, MASK)
                    idx[bt, slot, krl * 64:(krl + 1) * 64, qrl * 64:(qrl + 1) * 64] = blk
    return idx


_CONST_CACHE = {}


def _consts():
    if not _CONST_CACHE:
        C, S = _rope_tables()
        C64, S64, CN, SN = _dft_consts()
        _CONST_CACHE.update(dict(
            ropeC=C, ropeS=S, band=_band_mats(), C64=C64, S64=S64, CN=CN, SN=SN,
            identf=np.eye(128, dtype=np.float32), naidx=_na_index()))
    return _CONST_CACHE


def build_program(n_layers=2, debug=False, stop_after=None):
    global _LAST_NC
    nc = bass.Bass("TRN2", target_bir_lowering=False)
    _LAST_NC = nc
    es = contextlib.ExitStack()

    def din(name, shape, dt=F32):
        return nc.dram_tensor(name, list(shape), dt, kind="ExternalInput").ap()

    def dscr(name, shape, dt=F32):
        return nc.dram_tensor(name, list(shape), dt, kind="Internal").ap()

    x_in = din("x", [SEQ, D])
    ctx_in = din("ctx", [CTXL, D])
    cvec = din("cvec", [128, 8, 2])
    ada_w = din("ada_w", [2, D, 6 * D])
    ada_b = din("ada_b", [2, 6 * D])
    adab_col = din("adab_col", [2, 128, 48])
    ln_mix_g = din("ln_mix_g", [2, D]); ln_mix_b = din("ln_mix_b", [2, D])
    ln_ffn_g = din("ln_ffn_g", [2, D]); ln_ffn_b = din("ln_ffn_b", [2, D])
    w_out = din("w_out", [2, D, D])
    ev_w_in = din("ev_w_in", [D, 1536])
    ev_pool_w = din("ev_pool_w", [4, 64, 64])
    ev_pool_scale = din("ev_pool_scale", [256])
    ev_qk_gain = din("ev_qk_gain", [1024])
    ev_ffn_w1 = din("ev_ffn_w1", [D, FFN]); ev_ffn_w3 = din("ev_ffn_w3", [D, FFN]); ev_ffn_w2 = din("ev_ffn_w2", [FFN, D])
    if n_layers == 2:
        od_w_in = din("od_w_in", [D, 2560])
        od_fgain = din("od_fgain", [256])
        od_bias = din("od_bias", [12, 3, 8, 128, 512])
        od_router = din("od_router", [D, 8])
        od_w1 = din("od_w1", [NEXP, D, FFN]); od_w3 = din("od_w3", [NEXP, D, FFN]); od_w2 = din("od_w2", [NEXP, FFN, D])
    ropeC = din("ropeC", [128, NTL, 64]); ropeS = din("ropeS", [128, NTL, 64])
    band = din("band", [128, 4, 5, 128], BF16)
    if n_layers == 2:
        C64 = din("C64", [128, 128], BF16); S64 = din("S64", [128, 128], BF16)
        CN = din("CN", [SEQ, SEQ], BF16); SN = din("SN", [SEQ, SEQ], BF16)
    identf_in = din("identf", [128, 128])

    out_ap = nc.dram_tensor("out", [SEQ, D], F32, kind="ExternalOutput").ap()
    dbg = {}
    if debug:
        for nm, shp in (("dbg_x1", [NT * 128, D]), ("dbg_x2", [NT * 128, D]), ("dbg_x3", [SEQ, D])):
            dbg[nm] = nc.dram_tensor(nm, shp, F32, kind="ExternalOutput").ap()

    X1 = dscr("X1", [NT * 128, D]) if not debug else dbg["dbg_x1"]
    X2 = dscr("X2", [NT * 128, D]) if not debug else dbg["dbg_x2"]
    X3 = dscr("X3", [SEQ, D]) if not debug else dbg["dbg_x3"]
    X1b = [Buf(f"X1_{i}") for i in range(NT)]
    X2b = [Buf(f"X2_{i}") for i in range(NT)]
    X3b = [Buf(f"X3_{i}") for i in range(NTL)]

    T = Tracker(nc, es)

    def dump(name, ap, shape, dt, reads):
        if not debug:
            return
        d = nc.dram_tensor("dbg_" + name, list(shape), dt, kind="ExternalOutput").ap()
        T.dma("sp", T.semc("dbg"), d, ap, reads=reads, is_output=True)
    pe, act, dve, pool, sp = nc.tensor, nc.scalar, nc.vector, nc.gpsimd, nc.sync

    def sb(name, shape, dt, stack=None):
        return (stack or es).enter_context(nc.sbuf_tensor("s_" + name, list(shape), dt))

    def ps(name, shape, dt, stack):
        return stack.enter_context(nc.psum_tensor("p_" + name, list(shape), dt))

    def x_src(t):
        return x_in[t * 128:(t + 1) * 128, :] if t < NTL else ctx_in[(t - NTL) * 128:(t - NTL + 1) * 128, :]

    identf = sb("identf", [128, 128], F32); identf_b = Buf()
    identb = sb("identb", [128, 128], BF16); identb_b = Buf()
    onesf = sb("onesf", [128, 64], F32); ones_b = Buf()
    cst_s = T.semc("cst")
    T.dma("sp", cst_s, identf[:], identf_in[:, :], writes=[identf_b])
    T.dma("pool", cst_s, identb[:], identf_in[:, :], writes=[identb_b])
    T.op("dve", lambda: dve.memset(onesf[:], 1.0), writes=[ones_b])
    sel64 = sb("sel64", [128, 128], F32); sel_b = Buf()
    rcp_t = sb("rcp_t", [64, 512], F32); rcp_b = Buf()

    def _sel():
        dve.memset(sel64[:], 0.0)
        return dve.memset(sel64[64:65, :], 1.0)
    T.op("dve", _sel, writes=[sel_b])

    modcol = [sb(f"modcol{l}", [128, 4, 8, 2], F32) for l in range(2)]
    modcol_b = [Buf() for l in range(2)]
    epsc = sb("epsc", [128, 2], F32); eps_b = Buf()

    def _eps():
        dve.memset(epsc[:, 0:1], LN_EPS)
        return dve.memset(epsc[:, 1:2], RMS_EPS)
    T.op("dve", _eps, writes=[eps_b])
    AD = dscr("AD", [NT * 128, 256], BF16)
    AD_b = [Buf() for _ in range(NT)]

    def load_lnp(l, k, stack, sc):
        src = (ln_mix_g, ln_mix_b, ln_ffn_g, ln_ffn_b)[k]
        t = sb(f"lnp{l}{k}", [128, D], F32, stack)
        b = Buf()
        T.dma("sp", sc, t[:], src[l:l + 1, :].to_broadcast([128, D]), writes=[b])
        return t, b

    def compute_mod(l, lstack, mstack):
        nstream = 2 if l == 0 else 1
        gate = {}
        gate_b = {}
        for g in (1, 0):
            for s in range(nstream):
                gate[(s, g)] = sb(f"gate{l}{s}{g}", [128, D], F32, mstack if g == 0 else lstack)
                gate_b[(s, g)] = Buf()
        with contextlib.ExitStack() as st:
            cc = sb(f"cc{l}", [128, 8, 2], F32, st); cc_b = Buf()
            ccrep = sb(f"ccrep{l}", [128, 8, 2, 128], F32, st); ccrep_b = Buf()
            abcol = sb(f"abcol{l}", [128, 48], F32, st); abcol_b = Buf()
            abrow = sb(f"abrow{l}", [128, 2048], F32, st); abrow_b = Buf()
            ms_ = T.semc("mod")
            T.dma("sp", ms_, cc[:], cvec[:, :, :], writes=[cc_b])
            T.dma("sp", ms_, abcol[:], adab_col[l, :, :], writes=[abcol_b])
            for gi, seg in enumerate((2, 5)):
                T.dma("sp", ms_, abrow[:, gi * 1024:(gi + 1) * 1024],
                      ada_b[l:l + 1, seg * 1024:(seg + 1) * 1024].to_broadcast([128, 1024]), writes=[abrow_b])
            T.op("act", lambda: act.activation(out=cc[:], in_=cc[:], func=AF.Silu), reads=[cc_b], writes=[cc_b])

            def _rep():
                i = None
                for k in range(8):
                    for s in range(2):
                        i = dve.tensor_copy(out=ccrep[:, k, s, :], in_=cc[:, k, s:s + 1].to_broadcast([128, 128]))
                return i
            T.op("dve", _rep, reads=[cc_b], writes=[ccrep_b])
            wring = Ring(T, st, nc, f"adaw{l}", 2, [128, 8, 512], F32)
            ps_c = ps(f"ps_c{l}", [128, 512], F32, st); ps_c_b = Buf()
            ps_g = [ps(f"ps_g{l}{i}", [128, 512], F32, st) for i in range(2)]; ps_g_b = [Buf(), Buf()]
            for j in range(12):
                seg, hf = j // 2, j % 2
                wt, wb, ws = wring.next()
                T.dma("sp", ws, wt[:], ada_w[l, :, j * 512:(j + 1) * 512].rearrange("(k p) n -> p k n", p=128),
                      writes=[wb])
                if seg in (2, 5):
                    gi = 0 if seg == 2 else 1
                    for s in range(nstream):
                        def _mm(s=s, wt=wt):
                            i = None
                            for k in range(8):
                                i = pe.matmul(ps_g[s][:, :], lhsT=ccrep[:, k, s, :], rhs=wt[:, k, :],
                                              start=(k == 0), stop=(k == 7))
                            return i
                        T.op("pe", _mm, reads=[ccrep_b, wb], writes=[ps_g_b[s]])
                        T.op("dve", lambda s=s, gi=gi, hf=hf: dve.tensor_tensor(
                            out=gate[(s, gi)][:, hf * 512:(hf + 1) * 512], in0=ps_g[s][:, :],
                            in1=abrow[:, gi * 1024 + hf * 512: gi * 1024 + (hf + 1) * 512], op=ALU.add),
                            reads=[ps_g_b[s], abrow_b], writes=[gate_b[(s, gi)]])
                else:
                    vi = {0: 0, 1: 1, 3: 2, 4: 3}[seg]
                    for c4 in range(4):
                        def _mm(c4=c4, wt=wt):
                            i = None
                            for k in range(8):
                                i = pe.matmul(ps_c[:, 0:2], lhsT=wt[:, k, c4 * 128:(c4 + 1) * 128], rhs=cc[:, k, :],
                                              start=(k == 0), stop=(k == 7))
                            return i
                        T.op("pe", _mm, reads=[cc_b, wb], writes=[ps_c_b])
                        chunk = hf * 4 + c4
                        colidx = seg * 8 + chunk
                        T.op("dve", lambda vi=vi, chunk=chunk, colidx=colidx: dve.tensor_scalar(
                            out=modcol[l][:, vi, chunk, :], in0=ps_c[:, 0:2],
                            scalar1=abcol[:, colidx:colidx + 1], scalar2=(1.0 if vi in (1, 3) else 0.0),
                            op0=ALU.add, op1=ALU.add),
                            reads=[ps_c_b, abcol_b], writes=[modcol_b[l]])
        T.barrier()
        if l == 0:
            dump("modcol", modcol[0][:].rearrange("p a b c -> p (a b c)"), [128, 64], F32, [modcol_b[0]])
            dump("gate00", gate[(0, 0)][:], [128, D], F32, [gate_b[(0, 0)]])
        return gate, gate_b

    def make_hT(xt, xb, l, vsh, vsc, stream, ps_tr, ps_tr_b, hT_ap, hT_b, hTf_ap=None, hTf_b=None):
        def _tr():
            i = None
            for k in range(8):
                i = pe.transpose(ps_tr[:, k * 128:(k + 1) * 128], xt[:, k * 128:(k + 1) * 128], identf[:])
            return i
        T.op("pe", _tr, reads=[xb, identf_b], writes=[ps_tr_b])

        def _ev():
            i = None
            for k in range(8):
                i = dve.tensor_scalar(out=hT_ap(k), in0=ps_tr[:, k * 128:(k + 1) * 128],
                                      scalar1=modcol[l][:, vsc, k, stream:stream + 1],
                                      scalar2=modcol[l][:, vsh, k, stream:stream + 1], op0=ALU.mult, op1=ALU.add)
            return i
        T.op("dve", _ev, reads=[ps_tr_b, modcol_b[l]], writes=[hT_b])
        if hTf_ap is not None:
            def _ev2():
                i = None
                for k in range(8):
                    i = dve.tensor_scalar(out=hTf_ap(k), in0=ps_tr[:, k * 128:(k + 1) * 128],
                                          scalar1=modcol[l][:, vsc, k, stream:stream + 1],
                                          scalar2=modcol[l][:, vsh, k, stream:stream + 1], op0=ALU.mult, op1=ALU.add)
                return i
            T.op("dve", _ev2, reads=[ps_tr_b, modcol_b[l]], writes=[hTf_b])

    lnw = {}
    lnw["st"] = sb("ln_st", [128, 2, 6], F32); lnw["mv"] = sb("ln_mv", [128, 2], F32)
    lnw["sd"] = sb("ln_sd", [128, 1], F32); lnw["rs"] = sb("ln_rs", [128, 1], F32)
    lnw["b0"] = Buf(); lnw["b1"] = Buf(); lnw["b2"] = Buf(); lnw["b3"] = Buf()

    def resid_ln(ps_halves, ps_bufs, zt, zb, gate_t, gate_bf, lg, lgb, lb, lbb, tmp, tmp_b):
        stt, mv, sd, rs = lnw["st"], lnw["mv"], lnw["sd"], lnw["rs"]

        def _a():
            for h in range(2):
                dve.tensor_tensor(out=tmp[:, h * 512:(h + 1) * 512], in0=ps_halves[h],
                                  in1=gate_t[:, h * 512:(h + 1) * 512], op=ALU.mult)
            dve.scalar_tensor_tensor(out=zt[:], in0=zt[:], scalar=ALPHA, in1=tmp[:], op0=ALU.mult, op1=ALU.add)
            i = None
            for h in range(2):
                i = dve.bn_stats(out=stt[:, h, :], in_=zt[:, h * 512:(h + 1) * 512])
            return i
        T.op("dve", _a, reads=list(ps_bufs) + [gate_bf], writes=[zb, tmp_b, lnw["b0"]])
        T.op("dve", lambda: dve.bn_aggr(out=mv[:], in_=stt[:].rearrange("p a b -> p (a b)")),
             reads=[lnw["b0"]], writes=[lnw["b1"]], hard=True)
        T.op("act", lambda: act.activation(out=sd[:], in_=mv[:, 1:2], func=AF.Sqrt, bias=epsc[:, 0:1], scale=1.0),
             reads=[lnw["b1"], eps_b], writes=[lnw["b2"]])
        T.op("dve", lambda: dve.reciprocal(out=rs[:], in_=sd[:]), reads=[lnw["b2"]], writes=[lnw["b3"]])

        def _b():
            dve.tensor_scalar(out=zt[:], in0=zt[:], scalar1=mv[:, 0:1], scalar2=rs[:, 0:1],
                              op0=ALU.subtract, op1=ALU.mult)
            dve.tensor_tensor(out=zt[:], in0=zt[:], in1=lg[:], op=ALU.mult)
            return dve.tensor_tensor(out=zt[:], in0=zt[:], in1=lb[:], op=ALU.add)
        T.op("dve", _b, reads=[lnw["b3"], lnw["b1"], lgb, lbb], writes=[zb], hard=True)

    def rms_heads(src_ap, nh, sq, ms, rt, dst_ap, wbuf, reads, gain_t=None, gain_b=None):
        T.op("act", lambda: act.activation(out=sq[:, 0:nh * 64], in_=src_ap, func=AF.Square),
             reads=reads, writes=[wbuf["sq"]])
        T.op("dve", lambda: dve.tensor_reduce(out=ms[:, 0:nh], in_=sq[:, 0:nh * 64].rearrange("p (h d) -> p h d", d=64),
                                              axis=AX.X, op=ALU.add),
             reads=[wbuf["sq"]], writes=[wbuf["ms"]])
        T.op("act", lambda: act.activation(out=rt[:, 0:nh], in_=ms[:, 0:nh], func=AF.Sqrt, bias=epsc[:, 1:2],
                                           scale=1.0 / 64.0),
             reads=[wbuf["ms"], eps_b], writes=[wbuf["rt"]])

        T.op("dve", lambda: dve.reciprocal(out=rt[:, 0:nh], in_=rt[:, 0:nh]), reads=[wbuf["rt"]], writes=[wbuf["rt"]])

        def _n():
            i = dve.tensor_tensor(out=dst_ap, in0=src_ap.rearrange("p (h d) -> p h d", d=64),
                                  in1=rt[:, 0:nh].unsqueeze(2).to_broadcast([128, nh, 64]), op=ALU.mult)
            if gain_t is not None:
                i = dve.tensor_tensor(out=dst_ap, in0=dst_ap, in1=gain_t, op=ALU.mult)
            return i
        T.op("dve", _n, reads=list(reads) + [wbuf["rt"]] + ([gain_b] if gain_b else []), writes=[wbuf["dst"]], hard=True)

    acnt = [0]

    asteps = []
    apre = []

    def attn_head(qT_ap, kT_fn, v_fn, key_list, ps_s, ps_s_b, pT_ring, ps_o, ps_o_b, nq, reads_q, bias_fn=None,
                  pre=None, post=None):
        nk = len(key_list)
        hseq = len(apre)
        apre.append(pre)
        for i, kt in enumerate(key_list):
            sidx = acnt[0] % 2
            acnt[0] += 1

            def s_fn(kt=kt, sidx=sidx):
                kap, kb = kT_fn(kt)
                bi = bias_fn(kt) if bias_fn is not None else None

                def _s():
                    ins = pe.matmul(ps_s[sidx][:, 0:nq], lhsT=kap, rhs=qT_ap, start=True, stop=(bi is None))
                    if bi is not None:
                        ins = pe.matmul(ps_s[sidx][:, 0:nq], lhsT=identb[:], rhs=bi[0], start=False, stop=True)
                    return ins
                T.op("pe", _s, reads=[kb] + list(reads_q) + ([bi[1], identb_b] if bi else []), writes=[ps_s_b[sidx]])

            def epv_fn(kt=kt, sidx=sidx, i=i):
                pt, pb, _ = pT_ring.next()
                T.op("act", lambda: act.activation(out=pt[:, 0:nq], in_=ps_s[sidx][:, 0:nq], func=AF.Exp),
                     reads=[ps_s_b[sidx]], writes=[pb])
                vap, vb = v_fn(kt)
                T.op("pe", lambda: pe.matmul(ps_o[0:65, 0:nq], lhsT=vap, rhs=pt[:, 0:nq],
                                             start=(i == 0), stop=(i == nk - 1)),
                     reads=[vb, pb], writes=[ps_o_b])
            asteps.append((s_fn, epv_fn, post if i == nk - 1 else None, hseq))

    def attn_flush(lookahead=2):
        n = len(asteps)
        done_pre = [0]

        def run_pre(upto):
            while done_pre[0] <= min(upto, len(apre) - 1):
                f = apre[done_pre[0]]
                if f is not None:
                    f()
                done_pre[0] += 1
        pend = []
        for j in range(n):
            run_pre(asteps[j][3] + lookahead)
            if j == 0:
                asteps[0][0]()
            if j + 1 < n:
                run_pre(asteps[j + 1][3])
                asteps[j + 1][0]()
            asteps[j][1]()
            while pend and pend[0][0] <= j:
                pend.pop(0)[1]()
            if asteps[j][2] is not None:
                pend.append((j + 3, asteps[j][2]))
        for _, f in pend:
            f()
        asteps.clear()
        apre.clear()

    def attn_norm(ps_o, ps_o_b, osb, osb_b, ps_bc, ps_bc_b, dst_ap, dst_b, nq):
        T.op("dve", lambda: dve.tensor_copy(out=osb[0:65, 0:nq], in_=ps_o[0:65, 0:nq]), reads=[ps_o_b], writes=[osb_b])
        T.op("pe", lambda: pe.matmul(ps_bc[:, 0:nq], lhsT=sel64[:, :], rhs=osb[:, 0:nq], start=True, stop=True),
             reads=[osb_b, sel_b], writes=[ps_bc_b])
        def _n():
            dve.reciprocal(out=rcp_t[0:64, 0:nq], in_=ps_bc[0:64, 0:nq])
            return dve.tensor_tensor(out=dst_ap, in0=osb[0:64, 0:nq], in1=rcp_t[0:64, 0:nq], op=ALU.mult)
        T.op("dve", _n, reads=[osb_b, ps_bc_b], writes=[dst_b, rcp_b])

    def x_src(t):
        return x_in[t * 128:(t + 1) * 128, :] if t < NTL else ctx_in[(t - NTL) * 128:(t - NTL + 1) * 128, :]

    def qhead_pos(h):
        return (h % 3) + 3 * (h // 6), (h // 3) % 2

    L = 0
    with contextlib.ExitStack() as stL0:
        stR = contextlib.ExitStack()
        gate, gate_b = compute_mod(0, stL0, stR)
        QT = sb("QT", [128, 6, SEQ + CTXL], BF16, stR); QT_b = [Buf() for _ in range(NT)]
        KT = sb("KT", [128, 2, SEQ + CTXL], BF16, stR); KT_b = [Buf() for _ in range(NT)]
        VA = sb("VA", [128, NT, 4, 66], BF16, stR); VA_b = [Buf() for _ in range(NT)]
        bandt = sb("bandt", [128, 4, 5, 128], BF16, stR); band_b = Buf()
        poolw = sb("poolw", [64, 4, 64], BF16, stR); poolw_b = Buf()
        w0_s = T.semc("w0")
        T.op("dve", lambda: dve.memset(VA[:].rearrange("p t k d -> p (t k) d")[:, :, 64:65], 1.0), writes=VA_b)
        T.dma("sp", w0_s, bandt[:], band[:, :, :, :], writes=[band_b])

        with contextlib.ExitStack() as stA:
            win = sb("win0", [128, 8, 1536], BF16, stA); win_b = Buf()
            T.dma("pool", w0_s, win[:], ev_w_in.rearrange("(k p) n -> p k n", p=128), writes=[win_b])
            if n_layers == 2:
                W1s = dscr("W1s", [NEXP, 11, 128, 8, 256], BF16); W3s = dscr("W3s", [NEXP, 11, 128, 8, 256], BF16)
                W2s = dscr("W2s", [NEXP, 2, 128, NF, 512], BF16)
                W1s_b = [Buf() for _ in range(NEXP)]; W3s_b = [Buf() for _ in range(NEXP)]; W2s_b = [Buf() for _ in range(NEXP)]
                wc_s = T.semc("wcast")
                for e in range(NEXP):
                    for j in range(11):
                        T.dma("pool", wc_s, W1s[e, j], od_w1[e][:, j * 256:(j + 1) * 256].rearrange("(k p) n -> p k n", p=128),
                              writes=[W1s_b[e]])
                        T.dma("pool", wc_s, W3s[e, j], od_w3[e][:, j * 256:(j + 1) * 256].rearrange("(k p) n -> p k n", p=128),
                              writes=[W3s_b[e]])
                    for hh in range(2):
                        T.dma("pool", wc_s, W2s[e, hh], od_w2[e][:, hh * 512:(hh + 1) * 512].rearrange("(f p) n -> p f n", p=128),
                              writes=[W2s_b[e]])
            rring = Ring(T, stA, nc, "rope", 2, [128, 2, 64], F32)
            gaint = sb("gaint", [128, 16, 64], F32, stA); gain_b = Buf()
            T.dma("sp", w0_s, gaint[:].rearrange("p h d -> p (h d)"),
                  ev_qk_gain.rearrange("(o n) -> o n", o=1).to_broadcast([128, 1024]), writes=[gain_b])
            T.op("dve", lambda: dve.tensor_scalar(out=gaint[:, 0:12, :], in0=gaint[:, 0:12, :], scalar1=0.125,
                                                  scalar2=None, op0=ALU.mult), reads=[gain_b], writes=[gain_b])
            pwf = sb("pwf", [64, 4, 64], F32, stA); pws = sb("pws", [64, 4, 64], F32, stA); pw_b = Buf()
            T.dma("sp", w0_s, pwf[:], ev_pool_w.rearrange("g c d -> c g d"), writes=[pw_b])
            T.dma("sp", w0_s, pws[:].rearrange("p g d -> p (g d)"),
                  ev_pool_scale.rearrange("(o n) -> o n", o=1).to_broadcast([64, 256]), writes=[pw_b])
            T.op("dve", lambda: dve.tensor_tensor(out=poolw[:], in0=pwf[:], in1=pws[:], op=ALU.mult),
                 reads=[pw_b], writes=[poolw_b])

            xring = Ring(T, stA, nc, "xa", 2, [128, D], F32)
            aring = Ring(T, stA, nc, "aa", 2, [128, 256], BF16)
            hT = sb("hTa", [128, 8, 128], BF16, stA); hT_b = Buf()
            ms = sb("msa", [128, 16], F32, stA); rt = sb("rta", [128, 16], F32, stA)
            qn = sb("qna", [128, 16, 64], F32, stA)
            t1 = sb("t1a", [128, 16, 64], F32, stA); t2 = sb("t2a", [128, 16, 64], F32, stA)
            sq = t2[:].rearrange("p h d -> p (h d)")
            stg = sb("stga", [128, 1024], BF16, stA)
            t12_b = Buf(); stg_b = Buf()
            wb_ = {"sq": t12_b, "ms": Buf(), "rt": Buf(), "dst": Buf()}
            ps_tr = ps("ps_trA", [128, 1024], F32, stA); ps_tr_b = Buf()
            ps_pj = ps("ps_pjA", [128, 1536], F32, stA); ps_pj_b = Buf()
            ps_t2 = ps("ps_t2A", [128, 1024], BF16, stA); ps_t2_b = Buf()
            for t in range(NT):
                stream = 0 if t < NTL else 1
                xt, xb, xs = xring.next()
                T.dma("sp", xs, xt[:], x_src(t), writes=[xb])
                make_hT(xt, xb, L, 0, 1, stream, ps_tr, ps_tr_b, lambda k: hT[:, k, :], hT_b)

                def _pj():
                    i = None
                    for (c0, n, w0) in ((0, 512, 256), (512, 512, 768), (1024, 256, 0), (1280, 256, 1280)):
                        for k in range(8):
                            i = pe.matmul(ps_pj[:, c0:c0 + n], lhsT=hT[:, k, :],
                                          rhs=win[:, k, w0:w0 + n], start=(k == 0), stop=(k == 7))
                    return i
                T.op("pe", _pj, reads=[hT_b, win_b], writes=[ps_pj_b])
                if t == 0:
                    dump("hT", hT[:].rearrange("p a b -> p (a b)"), [128, 1024], BF16, [hT_b])
                at_, ab_, as_ = aring.next()

                def _av(t=t, at_=at_):
                    act.copy(out=at_[:], in_=ps_pj[:, 1024:1280])
                    return act.copy(out=VA[:, t, :, 0:64], in_=ps_pj[:, 1280:1536].rearrange("p (k d) -> p k d", d=64))
                T.op("act", _av, reads=[ps_pj_b], writes=[ab_, VA_b[t]])
                T.dma("sp", as_, AD[t * 128:(t + 1) * 128, :], at_[:], reads=[ab_], writes=[AD_b[t]])
                rms_heads(ps_pj[:, 0:1024], 16, sq, ms, rt, qn[:], wb_, [ps_pj_b], gaint[:], gain_b)
                if t == 0:
                    dump("ms", ms[:], [128, 16], F32, [wb_["ms"]])
                    dump("qn", qn[:].rearrange("p a b -> p (a b)"), [128, 1024], F32, [wb_["dst"]])
                stq = stg[:, 0:768].rearrange("p (jj r hf d) -> p jj r hf d", jj=2, r=3, hf=2)
                stk = stg[:, 768:1024].rearrange("p (k d) -> p k d", d=64)

                def qperm(tt):
                    return tt[:, 0:12, :].rearrange("p (jj hf r) d -> p jj r hf d", jj=2, hf=2, r=3)
                if t < NTL:
                    rt_, rb_, rs_ = rring.next()
                    T.dma("sp", rs_, rt_[:, 0, :], ropeC[:, t, :], writes=[rb_])
                    T.dma("sp", rs_, rt_[:, 1, :], ropeS[:, t, :], writes=[rb_])

                    def _rope(rt_=rt_):
                        cb = rt_[:, 0, :].unsqueeze(1).to_broadcast([128, 16, 64])
                        dve.tensor_tensor(out=t1[:], in0=qn[:], in1=cb, op=ALU.mult)
                        qv = qn[:].rearrange("p h (a f) -> p h a f", a=4)
                        tv = t2[:].rearrange("p h (a f) -> p h a f", a=4)
                        sv = rt_[:, 1, :].rearrange("p (a f) -> p a f", a=4)
                        i = None
                        for ax in range(2):
                            for hf in range(2):
                                a_dst = ax * 2 + hf
                                a_src = ax * 2 + (1 - hf)
                                i = dve.tensor_tensor(out=tv[:, :, a_dst, :], in0=qv[:, :, a_src, :],
                                                      in1=sv[:, a_dst, :].unsqueeze(1).to_broadcast([128, 16, 16]),
                                                      op=ALU.mult)
                        return i
                    T.op("dve", _rope, reads=[wb_["dst"], rb_], writes=[t12_b])

                    def _st():
                        for jj in range(2):
                            dve.tensor_tensor(out=stq[:, jj], in0=qperm(t1)[:, jj], in1=qperm(t2)[:, jj], op=ALU.add)
                        return dve.tensor_tensor(out=stk, in0=t1[:, 12:16, :], in1=t2[:, 12:16, :], op=ALU.add)
                    T.op("dve", _st, reads=[t12_b], writes=[stg_b])
                else:
                    def _st():
                        for jj in range(2):
                            dve.tensor_copy(out=stq[:, jj], in_=qperm(qn)[:, jj])
                        return dve.tensor_copy(out=stk, in_=qn[:, 12:16, :])
                    T.op("dve", _st, reads=[wb_["dst"]], writes=[stg_b])

                def _t2():
                    i = None
                    for j in range(8):
                        i = pe.transpose(ps_t2[:, j * 128:(j + 1) * 128], stg[:, j * 128:(j + 1) * 128], identb[:])
                    return i
                T.op("pe", _t2, reads=[stg_b, identb_b], writes=[ps_t2_b])

                def _e2(t=t):
                    act.copy(out=QT[:, :, t * 128:(t + 1) * 128],
                             in_=ps_t2[:, 0:768].rearrange("p (j n) -> p j n", n=128))
                    return act.copy(out=KT[:, :, t * 128:(t + 1) * 128],
                                    in_=ps_t2[:, 768:1024].rearrange("p (j n) -> p j n", n=128))
                T.op("act", _e2, reads=[ps_t2_b], writes=[QT_b[t], KT_b[t]])
                if t == 0:
                    dump("stg", stg[:], [128, 1024], BF16, [stg_b])
                    dump("QT0", QT[:, :, 0:128], [128, 6, 128], BF16, [QT_b[0]])
                    dump("KT0", KT[:, :, 0:128], [128, 2, 128], BF16, [KT_b[0]])
                    dump("VA0", VA[:, 0, :, :], [128, 4, 66], BF16, [VA_b[0]])

        T.barrier()
        with contextlib.ExitStack() as stB:
            wout = sb("wout0", [128, 16, D], BF16, stB); wout_b = Buf()
            T.op("pool", lambda: pool.memset(wout[64:128, :, :], 0.0), writes=[wout_b])
            T.dma("pool", w0_s, wout[0:64], w_out[0].rearrange("(c p) n -> p c n", p=64), writes=[wout_b])
            lg, lgb = load_lnp(0, 0, stB, w0_s)
            lb, lbb = load_lnp(0, 1, stB, w0_s)
            ps_s = [ps(f"ps_s{i}", [128, 512], F32, stB) for i in range(2)]; ps_s_b = [Buf(), Buf()]
            ps_o = [ps(f"ps_o{i}", [128, 512], F32, stB) for i in range(2)]; ps_o_b = [Buf(), Buf()]
            ps_bc = ps("ps_bc", [128, 512], F32, stB); ps_bc_b = Buf()
            ps_y = ps("ps_y", [128, 1024], F32, stB); ps_y_b = Buf()
            ps_pl = ps("ps_pl", [128, 512], F32, stB); ps_pl_b = Buf()
            pT_ring = Ring(T, stB, nc, "pT", 3, [128, 512], BF16)
            osb = [sb(f"osb{i}", [128, 512], F32, stB) for i in range(2)]; osb_b = [Buf(), Buf()]
            for i in range(2):
                T.op("pool", lambda i=i: pool.memset(osb[i][:], 0.0), writes=[osb_b[i]])
            OT = sb("OT0", [128, 12, 512], BF16, stB); OT_b = [Buf() for _ in range(12)]
            T.op("pool", lambda: pool.memset(OT[64:128, :, :], 0.0), writes=OT_b)
            mT = sb("mT", [64, 4, 128], BF16, stB); mT_b = Buf()
            PTt = sb("PTt", [128, 4, 128], BF16, stB); PT_b = Buf()
            T.op("pool", lambda: pool.memset(PTt[64:128, :, :], 0.0), writes=[PT_b])
            a3ring = Ring(T, stB, nc, "a3", 2, [128, 3, 256], BF16)
            zring = Ring(T, stB, nc, "zb", 2, [128, D], F32)
            tmp = sb("tmpb", [128, D], F32, stB); tmp_b = Buf()
            hcount = 0
            chunks = [(qc * 4, 4, list(range(NT))) for qc in range(8)] + [(NTL, 2, [NTL, NTL + 1])]
            qz = sb("Qz0", [128, 12, 512], BF16, stB); Qz_b = [Buf() for _ in range(12)]
            T.op("pool", lambda: pool.memset(qz[:], 0.0), writes=Qz_b)
            for ci, (t0, ntl, keys) in enumerate(chunks):
                nq = ntl * 128
                stream = 0 if t0 < NTL else 1
                for h in range(12):
                    ch, half = qhead_pos(h)
                    rows = slice(half * 64, (half + 1) * 64)
                    T.op("pool", lambda h=h, ch=ch, rows=rows, t0=t0, nq=nq: pool.tensor_copy(
                        out=qz[rows, h, 0:nq], in_=QT[rows, ch, t0 * 128:t0 * 128 + nq]),
                        reads=[QT_b[t0 + i] for i in range(ntl)], writes=[Qz_b[h]])
                for h in range(12):
                    ch, half = qhead_pos(h)
                    kv = h // 3
                    rows = slice(half * 64, (half + 1) * 64)
                    oi = hcount % 2
                    hcount += 1
                    attn_head(qz[:, h, 0:nq],
                              lambda kt, kv=kv: (KT[:, kv // 2, kt * 128:(kt + 1) * 128], KT_b[kt]),
                              lambda kt, kv=kv: (VA[:, kt, kv, 0:65], VA_b[kt]),
                              keys, ps_s, ps_s_b, pT_ring, ps_o[oi], ps_o_b[oi], nq,
                              [Qz_b[h]],
                              post=lambda oi=oi, h=h, nq=nq: attn_norm(ps_o[oi], ps_o_b[oi], osb[oi], osb_b[oi], ps_bc, ps_bc_b,
                                                                       OT[0:64, h, 0:nq], OT_b[h], nq))
                attn_flush()
                for il in range(ntl):
                    t = t0 + il
                    first = (t == 0) or (t == NTL)
                    last = (t == NTL - 1) or (t == NT - 1)
                    tlo = t if first else t - 1
                    thi = t if last else t + 1
                    a3, a3b, a3s = a3ring.next()
                    nsrc = thi - tlo + 1
                    T.dma("sp", a3s, a3[:, 0:nsrc, :], AD[tlo * 128:(thi + 1) * 128, :].rearrange("(j p) c -> p j c", p=128),
                          reads=[AD_b[i] for i in range(tlo, thi + 1)], writes=[a3b])
                    srcs = []
                    if not first:
                        srcs.append((t - 1 - tlo, 0))
                    srcs.append((t - tlo, 3 if first else (4 if last else 1)))
                    if not last:
                        srcs.append((t + 1 - tlo, 2))

                    def _pm(srcs=srcs, a3=a3):
                        i = None
                        for g in range(4):
                            for j, (sl, var) in enumerate(srcs):
                                i = pe.matmul(ps_pl[0:64, g * 128:(g + 1) * 128], lhsT=a3[:, sl, g * 64:(g + 1) * 64],
                                              rhs=bandt[:, g, var, :], start=(j == 0), stop=(j == len(srcs) - 1))
                        return i
                    T.op("pe", _pm, reads=[a3b, band_b], writes=[ps_pl_b])
                    T.op("act", lambda: act.copy(out=mT[:].rearrange("p g n -> p (g n)"), in_=ps_pl[0:64, :]),
                         reads=[ps_pl_b], writes=[mT_b])

                    def _pp():
                        i = None
                        for g in range(4):
                            i = pe.matmul(ps_pl[0:64, g * 128:(g + 1) * 128], lhsT=poolw[:, g, :], rhs=mT[:, g, :],
                                          start=True, stop=True)
                        return i
                    T.op("pe", _pp, reads=[mT_b, poolw_b], writes=[ps_pl_b])
                    T.op("act", lambda: act.copy(out=PTt[0:64].rearrange("p g n -> p (g n)"), in_=ps_pl[0:64, :]),
                         reads=[ps_pl_b], writes=[PT_b])

                    def _y(il=il):
                        i = None
                        for n in range(2):
                            for c in range(16):
                                lhs = PTt[:, c, :] if c < 4 else OT[:, c - 4, il * 128:(il + 1) * 128]
                                i = pe.matmul(ps_y[:, n * 512:(n + 1) * 512], lhsT=lhs, rhs=wout[:, c, n * 512:(n + 1) * 512],
                                              start=(c == 0), stop=(c == 15))
                        return i
                    T.op("pe", _y, reads=[PT_b, wout_b] + OT_b, writes=[ps_y_b])
                    if t == 0:
                        dump("OT", OT[0:64], [64, 12, 512], BF16, OT_b)
                        dump("PT", PTt[0:64], [64, 4, 128], BF16, [PT_b])
                    zt, zb, zs = zring.next()
                    T.dma("sp", zs, zt[:], x_src(t), writes=[zb])
                    resid_ln([ps_y[:, 0:512], ps_y[:, 512:1024]], [ps_y_b], zt, zb,
                             gate[(stream, 0)], gate_b[(stream, 0)], lg, lgb, lb, lbb, tmp, tmp_b)
                    T.dma("sp", zs, X1[t * 128:(t + 1) * 128, :], zt[:], reads=[zb], writes=[X1b[t]], is_output=debug)

        T.barrier()
        stR.close()
        with contextlib.ExitStack() as stF:
            w1 = sb("w1f", [128, 8, FFN], BF16, stF); w3 = sb("w3f", [128, 8, FFN], BF16, stF)
            w2 = sb("w2f", [128, NF, D], BF16, stF); wf_b = Buf(); wf_s = T.semc("wf")
            for k in range(8):
                T.dma("pool", wf_s, w1[:, k, :], ev_ffn_w1[k * 128:(k + 1) * 128, :], writes=[wf_b], max_dma_last_dim=4096)
                T.dma("pool", wf_s, w3[:, k, :], ev_ffn_w3[k * 128:(k + 1) * 128, :], writes=[wf_b], max_dma_last_dim=4096)
            for f in range(NF):
                T.dma("pool", wf_s, w2[:, f, :], ev_ffn_w2[f * 128:(f + 1) * 128, :], writes=[wf_b], max_dma_last_dim=4096)
            lg, lgb = load_lnp(0, 2, stF, wf_s)
            lb, lbb = load_lnp(0, 3, stF, wf_s)
            x1c = [sb(f"x1c{i}", [128, D], F32, stF) for i in range(4)]; x1c_b = [Buf() for _ in range(4)]
            x1c_s = [T.semc("x1c") for _ in range(4)]
            h2T = sb("h2T", [128, 8, 512], BF16, stF); h2T_b = [Buf() for _ in range(4)]
            AT = sb("ATf", [128, NF, 512], BF16, stF); AT_b = [Buf() for _ in range(NF)]
            sg = [sb(f"sgf{i}", [128, 512], BF16, stF) for i in range(2)]; sg_b = [Buf(), Buf()]
            tmp = sb("tmpf", [128, D], F32, stF); tmp_b = Buf()
            ps_tr = ps("ps_trF", [128, 1024], F32, stF); ps_tr_b = Buf()
            ps_g = [ps(f"ps_gF{i}", [128, 512], F32, stF) for i in range(2)]; ps_g_b = [Buf(), Buf()]
            ps_u = [ps(f"ps_uF{i}", [128, 512], F32, stF) for i in range(2)]; ps_u_b = [Buf(), Buf()]
            ps_o2 = [ps(f"ps_oF{i}", [128, 512], F32, stF) for i in range(2)]; ps_o2_b = [Buf(), Buf()]
            oc = 0
            chunks = [(qc * 4, 4) for qc in range(8)] + [(NTL, 2)]
            for (t0, ntl) in chunks:
                nq = ntl * 128
                stream = 0 if t0 < NTL else 1
                for il in range(ntl):
                    t = t0 + il
                    T.dma("sp", x1c_s[il], x1c[il][:], X1[t * 128:(t + 1) * 128, :], reads=[X1b[t]], writes=[x1c_b[il]])
                    make_hT(x1c[il], x1c_b[il], L, 2, 3, stream, ps_tr, ps_tr_b,
                            lambda k, il=il: h2T[:, k, il * 128:(il + 1) * 128], h2T_b[il])
                for f in range(NF):
                    gi = f % 2

                    def _gu(f=f, gi=gi, nq=nq):
                        i = None
                        for k in range(8):
                            i = pe.matmul(ps_g[gi][:, 0:nq], lhsT=w1[:, k, f * 128:(f + 1) * 128], rhs=h2T[:, k, 0:nq],
                                          start=(k == 0), stop=(k == 7))
                        for k in range(8):
                            i = pe.matmul(ps_u[gi][:, 0:nq], lhsT=w3[:, k, f * 128:(f + 1) * 128], rhs=h2T[:, k, 0:nq],
                                          start=(k == 0), stop=(k == 7))
                        return i
                    T.op("pe", _gu, reads=[wf_b] + h2T_b[0:ntl], writes=[ps_g_b[gi], ps_u_b[gi]])
                    T.op("act", lambda gi=gi, nq=nq: act.activation(out=sg[gi][:, 0:nq], in_=ps_g[gi][:, 0:nq], func=AF.Silu),
                         reads=[ps_g_b[gi]], writes=[sg_b[gi]])
                    T.op("dve", lambda gi=gi, f=f, nq=nq: dve.tensor_tensor(out=AT[:, f, 0:nq], in0=sg[gi][:, 0:nq],
                                                                           in1=ps_u[gi][:, 0:nq], op=ALU.mult),
                         reads=[sg_b[gi], ps_u_b[gi]], writes=[AT_b[f]])
                for il in range(ntl):
                    t = t0 + il
                    ois = []
                    for n in range(2):
                        oi = oc % 2
                        oc += 1
                        ois.append(oi)

                        def _o(il=il, n=n, oi=oi):
                            i = None
                            for f in range(NF):
                                i = pe.matmul(ps_o2[oi][:, :], lhsT=AT[:, f, il * 128:(il + 1) * 128],
                                              rhs=w2[:, f, n * 512:(n + 1) * 512], start=(f == 0), stop=(f == NF - 1))
                            return i
                        T.op("pe", _o, reads=AT_b + [wf_b], writes=[ps_o2_b[oi]])
                    resid_ln([ps_o2[ois[0]][:, :], ps_o2[ois[1]][:, :]], [ps_o2_b[ois[0]], ps_o2_b[ois[1]]],
                             x1c[il], x1c_b[il], gate[(stream, 1)], gate_b[(stream, 1)], lg, lgb, lb, lbb, tmp, tmp_b)
                    if n_layers == 1 and t < NTL:
                        T.dma("sp", x1c_s[il], out_ap[t * 128:(t + 1) * 128, :], x1c[il][:], reads=[x1c_b[il]], is_output=True)
                    T.dma("sp", x1c_s[il], X2[t * 128:(t + 1) * 128, :], x1c[il][:], reads=[x1c_b[il]], writes=[X2b[t]],
                          is_output=debug)

    T.barrier()
    if n_layers == 1:
        T.finish()
        es.close()
        return nc

    L = 1
    QD = dscr("QD", [128, 6, SEQ], BF16); QD_b = [Buf() for _ in range(NTL)]
    KD = dscr("KD", [128, 6, SEQ + CTXL], BF16); KD_b = [Buf() for _ in range(NT)]
    VD = dscr("VD", [NT, 128, 12 * 66], BF16); VD_b = [Buf() for _ in range(NT)]
    with contextlib.ExitStack() as stL1:
        stR = contextlib.ExitStack()
        gate, gate_b = compute_mod(1, stL1, stR)
        YT = sb("YT", [128, 2, SEQ], BF16, stR); YT_b = [Buf() for _ in range(8)]
        w1_s = T.semc("w1")
        stU = contextlib.ExitStack()
        UCS = sb("UCS", [128, NTL, 512], BF16, stU); UCS_b = [Buf() for _ in range(NTL)]
        with contextlib.ExitStack() as stA:
            win = sb("win1", [128, 8, 2560], BF16, stA); win_b = Buf()
            for k in range(8):
                T.dma("pool", w1_s, win[:, k, :], od_w_in[k * 128:(k + 1) * 128, :], writes=[win_b], max_dma_last_dim=4096)
            fg = sb("fg1", [128, 4, 64], F32, stA); fg_b = Buf()
            T.dma("sp", w1_s, fg[:].rearrange("p g d -> p (g d)"),
                  od_fgain.rearrange("(o n) -> o n", o=1).to_broadcast([128, 256]), writes=[fg_b])
            c64 = sb("c64", [128, 2, 128], BF16, stA); c64_b = Buf()
            T.dma("sp", w1_s, c64[:, 0, :], C64[:, :], writes=[c64_b])
            T.dma("sp", w1_s, c64[:, 1, :], S64[:, :], writes=[c64_b])
            xring = Ring(T, stA, nc, "xa1", 2, [128, D], F32)
            hT = sb("hTa1", [128, 8, 128], BF16, stA); hT_b = Buf()
            sq = sb("sqa1", [128, 256], F32, stA); ms = sb("msa1", [128, 4], F32, stA); rt = sb("rta1", [128, 4], F32, stA)
            un = sb("una1", [128, 4, 64], F32, stA)
            wb_ = {"sq": Buf(), "ms": Buf(), "rt": Buf(), "dst": Buf()}
            stg = sb("stga1", [128, 14 * 128], BF16, stA); stg_b = Buf()
            qkT = Ring(T, stA, nc, "qkT1", 2, [128, 12, 128], BF16)
            uT = sb("uT1", [128, 2, 128], BF16, stA); uT_b = Buf()
            vring = Ring(T, stA, nc, "va1", 2, [128, 12, 66], BF16)
            for i_ in range(2):
                T.op("dve", lambda i_=i_: dve.memset(vring.t[i_][:, :, 64:66], 1.0), writes=[vring.b[i_]])
            ps_tr = ps("ps_trA1", [128, 1024], F32, stA); ps_tr_b = Buf()
            ps_pj = ps("ps_pjA1", [128, 2048], F32, stA); ps_pj_b = Buf()
            ps_t2 = ps("ps_t2A1", [128, 2048], BF16, stA); ps_t2_b = Buf()
            for t in range(NT):
                lat = t < NTL
                stream = 0 if lat else 1
                xt, xb, xs = xring.next()
                T.dma("sp", xs, xt[:], X2[t * 128:(t + 1) * 128, :], reads=[X2b[t]], writes=[xb])
                make_hT(xt, xb, L, 0, 1, stream, ps_tr, ps_tr_b, lambda k: hT[:, k, :], hT_b)

                def _pj(lat=lat):
                    i = None
                    groups = [(1024, 512, 1024), (1536, 256, 1536)]
                    if lat:
                        groups = [(0, 512, 256), (512, 256, 768), (768, 256, 0)] + groups
                    for (c0, n, w0) in groups:
                        for k in range(8):
                            i = pe.matmul(ps_pj[:, c0:c0 + n], lhsT=hT[:, k, :], rhs=win[:, k, w0:w0 + n],
                                          start=(k == 0), stop=(k == 7))
                    return i
                T.op("pe", _pj, reads=[hT_b, win_b], writes=[ps_pj_b])
                if lat:
                    rms_heads(ps_pj[:, 768:1024], 4, sq, ms, rt, un[:], wb_, [ps_pj_b], fg[:], fg_b)

                def _stq(lat=lat):
                    i = act.copy(out=stg[:, 768:1536], in_=ps_pj[:, 1024:1792])
                    if lat:
                        i = act.mul(out=stg[:, 0:768], in_=ps_pj[:, 0:768], mul=0.125)
                    return i
                T.op("act", _stq, reads=[ps_pj_b] + ([wb_["dst"]] if lat else []), writes=[stg_b])
                if lat:
                    T.op("dve", lambda: dve.tensor_copy(out=stg[:, 1536:1792], in_=un[:].rearrange("p g d -> p (g d)")),
                         reads=[wb_["dst"]], writes=[stg_b])

                def _t2(lat=lat):
                    i = None
                    for j in (range(14) if lat else range(6, 12)):
                        i = pe.transpose(ps_t2[:, j * 128:(j + 1) * 128], stg[:, j * 128:(j + 1) * 128], identb[:])
                    return i
                T.op("pe", _t2, reads=[stg_b, identb_b], writes=[ps_t2_b])
                qk, qkb, qks = qkT.next()

                def _e2(lat=lat, qk=qk):
                    i = act.copy(out=qk[:, 6:12, :], in_=ps_t2[:, 768:1536].rearrange("p (j n) -> p j n", n=128))
                    if lat:
                        i = act.copy(out=qk[:, 0:6, :], in_=ps_t2[:, 0:768].rearrange("p (j n) -> p j n", n=128))
                    return i
                T.op("act", _e2, reads=[ps_t2_b], writes=[qkb])
                T.dma("sp", qks, KD[:, :, t * 128:(t + 1) * 128], qk[:, 6:12, :], reads=[qkb], writes=[KD_b[t]])
                if lat:
                    T.dma("sp", qks, QD[:, :, t * 128:(t + 1) * 128], qk[:, 0:6, :], reads=[qkb], writes=[QD_b[t]])
                    T.op("dve", lambda: dve.tensor_copy(out=uT[:].rearrange("p a n -> p (a n)"), in_=ps_t2[:, 1536:1792]),
                         reads=[ps_t2_b], writes=[uT_b])

                    def _uc():
                        i = None
                        for cs_ in range(2):
                            for c2 in range(2):
                                i = pe.matmul(ps_tr[:, (cs_ * 2 + c2) * 128:(cs_ * 2 + c2 + 1) * 128], lhsT=uT[:, c2, :],
                                              rhs=c64[:, cs_, :], start=True, stop=True)
                        return i
                    T.op("pe", _uc, reads=[uT_b, c64_b], writes=[ps_tr_b])
                    T.op("dve", lambda t=t: dve.tensor_copy(out=UCS[:, t, :], in_=ps_tr[:, 0:512]),
                         reads=[ps_tr_b], writes=[UCS_b[t]])
                def _pv():
                    i = None
                    for (c0, n, w0) in ((0, 512, 1792), (512, 256, 2304)):
                        for k in range(8):
                            i = pe.matmul(ps_pj[:, c0:c0 + n], lhsT=hT[:, k, :], rhs=win[:, k, w0:w0 + n],
                                          start=(k == 0), stop=(k == 7))
                    return i
                T.op("pe", _pv, reads=[hT_b, win_b], writes=[ps_pj_b])
                vt, vb, vs = vring.next()
                T.op("act", lambda vt=vt: act.copy(out=vt[:, :, 0:64], in_=ps_pj[:, 0:768].rearrange("p (h d) -> p h d", d=64)),
                     reads=[ps_pj_b], writes=[vb])
                T.dma("sp", vs, VD[t], vt[:].rearrange("p h d -> p (h d)"), reads=[vb], writes=[VD_b[t]])
        T.barrier()
        if stop_after == "A1":
            T.finish(); stU.close(); stR.close(); return nc
        with contextlib.ExitStack() as stFo:
            tring = Ring(T, stFo, nc, "dft", 3, [128, 2, 8, 512], BF16)
            ps_f = [ps(f"ps_f{i}", [128, 512], F32, stFo) for i in range(4)]; ps_f_b = [Buf() for _ in range(4)]
            for tc in range(8):
                pb_ = (tc % 2) * 2
                for sp_ in range(4):
                    tt, tb, ts_ = tring.next()
                    for ci, src in enumerate((CN, SN)):
                        T.dma("sp", ts_, tt[:, ci, :, :],
                              src[sp_ * 1024:(sp_ + 1) * 1024, tc * 512:(tc + 1) * 512].rearrange("(j p) n -> p j n", p=128),
                              writes=[tb])

                    def _f(tt=tt, sp_=sp_, pb_=pb_):
                        i = None
                        for c2 in range(2):
                            for j in range(8):
                                s_ = sp_ * 8 + j
                                for ci in range(2):
                                    i = pe.matmul(ps_f[pb_ + c2][:, :], lhsT=UCS[:, s_, (ci * 2 + c2) * 128:(ci * 2 + c2 + 1) * 128],
                                                  rhs=tt[:, ci, j, :], start=(s_ == 0 and ci == 0), stop=(s_ == 31 and ci == 1))
                        return i
                    T.op("pe", _f, reads=[tb] + UCS_b[sp_ * 8:(sp_ + 1) * 8], writes=[ps_f_b[pb_], ps_f_b[pb_ + 1]])

                def _fe(tc=tc, pb_=pb_):
                    i = None
                    for c2 in range(2):
                        i = act.copy(out=YT[:, c2, tc * 512:(tc + 1) * 512], in_=ps_f[pb_ + c2][:, :])
                    return i
                T.op("act", _fe, reads=[ps_f_b[pb_], ps_f_b[pb_ + 1]], writes=[YT_b[tc]])
        T.barrier()
        if stop_after == "FO":
            T.finish(); stU.close(); stR.close(); return nc
        stU.close()

        with contextlib.ExitStack() as stB:
            wout = sb("wout1", [128, 12, D], BF16, stB); woutF = sb("woutF1", [128, 2, D], BF16, stB); wout_b = Buf()
            T.op("pool", lambda: pool.memset(wout[64:128, :, :], 0.0), writes=[wout_b])
            T.dma("pool", w1_s, wout[0:64], w_out[1, 256:1024, :].rearrange("(c p) n -> p c n", p=64), writes=[wout_b])
            T.dma("pool", w1_s, woutF[:], w_out[1, 0:256, :].rearrange("(c p) n -> p c n", p=128), writes=[wout_b])
            lg, lgb = load_lnp(1, 0, stB, w1_s)
            lb, lbb = load_lnp(1, 1, stB, w1_s)
            KTc = sb("KTc", [128, 6, 256], BF16, stB); Vc = sb("Vc", [128, 2, 12 * 66], BF16, stB); kvc_b = Buf()
            T.dma("sp", w1_s, KTc[:], KD[:, :, SEQ:SEQ + CTXL], reads=[KD_b[32], KD_b[33]], writes=[kvc_b])
            T.dma("sp", w1_s, Vc[:], VD[NTL:NT].rearrange("j p n -> p j n"), reads=[VD_b[32], VD_b[33]], writes=[kvc_b])
            qz1 = sb("Qz1", [128, 12, 512], BF16, stB); Qz1_b = [Buf() for _ in range(12)]; qz1_s = T.semc("qz1")
            T.op("pool", lambda: pool.memset(qz1[:], 0.0), writes=Qz1_b)
            kring = Ring(T, stB, nc, "kb1", 2, [128, 6, 1024], BF16)
            vwring = Ring(T, stB, nc, "vb1", 2, [128, 8, 12 * 66], BF16)
            bring = Ring(T, stB, nc, "bias1", 3, [128, 8, 512], BF16)
            ps_s = [ps(f"ps_s1{i}", [128, 512], F32, stB) for i in range(2)]; ps_s_b = [Buf(), Buf()]
            ps_o = [ps(f"ps_o1{i}", [128, 512], F32, stB) for i in range(2)]; ps_o_b = [Buf(), Buf()]
            ps_bc = ps("ps_bc1", [128, 512], F32, stB); ps_bc_b = Buf()
            ps_y = ps("ps_y1", [128, 1024], F32, stB); ps_y_b = Buf()
            pT_ring = Ring(T, stB, nc, "pT1", 3, [128, 512], BF16)
            osb = [sb(f"osb1{i}", [128, 512], F32, stB) for i in range(2)]; osb_b = [Buf(), Buf()]
            for i in range(2):
                T.op("pool", lambda i=i: pool.memset(osb[i][:], 0.0), writes=[osb_b[i]])
            OT = sb("OT1", [128, 12, 512], BF16, stB); OT_b = [Buf() for _ in range(12)]
            T.op("pool", lambda: pool.memset(OT[64:128, :, :], 0.0), writes=OT_b)
            zring = Ring(T, stB, nc, "zb1", 2, [128, D], F32)
            tmp = sb("tmpb1", [128, D], F32, stB); tmp_b = Buf()
            hcount = 0
            for qb in range(8):
                t0 = 4 * qb
                bt = 0 if qb == 0 else (2 if qb == 7 else 1)
                klo = max(0, t0 - 2)
                khi = min(NTL, t0 + 6)
                nk = khi - klo
                slot0 = klo - (t0 - 2)
                for h in range(12):
                    ch, half = h // 2, h % 2
                    rows = slice(half * 64, (half + 1) * 64)
                    T.dma("sp", qz1_s, qz1[rows, h, :], QD[rows, ch, t0 * 128:(t0 + 4) * 128], reads=QD_b[t0:t0 + 4],
                          writes=[Qz1_b[h]])
                kt_, kb_, ks_ = kring.next()
                T.dma("sp", ks_, kt_[:, :, 0:nk * 128], KD[:, :, klo * 128:khi * 128], reads=KD_b[klo:khi], writes=[kb_])
                vt_, vb_, vs_ = vwring.next()
                T.dma("sp", vs_, vt_[:, 0:nk, :], VD[klo:khi].rearrange("j p n -> p j n"), reads=VD_b[klo:khi], writes=[vb_])
                keys = list(range(klo, khi)) + [NTL, NTL + 1]
                for h in range(12):
                    ch, half = h // 2, h % 2
                    rows = slice(half * 64, (half + 1) * 64)
                    oi = hcount % 2
                    hcount += 1
                    bt_, bb_, bs_ = bring.next()

                    def bpre(bt_=bt_, bb_=bb_, bs_=bs_, h=h, bt=bt, slot0=slot0, nk=nk):
                        T.dma("pool", bs_, bt_[:, 0:nk, :], od_bias[h, bt, slot0:slot0 + nk].rearrange("j p n -> p j n"),
                              writes=[bb_])

                    def kfn(kt, rows=rows, ch=ch, kt_=kt_, kb_=kb_, klo=klo):
                        if kt >= NTL:
                            return KTc[:, ch, (kt - NTL) * 128:(kt - NTL + 1) * 128], kvc_b
                        return kt_[:, ch, (kt - klo) * 128:(kt - klo + 1) * 128], kb_

                    def vfn(kt, h=h, vt_=vt_, vb_=vb_, klo=klo):
                        if kt >= NTL:
                            return Vc[:, kt - NTL, h * 66:h * 66 + 65], kvc_b
                        return vt_[:, kt - klo, h * 66:h * 66 + 65], vb_

                    def bfn(kt, bt_=bt_, bb_=bb_, klo=klo):
                        if kt >= NTL:
                            return None
                        return bt_[:, kt - klo, :], bb_
                    attn_head(qz1[:, h, :], kfn, vfn, keys, ps_s, ps_s_b, pT_ring, ps_o[oi], ps_o_b[oi], 512,
                              [Qz1_b[h]], bias_fn=bfn, pre=bpre,
                              post=lambda oi=oi, h=h: attn_norm(ps_o[oi], ps_o_b[oi], osb[oi], osb_b[oi], ps_bc, ps_bc_b,
                                                                OT[0:64, h, :], OT_b[h], 512))
                attn_flush()
                for il in range(4):
                    t = t0 + il

                    def _y(il=il, t=t):
                        i = None
                        for n in range(2):
                            for c in range(14):
                                if c < 2:
                                    lhs, rhs = YT[:, c, t * 128:(t + 1) * 128], woutF[:, c, n * 512:(n + 1) * 512]
                                else:
                                    lhs, rhs = OT[:, c - 2, il * 128:(il + 1) * 128], wout[:, c - 2, n * 512:(n + 1) * 512]
                                i = pe.matmul(ps_y[:, n * 512:(n + 1) * 512], lhsT=lhs, rhs=rhs, start=(c == 0), stop=(c == 13))
                        return i
                    T.op("pe", _y, reads=[YT_b[t // 4], wout_b] + OT_b, writes=[ps_y_b])
                    zt, zb, zs = zring.next()
                    T.dma("sp", zs, zt[:], X2[t * 128:(t + 1) * 128, :], reads=[X2b[t]], writes=[zb])
                    resid_ln([ps_y[:, 0:512], ps_y[:, 512:1024]], [ps_y_b], zt, zb,
                             gate[(0, 0)], gate_b[(0, 0)], lg, lgb, lb, lbb, tmp, tmp_b)
                    T.dma("sp", zs, X3[t * 128:(t + 1) * 128, :], zt[:], reads=[zb], writes=[X3b[t]], is_output=debug)
        T.barrier()
        if stop_after == "NA":
            T.finish(); stR.close(); return nc
        stR.close()

        with contextlib.ExitStack() as stM:
            lg, lgb = load_lnp(1, 2, stM, w1_s)
            lb, lbb = load_lnp(1, 3, stM, w1_s)
            wr = sb("wr", [128, 8, 8], F32, stM); wr_b = Buf()
            T.dma("sp", w1_s, wr[:], od_router.rearrange("(k p) e -> p k e", p=128), writes=[wr_b])
            x3c = [sb(f"x3c{i}", [128, D], F32, stM) for i in range(4)]; x3c_b = [Buf() for _ in range(4)]
            x3c_s = [T.semc("x3c") for _ in range(4)]
            acc = [sb(f"acc{i}", [128, D], F32, stM) for i in range(4)]; acc_b = [Buf() for _ in range(4)]
            h2T = sb("h2Tm", [128, 8, 512], BF16, stM); h2T_b = [Buf() for _ in range(4)]
            h2Tf = sb("h2Tf", [128, 8, 128], F32, stM); h2Tf_b = Buf()
            AT = sb("ATm", [128, NF, 512], BF16, stM); AT_b = [Buf() for _ in range(NF)]
            sg = [sb(f"sgm{i}", [128, 512], BF16, stM) for i in range(2)]; sg_b = [Buf(), Buf()]
            tmp = sb("tmpm", [128, D], F32, stM); tmp_b = Buf()
            lgt = sb("lgt", [128, 8], F32, stM); mx = sb("mxm", [128, 8], F32, stM); dd = sb("ddm", [128, 2], F32, stM)
            eq = sb("eqm", [128, 2, 8], F32, stM)
            gts = sb("gts", [128, 4, 8], F32, stM); gts_b = [Buf() for _ in range(4)]
            gb = [Buf() for _ in range(5)]
            w13 = Ring(T, stM, nc, "w13", 3, [128, 2, 8, 256], BF16)
            w2r = Ring(T, stM, nc, "w2r", 3, [128, NF, 512], BF16)
            ps_tr = ps("ps_trM", [128, 1024], F32, stM); ps_tr_b = Buf()
            ps_g = [ps(f"ps_gM{i}", [128, 512], F32, stM) for i in range(2)]; ps_g_b = [Buf(), Buf()]
            ps_u = [ps(f"ps_uM{i}", [128, 512], F32, stM) for i in range(2)]; ps_u_b = [Buf(), Buf()]
            ps_o2 = [ps(f"ps_oM{i}", [128, 512], F32, stM) for i in range(2)]; ps_o2_b = [Buf(), Buf()]
            oc = 0
            if stop_after == "M0":
                T.barrier(); T.finish(); stM.close(); return nc
            for tcn in range(8):
                t0 = tcn * 4
                for il in range(4):
                    t = t0 + il
                    if stop_after == "M1" and il == 1:
                        dump("h2Tf", h2Tf[:].rearrange("p a b -> p (a b)"), [128, 1024], F32, [h2Tf_b])
                        dump("lgt", lgt[:], [128, 8], F32, [gb[0]])
                        T.barrier(); T.finish(); stM.close(); return nc
                    T.dma("sp", x3c_s[il], x3c[il][:], X3[t * 128:(t + 1) * 128, :], reads=[X3b[t]], writes=[x3c_b[il]])
                    make_hT(x3c[il], x3c_b[il], L, 2, 3, 0, ps_tr, ps_tr_b,
                            lambda k, il=il: h2T[:, k, il * 128:(il + 1) * 128], h2T_b[il],
                            lambda k: h2Tf[:, k, :], h2Tf_b)
                    def chk(stage):
                        if stop_after == "S" + str(il * 10 + stage):
                            T.barrier(); T.finish(); stM.close()
                            raise StopIteration
                    chk(1)
                    oi = oc % 2
                    oc += 1

                    def _r(oi=oi):
                        i = None
                        for k in range(8):
                            i = pe.matmul(ps_o2[oi][:, 0:8], lhsT=h2Tf[:, k, :], rhs=wr[:, k, :], start=(k == 0), stop=(k == 7))
                        return i
                    T.op("pe", _r, reads=[h2Tf_b, wr_b], writes=[ps_o2_b[oi]])
                    chk(2)
                    T.op("dve", lambda oi=oi: dve.tensor_copy(out=lgt[:], in_=ps_o2[oi][:, 0:8]), reads=[ps_o2_b[oi]], writes=[gb[0]])
                    chk(3)
                    T.op("dve", lambda: dve.max(out=mx[:], in_=lgt[:]), reads=[gb[0]], writes=[gb[1]], hard=True)
                    chk(4)
                    T.op("dve", lambda: dve.tensor_tensor(out=dd[:, 0:1], in0=mx[:, 0:1], in1=mx[:, 1:2], op=ALU.subtract),
                         reads=[gb[1]], writes=[gb[2]], hard=True)
                    T.op("act", lambda: act.activation(out=dd[:, 0:1], in_=dd[:, 0:1], func=AF.Sigmoid), reads=[gb[2]], writes=[gb[2]])

                    chk(5)

                    def _g1():
                        dve.tensor_scalar(out=dd[:, 1:2], in0=dd[:, 0:1], scalar1=-1.0, scalar2=1.0, op0=ALU.mult, op1=ALU.add)
                        dve.tensor_scalar(out=eq[:, 0, :], in0=lgt[:], scalar1=mx[:, 0:1], scalar2=None, op0=ALU.is_equal)
                        return dve.tensor_scalar(out=eq[:, 1, :], in0=lgt[:], scalar1=mx[:, 1:2], scalar2=None, op0=ALU.is_equal)
                    T.op("dve", _g1, reads=[gb[2], gb[1], gb[0]], writes=[gb[3]], hard=True)

                    chk(6)

                    def _g2(il=il):
                        dve.tensor_scalar(out=eq[:, 0, :], in0=eq[:, 0, :], scalar1=dd[:, 0:1], scalar2=None, op0=ALU.mult)
                        return dve.scalar_tensor_tensor(out=gts[:, il, :], in0=eq[:, 1, :], scalar=dd[:, 1:2], in1=eq[:, 0, :],
                                                        op0=ALU.mult, op1=ALU.add)
                    T.op("dve", _g2, reads=[gb[3]], writes=[gts_b[il], gb[4]], hard=True)
                    chk(7)
                if stop_after == "MG":
                    dump("gts", gts[:].rearrange("p a b -> p (a b)"), [128, 32], F32, gts_b)
                    T.barrier(); T.finish(); stM.close(); return nc
                for e in range(NEXP):
                    if stop_after == "ME" and e == 1:
                        dump("acc0", acc[0][:], [128, D], F32, [acc_b[0]])
                        T.barrier(); T.finish(); stM.close(); return nc
                    for fp in range(11):
                        wt_, wb2_, ws2_ = w13.next()
                        T.dma("sp", ws2_, wt_[:, 0, :, :], W1s[e, fp], reads=[W1s_b[e]], writes=[wb2_])
                        T.dma("sp", ws2_, wt_[:, 1, :, :], W3s[e, fp], reads=[W3s_b[e]], writes=[wb2_])
                        for f2 in range(2):
                            f = fp * 2 + f2
                            gi = f % 2

                            def _gu(f2=f2, gi=gi, wt_=wt_):
                                i = None
                                for k in range(8):
                                    i = pe.matmul(ps_g[gi][:, :], lhsT=wt_[:, 0, k, f2 * 128:(f2 + 1) * 128], rhs=h2T[:, k, :],
                                                  start=(k == 0), stop=(k == 7))
                                for k in range(8):
                                    i = pe.matmul(ps_u[gi][:, :], lhsT=wt_[:, 1, k, f2 * 128:(f2 + 1) * 128], rhs=h2T[:, k, :],
                                                  start=(k == 0), stop=(k == 7))
                                return i
                            T.op("pe", _gu, reads=[wb2_] + h2T_b, writes=[ps_g_b[gi], ps_u_b[gi]])
                            T.op("act", lambda gi=gi: act.activation(out=sg[gi][:, :], in_=ps_g[gi][:, :], func=AF.Silu),
                                 reads=[ps_g_b[gi]], writes=[sg_b[gi]])
                            T.op("dve", lambda gi=gi, f=f: dve.tensor_tensor(out=AT[:, f, :], in0=sg[gi][:, :], in1=ps_u[gi][:, :],
                                                                             op=ALU.mult),
                                 reads=[sg_b[gi], ps_u_b[gi]], writes=[AT_b[f]])
                    for n in range(2):
                        w2t, w2b, w2s = w2r.next()
                        T.dma("sp", w2s, w2t[:].rearrange("p f n -> p (f n)"), W2s[e, n].rearrange("p f n -> p (f n)"),
                              reads=[W2s_b[e]], writes=[w2b])
                        for il in range(4):
                            oi = oc % 2
                            oc += 1

                            def _o(il=il, oi=oi, w2t=w2t):
                                i = None
                                for f in range(NF):
                                    i = pe.matmul(ps_o2[oi][:, :], lhsT=AT[:, f, il * 128:(il + 1) * 128], rhs=w2t[:, f, :],
                                                  start=(f == 0), stop=(f == NF - 1))
                                return i
                            T.op("pe", _o, reads=AT_b + [w2b], writes=[ps_o2_b[oi]])
                            dst = acc[il][:, n * 512:(n + 1) * 512]
                            if e == 0:
                                T.op("dve", lambda oi=oi, il=il, dst=dst: dve.tensor_scalar(
                                    out=dst, in0=ps_o2[oi][:, :], scalar1=gts[:, il, 0:1], scalar2=None, op0=ALU.mult),
                                    reads=[ps_o2_b[oi], gts_b[il]], writes=[acc_b[il]])
                            else:
                                T.op("dve", lambda oi=oi, il=il, dst=dst, e=e: dve.scalar_tensor_tensor(
                                    out=dst, in0=ps_o2[oi][:, :], scalar=gts[:, il, e:e + 1], in1=dst, op0=ALU.mult, op1=ALU.add),
                                    reads=[ps_o2_b[oi], gts_b[il]], writes=[acc_b[il]])
                for il in range(4):
                    t = t0 + il
                    resid_ln([acc[il][:, 0:512], acc[il][:, 512:1024]], [acc_b[il]], x3c[il], x3c_b[il],
                             gate[(0, 1)], gate_b[(0, 1)], lg, lgb, lb, lbb, tmp, tmp_b)
                    T.dma("sp", x3c_s[il], out_ap[t * 128:(t + 1) * 128, :], x3c[il][:], reads=[x3c_b[il]], is_output=True)
    T.barrier()
    T.finish()
    es.close()
    return nc


_PROG = {}


def _get_prog(n_layers=2, debug=False):
    key = (n_layers, debug)
    if key not in _PROG:
        _PROG[key] = build_program(n_layers, debug)
    return _PROG[key]


L1_KEYS = ("od_w_in", "od_fgain", "od_bias", "od_router", "od_w1", "od_w3", "od_w2", "C64", "S64", "CN", "SN")


def make_in_maps(inp, n_cores=N_CORES, n_layers=2):
    cs = _consts()
    f32 = lambda a: np.ascontiguousarray(np.asarray(a, dtype=np.float32))
    x = f32(inp["x"]); c = f32(inp["c"]); ctx = f32(inp["ctx"]); c_ctx = f32(inp["c_ctx"])
    ada_b = f32(inp["ada_b"])
    adab_col = np.ascontiguousarray(ada_b.reshape(2, 48, 128).transpose(0, 2, 1))
    qk_gain = np.concatenate([np.tile(f32(inp["ev_q_gain"])[0], 12), np.tile(f32(inp["ev_k_gain"])[0], 4)])
    rpb = f32(inp["od_rpb"])[0]
    rpb_ext = np.concatenate([rpb.reshape(12, -1), np.full((12, 1), NEG, np.float32)], axis=1)
    od_bias = np.ascontiguousarray(rpb_ext[:, cs["naidx"]])
    shared = dict(
        ada_w=f32(inp["ada_w"]), ada_b=ada_b, adab_col=adab_col,
        ln_mix_g=f32(inp["ln_mix_g"]), ln_mix_b=f32(inp["ln_mix_b"]),
        ln_ffn_g=f32(inp["ln_ffn_g"]), ln_ffn_b=f32(inp["ln_ffn_b"]),
        w_out=f32(inp["w_out"]), ev_w_in=f32(inp["ev_w_in"])[0], ev_pool_w=f32(inp["ev_pool_w"])[0],
        ev_pool_scale=f32(inp["ev_pool_scale"])[0], ev_qk_gain=f32(qk_gain),
        ev_ffn_w1=f32(inp["ev_ffn_w1"])[0], ev_ffn_w3=f32(inp["ev_ffn_w3"])[0], ev_ffn_w2=f32(inp["ev_ffn_w2"])[0],
        od_w_in=f32(inp["od_w_in"])[0], od_fgain=f32(inp["od_fourier_gain"])[0].reshape(256),
        od_bias=od_bias, od_router=f32(inp["od_router"])[0],
        od_w1=f32(inp["od_exp_w1"])[0], od_w3=f32(inp["od_exp_w3"])[0], od_w2=f32(inp["od_exp_w2"])[0],
        ropeC=cs["ropeC"], ropeS=cs["ropeS"], band=cs["band"], C64=cs["C64"], S64=cs["S64"],
        CN=cs["CN"], SN=cs["SN"], identf=cs["identf"],
    )
    if n_layers < 2:
        for k in L1_KEYS:
            shared.pop(k)
    maps = []
    for b in range(n_cores):
        cv = np.stack([c[b].reshape(8, 128).T, c_ctx.reshape(8, 128).T], axis=-1)
        m = dict(shared)
        m.update(x=x[b], ctx=ctx[b], cvec=np.ascontiguousarray(cv.astype(np.float32)))
        maps.append(m)
    return maps


def kernel(**inputs):
    nc = _get_prog(2, False)
    maps = make_in_maps(inputs)
    res = run_bass_kernel_spmd(nc, maps, core_ids=list(range(N_CORES)))
    return np.stack([np.asarray(r["out"], dtype=np.float32) for r in res.results], axis=0)
```

```python
import contextlib
import math
import numpy as np
import ml_dtypes
import concourse.bass as bass
import concourse.mybir as mybir
from concourse.bass_utils import run_bass_kernel_spmd

F32 = mybir.dt.float32
BF16 = mybir.dt.bfloat16
I32 = mybir.dt.int32
AF = mybir.ActivationFunctionType
ALU = mybir.AluOpType
AX = mybir.AxisListType

D = 1024
SEQ = 4096
CTXL = 256
NT = 34
NTL = 32
FFN = 2816
NF = 22
NEXP = 8
ALPHA = 4.0 ** 0.25
LN_EPS = 1e-6
RMS_EPS = 1e-6
NEG = -30000.0
N_CORES = 8
DEFER_L0 = True


class Buf:
    __slots__ = ("name", "w", "r")

    def __init__(self, name=""):
        self.name = name
        self.w = None
        self.r = {}


class SemC:
    __slots__ = ("sem", "cnt")

    def __init__(self, sem):
        self.sem = sem
        self.cnt = 0


class Tracker:
    def __init__(self, nc, es):
        self.nc = nc
        self.es = es
        self.E = {"pe": nc.tensor, "act": nc.scalar, "dve": nc.vector, "pool": nc.gpsimd, "sp": nc.sync}
        self.esem = {}
        self.ecnt = {}
        self.own = {k: set() for k in self.E}
        self.seen = {k: {} for k in self.E}
        self.nsem = 0
        self.dsem = {}
        for k in self.E:
            self._new_esem(k)
        self.out_events = []

    def new_sem(self, name):
        self.nsem += 1
        return self.es.enter_context(self.nc.semaphore(f"{name}_{self.nsem}"))

    def semc(self, name="d"):
        sc = SemC(self.new_sem(name))
        self.dsem[sc.sem.num] = sc
        return sc

    def _new_esem(self, k):
        s = self.new_sem("e" + k)
        self.esem[k] = s
        self.ecnt[k] = 0
        self.own[k].add(s.num)

    def _wait_all(self, eng, evs, allow_own=False):
        best = {}
        for ev in evs:
            if ev is None:
                continue
            s, v = ev
            if s.num in self.own[eng] and not allow_own:
                continue
            if s.num not in best or best[s.num][1] < v:
                best[s.num] = (s, v)
        for num, (s, v) in best.items():
            if num in self.dsem:
                v = self.dsem[num].cnt
            if self.seen[eng].get(num, 0) < v:
                self.E[eng].wait_ge(s, v)
                self.seen[eng][num] = v

    def _collect(self, reads, writes, skip_num=None):
        evs = []
        for b in reads:
            evs.append(b.w)
        for b in writes:
            if b.w is not None and (skip_num is None or b.w[0].num != skip_num):
                evs.append(b.w)
            evs.extend(b.r.values())
        return evs

    def _update(self, ev, reads, writes):
        for b in reads:
            old = b.r.get(ev[0].num)
            if old is None or old[1] < ev[1]:
                b.r[ev[0].num] = ev
        for b in writes:
            b.w = ev
            b.r = {}

    def op(self, eng, fn, reads=(), writes=(), hard=False):
        self._wait_all(eng, self._collect(reads, writes), allow_own=hard)
        inst = fn()
        own = self.esem[eng]
        self.ecnt[eng] += 1
        inst.then_inc(own, 1)
        ev = (own, self.ecnt[eng])
        self._update(ev, reads, writes)
        if self.ecnt[eng] >= 12000:
            self._new_esem(eng)
        return ev

    def dma(self, q, sc, out, in_, reads=(), writes=(), is_output=False, **kw):
        self._wait_all(q, self._collect(reads, writes, skip_num=sc.sem.num))
        inst = self.E[q].dma_start(out=out, in_=in_, **kw)
        sc.cnt += 16
        inst.then_inc(sc.sem, 16)
        ev = (sc.sem, sc.cnt)
        self._update(ev, reads, writes)
        if is_output:
            self.out_events.append(ev)
        return ev

    def idma(self, sc, reads=(), writes=(), is_output=False, **kw):
        self._wait_all("pool", self._collect(reads, writes, skip_num=sc.sem.num))
        inst = self.E["pool"].indirect_dma_start(**kw)
        sc.cnt += 16
        inst.then_inc(sc.sem, 16)
        ev = (sc.sem, sc.cnt)
        self._update(ev, reads, writes)
        if is_output:
            self.out_events.append(ev)
        return ev

    def barrier(self):
        evs = []
        for k in self.E:
            if self.ecnt[k] > 0:
                evs.append((self.esem[k], self.ecnt[k]))
        for sc in self.dsem.values():
            if sc.cnt > 0:
                evs.append((sc.sem, sc.cnt))
        for k in self.E:
            self._wait_all(k, evs)

    def finish(self):
        self._wait_all("sp", self.out_events)


class Ring:
    def __init__(self, T, es, nc, name, n, shape, dtype):
        self.n = n
        self.t = [es.enter_context(nc.sbuf_tensor(f"r_{name}{i}", shape, dtype)) for i in range(n)]
        self.b = [Buf(f"{name}{i}") for i in range(n)]
        self.s = [T.semc(name) for i in range(n)]
        self.i = 0

    def next(self):
        k = self.i % self.n
        self.i += 1
        return self.t[k], self.b[k], self.s[k]


def _rope_tables():
    t = np.arange(SEQ)
    row = (t // 64).astype(np.float32)
    col = (t % 64).astype(np.float32)
    inv = np.power(np.float32(10000.0), -np.arange(16, dtype=np.float32) / np.float32(16)).astype(np.float32)
    ang = np.stack([row[:, None] * inv, col[:, None] * inv], axis=1).astype(np.float32)
    cs, sn = np.cos(ang).astype(np.float32), np.sin(ang).astype(np.float32)
    C = np.zeros((SEQ, 2, 2, 16), np.float32)
    S = np.zeros((SEQ, 2, 2, 16), np.float32)
    C[:, :, 0, :] = cs
    C[:, :, 1, :] = cs
    S[:, :, 0, :] = -sn
    S[:, :, 1, :] = sn
    C = C.reshape(NTL, 128, 64).transpose(1, 0, 2)
    S = S.reshape(NTL, 128, 64).transpose(1, 0, 2)
    return np.ascontiguousarray(C), np.ascontiguousarray(S)


def _band_mats():
    out = np.zeros((4, 5, 128, 128), np.float32)
    n = 1024
    for g, w in enumerate((2, 4, 8, 16)):
        B = np.zeros((n, n), np.float64)
        for t in range(n):
            lo = min(max(t - w // 2, 0), n)
            hi = min(max(t - w // 2 + w, 0), n)
            B[lo:hi, t] += 1.0 / (hi - lo)
            B[t, t] -= 1.0
        i = 3
        out[g, 0] = B[(i - 1) * 128:i * 128, i * 128:(i + 1) * 128]
        out[g, 1] = B[i * 128:(i + 1) * 128, i * 128:(i + 1) * 128]
        out[g, 2] = B[(i + 1) * 128:(i + 2) * 128, i * 128:(i + 1) * 128]
        out[g, 3] = B[0:128, 0:128]
        out[g, 4] = B[n - 128:n, n - 128:n]
    return np.ascontiguousarray(out.transpose(2, 0, 1, 3)).astype(ml_dtypes.bfloat16)


def _dft_consts():
    c = np.arange(64)
    ang = 2.0 * np.pi * ((c[:, None] * c[None, :]) % 64) / 64.0
    C64 = np.zeros((128, 128), np.float64)
    S64 = np.zeros((128, 128), np.float64)
    for a in range(2):
        C64[a * 64:(a + 1) * 64, a * 64:(a + 1) * 64] = np.cos(ang)
        S64[a * 64:(a + 1) * 64, a * 64:(a + 1) * 64] = np.sin(ang)
    s = np.arange(SEQ, dtype=np.int64)
    m = (s[:, None] * s[None, :]) % SEQ
    angn = (2.0 * np.pi / SEQ) * m
    CN = (np.cos(angn) / 512.0).astype(ml_dtypes.bfloat16)
    SN = (-np.sin(angn) / 512.0).astype(ml_dtypes.bfloat16)
    return C64.astype(ml_dtypes.bfloat16), S64.astype(ml_dtypes.bfloat16), CN, SN


def _na_index():
    MASK = 15 * 31
    idx = np.full((3, 8, 128, 512), MASK, np.int64)
    for bt, qb in enumerate((0, 3, 7)):
        for slot in range(8):
            kt = 4 * qb - 2 + slot
            if kt < 0 or kt >= 32:
                continue
            for krl in range(2):
                kr = 2 * kt + krl
                for qrl in range(8):
                    qr = 8 * qb + qrl
                    rs = min(max(qr - 4, 0), 56)
                    if not (rs <= kr < rs + 8):
                        continue
                    dr = kr - qr + 7
                    qc = np.arange(64)
                    cs = np.clip(qc - 8, 0, 48)
                    kc = np.arange(64)
                    valid = (kc[:, None] >= cs[None, :]) & (kc[:, None] < cs[None, :] + 16)
                    dc = kc[:, None] - qc[None, :] + 15
                    blk = np.where(valid, dr * 31 + dc, MASK)
                    idx[bt, slot, krl * 64:(krl + 1) * 64, qrl * 64:(qrl + 1) * 64] = blk
    return idx


_CONST_CACHE = {}


def _consts():
    if not _CONST_CACHE:
        C, S = _rope_tables()
        C64, S64, CN, SN = _dft_consts()
        _CONST_CACHE.update(dict(
            ropeC=C, ropeS=S, band=_band_mats(), C64=C64, S64=S64, CN=CN, SN=SN,
            identf=np.eye(128, dtype=np.float32), naidx=_na_index(),
            pidx=np.arange(128, dtype=np.float32).reshape(128, 1),
            utri=np.triu(np.ones((128, 128), np.float32), 1)))
    return _CONST_CACHE


def build_program(n_layers=2, debug=False, stop_after=None):
    global _LAST_NC
    nc = bass.Bass("TRN2", target_bir_lowering=False)
    _LAST_NC = nc
    es = contextlib.ExitStack()

    def din(name, shape, dt=F32):
        return nc.dram_tensor(name, list(shape), dt, kind="ExternalInput").ap()

    def dscr(name, shape, dt=F32):
        return nc.dram_tensor(name, list(shape), dt, kind="Internal").ap()

    x_in = din("x", [SEQ, D])
    ctx_in = din("ctx", [CTXL, D])
    cvec = din("cvec", [128, 8, 2])
    ada_w = din("ada_w", [2, D, 6 * D])
    ada_b = din("ada_b", [2, 6 * D])
    adab_col = din("adab_col", [2, 128, 48])
    ln_mix_g = din("ln_mix_g", [2, D]); ln_mix_b = din("ln_mix_b", [2, D])
    ln_ffn_g = din("ln_ffn_g", [2, D]); ln_ffn_b = din("ln_ffn_b", [2, D])
    w_out = din("w_out", [2, D, D])
    ev_w_in = din("ev_w_in", [D, 1536])
    ev_pool_w = din("ev_pool_w", [4, 64, 64])
    ev_pool_scale = din("ev_pool_scale", [256])
    ev_qk_gain = din("ev_qk_gain", [1024])
    ev_ffn_w1 = din("ev_ffn_w1", [D, FFN]); ev_ffn_w3 = din("ev_ffn_w3", [D, FFN]); ev_ffn_w2 = din("ev_ffn_w2", [FFN, D])
    if n_layers == 2:
        od_w_in = din("od_w_in", [D, 2560])
        od_fgain = din("od_fgain", [256])
        od_bias = din("od_bias", [12, 3, 8, 128, 512])
        od_router = din("od_router", [D, 8])
        od_w1 = din("od_w1", [NEXP, D, FFN]); od_w3 = din("od_w3", [NEXP, D, FFN]); od_w2 = din("od_w2", [NEXP, FFN, D])
    ropeC = din("ropeC", [128, NTL, 64]); ropeS = din("ropeS", [128, NTL, 64])
    band = din("band", [128, 4, 5, 128], BF16)
    if n_layers == 2:
        C64 = din("C64", [128, 128], BF16); S64 = din("S64", [128, 128], BF16)
        CN = din("CN", [SEQ, SEQ], BF16); SN = din("SN", [SEQ, SEQ], BF16)
    identf_in = din("identf", [128, 128])
    if n_layers == 2:
        pidx_in = din("pidx", [128, 1]); utri_in = din("utri", [128, 128])

    out_ap = nc.dram_tensor("out", [SEQ, D], F32, kind="ExternalOutput").ap()
    dbg = {}
    if debug:
        for nm, shp in (("dbg_x1", [NT * 128, D]), ("dbg_x2", [NT * 128, D]), ("dbg_x3", [SEQ, D])):
            dbg[nm] = nc.dram_tensor(nm, shp, F32, kind="ExternalOutput").ap()

    X1 = dscr("X1", [NT * 128, D]) if not debug else dbg["dbg_x1"]
    X2 = dscr("X2", [NT * 128, D]) if not debug else dbg["dbg_x2"]
    X3 = dscr("X3", [SEQ, D]) if not debug else dbg["dbg_x3"]
    X1b = [Buf(f"X1_{i}") for i in range(NT)]
    X2b = [Buf(f"X2_{i}") for i in range(NT)]
    X3b = [Buf(f"X3_{i}") for i in range(NTL)]

    T = Tracker(nc, es)

    def dump(name, ap, shape, dt, reads):
        if not debug:
            return
        d = nc.dram_tensor("dbg_" + name, list(shape), dt, kind="ExternalOutput").ap()
        T.dma("sp", T.semc("dbg"), d, ap, reads=reads, is_output=True)
    pe, act, dve, pool, sp = nc.tensor, nc.scalar, nc.vector, nc.gpsimd, nc.sync

    def sb(name, shape, dt, stack=None):
        return (stack or es).enter_context(nc.sbuf_tensor("s_" + name, list(shape), dt))

    def ps(name, shape, dt, stack):
        return stack.enter_context(nc.psum_tensor("p_" + name, list(shape), dt))

    def x_src(t):
        return x_in[t * 128:(t + 1) * 128, :] if t < NTL else ctx_in[(t - NTL) * 128:(t - NTL + 1) * 128, :]

    identf = sb("identf", [128, 128], F32); identf_b = Buf()
    identb = sb("identb", [128, 128], BF16); identb_b = Buf()
    onesf = sb("onesf", [128, 64], F32); ones_b = Buf()
    cst_s = T.semc("cst")
    T.dma("sp", cst_s, identf[:], identf_in[:, :], writes=[identf_b])
    T.dma("pool", cst_s, identb[:], identf_in[:, :], writes=[identb_b])
    T.op("dve", lambda: dve.memset(onesf[:], 1.0), writes=[ones_b])
    sel64 = sb("sel64", [128, 128], F32); sel_b = Buf()
    rcp_t = sb("rcp_t", [64, 512], F32); rcp_b = Buf()

    def _sel():
        dve.memset(sel64[:], 0.0)
        return dve.memset(sel64[64:65, :], 1.0)
    T.op("dve", _sel, writes=[sel_b])

    modcol = [sb(f"modcol{l}", [128, 4, 8, 2], F32) for l in range(2)]
    modcol_b = [Buf() for l in range(2)]
    epsc = sb("epsc", [128, 2], F32); eps_b = Buf()

    def _eps():
        dve.memset(epsc[:, 0:1], LN_EPS)
        return dve.memset(epsc[:, 1:2], RMS_EPS)
    T.op("dve", _eps, writes=[eps_b])
    AD = dscr("AD", [NT * 128, 256], BF16)
    AD_b = [Buf() for _ in range(NT)]

    def load_lnp(l, k, stack, sc):
        src = (ln_mix_g, ln_mix_b, ln_ffn_g, ln_ffn_b)[k]
        t = sb(f"lnp{l}{k}", [128, D], F32, stack)
        b = Buf()
        T.dma("sp", sc, t[:], src[l:l + 1, :].to_broadcast([128, D]), writes=[b])
        return t, b

    def compute_mod(l, lstack, mstack):
        nstream = 2 if l == 0 else 1
        gate = {}
        gate_b = {}
        for g in (1, 0):
            for s in range(nstream):
                gate[(s, g)] = sb(f"gate{l}{s}{g}", [128, D], F32, mstack if g == 0 else lstack)
                gate_b[(s, g)] = Buf()
        with contextlib.ExitStack() as st:
            cc = sb(f"cc{l}", [128, 8, 2], F32, st); cc_b = Buf()
            ccrep = sb(f"ccrep{l}", [128, 8, 2, 128], F32, st); ccrep_b = Buf()
            abcol = sb(f"abcol{l}", [128, 48], F32, st); abcol_b = Buf()
            abrow = sb(f"abrow{l}", [128, 2048], F32, st); abrow_b = Buf()
            ms_ = T.semc("mod")
            T.dma("sp", ms_, cc[:], cvec[:, :, :], writes=[cc_b])
            T.dma("sp", ms_, abcol[:], adab_col[l, :, :], writes=[abcol_b])
            for gi, seg in enumerate((2, 5)):
                T.dma("sp", ms_, abrow[:, gi * 1024:(gi + 1) * 1024],
                      ada_b[l:l + 1, seg * 1024:(seg + 1) * 1024].to_broadcast([128, 1024]), writes=[abrow_b])
            T.op("act", lambda: act.activation(out=cc[:], in_=cc[:], func=AF.Silu), reads=[cc_b], writes=[cc_b])

            def _rep():
                i = None
                for k in range(8):
                    for s in range(2):
                        i = dve.tensor_copy(out=ccrep[:, k, s, :], in_=cc[:, k, s:s + 1].to_broadcast([128, 128]))
                return i
            T.op("dve", _rep, reads=[cc_b], writes=[ccrep_b])
            wring = Ring(T, st, nc, f"adaw{l}", 2, [128, 8, 512], F32)
            ps_c = ps(f"ps_c{l}", [128, 512], F32, st); ps_c_b = Buf()
            ps_g = [ps(f"ps_g{l}{i}", [128, 512], F32, st) for i in range(2)]; ps_g_b = [Buf(), Buf()]
            for j in range(12):
                seg, hf = j // 2, j % 2
                wt, wb, ws = wring.next()
                T.dma("sp", ws, wt[:], ada_w[l, :, j * 512:(j + 1) * 512].rearrange("(k p) n -> p k n", p=128),
                      writes=[wb])
                if seg in (2, 5):
                    gi = 0 if seg == 2 else 1
                    for s in range(nstream):
                        def _mm(s=s, wt=wt):
                            i = None
                            for k in range(8):
                                i = pe.matmul(ps_g[s][:, :], lhsT=ccrep[:, k, s, :], rhs=wt[:, k, :],
                                              start=(k == 0), stop=(k == 7))
                            return i
                        T.op("pe", _mm, reads=[ccrep_b, wb], writes=[ps_g_b[s]])
                        T.op("dve", lambda s=s, gi=gi, hf=hf: dve.tensor_tensor(
                            out=gate[(s, gi)][:, hf * 512:(hf + 1) * 512], in0=ps_g[s][:, :],
                            in1=abrow[:, gi * 1024 + hf * 512: gi * 1024 + (hf + 1) * 512], op=ALU.add),
                            reads=[ps_g_b[s], abrow_b], writes=[gate_b[(s, gi)]])
                else:
                    vi = {0: 0, 1: 1, 3: 2, 4: 3}[seg]
                    for c4 in range(4):
                        def _mm(c4=c4, wt=wt):
                            i = None
                            for k in range(8):
                                i = pe.matmul(ps_c[:, 0:2], lhsT=wt[:, k, c4 * 128:(c4 + 1) * 128], rhs=cc[:, k, :],
                                              start=(k == 0), stop=(k == 7))
                            return i
                        T.op("pe", _mm, reads=[cc_b, wb], writes=[ps_c_b])
                        chunk = hf * 4 + c4
                        colidx = seg * 8 + chunk
                        T.op("dve", lambda vi=vi, chunk=chunk, colidx=colidx: dve.tensor_scalar(
                            out=modcol[l][:, vi, chunk, :], in0=ps_c[:, 0:2],
                            scalar1=abcol[:, colidx:colidx + 1], scalar2=(1.0 if vi in (1, 3) else 0.0),
                            op0=ALU.add, op1=ALU.add),
                            reads=[ps_c_b, abcol_b], writes=[modcol_b[l]])
        T.barrier()
        if l == 0:
            dump("modcol", modcol[0][:].rearrange("p a b c -> p (a b c)"), [128, 64], F32, [modcol_b[0]])
            dump("gate00", gate[(0, 0)][:], [128, D], F32, [gate_b[(0, 0)]])
        return gate, gate_b

    def make_hT(xt, xb, l, vsh, vsc, stream, ps_tr, ps_tr_b, hT_ap, hT_b, hTf_ap=None, hTf_b=None):
        def _tr():
            i = None
            for k in range(8):
                i = pe.transpose(ps_tr[:, k * 128:(k + 1) * 128], xt[:, k * 128:(k + 1) * 128], identf[:])
            return i
        T.op("pe", _tr, reads=[xb, identf_b], writes=[ps_tr_b])

        def _ev():
            i = None
            for k in range(8):
                i = dve.tensor_scalar(out=hT_ap(k), in0=ps_tr[:, k * 128:(k + 1) * 128],
                                      scalar1=modcol[l][:, vsc, k, stream:stream + 1],
                                      scalar2=modcol[l][:, vsh, k, stream:stream + 1], op0=ALU.mult, op1=ALU.add)
            return i
        T.op("dve", _ev, reads=[ps_tr_b, modcol_b[l]], writes=[hT_b])
        if hTf_ap is not None:
            def _ev2():
                i = None
                for k in range(8):
                    i = dve.tensor_scalar(out=hTf_ap(k), in0=ps_tr[:, k * 128:(k + 1) * 128],
                                          scalar1=modcol[l][:, vsc, k, stream:stream + 1],
                                          scalar2=modcol[l][:, vsh, k, stream:stream + 1], op0=ALU.mult, op1=ALU.add)
                return i
            T.op("dve", _ev2, reads=[ps_tr_b, modcol_b[l]], writes=[hTf_b])

    lnw = {}
    lnw["st"] = sb("ln_st", [128, 2, 6], F32); lnw["mv"] = sb("ln_mv", [128, 2], F32)
    lnw["sd"] = sb("ln_sd", [128, 1], F32); lnw["rs"] = sb("ln_rs", [128, 1], F32)
    lnw["b0"] = Buf(); lnw["b1"] = Buf(); lnw["b2"] = Buf(); lnw["b3"] = Buf()

    def resid_ln(ps_halves, ps_bufs, zt, zb, gate_t, gate_bf, lg, lgb, lb, lbb, tmp, tmp_b):
        stt, mv, sd, rs = lnw["st"], lnw["mv"], lnw["sd"], lnw["rs"]

        def _a():
            for h in range(2):
                dve.tensor_tensor(out=tmp[:, h * 512:(h + 1) * 512], in0=ps_halves[h],
                                  in1=gate_t[:, h * 512:(h + 1) * 512], op=ALU.mult)
            dve.scalar_tensor_tensor(out=zt[:], in0=zt[:], scalar=ALPHA, in1=tmp[:], op0=ALU.mult, op1=ALU.add)
            i = None
            for h in range(2):
                i = dve.bn_stats(out=stt[:, h, :], in_=zt[:, h * 512:(h + 1) * 512])
            return i
        T.op("dve", _a, reads=list(ps_bufs) + [gate_bf], writes=[zb, tmp_b, lnw["b0"]])
        T.op("dve", lambda: dve.bn_aggr(out=mv[:], in_=stt[:].rearrange("p a b -> p (a b)")),
             reads=[lnw["b0"]], writes=[lnw["b1"]], hard=True)
        T.op("act", lambda: act.activation(out=sd[:], in_=mv[:, 1:2], func=AF.Sqrt, bias=epsc[:, 0:1], scale=1.0),
             reads=[lnw["b1"], eps_b], writes=[lnw["b2"]])
        T.op("dve", lambda: dve.reciprocal(out=rs[:], in_=sd[:]), reads=[lnw["b2"]], writes=[lnw["b3"]])

        def _b():
            dve.tensor_scalar(out=zt[:], in0=zt[:], scalar1=mv[:, 0:1], scalar2=rs[:, 0:1],
                              op0=ALU.subtract, op1=ALU.mult)
            dve.tensor_tensor(out=zt[:], in0=zt[:], in1=lg[:], op=ALU.mult)
            return dve.tensor_tensor(out=zt[:], in0=zt[:], in1=lb[:], op=ALU.add)
        T.op("dve", _b, reads=[lnw["b3"], lnw["b1"], lgb, lbb], writes=[zb], hard=True)

    def rms_heads(src_ap, nh, sq, ms, rt, dst_ap, wbuf, reads, gain_t=None, gain_b=None):
        T.op("act", lambda: act.activation(out=sq[:, 0:nh * 64], in_=src_ap, func=AF.Square),
             reads=reads, writes=[wbuf["sq"]])
        T.op("dve", lambda: dve.tensor_reduce(out=ms[:, 0:nh], in_=sq[:, 0:nh * 64].rearrange("p (h d) -> p h d", d=64),
                                              axis=AX.X, op=ALU.add),
             reads=[wbuf["sq"]], writes=[wbuf["ms"]])
        T.op("act", lambda: act.activation(out=rt[:, 0:nh], in_=ms[:, 0:nh], func=AF.Sqrt, bias=epsc[:, 1:2],
                                           scale=1.0 / 64.0),
             reads=[wbuf["ms"], eps_b], writes=[wbuf["rt"]])

        T.op("dve", lambda: dve.reciprocal(out=rt[:, 0:nh], in_=rt[:, 0:nh]), reads=[wbuf["rt"]], writes=[wbuf["rt"]])

        def _n():
            i = dve.tensor_tensor(out=dst_ap, in0=src_ap.rearrange("p (h d) -> p h d", d=64),
                                  in1=rt[:, 0:nh].unsqueeze(2).to_broadcast([128, nh, 64]), op=ALU.mult)
            if gain_t is not None:
                i = dve.tensor_tensor(out=dst_ap, in0=dst_ap, in1=gain_t, op=ALU.mult)
            return i
        T.op("dve", _n, reads=list(reads) + [wbuf["rt"]] + ([gain_b] if gain_b else []), writes=[wbuf["dst"]], hard=True)

    acnt = [0]

    asteps = []
    apre = []

    def attn_head(qT_ap, kT_fn, v_fn, key_list, ps_s, ps_s_b, pT_ring, ps_o, ps_o_b, nq, reads_q, bias_fn=None,
                  pre=None, post=None):
        nk = len(key_list)
        hseq = len(apre)
        apre.append(pre)
        for i, kt in enumerate(key_list):
            sidx = acnt[0] % 2
            acnt[0] += 1

            def s_fn(kt=kt, sidx=sidx):
                kap, kb = kT_fn(kt)
                bi = bias_fn(kt) if bias_fn is not None else None

                def _s():
                    ins = pe.matmul(ps_s[sidx][:, 0:nq], lhsT=kap, rhs=qT_ap, start=True, stop=(bi is None))
                    if bi is not None:
                        ins = pe.matmul(ps_s[sidx][:, 0:nq], lhsT=identb[:], rhs=bi[0], start=False, stop=True)
                    return ins
                T.op("pe", _s, reads=[kb] + list(reads_q) + ([bi[1], identb_b] if bi else []), writes=[ps_s_b[sidx]])

            def epv_fn(kt=kt, sidx=sidx, i=i):
                pt, pb, _ = pT_ring.next()
                T.op("act", lambda: act.activation(out=pt[:, 0:nq], in_=ps_s[sidx][:, 0:nq], func=AF.Exp),
                     reads=[ps_s_b[sidx]], writes=[pb])
                vap, vb = v_fn(kt)
                T.op("pe", lambda: pe.matmul(ps_o[0:65, 0:nq], lhsT=vap, rhs=pt[:, 0:nq],
                                             start=(i == 0), stop=(i == nk - 1)),
                     reads=[vb, pb], writes=[ps_o_b])
            asteps.append((s_fn, epv_fn, post if i == nk - 1 else None, hseq))

    def attn_flush(lookahead=2, fillers=()):
        n = len(asteps)
        done_pre = [0]
        nk0 = next((j for j in range(n) if asteps[j][2] is not None), n - 1) + 1
        fill_at = {}
        for i, f in enumerate(fillers):
            fill_at.setdefault((i * (nk0 + 1)) // max(1, len(fillers)), []).append(f)

        def run_pre(upto):
            while done_pre[0] <= min(upto, len(apre) - 1):
                f = apre[done_pre[0]]
                if f is not None:
                    f()
                done_pre[0] += 1
        pend = []
        for j in range(n):
            for f in fill_at.pop(j, []):
                f()
            run_pre(asteps[j][3] + lookahead)
            if j == 0:
                asteps[0][0]()
            if j + 1 < n:
                run_pre(asteps[j + 1][3])
                asteps[j + 1][0]()
            asteps[j][1]()
            while pend and pend[0][0] <= j:
                pend.pop(0)[1]()
            if asteps[j][2] is not None:
                pend.append((j + 3, asteps[j][2]))
        for _, f in pend:
            f()
        for j in sorted(fill_at):
            for f in fill_at[j]:
                f()
        asteps.clear()
        apre.clear()

    def attn_norm(ps_o, ps_o_b, osb, osb_b, ps_bc, ps_bc_b, dst_ap, dst_b, nq):
        T.op("dve", lambda: dve.tensor_copy(out=osb[0:65, 0:nq], in_=ps_o[0:65, 0:nq]), reads=[ps_o_b], writes=[osb_b])
        T.op("pe", lambda: pe.matmul(ps_bc[:, 0:nq], lhsT=sel64[:, :], rhs=osb[:, 0:nq], start=True, stop=True),
             reads=[osb_b, sel_b], writes=[ps_bc_b])
        def _n():
            dve.reciprocal(out=rcp_t[0:64, 0:nq], in_=ps_bc[0:64, 0:nq])
            return dve.tensor_tensor(out=dst_ap, in0=osb[0:64, 0:nq], in1=rcp_t[0:64, 0:nq], op=ALU.mult)
        T.op("dve", _n, reads=[osb_b, ps_bc_b], writes=[dst_b, rcp_b])

    def x_src(t):
        return x_in[t * 128:(t + 1) * 128, :] if t < NTL else ctx_in[(t - NTL) * 128:(t - NTL + 1) * 128, :]

    def qhead_pos(h):
        return (h % 3) + 3 * (h // 6), (h // 3) % 2

    L = 0
    with contextlib.ExitStack() as stL0:
        stR = contextlib.ExitStack()
        gate, gate_b = compute_mod(0, stL0, stR)
        QT = sb("QT", [128, 6, SEQ + CTXL], BF16, stR); QT_b = [Buf() for _ in range(NT)]
        KT = sb("KT", [128, 2, SEQ + CTXL], BF16, stR); KT_b = [Buf() for _ in range(NT)]
        VA = sb("VA", [128, NT, 4, 66], BF16, stR); VA_b = [Buf() for _ in range(NT)]
        bandt = sb("bandt", [128, 4, 5, 128], BF16, stR); band_b = Buf()
        poolw = sb("poolw", [128, 4, 128], BF16, stR); poolw_b = Buf()
        T.op("pool", lambda: pool.memset(poolw[:], 0.0), writes=[poolw_b])
        w0_s = T.semc("w0")
        T.op("dve", lambda: dve.memset(VA[:].rearrange("p t k d -> p (t k) d")[:, :, 64:65], 1.0), writes=VA_b)
        T.dma("sp", w0_s, bandt[:], band[:, :, :, :], writes=[band_b])

        with contextlib.ExitStack() as stA:
            win = sb("win0", [128, 8, 1536], BF16, stA); win_b = Buf()
            T.dma("pool", w0_s, win[:], ev_w_in.rearrange("(k p) n -> p k n", p=128), writes=[win_b])
            if n_layers == 2:
                W1s = dscr("W1s", [NEXP, 11, 128, 8, 256], BF16); W3s = dscr("W3s", [NEXP, 11, 128, 8, 256], BF16)
                W2s = dscr("W2s", [NEXP, 2, 128, NF, 512], BF16)
                W1s_b = [Buf() for _ in range(NEXP)]; W3s_b = [Buf() for _ in range(NEXP)]; W2s_b = [Buf() for _ in range(NEXP)]
                wc_s = T.semc("wcast")
                for e in range(NEXP):
                    for j in range(11):
                        T.dma("pool", wc_s, W1s[e, j], od_w1[e][:, j * 256:(j + 1) * 256].rearrange("(k p) n -> p k n", p=128),
                              writes=[W1s_b[e]])
                        T.dma("pool", wc_s, W3s[e, j], od_w3[e][:, j * 256:(j + 1) * 256].rearrange("(k p) n -> p k n", p=128),
                              writes=[W3s_b[e]])
                    for hh in range(2):
                        T.dma("pool", wc_s, W2s[e, hh], od_w2[e][:, hh * 512:(hh + 1) * 512].rearrange("(f p) n -> p f n", p=128),
                              writes=[W2s_b[e]])
            rring = Ring(T, stA, nc, "rope", 2, [128, 2, 64], F32)
            gaint = sb("gaint", [128, 16, 64], F32, stA); gain_b = Buf()
            T.dma("sp", w0_s, gaint[:].rearrange("p h d -> p (h d)"),
                  ev_qk_gain.rearrange("(o n) -> o n", o=1).to_broadcast([128, 1024]), writes=[gain_b])
            T.op("dve", lambda: dve.tensor_scalar(out=gaint[:, 0:12, :], in0=gaint[:, 0:12, :], scalar1=0.125,
                                                  scalar2=None, op0=ALU.mult), reads=[gain_b], writes=[gain_b])
            pwf = sb("pwf", [64, 4, 64], F32, stA); pws = sb("pws", [64, 4, 64], F32, stA); pw_b = Buf()
            T.dma("sp", w0_s, pwf[:], ev_pool_w.rearrange("g c d -> c g d"), writes=[pw_b])
            T.dma("sp", w0_s, pws[:].rearrange("p g d -> p (g d)"),
                  ev_pool_scale.rearrange("(o n) -> o n", o=1).to_broadcast([64, 256]), writes=[pw_b])
            T.op("dve", lambda: dve.tensor_tensor(out=poolw[0:64, :, 0:64], in0=pwf[:], in1=pws[:], op=ALU.mult),
                 reads=[pw_b], writes=[poolw_b])

            xring = Ring(T, stA, nc, "xa", 2, [128, D], F32)
            aring = Ring(T, stA, nc, "aa", 2, [128, 256], BF16)
            hT = sb("hTa", [128, 8, 128], BF16, stA); hT_b = Buf()
            ms = sb("msa", [128, 16], F32, stA); rt = sb("rta", [128, 16], F32, stA)
            qn = sb("qna", [128, 16, 64], F32, stA)
            t1 = sb("t1a", [128, 16, 64], F32, stA); t2 = sb("t2a", [128, 16, 64], F32, stA)
            sq = t2[:].rearrange("p h d -> p (h d)")
            stg = sb("stga", [128, 1024], BF16, stA)
            t12_b = Buf(); stg_b = Buf()
            wb_ = {"sq": t12_b, "ms": Buf(), "rt": Buf(), "dst": Buf()}
            ps_tr = ps("ps_trA", [128, 1024], F32, stA); ps_tr_b = Buf()
            ps_pj = ps("ps_pjA", [128, 1536], F32, stA); ps_pj_b = Buf()
            ps_t2 = ps("ps_t2A", [128, 1024], BF16, stA); ps_t2_b = Buf()
            for t in range(NT):
                stream = 0 if t < NTL else 1
                xt, xb, xs = xring.next()
                T.dma("sp", xs, xt[:], x_src(t), writes=[xb])
                make_hT(xt, xb, L, 0, 1, stream, ps_tr, ps_tr_b, lambda k: hT[:, k, :], hT_b)

                def _pj():
                    i = None
                    for (c0, n, w0) in ((0, 512, 256), (512, 512, 768), (1024, 256, 0), (1280, 256, 1280)):
                        for k in range(8):
                            i = pe.matmul(ps_pj[:, c0:c0 + n], lhsT=hT[:, k, :],
                                          rhs=win[:, k, w0:w0 + n], start=(k == 0), stop=(k == 7))
                    return i
                T.op("pe", _pj, reads=[hT_b, win_b], writes=[ps_pj_b])
                if t == 0:
                    dump("hT", hT[:].rearrange("p a b -> p (a b)"), [128, 1024], BF16, [hT_b])
                at_, ab_, as_ = aring.next()

                def _av(t=t, at_=at_):
                    act.copy(out=at_[:], in_=ps_pj[:, 1024:1280])
                    return act.copy(out=VA[:, t, :, 0:64], in_=ps_pj[:, 1280:1536].rearrange("p (k d) -> p k d", d=64))
                T.op("act", _av, reads=[ps_pj_b], writes=[ab_, VA_b[t]])
                T.dma("sp", as_, AD[t * 128:(t + 1) * 128, :], at_[:], reads=[ab_], writes=[AD_b[t]])
                rms_heads(ps_pj[:, 0:1024], 16, sq, ms, rt, qn[:], wb_, [ps_pj_b], gaint[:], gain_b)
                if t == 0:
                    dump("ms", ms[:], [128, 16], F32, [wb_["ms"]])
                    dump("qn", qn[:].rearrange("p a b -> p (a b)"), [128, 1024], F32, [wb_["dst"]])
                stq = stg[:, 0:768].rearrange("p (jj r hf d) -> p jj r hf d", jj=2, r=3, hf=2)
                stk = stg[:, 768:1024].rearrange("p (k d) -> p k d", d=64)

                def qperm(tt):
                    return tt[:, 0:12, :].rearrange("p (jj hf r) d -> p jj r hf d", jj=2, hf=2, r=3)
                if t < NTL:
                    rt_, rb_, rs_ = rring.next()
                    T.dma("sp", rs_, rt_[:, 0, :], ropeC[:, t, :], writes=[rb_])
                    T.dma("sp", rs_, rt_[:, 1, :], ropeS[:, t, :], writes=[rb_])

                    def _rope(rt_=rt_):
                        cb = rt_[:, 0, :].unsqueeze(1).to_broadcast([128, 16, 64])
                        dve.tensor_tensor(out=t1[:], in0=qn[:], in1=cb, op=ALU.mult)
                        qv = qn[:].rearrange("p h (a f) -> p h a f", a=4)
                        tv = t2[:].rearrange("p h (a f) -> p h a f", a=4)
                        sv = rt_[:, 1, :].rearrange("p (a f) -> p a f", a=4)
                        i = None
                        for ax in range(2):
                            for hf in range(2):
                                a_dst = ax * 2 + hf
                                a_src = ax * 2 + (1 - hf)
                                i = dve.tensor_tensor(out=tv[:, :, a_dst, :], in0=qv[:, :, a_src, :],
                                                      in1=sv[:, a_dst, :].unsqueeze(1).to_broadcast([128, 16, 16]),
                                                      op=ALU.mult)
                        return i
                    T.op("dve", _rope, reads=[wb_["dst"], rb_], writes=[t12_b])

                    def _st():
                        for jj in range(2):
                            dve.tensor_tensor(out=stq[:, jj], in0=qperm(t1)[:, jj], in1=qperm(t2)[:, jj], op=ALU.add)
                        return dve.tensor_tensor(out=stk, in0=t1[:, 12:16, :], in1=t2[:, 12:16, :], op=ALU.add)
                    T.op("dve", _st, reads=[t12_b], writes=[stg_b])
                else:
                    def _st():
                        for jj in range(2):
                            dve.tensor_copy(out=stq[:, jj], in_=qperm(qn)[:, jj])
                        return dve.tensor_copy(out=stk, in_=qn[:, 12:16, :])
                    T.op("dve", _st, reads=[wb_["dst"]], writes=[stg_b])

                def _t2():
                    i = None
                    for j in range(8):
                        i = pe.transpose(ps_t2[:, j * 128:(j + 1) * 128], stg[:, j * 128:(j + 1) * 128], identb[:])
                    return i
                T.op("pe", _t2, reads=[stg_b, identb_b], writes=[ps_t2_b])

                def _e2(t=t):
                    act.copy(out=QT[:, :, t * 128:(t + 1) * 128],
                             in_=ps_t2[:, 0:768].rearrange("p (j n) -> p j n", n=128))
                    return act.copy(out=KT[:, :, t * 128:(t + 1) * 128],
                                    in_=ps_t2[:, 768:1024].rearrange("p (j n) -> p j n", n=128))
                T.op("act", _e2, reads=[ps_t2_b], writes=[QT_b[t], KT_b[t]])
                if t == 0:
                    dump("stg", stg[:], [128, 1024], BF16, [stg_b])
                    dump("QT0", QT[:, :, 0:128], [128, 6, 128], BF16, [QT_b[0]])
                    dump("KT0", KT[:, :, 0:128], [128, 2, 128], BF16, [KT_b[0]])
                    dump("VA0", VA[:, 0, :, :], [128, 4, 66], BF16, [VA_b[0]])

        T.barrier()
        with contextlib.ExitStack() as stB:
            wout = sb("wout0", [128, 16, D], BF16, stB); wout_b = Buf()
            T.op("pool", lambda: pool.memset(wout[64:128, :, :], 0.0), writes=[wout_b])
            T.dma("pool", w0_s, wout[0:64], w_out[0].rearrange("(c p) n -> p c n", p=64), writes=[wout_b])
            lg, lgb = load_lnp(0, 0, stB, w0_s)
            lb, lbb = load_lnp(0, 1, stB, w0_s)
            ps_s = [ps(f"ps_s{i}", [128, 512], F32, stB) for i in range(2)]; ps_s_b = [Buf(), Buf()]
            ps_o = [ps(f"ps_o{i}", [128, 512], F32, stB) for i in range(2)]; ps_o_b = [Buf(), Buf()]
            ps_bc = ps("ps_bc", [128, 512], F32, stB); ps_bc_b = Buf()
            ps_y = ps("ps_y", [128, 1024], F32, stB); ps_y_b = Buf()
            ps_pl = ps("ps_pl", [128, 512], F32, stB); ps_pl_b = Buf()
            pT_ring = Ring(T, stB, nc, "pT", 3, [128, 512], BF16)
            osb = [sb(f"osb{i}", [128, 512], F32, stB) for i in range(2)]; osb_b = [Buf(), Buf()]
            for i in range(2):
                T.op("pool", lambda i=i: pool.memset(osb[i][:], 0.0), writes=[osb_b[i]])
            OT = sb("OT0", [128, 12, 512], BF16, stB); OT_b = [Buf() for _ in range(12)]
            T.op("pool", lambda: pool.memset(OT[64:128, :, :], 0.0), writes=OT_b)
            mT = sb("mT", [128, 4, 128], BF16, stB); mT_b = Buf()
            T.op("pool", lambda: pool.memset(mT[64:128, :, :], 0.0), writes=[mT_b])
            PTt = sb("PTt", [128, 4, 128], BF16, stB); PT_b = Buf()
            T.op("pool", lambda: pool.memset(PTt[64:128, :, :], 0.0), writes=[PT_b])
            a3ring = Ring(T, stB, nc, "a3", 2, [128, 3 * 256 + 64], BF16)
            for a3t_, a3b_ in zip(a3ring.t, a3ring.b):
                T.op("pool", lambda a3t_=a3t_: pool.memset(a3t_[:], 0.0), writes=[a3b_])
            zring = Ring(T, stB, nc, "zb", 2, [128, D], F32)
            tmp = sb("tmpb", [128, D], F32, stB); tmp_b = Buf()
            hcount = 0
            chunks = [(qc * 4, 4, list(range(NT))) for qc in range(8)] + [(NTL, 2, [NTL, NTL + 1])]
            qz = sb("Qz0", [128, 12, 512], BF16, stB); Qz_b = [Buf() for _ in range(12)]
            T.op("pool", lambda: pool.memset(qz[:], 0.0), writes=Qz_b)
            tails = []
            for ci, (t0, ntl, keys) in enumerate(chunks):
                nq = ntl * 128
                stream = 0 if t0 < NTL else 1
                for h in range(12):
                    ch, half = qhead_pos(h)
                    rows = slice(half * 64, (half + 1) * 64)
                    T.op("pool", lambda h=h, ch=ch, rows=rows, t0=t0, nq=nq: pool.tensor_copy(
                        out=qz[rows, h, 0:nq], in_=QT[rows, ch, t0 * 128:t0 * 128 + nq]),
                        reads=[QT_b[t0 + i] for i in range(ntl)], writes=[Qz_b[h]])
                for h in range(12):
                    ch, half = qhead_pos(h)
                    kv = h // 3
                    rows = slice(half * 64, (half + 1) * 64)
                    oi = hcount % 2
                    hcount += 1
                    attn_head(qz[:, h, 0:nq],
                              lambda kt, kv=kv: (KT[:, kv // 2, kt * 128:(kt + 1) * 128], KT_b[kt]),
                              lambda kt, kv=kv: (VA[:, kt, kv, 0:65], VA_b[kt]),
                              keys, ps_s, ps_s_b, pT_ring, ps_o[oi], ps_o_b[oi], nq,
                              [Qz_b[h]],
                              post=lambda oi=oi, h=h, nq=nq: attn_norm(ps_o[oi], ps_o_b[oi], osb[oi], osb_b[oi], ps_bc, ps_bc_b,
                                                                       OT[0:64, h, 0:nq], OT_b[h], nq))
                attn_flush(fillers=tails)
                tails = []
                for il in range(ntl):
                    def _tail(il=il, t0=t0, stream=stream):
                        t = t0 + il
                        first = (t == 0) or (t == NTL)
                        last = (t == NTL - 1) or (t == NT - 1)
                        tlo = t if first else t - 1
                        thi = t if last else t + 1
                        a3, a3b, a3s = a3ring.next()
                        nsrc = thi - tlo + 1
                        T.dma("sp", a3s, a3[:, 0:nsrc * 256].rearrange("p (j c) -> p j c", c=256),
                              AD[tlo * 128:(thi + 1) * 128, :].rearrange("(j p) c -> p j c", p=128),
                              reads=[AD_b[i] for i in range(tlo, thi + 1)], writes=[a3b])
                        srcs = []
                        if not first:
                            srcs.append((t - 1 - tlo, 0))
                        srcs.append((t - tlo, 3 if first else (4 if last else 1)))
                        if not last:
                            srcs.append((t + 1 - tlo, 2))

                        def _pm(srcs=srcs, a3=a3):
                            i = None
                            for g in range(4):
                                for j, (sl, var) in enumerate(srcs):
                                    i = pe.matmul(ps_pl[:, g * 128:(g + 1) * 128], lhsT=a3[:, sl * 256 + g * 64:sl * 256 + g * 64 + 128],
                                                  rhs=bandt[:, g, var, :], start=(j == 0), stop=(j == len(srcs) - 1))
                            return i
                        T.op("pe", _pm, reads=[a3b, band_b], writes=[ps_pl_b])
                        T.op("act", lambda: act.copy(out=mT[0:64].rearrange("p g n -> p (g n)"), in_=ps_pl[0:64, :]),
                             reads=[ps_pl_b], writes=[mT_b])

                        def _pp():
                            i = None
                            for g in range(4):
                                i = pe.matmul(ps_pl[:, g * 128:(g + 1) * 128], lhsT=poolw[:, g, :], rhs=mT[:, g, :],
                                              start=True, stop=True)
                            return i
                        T.op("pe", _pp, reads=[mT_b, poolw_b], writes=[ps_pl_b])
                        T.op("act", lambda: act.copy(out=PTt[0:64].rearrange("p g n -> p (g n)"), in_=ps_pl[0:64, :]),
                             reads=[ps_pl_b], writes=[PT_b])

                        def _y(il=il):
                            i = None
                            for n in range(2):
                                for c in range(16):
                                    lhs = PTt[:, c, :] if c < 4 else OT[:, c - 4, il * 128:(il + 1) * 128]
                                    i = pe.matmul(ps_y[:, n * 512:(n + 1) * 512], lhsT=lhs, rhs=wout[:, c, n * 512:(n + 1) * 512],
                                                  start=(c == 0), stop=(c == 15))
                            return i
                        T.op("pe", _y, reads=[PT_b, wout_b] + OT_b, writes=[ps_y_b])
                        if t == 0:
                            dump("OT", OT[0:64], [64, 12, 512], BF16, OT_b)
                            dump("PT", PTt[0:64], [64, 4, 128], BF16, [PT_b])
                        zt, zb, zs = zring.next()
                        T.dma("sp", zs, zt[:], x_src(t), writes=[zb])
                        resid_ln([ps_y[:, 0:512], ps_y[:, 512:1024]], [ps_y_b], zt, zb,
                                 gate[(stream, 0)], gate_b[(stream, 0)], lg, lgb, lb, lbb, tmp, tmp_b)
                        T.dma("sp", zs, X1[t * 128:(t + 1) * 128, :], zt[:], reads=[zb], writes=[X1b[t]], is_output=debug)
                    tails.append(_tail)
                if not DEFER_L0:
                    for f in tails:
                        f()
                    tails = []
            for f in tails:
                f()

        T.barrier()
        stR.close()
        with contextlib.ExitStack() as stF:
            w1 = sb("w1f", [128, 8, FFN], BF16, stF); w3 = sb("w3f", [128, 8, FFN], BF16, stF)
            w2 = sb("w2f", [128, NF, D], BF16, stF); wf_b = Buf(); wf_s = T.semc("wf")
            for k in range(8):
                T.dma("pool", wf_s, w1[:, k, :], ev_ffn_w1[k * 128:(k + 1) * 128, :], writes=[wf_b], max_dma_last_dim=4096)
                T.dma("pool", wf_s, w3[:, k, :], ev_ffn_w3[k * 128:(k + 1) * 128, :], writes=[wf_b], max_dma_last_dim=4096)
            for f in range(NF):
                T.dma("pool", wf_s, w2[:, f, :], ev_ffn_w2[f * 128:(f + 1) * 128, :], writes=[wf_b], max_dma_last_dim=4096)
            lg, lgb = load_lnp(0, 2, stF, wf_s)
            lb, lbb = load_lnp(0, 3, stF, wf_s)
            x1c = [sb(f"x1c{i}", [128, D], F32, stF) for i in range(4)]; x1c_b = [Buf() for _ in range(4)]
            x1c_s = [T.semc("x1c") for _ in range(4)]
            h2T = sb("h2T", [128, 8, 512], BF16, stF); h2T_b = [Buf() for _ in range(4)]
            AT = sb("ATf", [128, NF, 512], BF16, stF); AT_b = [Buf() for _ in range(NF)]
            sg = [sb(f"sgf{i}", [128, 512], BF16, stF) for i in range(2)]; sg_b = [Buf(), Buf()]
            tmp = sb("tmpf", [128, D], F32, stF); tmp_b = Buf()
            ps_tr = ps("ps_trF", [128, 1024], F32, stF); ps_tr_b = Buf()
            ps_g = [ps(f"ps_gF{i}", [128, 512], F32, stF) for i in range(2)]; ps_g_b = [Buf(), Buf()]
            ps_u = [ps(f"ps_uF{i}", [128, 512], F32, stF) for i in range(2)]; ps_u_b = [Buf(), Buf()]
            ps_o2 = [ps(f"ps_oF{i}", [128, 512], F32, stF) for i in range(2)]; ps_o2_b = [Buf(), Buf()]
            oc = 0
            chunks = [(qc * 4, 4) for qc in range(8)] + [(NTL, 2)]
            for (t0, ntl) in chunks:
                nq = ntl * 128
                stream = 0 if t0 < NTL else 1
                for il in range(ntl):
                    t = t0 + il
                    T.dma("sp", x1c_s[il], x1c[il][:], X1[t * 128:(t + 1) * 128, :], reads=[X1b[t]], writes=[x1c_b[il]])
                    make_hT(x1c[il], x1c_b[il], L, 2, 3, stream, ps_tr, ps_tr_b,
                            lambda k, il=il: h2T[:, k, il * 128:(il + 1) * 128], h2T_b[il])
                for f in range(NF):
                    gi = f % 2

                    def _gu(f=f, gi=gi, nq=nq):
                        i = None
                        for k in range(8):
                            i = pe.matmul(ps_g[gi][:, 0:nq], lhsT=w1[:, k, f * 128:(f + 1) * 128], rhs=h2T[:, k, 0:nq],
                                          start=(k == 0), stop=(k == 7))
                        for k in range(8):
                            i = pe.matmul(ps_u[gi][:, 0:nq], lhsT=w3[:, k, f * 128:(f + 1) * 128], rhs=h2T[:, k, 0:nq],
                                          start=(k == 0), stop=(k == 7))
                        return i
                    T.op("pe", _gu, reads=[wf_b] + h2T_b[0:ntl], writes=[ps_g_b[gi], ps_u_b[gi]])
                    T.op("act", lambda gi=gi, nq=nq: act.activation(out=sg[gi][:, 0:nq], in_=ps_g[gi][:, 0:nq], func=AF.Silu),
                         reads=[ps_g_b[gi]], writes=[sg_b[gi]])
                    T.op("dve", lambda gi=gi, f=f, nq=nq: dve.tensor_tensor(out=AT[:, f, 0:nq], in0=sg[gi][:, 0:nq],
                                                                           in1=ps_u[gi][:, 0:nq], op=ALU.mult),
                         reads=[sg_b[gi], ps_u_b[gi]], writes=[AT_b[f]])
                for il in range(ntl):
                    t = t0 + il
                    ois = []
                    for n in range(2):
                        oi = oc % 2
                        oc += 1
                        ois.append(oi)

                        def _o(il=il, n=n, oi=oi):
                            i = None
                            for f in range(NF):
                                i = pe.matmul(ps_o2[oi][:, :], lhsT=AT[:, f, il * 128:(il + 1) * 128],
                                              rhs=w2[:, f, n * 512:(n + 1) * 512], start=(f == 0), stop=(f == NF - 1))
                            return i
                        T.op("pe", _o, reads=AT_b + [wf_b], writes=[ps_o2_b[oi]])
                    resid_ln([ps_o2[ois[0]][:, :], ps_o2[ois[1]][:, :]], [ps_o2_b[ois[0]], ps_o2_b[ois[1]]],
                             x1c[il], x1c_b[il], gate[(stream, 1)], gate_b[(stream, 1)], lg, lgb, lb, lbb, tmp, tmp_b)
                    if n_layers == 1 and t < NTL:
                        T.dma("sp", x1c_s[il], out_ap[t * 128:(t + 1) * 128, :], x1c[il][:], reads=[x1c_b[il]], is_output=True)
                    T.dma("sp", x1c_s[il], X2[t * 128:(t + 1) * 128, :], x1c[il][:], reads=[x1c_b[il]], writes=[X2b[t]],
                          is_output=debug)

    T.barrier()
    if n_layers == 1:
        T.finish()
        es.close()
        return nc

    L = 1
    QD = dscr("QD", [128, 6, SEQ], BF16); QD_b = [Buf() for _ in range(NTL)]
    KD = dscr("KD", [128, 6, SEQ + CTXL], BF16); KD_b = [Buf() for _ in range(NT)]
    VD = dscr("VD", [NT, 128, 12 * 66], BF16); VD_b = [Buf() for _ in range(NT)]
    with contextlib.ExitStack() as stL1:
        stR = contextlib.ExitStack()
        gate, gate_b = compute_mod(1, stL1, stR)
        YT = sb("YT", [128, 2, SEQ], BF16, stR); YT_b = [Buf() for _ in range(8)]
        w1_s = T.semc("w1")
        stU = contextlib.ExitStack()
        UCS = sb("UCS", [128, NTL, 512], BF16, stU); UCS_b = [Buf() for _ in range(NTL)]
        with contextlib.ExitStack() as stA:
            win = sb("win1", [128, 8, 2560], BF16, stA); win_b = Buf()
            for k in range(8):
                T.dma("pool", w1_s, win[:, k, :], od_w_in[k * 128:(k + 1) * 128, :], writes=[win_b], max_dma_last_dim=4096)
            fg = sb("fg1", [128, 4, 64], F32, stA); fg_b = Buf()
            T.dma("sp", w1_s, fg[:].rearrange("p g d -> p (g d)"),
                  od_fgain.rearrange("(o n) -> o n", o=1).to_broadcast([128, 256]), writes=[fg_b])
            c64 = sb("c64", [128, 2, 128], BF16, stA); c64_b = Buf()
            T.dma("sp", w1_s, c64[:, 0, :], C64[:, :], writes=[c64_b])
            T.dma("sp", w1_s, c64[:, 1, :], S64[:, :], writes=[c64_b])
            xring = Ring(T, stA, nc, "xa1", 2, [128, D], F32)
            hT = sb("hTa1", [128, 8, 128], BF16, stA); hT_b = Buf()
            sq = sb("sqa1", [128, 256], F32, stA); ms = sb("msa1", [128, 4], F32, stA); rt = sb("rta1", [128, 4], F32, stA)
            un = sb("una1", [128, 4, 64], F32, stA)
            wb_ = {"sq": Buf(), "ms": Buf(), "rt": Buf(), "dst": Buf()}
            stg = sb("stga1", [128, 14 * 128], BF16, stA); stg_b = Buf()
            qkT = Ring(T, stA, nc, "qkT1", 2, [128, 12, 128], BF16)
            uT = sb("uT1", [128, 2, 128], BF16, stA); uT_b = Buf()
            vring = Ring(T, stA, nc, "va1", 2, [128, 12, 66], BF16)
            for i_ in range(2):
                T.op("dve", lambda i_=i_: dve.memset(vring.t[i_][:, :, 64:66], 1.0), writes=[vring.b[i_]])
            ps_tr = ps("ps_trA1", [128, 1024], F32, stA); ps_tr_b = Buf()
            ps_pj = ps("ps_pjA1", [128, 2048], F32, stA); ps_pj_b = Buf()
            ps_t2 = ps("ps_t2A1", [128, 2048], BF16, stA); ps_t2_b = Buf()
            for t in range(NT):
                lat = t < NTL
                stream = 0 if lat else 1
                xt, xb, xs = xring.next()
                T.dma("sp", xs, xt[:], X2[t * 128:(t + 1) * 128, :], reads=[X2b[t]], writes=[xb])
                make_hT(xt, xb, L, 0, 1, stream, ps_tr, ps_tr_b, lambda k: hT[:, k, :], hT_b)

                def _pj(lat=lat):
                    i = None
                    groups = [(1024, 512, 1024), (1536, 256, 1536)]
                    if lat:
                        groups = [(0, 512, 256), (512, 256, 768), (768, 256, 0)] + groups
                    for (c0, n, w0) in groups:
                        for k in range(8):
                            i = pe.matmul(ps_pj[:, c0:c0 + n], lhsT=hT[:, k, :], rhs=win[:, k, w0:w0 + n],
                                          start=(k == 0), stop=(k == 7))
                    return i
                T.op("pe", _pj, reads=[hT_b, win_b], writes=[ps_pj_b])
                if lat:
                    rms_heads(ps_pj[:, 768:1024], 4, sq, ms, rt, un[:], wb_, [ps_pj_b], fg[:], fg_b)

                def _stq(lat=lat):
                    i = act.copy(out=stg[:, 768:1536], in_=ps_pj[:, 1024:1792])
                    if lat:
                        i = act.mul(out=stg[:, 0:768], in_=ps_pj[:, 0:768], mul=0.125)
                    return i
                T.op("act", _stq, reads=[ps_pj_b] + ([wb_["dst"]] if lat else []), writes=[stg_b])
                if lat:
                    T.op("dve", lambda: dve.tensor_copy(out=stg[:, 1536:1792], in_=un[:].rearrange("p g d -> p (g d)")),
                         reads=[wb_["dst"]], writes=[stg_b])

                def _t2(lat=lat):
                    i = None
                    for j in (range(14) if lat else range(6, 12)):
                        i = pe.transpose(ps_t2[:, j * 128:(j + 1) * 128], stg[:, j * 128:(j + 1) * 128], identb[:])
                    return i
                T.op("pe", _t2, reads=[stg_b, identb_b], writes=[ps_t2_b])
                qk, qkb, qks = qkT.next()

                def _e2(lat=lat, qk=qk):
                    i = act.copy(out=qk[:, 6:12, :], in_=ps_t2[:, 768:1536].rearrange("p (j n) -> p j n", n=128))
                    if lat:
                        i = act.copy(out=qk[:, 0:6, :], in_=ps_t2[:, 0:768].rearrange("p (j n) -> p j n", n=128))
                    return i
                T.op("act", _e2, reads=[ps_t2_b], writes=[qkb])
                T.dma("sp", qks, KD[:, :, t * 128:(t + 1) * 128], qk[:, 6:12, :], reads=[qkb], writes=[KD_b[t]])
                if lat:
                    T.dma("sp", qks, QD[:, :, t * 128:(t + 1) * 128], qk[:, 0:6, :], reads=[qkb], writes=[QD_b[t]])
                    T.op("dve", lambda: dve.tensor_copy(out=uT[:].rearrange("p a n -> p (a n)"), in_=ps_t2[:, 1536:1792]),
                         reads=[ps_t2_b], writes=[uT_b])

                    def _uc():
                        i = None
                        for cs_ in range(2):
                            for c2 in range(2):
                                i = pe.matmul(ps_tr[:, (cs_ * 2 + c2) * 128:(cs_ * 2 + c2 + 1) * 128], lhsT=uT[:, c2, :],
                                              rhs=c64[:, cs_, :], start=True, stop=True)
                        return i
                    T.op("pe", _uc, reads=[uT_b, c64_b], writes=[ps_tr_b])
                    T.op("dve", lambda t=t: dve.tensor_copy(out=UCS[:, t, :], in_=ps_tr[:, 0:512]),
                         reads=[ps_tr_b], writes=[UCS_b[t]])
                def _pv():
                    i = None
                    for (c0, n, w0) in ((0, 512, 1792), (512, 256, 2304)):
                        for k in range(8):
                            i = pe.matmul(ps_pj[:, c0:c0 + n], lhsT=hT[:, k, :], rhs=win[:, k, w0:w0 + n],
                                          start=(k == 0), stop=(k == 7))
                    return i
                T.op("pe", _pv, reads=[hT_b, win_b], writes=[ps_pj_b])
                vt, vb, vs = vring.next()
                T.op("act", lambda vt=vt: act.copy(out=vt[:, :, 0:64], in_=ps_pj[:, 0:768].rearrange("p (h d) -> p h d", d=64)),
                     reads=[ps_pj_b], writes=[vb])
                T.dma("sp", vs, VD[t], vt[:].rearrange("p h d -> p (h d)"), reads=[vb], writes=[VD_b[t]])
        T.barrier()
        if stop_after == "A1":
            T.finish(); stU.close(); stR.close(); return nc
        with contextlib.ExitStack() as stFo:
            tring = Ring(T, stFo, nc, "dft", 3, [128, 2, 8, 512], BF16)
            ps_f = [ps(f"ps_f{i}", [128, 512], F32, stFo) for i in range(4)]; ps_f_b = [Buf() for _ in range(4)]
            for tc in range(8):
                pb_ = (tc % 2) * 2
                for sp_ in range(4):
                    tt, tb, ts_ = tring.next()
                    for ci, src in enumerate((CN, SN)):
                        T.dma("sp", ts_, tt[:, ci, :, :],
                              src[sp_ * 1024:(sp_ + 1) * 1024, tc * 512:(tc + 1) * 512].rearrange("(j p) n -> p j n", p=128),
                              writes=[tb])

                    def _f(tt=tt, sp_=sp_, pb_=pb_):
                        i = None
                        for c2 in range(2):
                            for j in range(8):
                                s_ = sp_ * 8 + j
                                for ci in range(2):
                                    i = pe.matmul(ps_f[pb_ + c2][:, :], lhsT=UCS[:, s_, (ci * 2 + c2) * 128:(ci * 2 + c2 + 1) * 128],
                                                  rhs=tt[:, ci, j, :], start=(s_ == 0 and ci == 0), stop=(s_ == 31 and ci == 1))
                        return i
                    T.op("pe", _f, reads=[tb] + UCS_b[sp_ * 8:(sp_ + 1) * 8], writes=[ps_f_b[pb_], ps_f_b[pb_ + 1]])

                def _fe(tc=tc, pb_=pb_):
                    i = None
                    for c2 in range(2):
                        i = act.copy(out=YT[:, c2, tc * 512:(tc + 1) * 512], in_=ps_f[pb_ + c2][:, :])
                    return i
                T.op("act", _fe, reads=[ps_f_b[pb_], ps_f_b[pb_ + 1]], writes=[YT_b[tc]])
        T.barrier()
        if stop_after == "FO":
            T.finish(); stU.close(); stR.close(); return nc
        stU.close()

        with contextlib.ExitStack() as stB:
            wout = sb("wout1", [128, 12, D], BF16, stB); woutF = sb("woutF1", [128, 2, D], BF16, stB); wout_b = Buf()
            T.op("pool", lambda: pool.memset(wout[64:128, :, :], 0.0), writes=[wout_b])
            T.dma("pool", w1_s, wout[0:64], w_out[1, 256:1024, :].rearrange("(c p) n -> p c n", p=64), writes=[wout_b])
            T.dma("pool", w1_s, woutF[:], w_out[1, 0:256, :].rearrange("(c p) n -> p c n", p=128), writes=[wout_b])
            lg, lgb = load_lnp(1, 0, stB, w1_s)
            lb, lbb = load_lnp(1, 1, stB, w1_s)
            KTc = sb("KTc", [128, 6, 256], BF16, stB); Vc = sb("Vc", [128, 2, 12 * 66], BF16, stB); kvc_b = Buf()
            T.dma("sp", w1_s, KTc[:], KD[:, :, SEQ:SEQ + CTXL], reads=[KD_b[32], KD_b[33]], writes=[kvc_b])
            T.dma("sp", w1_s, Vc[:], VD[NTL:NT].rearrange("j p n -> p j n"), reads=[VD_b[32], VD_b[33]], writes=[kvc_b])
            qz1 = sb("Qz1", [128, 12, 512], BF16, stB); Qz1_b = [Buf() for _ in range(12)]; qz1_s = T.semc("qz1")
            T.op("pool", lambda: pool.memset(qz1[:], 0.0), writes=Qz1_b)
            kring = Ring(T, stB, nc, "kb1", 2, [128, 6, 1024], BF16)
            vwring = Ring(T, stB, nc, "vb1", 2, [128, 8, 12 * 66], BF16)
            bring = Ring(T, stB, nc, "bias1", 3, [128, 8, 512], BF16)
            ps_s = [ps(f"ps_s1{i}", [128, 512], F32, stB) for i in range(2)]; ps_s_b = [Buf(), Buf()]
            ps_o = [ps(f"ps_o1{i}", [128, 512], F32, stB) for i in range(2)]; ps_o_b = [Buf(), Buf()]
            ps_bc = ps("ps_bc1", [128, 512], F32, stB); ps_bc_b = Buf()
            ps_y = ps("ps_y1", [128, 1024], F32, stB); ps_y_b = Buf()
            pT_ring = Ring(T, stB, nc, "pT1", 3, [128, 512], BF16)
            osb = [sb(f"osb1{i}", [128, 512], F32, stB) for i in range(2)]; osb_b = [Buf(), Buf()]
            for i in range(2):
                T.op("pool", lambda i=i: pool.memset(osb[i][:], 0.0), writes=[osb_b[i]])
            OT = sb("OT1", [128, 12, 512], BF16, stB); OT_b = [Buf() for _ in range(12)]
            T.op("pool", lambda: pool.memset(OT[64:128, :, :], 0.0), writes=OT_b)
            zring = Ring(T, stB, nc, "zb1", 2, [128, D], F32)
            tmp = sb("tmpb1", [128, D], F32, stB); tmp_b = Buf()
            hcount = 0
            tails = []
            for qb in range(8):
                t0 = 4 * qb
                bt = 0 if qb == 0 else (2 if qb == 7 else 1)
                klo = max(0, t0 - 2)
                khi = min(NTL, t0 + 6)
                nk = khi - klo
                slot0 = klo - (t0 - 2)
                for h in range(12):
                    ch, half = h // 2, h % 2
                    rows = slice(half * 64, (half + 1) * 64)
                    T.dma("sp", qz1_s, qz1[rows, h, :], QD[rows, ch, t0 * 128:(t0 + 4) * 128], reads=QD_b[t0:t0 + 4],
                          writes=[Qz1_b[h]])
                kt_, kb_, ks_ = kring.next()
                T.dma("sp", ks_, kt_[:, :, 0:nk * 128], KD[:, :, klo * 128:khi * 128], reads=KD_b[klo:khi], writes=[kb_])
                vt_, vb_, vs_ = vwring.next()
                T.dma("sp", vs_, vt_[:, 0:nk, :], VD[klo:khi].rearrange("j p n -> p j n"), reads=VD_b[klo:khi], writes=[vb_])
                keys = list(range(klo, khi)) + [NTL, NTL + 1]
                for h in range(12):
                    ch, half = h // 2, h % 2
                    rows = slice(half * 64, (half + 1) * 64)
                    oi = hcount % 2
                    hcount += 1
                    bt_, bb_, bs_ = bring.next()

                    def bpre(bt_=bt_, bb_=bb_, bs_=bs_, h=h, bt=bt, slot0=slot0, nk=nk):
                        T.dma("pool", bs_, bt_[:, 0:nk, :], od_bias[h, bt, slot0:slot0 + nk].rearrange("j p n -> p j n"),
                              writes=[bb_])

                    def kfn(kt, rows=rows, ch=ch, kt_=kt_, kb_=kb_, klo=klo):
                        if kt >= NTL:
                            return KTc[:, ch, (kt - NTL) * 128:(kt - NTL + 1) * 128], kvc_b
                        return kt_[:, ch, (kt - klo) * 128:(kt - klo + 1) * 128], kb_

                    def vfn(kt, h=h, vt_=vt_, vb_=vb_, klo=klo):
                        if kt >= NTL:
                            return Vc[:, kt - NTL, h * 66:h * 66 + 65], kvc_b
                        return vt_[:, kt - klo, h * 66:h * 66 + 65], vb_

                    def bfn(kt, bt_=bt_, bb_=bb_, klo=klo):
                        if kt >= NTL:
                            return None
                        return bt_[:, kt - klo, :], bb_
                    attn_head(qz1[:, h, :], kfn, vfn, keys, ps_s, ps_s_b, pT_ring, ps_o[oi], ps_o_b[oi], 512,
                              [Qz1_b[h]], bias_fn=bfn, pre=bpre,
                              post=lambda oi=oi, h=h: attn_norm(ps_o[oi], ps_o_b[oi], osb[oi], osb_b[oi], ps_bc, ps_bc_b,
                                                                OT[0:64, h, :], OT_b[h], 512))
                attn_flush(fillers=tails)
                tails = []
                for il in range(4):
                    def _tail(il=il, t0=t0):
                        t = t0 + il

                        def _y(il=il, t=t):
                            i = None
                            for n in range(2):
                                for c in range(14):
                                    if c < 2:
                                        lhs, rhs = YT[:, c, t * 128:(t + 1) * 128], woutF[:, c, n * 512:(n + 1) * 512]
                                    else:
                                        lhs, rhs = OT[:, c - 2, il * 128:(il + 1) * 128], wout[:, c - 2, n * 512:(n + 1) * 512]
                                    i = pe.matmul(ps_y[:, n * 512:(n + 1) * 512], lhsT=lhs, rhs=rhs, start=(c == 0), stop=(c == 13))
                            return i
                        T.op("pe", _y, reads=[YT_b[t // 4], wout_b] + OT_b, writes=[ps_y_b])
                        zt, zb, zs = zring.next()
                        T.dma("sp", zs, zt[:], X2[t * 128:(t + 1) * 128, :], reads=[X2b[t]], writes=[zb])
                        resid_ln([ps_y[:, 0:512], ps_y[:, 512:1024]], [ps_y_b], zt, zb,
                                 gate[(0, 0)], gate_b[(0, 0)], lg, lgb, lb, lbb, tmp, tmp_b)
                        T.dma("sp", zs, X3[t * 128:(t + 1) * 128, :], zt[:], reads=[zb], writes=[X3b[t]], is_output=debug)
                    tails.append(_tail)
            for f in tails:
                f()

        T.barrier()
        if stop_after == "NA":
            T.finish(); stR.close(); return nc
        stR.close()

        with contextlib.ExitStack() as stM:
            NG = 23
            NROW = NG * 512
            Xs = dscr("Xs", [NROW, D]); Ys = dscr("Ys", [NROW, D])
            Xs_b = Buf(); Ys_b = Buf()
            xs_s = T.semc("xs"); ys_s = T.semc("ys"); wg_s = T.semc("wg"); yg_s = T.semc("yg")
            W1r = W1s.rearrange("e j p k n -> (e j p) (k n)")
            W3r = W3s.rearrange("e j p k n -> (e j p) (k n)")
            W2r = W2s.rearrange("e h p f n -> (e h p) (f n)")
            lg, lgb = load_lnp(1, 2, stM, w1_s)
            lb, lbb = load_lnp(1, 3, stM, w1_s)
            wr = sb("wr", [128, 8, 8], F32, stM); wr_b = Buf()
            T.dma("sp", w1_s, wr[:], od_router.rearrange("(k p) e -> p k e", p=128), writes=[wr_b])
            pidx = sb("pidx", [128, 1], F32, stM); pidx_b = Buf()
            T.dma("sp", w1_s, pidx[:], pidx_in[:, :], writes=[pidx_b])
            utri = sb("utri", [128, 128], BF16, stM); utri_b = Buf()
            T.dma("pool", w1_s, utri[:], utri_in[:, :], writes=[utri_b])
            onesb = sb("onesb", [128, 128], BF16, stM); onesb_b = Buf()
            T.op("dve", lambda: dve.memset(onesb[:], 1.0), writes=[onesb_b])
            x3c = [sb(f"x3c{i}", [128, D], F32, stM) for i in range(4)]; x3c_b = [Buf() for _ in range(4)]
            x3c_s = [T.semc("x3c") for _ in range(4)]
            acc = [sb(f"acc{i}", [128, D], F32, stM) for i in range(4)]; acc_b = [Buf() for _ in range(4)]
            h2T = sb("h2Tm", [128, 8, 512], BF16, stM); h2T_b = [Buf() for _ in range(4)]
            h2Tf = sb("h2Tf", [128, 8, 128], F32, stM); h2Tf_b = Buf()
            AT = sb("ATm", [128, NF, 512], BF16, stM); AT_b = [Buf() for _ in range(NF)]
            sg = [sb(f"sgm{i}", [128, 512], BF16, stM) for i in range(2)]; sg_b = [Buf(), Buf()]
            tmp = sb("tmpm", [128, D], F32, stM); tmp_b = Buf()
            lgt = sb("lgt", [128, 8], F32, stM); mx = sb("mxm", [128, 8], F32, stM)
            indb = sb("indb", [128, 8], BF16, stM)
            eqs = sb("eqs", [128, NTL, 2, 8], F32, stM)
            wts = sb("wts", [128, NTL, 2], F32, stM)
            posl = sb("posl", [128, NTL, 8], F32, stM)
            off = sb("offm", [128, 8], F32, stM)
            gb = [Buf() for _ in range(6)]
            route_b = Buf()
            w13 = Ring(T, stM, nc, "w13", 3, [128, 2, 8 * 256], BF16)
            w2r = Ring(T, stM, nc, "w2r", 3, [128, NF * 512], BF16)
            ps_tr = ps("ps_trM", [128, 1024], F32, stM); ps_tr_b = Buf()
            ps_g = [ps(f"ps_gM{i}", [128, 512], F32, stM) for i in range(2)]; ps_g_b = [Buf(), Buf()]
            ps_u = [ps(f"ps_uM{i}", [128, 512], F32, stM) for i in range(2)]; ps_u_b = [Buf(), Buf()]
            ps_o2 = [ps(f"ps_oM{i}", [128, 512], F32, stM) for i in range(2)]; ps_o2_b = [Buf(), Buf()]
            oc = 0
            T.op("dve", lambda: dve.memset(off[:], 0.0), writes=[gb[5]])

            for t in range(NTL):
                il = t % 4
                T.dma("sp", x3c_s[il], x3c[il][:], X3[t * 128:(t + 1) * 128, :], reads=[X3b[t]], writes=[x3c_b[il]])
                make_hT(x3c[il], x3c_b[il], L, 2, 3, 0, ps_tr, ps_tr_b,
                        lambda k: h2T[:, k, 0:128], h2T_b[0], lambda k: h2Tf[:, k, :], h2Tf_b)
                oi = oc % 2
                oc += 1

                def _r(oi=oi):
                    i = None
                    for k in range(8):
                        i = pe.matmul(ps_o2[oi][:, 0:8], lhsT=h2Tf[:, k, :], rhs=wr[:, k, :], start=(k == 0), stop=(k == 7))
                    return i
                T.op("pe", _r, reads=[h2Tf_b, wr_b], writes=[ps_o2_b[oi]])
                T.op("dve", lambda oi=oi: dve.tensor_copy(out=lgt[:], in_=ps_o2[oi][:, 0:8]), reads=[ps_o2_b[oi]], writes=[gb[0]])
                T.op("dve", lambda: dve.max(out=mx[:], in_=lgt[:]), reads=[gb[0]], writes=[gb[1]], hard=True)
                T.op("dve", lambda t=t: dve.tensor_tensor(out=wts[:, t, 0:1], in0=mx[:, 0:1], in1=mx[:, 1:2], op=ALU.subtract),
                     reads=[gb[1]], writes=[gb[2]], hard=True)
                T.op("act", lambda t=t: act.activation(out=wts[:, t, 0:1], in_=wts[:, t, 0:1], func=AF.Sigmoid),
                     reads=[gb[2]], writes=[gb[2]])

                def _g1(t=t):
                    dve.tensor_scalar(out=wts[:, t, 1:2], in0=wts[:, t, 0:1], scalar1=-1.0, scalar2=1.0, op0=ALU.mult, op1=ALU.add)
                    dve.tensor_scalar(out=eqs[:, t, 0, :], in0=lgt[:], scalar1=mx[:, 0:1], scalar2=None, op0=ALU.is_equal)
                    return dve.tensor_scalar(out=eqs[:, t, 1, :], in0=lgt[:], scalar1=mx[:, 1:2], scalar2=None, op0=ALU.is_equal)
                T.op("dve", _g1, reads=[gb[2], gb[1], gb[0]], writes=[gb[3]], hard=True)
                T.op("dve", lambda t=t: dve.tensor_tensor(out=indb[:], in0=eqs[:, t, 0, :], in1=eqs[:, t, 1, :], op=ALU.add),
                     reads=[gb[3]], writes=[gb[4]], hard=True)

                def _pf(oi=oi):
                    pe.matmul(ps_o2[oi][:, 8:16], lhsT=utri[:, :], rhs=indb[:, :], start=True, stop=True)
                    return pe.matmul(ps_o2[oi][:, 16:24], lhsT=onesb[:, :], rhs=indb[:, :], start=True, stop=True)
                T.op("pe", _pf, reads=[gb[4], utri_b, onesb_b], writes=[ps_o2_b[oi]])

                def _po(oi=oi, t=t):
                    dve.tensor_tensor(out=posl[:, t, :], in0=ps_o2[oi][:, 8:16], in1=off[:], op=ALU.add)
                    return dve.tensor_tensor(out=off[:], in0=off[:], in1=ps_o2[oi][:, 16:24], op=ALU.add)
                T.op("dve", _po, reads=[ps_o2_b[oi], gb[5]], writes=[gb[5], route_b], hard=True)

            thr = sb("thr", [128, 8, 8], F32, stM); gidx = sb("gidx", [128, NG, 8], F32, stM)
            cmp8 = sb("cmp8", [128, 8, 8], F32, stM); cmpg = sb("cmpg", [128, NG, 8], F32, stM)
            ngr = sb("ngr", [128, 8], F32, stM); gend = sb("gend", [128, 8], F32, stM); base = sb("basem", [128, 8], F32, stM)
            eg = sb("egm", [128, NG], F32, stM)
            jp1 = sb("jp1", [128, 11], F32, stM); jp2 = sb("jp2", [128, 2], F32, stM)
            w1if = sb("w1if", [128, NG, 11], F32, stM); w2if = sb("w2if", [128, NG, 2], F32, stM)
            w1idx = sb("w1idx", [128, NG, 11], I32, stM); w2idx = sb("w2idx", [128, NG, 2], I32, stM)
            posf = sb("posf", [128, NTL, 2, 8], F32, stM); pab = sb("pab", [128, NTL, 2], F32, stM)
            idxab = sb("idxab", [128, NTL, 2], I32, stM)
            idx_b = Buf()

            def dv(fn, extra=()):
                T.op("dve", fn, reads=[idx_b] + list(extra), writes=[idx_b], hard=True)
            for k in range(8):
                dv(lambda k=k: dve.memset(thr[:, :, k:k + 1], 512.0 * k))
            for g in range(NG):
                dv(lambda g=g: dve.memset(gidx[:, g, :], float(g)))
            for j in range(11):
                dv(lambda j=j: dve.tensor_scalar(out=jp1[:, j:j + 1], in0=pidx[:, 0:1], scalar1=128.0 * j, scalar2=None,
                                                 op0=ALU.add), [pidx_b])
            for n in range(2):
                dv(lambda n=n: dve.tensor_scalar(out=jp2[:, n:n + 1], in0=pidx[:, 0:1], scalar1=128.0 * n, scalar2=None,
                                                 op0=ALU.add), [pidx_b])
            dv(lambda: dve.tensor_tensor(out=cmp8[:], in0=off[:].unsqueeze(2).to_broadcast([128, 8, 8]), in1=thr[:], op=ALU.is_gt),
               [route_b, gb[5]])
            dv(lambda: dve.tensor_reduce(out=ngr[:], in_=cmp8[:], axis=AX.X, op=ALU.add))
            dv(lambda: dve.tensor_copy(out=gend[:, 0:1], in_=ngr[:, 0:1]))
            for e in range(1, 8):
                dv(lambda e=e: dve.tensor_tensor(out=gend[:, e:e + 1], in0=gend[:, e - 1:e], in1=ngr[:, e:e + 1], op=ALU.add))
            dv(lambda: dve.tensor_tensor(out=base[:], in0=gend[:], in1=ngr[:], op=ALU.subtract))
            dv(lambda: dve.tensor_scalar(out=base[:], in0=base[:], scalar1=512.0, scalar2=None, op0=ALU.mult))
            dv(lambda: dve.tensor_tensor(out=cmpg[:], in0=gend[:].unsqueeze(1).to_broadcast([128, NG, 8]), in1=gidx[:], op=ALU.is_le))
            dv(lambda: dve.tensor_reduce(out=eg[:], in_=cmpg[:], axis=AX.X, op=ALU.add))
            dv(lambda: dve.tensor_scalar(out=eg[:], in0=eg[:], scalar1=7.0, scalar2=None, op0=ALU.min))
            dv(lambda: dve.tensor_scalar(out=w1if[:], in0=eg[:].unsqueeze(2).to_broadcast([128, NG, 11]), scalar1=1408.0,
                                         scalar2=None, op0=ALU.mult))
            dv(lambda: dve.tensor_tensor(out=w1if[:], in0=w1if[:], in1=jp1[:].unsqueeze(1).to_broadcast([128, NG, 11]), op=ALU.add))
            dv(lambda: dve.tensor_scalar(out=w2if[:], in0=eg[:].unsqueeze(2).to_broadcast([128, NG, 2]), scalar1=256.0,
                                         scalar2=None, op0=ALU.mult))
            dv(lambda: dve.tensor_tensor(out=w2if[:], in0=w2if[:], in1=jp2[:].unsqueeze(1).to_broadcast([128, NG, 2]), op=ALU.add))
            dv(lambda: dve.tensor_copy(out=w1idx[:], in_=w1if[:]))
            dv(lambda: dve.tensor_copy(out=w2idx[:], in_=w2if[:]))
            dv(lambda: dve.tensor_tensor(out=posl[:], in0=posl[:], in1=base[:].unsqueeze(1).to_broadcast([128, NTL, 8]), op=ALU.add),
               [route_b])
            dv(lambda: dve.tensor_tensor(out=posf[:], in0=eqs[:], in1=posl[:].unsqueeze(2).to_broadcast([128, NTL, 2, 8]),
                                         op=ALU.mult), [gb[3]])
            dv(lambda: dve.tensor_reduce(out=pab[:], in_=posf[:], axis=AX.X, op=ALU.add))
            dv(lambda: dve.tensor_copy(out=idxab[:], in_=pab[:]))
            if debug:
                dump("idxab", idxab[:].rearrange("p a b -> p (a b)"), [128, NTL * 2], I32, [idx_b])
                dump("w1idx", w1idx[:].rearrange("p a b -> p (a b)"), [128, NG * 11], I32, [idx_b])
                dump("offm", off[:], [128, 8], F32, [idx_b])
                dump("egm", eg[:], [128, NG], F32, [idx_b])
                dump("wts", wts[:].rearrange("p a b -> p (a b)"), [128, NTL * 2], F32, [idx_b])

            for t in range(NTL):
                il = t % 4
                T.dma("sp", x3c_s[il], x3c[il][:], X3[t * 128:(t + 1) * 128, :], reads=[X3b[t]], writes=[x3c_b[il]])
                for a in range(2):
                    T.idma(xs_s, reads=[x3c_b[il], idx_b], writes=[Xs_b], out=Xs[:, :],
                           out_offset=bass.IndirectOffsetOnAxis(ap=idxab[:, t, a:a + 1], axis=0), in_=x3c[il][:, :], in_offset=None)

            for g in range(NG):
                for il in range(4):
                    r0 = (g * 4 + il) * 128
                    T.dma("sp", x3c_s[il], x3c[il][:], Xs[r0:r0 + 128, :], reads=[Xs_b], writes=[x3c_b[il]])
                    make_hT(x3c[il], x3c_b[il], L, 2, 3, 0, ps_tr, ps_tr_b,
                            lambda k, il=il: h2T[:, k, il * 128:(il + 1) * 128], h2T_b[il])
                for fp in range(11):
                    wt_, wb2_, ws2_ = w13.next()
                    T.idma(ws2_, reads=[W1s_b[0], idx_b], writes=[wb2_], out=wt_[:, 0, :], out_offset=None, in_=W1r[:, :],
                           in_offset=bass.IndirectOffsetOnAxis(ap=w1idx[:, g, fp:fp + 1], axis=0))
                    T.idma(ws2_, reads=[W3s_b[0], idx_b], writes=[wb2_], out=wt_[:, 1, :], out_offset=None, in_=W3r[:, :],
                           in_offset=bass.IndirectOffsetOnAxis(ap=w1idx[:, g, fp:fp + 1], axis=0))
                    for f2 in range(2):
                        f = fp * 2 + f2
                        gi = f % 2

                        def _gu(f2=f2, gi=gi, wt_=wt_):
                            i = None
                            for k in range(8):
                                c0 = k * 256 + f2 * 128
                                i = pe.matmul(ps_g[gi][:, :], lhsT=wt_[:, 0, c0:c0 + 128], rhs=h2T[:, k, :],
                                              start=(k == 0), stop=(k == 7))
                            for k in range(8):
                                c0 = k * 256 + f2 * 128
                                i = pe.matmul(ps_u[gi][:, :], lhsT=wt_[:, 1, c0:c0 + 128], rhs=h2T[:, k, :],
                                              start=(k == 0), stop=(k == 7))
                            return i
                        T.op("pe", _gu, reads=[wb2_] + h2T_b, writes=[ps_g_b[gi], ps_u_b[gi]])
                        T.op("act", lambda gi=gi: act.activation(out=sg[gi][:, :], in_=ps_g[gi][:, :], func=AF.Silu),
                             reads=[ps_g_b[gi]], writes=[sg_b[gi]])
                        T.op("dve", lambda gi=gi, f=f: dve.tensor_tensor(out=AT[:, f, :], in0=sg[gi][:, :], in1=ps_u[gi][:, :],
                                                                         op=ALU.mult),
                             reads=[sg_b[gi], ps_u_b[gi]], writes=[AT_b[f]])
                for n in range(2):
                    w2t, w2b, w2s = w2r.next()
                    T.idma(w2s, reads=[W2s_b[0], idx_b], writes=[w2b], out=w2t[:, :], out_offset=None, in_=W2r[:, :],
                           in_offset=bass.IndirectOffsetOnAxis(ap=w2idx[:, g, n:n + 1], axis=0))
                    for il in range(4):
                        oi = oc % 2
                        oc += 1

                        def _o(il=il, oi=oi, w2t=w2t):
                            i = None
                            for f in range(NF):
                                i = pe.matmul(ps_o2[oi][:, :], lhsT=AT[:, f, il * 128:(il + 1) * 128],
                                              rhs=w2t[:, f * 512:(f + 1) * 512], start=(f == 0), stop=(f == NF - 1))
                            return i
                        T.op("pe", _o, reads=AT_b + [w2b], writes=[ps_o2_b[oi]])
                        T.op("act", lambda oi=oi, il=il, n=n: act.copy(out=acc[il][:, n * 512:(n + 1) * 512], in_=ps_o2[oi][:, :]),
                             reads=[ps_o2_b[oi]], writes=[acc_b[il]])
                for il in range(4):
                    r0 = (g * 4 + il) * 128
                    T.dma("sp", ys_s, Ys[r0:r0 + 128, :], acc[il][:], reads=[acc_b[il]], writes=[Ys_b])

            for t in range(NTL):
                il = t % 4
                ya, yb = acc[(t % 2) * 2], acc[(t % 2) * 2 + 1]
                ya_b, yb_b = acc_b[(t % 2) * 2], acc_b[(t % 2) * 2 + 1]
                T.dma("sp", x3c_s[il], x3c[il][:], X3[t * 128:(t + 1) * 128, :], reads=[X3b[t]], writes=[x3c_b[il]])
                T.idma(yg_s, reads=[Ys_b, idx_b], writes=[ya_b], out=ya[:, :], out_offset=None, in_=Ys[:, :],
                       in_offset=bass.IndirectOffsetOnAxis(ap=idxab[:, t, 0:1], axis=0))
                T.idma(yg_s, reads=[Ys_b, idx_b], writes=[yb_b], out=yb[:, :], out_offset=None, in_=Ys[:, :],
                       in_offset=bass.IndirectOffsetOnAxis(ap=idxab[:, t, 1:2], axis=0))

                def _cmb(t=t, ya=ya, yb=yb):
                    dve.tensor_scalar(out=ya[:], in0=ya[:], scalar1=wts[:, t, 0:1], scalar2=None, op0=ALU.mult)
                    return dve.scalar_tensor_tensor(out=ya[:], in0=yb[:], scalar=wts[:, t, 1:2], in1=ya[:], op0=ALU.mult, op1=ALU.add)
                T.op("dve", _cmb, reads=[ya_b, yb_b, route_b, gb[3]], writes=[ya_b])
                resid_ln([ya[:, 0:512], ya[:, 512:1024]], [ya_b], x3c[il], x3c_b[il],
                         gate[(0, 1)], gate_b[(0, 1)], lg, lgb, lb, lbb, tmp, tmp_b)
                T.dma("sp", x3c_s[il], out_ap[t * 128:(t + 1) * 128, :], x3c[il][:], reads=[x3c_b[il]], is_output=True)
    T.barrier()
    T.finish()
    es.close()
    return nc


_PROG = {}


def _get_prog(n_layers=2, debug=False):
    key = (n_layers, debug)
    if key not in _PROG:
        _PROG[key] = build_program(n_layers, debug)
    return _PROG[key]


L1_KEYS = ("pidx", "utri", "od_w_in", "od_fgain", "od_bias", "od_router", "od_w1", "od_w3", "od_w2", "C64", "S64", "CN", "SN")


def make_in_maps(inp, n_cores=N_CORES, n_layers=2):
    cs = _consts()
    f32 = lambda a: np.ascontiguousarray(np.asarray(a, dtype=np.float32))
    x = f32(inp["x"]); c = f32(inp["c"]); ctx = f32(inp["ctx"]); c_ctx = f32(inp["c_ctx"])
    ada_b = f32(inp["ada_b"])
    adab_col = np.ascontiguousarray(ada_b.reshape(2, 48, 128).transpose(0, 2, 1))
    qk_gain = np.concatenate([np.tile(f32(inp["ev_q_gain"])[0], 12), np.tile(f32(inp["ev_k_gain"])[0], 4)])
    rpb = f32(inp["od_rpb"])[0]
    rpb_ext = np.concatenate([rpb.reshape(12, -1), np.full((12, 1), NEG, np.float32)], axis=1)
    od_bias = np.ascontiguousarray(rpb_ext[:, cs["naidx"]])
    shared = dict(
        ada_w=f32(inp["ada_w"]), ada_b=ada_b, adab_col=adab_col,
        ln_mix_g=f32(inp["ln_mix_g"]), ln_mix_b=f32(inp["ln_mix_b"]),
        ln_ffn_g=f32(inp["ln_ffn_g"]), ln_ffn_b=f32(inp["ln_ffn_b"]),
        w_out=f32(inp["w_out"]), ev_w_in=f32(inp["ev_w_in"])[0], ev_pool_w=f32(inp["ev_pool_w"])[0],
        ev_pool_scale=f32(inp["ev_pool_scale"])[0], ev_qk_gain=f32(qk_gain),
        ev_ffn_w1=f32(inp["ev_ffn_w1"])[0], ev_ffn_w3=f32(inp["ev_ffn_w3"])[0], ev_ffn_w2=f32(inp["ev_ffn_w2"])[0],
        od_w_in=f32(inp["od_w_in"])[0], od_fgain=f32(inp["od_fourier_gain"])[0].reshape(256),
        od_bias=od_bias, od_router=f32(inp["od_router"])[0],
        od_w1=f32(inp["od_exp_w1"])[0], od_w3=f32(inp["od_exp_w3"])[0], od_w2=f32(inp["od_exp_w2"])[0],
        ropeC=cs["ropeC"], ropeS=cs["ropeS"], band=cs["band"], C64=cs["C64"], S64=cs["S64"],
        CN=cs["CN"], SN=cs["SN"], identf=cs["identf"], pidx=cs["pidx"], utri=cs["utri"],
    )
    if n_layers < 2:
        for k in L1_KEYS:
            shared.pop(k)
    maps = []
    for b in range(n_cores):
        cv = np.stack([c[b].reshape(8, 128).T, c_ctx.reshape(8, 128).T], axis=-1)
        m = dict(shared)
        m.update(x=x[b], ctx=ctx[b], cvec=np.ascontiguousarray(cv.astype(np.float32)))
        maps.append(m)
    return maps


def kernel(**inputs):
    nc = _get_prog(2, False)
    maps = make_in_maps(inputs)
    res = run_bass_kernel_spmd(nc, maps, core_ids=list(range(N_CORES)))
    return np.stack([np.asarray(r["out"], dtype=np.float32) for r in res.results], axis=0)
```

```python
import contextlib
import math
import numpy as np
import ml_dtypes
import concourse.bass as bass
import concourse.mybir as mybir
from concourse.bass_utils import run_bass_kernel_spmd

F32 = mybir.dt.float32
BF16 = mybir.dt.bfloat16
I32 = mybir.dt.int32
AF = mybir.ActivationFunctionType
ALU = mybir.AluOpType
AX = mybir.AxisListType

D = 1024
SEQ = 4096
CTXL = 256
NT = 34
NTL = 32
FFN = 2816
NF = 22
NEXP = 8
ALPHA = 4.0 ** 0.25
LN_EPS = 1e-6
RMS_EPS = 1e-6
NEG = -30000.0
N_CORES = 8
DEFER_L0 = True


class Buf:
    __slots__ = ("name", "w", "r")

    def __init__(self, name=""):
        self.name = name
        self.w = None
        self.r = {}


class SemC:
    __slots__ = ("sem", "cnt")

    def __init__(self, sem):
        self.sem = sem
        self.cnt = 0


class Tracker:
    def __init__(self, nc, es):
        self.nc = nc
        self.es = es
        self.E = {"pe": nc.tensor, "act": nc.scalar, "dve": nc.vector, "pool": nc.gpsimd, "sp": nc.sync}
        self.esem = {}
        self.ecnt = {}
        self.own = {k: set() for k in self.E}
        self.seen = {k: {} for k in self.E}
        self.nsem = 0
        self.dsem = {}
        for k in self.E:
            self._new_esem(k)
        self.out_events = []

    def new_sem(self, name):
        self.nsem += 1
        return self.es.enter_context(self.nc.semaphore(f"{name}_{self.nsem}"))

    def semc(self, name="d"):
        sc = SemC(self.new_sem(name))
        self.dsem[sc.sem.num] = sc
        return sc

    def _new_esem(self, k):
        s = self.new_sem("e" + k)
        self.esem[k] = s
        self.ecnt[k] = 0
        self.own[k].add(s.num)

    def _wait_all(self, eng, evs, allow_own=False):
        best = {}
        for ev in evs:
            if ev is None:
                continue
            s, v = ev
            if s.num in self.own[eng] and not allow_own:
                continue
            if s.num not in best or best[s.num][1] < v:
                best[s.num] = (s, v)
        for num, (s, v) in best.items():
            if num in self.dsem:
                v = self.dsem[num].cnt
            if self.seen[eng].get(num, 0) < v:
                self.E[eng].wait_ge(s, v)
                self.seen[eng][num] = v

    def _collect(self, reads, writes, skip_num=None):
        evs = []
        for b in reads:
            evs.append(b.w)
        for b in writes:
            if b.w is not None and (skip_num is None or b.w[0].num != skip_num):
                evs.append(b.w)
            evs.extend(b.r.values())
        return evs

    def _update(self, ev, reads, writes):
        for b in reads:
            old = b.r.get(ev[0].num)
            if old is None or old[1] < ev[1]:
                b.r[ev[0].num] = ev
        for b in writes:
            b.w = ev
            b.r = {}

    def op(self, eng, fn, reads=(), writes=(), hard=False):
        self._wait_all(eng, self._collect(reads, writes), allow_own=hard)
        inst = fn()
        own = self.esem[eng]
        self.ecnt[eng] += 1
        inst.then_inc(own, 1)
        ev = (own, self.ecnt[eng])
        self._update(ev, reads, writes)
        if self.ecnt[eng] >= 12000:
            self._new_esem(eng)
        return ev

    def dma(self, q, sc, out, in_, reads=(), writes=(), is_output=False, **kw):
        self._wait_all(q, self._collect(reads, writes, skip_num=sc.sem.num))
        inst = self.E[q].dma_start(out=out, in_=in_, **kw)
        sc.cnt += 16
        inst.then_inc(sc.sem, 16)
        ev = (sc.sem, sc.cnt)
        self._update(ev, reads, writes)
        if is_output:
            self.out_events.append(ev)
        return ev

    def idma(self, sc, reads=(), writes=(), is_output=False, **kw):
        self._wait_all("pool", self._collect(reads, writes, skip_num=sc.sem.num))
        inst = self.E["pool"].indirect_dma_start(**kw)
        sc.cnt += 16
        inst.then_inc(sc.sem, 16)
        ev = (sc.sem, sc.cnt)
        self._update(ev, reads, writes)
        if is_output:
            self.out_events.append(ev)
        return ev

    def barrier(self):
        evs = []
        for k in self.E:
            if self.ecnt[k] > 0:
                evs.append((self.esem[k], self.ecnt[k]))
        for sc in self.dsem.values():
            if sc.cnt > 0:
                evs.append((sc.sem, sc.cnt))
        for k in self.E:
            self._wait_all(k, evs)

    def finish(self):
        self._wait_all("sp", self.out_events)


class Ring:
    def __init__(self, T, es, nc, name, n, shape, dtype):
        self.n = n
        self.t = [es.enter_context(nc.sbuf_tensor(f"r_{name}{i}", shape, dtype)) for i in range(n)]
        self.b = [Buf(f"{name}{i}") for i in range(n)]
        self.s = [T.semc(name) for i in range(n)]
        self.i = 0

    def next(self):
        k = self.i % self.n
        self.i += 1
        return self.t[k], self.b[k], self.s[k]


def _rope_tables():
    t = np.arange(SEQ)
    row = (t // 64).astype(np.float32)
    col = (t % 64).astype(np.float32)
    inv = np.power(np.float32(10000.0), -np.arange(16, dtype=np.float32) / np.float32(16)).astype(np.float32)
    ang = np.stack([row[:, None] * inv, col[:, None] * inv], axis=1).astype(np.float32)
    cs, sn = np.cos(ang).astype(np.float32), np.sin(ang).astype(np.float32)
    C = np.zeros((SEQ, 2, 2, 16), np.float32)
    S = np.zeros((SEQ, 2, 2, 16), np.float32)
    C[:, :, 0, :] = cs
    C[:, :, 1, :] = cs
    S[:, :, 0, :] = -sn
    S[:, :, 1, :] = sn
    C = C.reshape(NTL, 128, 64).transpose(1, 0, 2)
    S = S.reshape(NTL, 128, 64).transpose(1, 0, 2)
    return np.ascontiguousarray(C), np.ascontiguousarray(S)


def _band_mats():
    out = np.zeros((4, 5, 128, 128), np.float32)
    n = 1024
    for g, w in enumerate((2, 4, 8, 16)):
        B = np.zeros((n, n), np.float64)
        for t in range(n):
            lo = min(max(t - w // 2, 0), n)
            hi = min(max(t - w // 2 + w, 0), n)
            B[lo:hi, t] += 1.0 / (hi - lo)
            B[t, t] -= 1.0
        i = 3
        out[g, 0] = B[(i - 1) * 128:i * 128, i * 128:(i + 1) * 128]
        out[g, 1] = B[i * 128:(i + 1) * 128, i * 128:(i + 1) * 128]
        out[g, 2] = B[(i + 1) * 128:(i + 2) * 128, i * 128:(i + 1) * 128]
        out[g, 3] = B[0:128, 0:128]
        out[g, 4] = B[n - 128:n, n - 128:n]
    return np.ascontiguousarray(out.transpose(2, 0, 1, 3)).astype(ml_dtypes.bfloat16)


def _dft_consts():
    c = np.arange(64)
    ang = 2.0 * np.pi * ((c[:, None] * c[None, :]) % 64) / 64.0
    C64 = np.zeros((128, 128), np.float64)
    S64 = np.zeros((128, 128), np.float64)
    for a in range(2):
        C64[a * 64:(a + 1) * 64, a * 64:(a + 1) * 64] = np.cos(ang)
        S64[a * 64:(a + 1) * 64, a * 64:(a + 1) * 64] = np.sin(ang)
    s = np.arange(SEQ, dtype=np.int64)
    m = (s[:, None] * s[None, :]) % SEQ
    angn = (2.0 * np.pi / SEQ) * m
    CN = (np.cos(angn) / 512.0).astype(ml_dtypes.bfloat16)
    SN = (-np.sin(angn) / 512.0).astype(ml_dtypes.bfloat16)
    return C64.astype(ml_dtypes.bfloat16), S64.astype(ml_dtypes.bfloat16), CN, SN


def _na_index():
    MASK = 15 * 31
    idx = np.full((3, 8, 128, 512), MASK, np.int64)
    for bt, qb in enumerate((0, 3, 7)):
        for slot in range(8):
            kt = 4 * qb - 2 + slot
            if kt < 0 or kt >= 32:
                continue
            for krl in range(2):
                kr = 2 * kt + krl
                for qrl in range(8):
                    qr = 8 * qb + qrl
                    rs = min(max(qr - 4, 0), 56)
                    if not (rs <= kr < rs + 8):
                        continue
                    dr = kr - qr + 7
                    qc = np.arange(64)
                    cs = np.clip(qc - 8, 0, 48)
                    kc = np.arange(64)
                    valid = (kc[:, None] >= cs[None, :]) & (kc[:, None] < cs[None, :] + 16)
                    dc = kc[:, None] - qc[None, :] + 15
                    blk = np.where(valid, dr * 31 + dc, MASK)
                    idx[bt, slot, krl * 64:(krl + 1) * 64, qrl * 64:(qrl + 1) * 64] = blk
    return idx


_CONST_CACHE = {}


def _consts():
    if not _CONST_CACHE:
        C, S = _rope_tables()
        C64, S64, CN, SN = _dft_consts()
        _CONST_CACHE.update(dict(
            ropeC=C, ropeS=S, band=_band_mats(), C64=C64, S64=S64, CN=CN, SN=SN,
            identf=np.eye(128, dtype=np.float32), naidx=_na_index(),
            pidx=np.arange(128, dtype=np.float32).reshape(128, 1),
            utri=np.triu(np.ones((128, 128), np.float32), 1)))
    return _CONST_CACHE


def build_program(n_layers=2, debug=False, stop_after=None):
    global _LAST_NC
    nc = bass.Bass("TRN2", target_bir_lowering=False)
    _LAST_NC = nc
    es = contextlib.ExitStack()

    def din(name, shape, dt=F32):
        return nc.dram_tensor(name, list(shape), dt, kind="ExternalInput").ap()

    def dscr(name, shape, dt=F32):
        return nc.dram_tensor(name, list(shape), dt, kind="Internal").ap()

    x_in = din("x", [SEQ, D])
    ctx_in = din("ctx", [CTXL, D])
    cvec = din("cvec", [128, 8, 2])
    ada_w = din("ada_w", [2, D, 6 * D])
    ada_b = din("ada_b", [2, 6 * D])
    adab_col = din("adab_col", [2, 128, 48])
    ln_mix_g = din("ln_mix_g", [2, D]); ln_mix_b = din("ln_mix_b", [2, D])
    ln_ffn_g = din("ln_ffn_g", [2, D]); ln_ffn_b = din("ln_ffn_b", [2, D])
    w_out = din("w_out", [2, D, D])
    ev_w_in = din("ev_w_in", [D, 1536])
    ev_pool_w = din("ev_pool_w", [4, 64, 64])
    ev_pool_scale = din("ev_pool_scale", [256])
    ev_qk_gain = din("ev_qk_gain", [1024])
    ev_ffn_w1 = din("ev_ffn_w1", [D, FFN]); ev_ffn_w3 = din("ev_ffn_w3", [D, FFN]); ev_ffn_w2 = din("ev_ffn_w2", [FFN, D])
    if n_layers == 2:
        od_w_in = din("od_w_in", [D, 2560])
        od_fgain = din("od_fgain", [256])
        od_bias = din("od_bias", [12, 3, 8, 128, 512])
        od_router = din("od_router", [D, 8])
        od_w1 = din("od_w1", [NEXP, D, FFN]); od_w3 = din("od_w3", [NEXP, D, FFN]); od_w2 = din("od_w2", [NEXP, FFN, D])
    ropeC = din("ropeC", [128, NTL, 64]); ropeS = din("ropeS", [128, NTL, 64])
    band = din("band", [128, 4, 5, 128], BF16)
    if n_layers == 2:
        C64 = din("C64", [128, 128], BF16); S64 = din("S64", [128, 128], BF16)
        CN = din("CN", [SEQ, SEQ], BF16); SN = din("SN", [SEQ, SEQ], BF16)
    identf_in = din("identf", [128, 128])
    if n_layers == 2:
        pidx_in = din("pidx", [128, 1]); utri_in = din("utri", [128, 128])

    out_ap = nc.dram_tensor("out", [SEQ, D], F32, kind="ExternalOutput").ap()
    dbg = {}
    if debug:
        for nm, shp in (("dbg_x1", [NT * 128, D]), ("dbg_x2", [NT * 128, D]), ("dbg_x3", [SEQ, D])):
            dbg[nm] = nc.dram_tensor(nm, shp, F32, kind="ExternalOutput").ap()

    X1 = dscr("X1", [NT * 128, D]) if not debug else dbg["dbg_x1"]
    X2 = dscr("X2", [NT * 128, D]) if not debug else dbg["dbg_x2"]
    X3 = dscr("X3", [SEQ, D]) if not debug else dbg["dbg_x3"]
    X1b = [Buf(f"X1_{i}") for i in range(NT)]
    X2b = [Buf(f"X2_{i}") for i in range(NT)]
    X3b = [Buf(f"X3_{i}") for i in range(NTL)]

    T = Tracker(nc, es)

    def dump(name, ap, shape, dt, reads):
        if not debug:
            return
        d = nc.dram_tensor("dbg_" + name, list(shape), dt, kind="ExternalOutput").ap()
        T.dma("sp", T.semc("dbg"), d, ap, reads=reads, is_output=True)
    pe, act, dve, pool, sp = nc.tensor, nc.scalar, nc.vector, nc.gpsimd, nc.sync

    def sb(name, shape, dt, stack=None):
        return (stack or es).enter_context(nc.sbuf_tensor("s_" + name, list(shape), dt))

    def ps(name, shape, dt, stack):
        return stack.enter_context(nc.psum_tensor("p_" + name, list(shape), dt))

    def x_src(t):
        return x_in[t * 128:(t + 1) * 128, :] if t < NTL else ctx_in[(t - NTL) * 128:(t - NTL + 1) * 128, :]

    identf = sb("identf", [128, 128], F32); identf_b = Buf()
    identb = sb("identb", [128, 128], BF16); identb_b = Buf()
    onesf = sb("onesf", [128, 64], F32); ones_b = Buf()
    cst_s = T.semc("cst")
    T.dma("sp", cst_s, identf[:], identf_in[:, :], writes=[identf_b])
    T.dma("pool", cst_s, identb[:], identf_in[:, :], writes=[identb_b])
    T.op("dve", lambda: dve.memset(onesf[:], 1.0), writes=[ones_b])
    sel64 = sb("sel64", [128, 128], F32); sel_b = Buf()
    rcp_t = sb("rcp_t", [64, 512], F32); rcp_b = Buf()

    def _sel():
        dve.memset(sel64[:], 0.0)
        return dve.memset(sel64[64:65, :], 1.0)
    T.op("dve", _sel, writes=[sel_b])

    modcol = [sb(f"modcol{l}", [128, 4, 8, 2], F32) for l in range(2)]
    modcol_b = [Buf() for l in range(2)]
    epsc = sb("epsc", [128, 2], F32); eps_b = Buf()

    def _eps():
        dve.memset(epsc[:, 0:1], LN_EPS)
        return dve.memset(epsc[:, 1:2], RMS_EPS)
    T.op("dve", _eps, writes=[eps_b])
    AD = dscr("AD", [NT * 128, 256], BF16)
    AD_b = [Buf() for _ in range(NT)]

    def load_lnp(l, k, stack, sc):
        src = (ln_mix_g, ln_mix_b, ln_ffn_g, ln_ffn_b)[k]
        t = sb(f"lnp{l}{k}", [128, D], F32, stack)
        b = Buf()
        T.dma("sp", sc, t[:], src[l:l + 1, :].to_broadcast([128, D]), writes=[b])
        return t, b

    def compute_mod(l, lstack, mstack):
        nstream = 2 if l == 0 else 1
        gate = {}
        gate_b = {}
        for g in (1, 0):
            for s in range(nstream):
                gate[(s, g)] = sb(f"gate{l}{s}{g}", [128, D], F32, mstack if g == 0 else lstack)
                gate_b[(s, g)] = Buf()
        with contextlib.ExitStack() as st:
            cc = sb(f"cc{l}", [128, 8, 2], F32, st); cc_b = Buf()
            ccrep = sb(f"ccrep{l}", [128, 8, 2, 128], F32, st); ccrep_b = Buf()
            abcol = sb(f"abcol{l}", [128, 48], F32, st); abcol_b = Buf()
            abrow = sb(f"abrow{l}", [128, 2048], F32, st); abrow_b = Buf()
            ms_ = T.semc("mod")
            T.dma("sp", ms_, cc[:], cvec[:, :, :], writes=[cc_b])
            T.dma("sp", ms_, abcol[:], adab_col[l, :, :], writes=[abcol_b])
            for gi, seg in enumerate((2, 5)):
                T.dma("sp", ms_, abrow[:, gi * 1024:(gi + 1) * 1024],
                      ada_b[l:l + 1, seg * 1024:(seg + 1) * 1024].to_broadcast([128, 1024]), writes=[abrow_b])
            T.op("act", lambda: act.activation(out=cc[:], in_=cc[:], func=AF.Silu), reads=[cc_b], writes=[cc_b])

            def _rep():
                i = None
                for k in range(8):
                    for s in range(2):
                        i = dve.tensor_copy(out=ccrep[:, k, s, :], in_=cc[:, k, s:s + 1].to_broadcast([128, 128]))
                return i
            T.op("dve", _rep, reads=[cc_b], writes=[ccrep_b])
            wring = Ring(T, st, nc, f"adaw{l}", 2, [128, 8, 512], F32)
            ps_c = ps(f"ps_c{l}", [128, 512], F32, st); ps_c_b = Buf()
            ps_g = [ps(f"ps_g{l}{i}", [128, 512], F32, st) for i in range(2)]; ps_g_b = [Buf(), Buf()]
            for j in range(12):
                seg, hf = j // 2, j % 2
                wt, wb, ws = wring.next()
                T.dma("sp", ws, wt[:], ada_w[l, :, j * 512:(j + 1) * 512].rearrange("(k p) n -> p k n", p=128),
                      writes=[wb])
                if seg in (2, 5):
                    gi = 0 if seg == 2 else 1
                    for s in range(nstream):
                        def _mm(s=s, wt=wt):
                            i = None
                            for k in range(8):
                                i = pe.matmul(ps_g[s][:, :], lhsT=ccrep[:, k, s, :], rhs=wt[:, k, :],
                                              start=(k == 0), stop=(k == 7))
                            return i
                        T.op("pe", _mm, reads=[ccrep_b, wb], writes=[ps_g_b[s]])
                        T.op("dve", lambda s=s, gi=gi, hf=hf: dve.tensor_tensor(
                            out=gate[(s, gi)][:, hf * 512:(hf + 1) * 512], in0=ps_g[s][:, :],
                            in1=abrow[:, gi * 1024 + hf * 512: gi * 1024 + (hf + 1) * 512], op=ALU.add),
                            reads=[ps_g_b[s], abrow_b], writes=[gate_b[(s, gi)]])
                else:
                    vi = {0: 0, 1: 1, 3: 2, 4: 3}[seg]
                    for c4 in range(4):
                        def _mm(c4=c4, wt=wt):
                            i = None
                            for k in range(8):
                                i = pe.matmul(ps_c[:, 0:2], lhsT=wt[:, k, c4 * 128:(c4 + 1) * 128], rhs=cc[:, k, :],
                                              start=(k == 0), stop=(k == 7))
                            return i
                        T.op("pe", _mm, reads=[cc_b, wb], writes=[ps_c_b])
                        chunk = hf * 4 + c4
                        colidx = seg * 8 + chunk
                        T.op("dve", lambda vi=vi, chunk=chunk, colidx=colidx: dve.tensor_scalar(
                            out=modcol[l][:, vi, chunk, :], in0=ps_c[:, 0:2],
                            scalar1=abcol[:, colidx:colidx + 1], scalar2=(1.0 if vi in (1, 3) else 0.0),
                            op0=ALU.add, op1=ALU.add),
                            reads=[ps_c_b, abcol_b], writes=[modcol_b[l]])
        T.barrier()
        if l == 0:
            dump("modcol", modcol[0][:].rearrange("p a b c -> p (a b c)"), [128, 64], F32, [modcol_b[0]])
            dump("gate00", gate[(0, 0)][:], [128, D], F32, [gate_b[(0, 0)]])
        return gate, gate_b

    def make_hT(xt, xb, l, vsh, vsc, stream, ps_tr, ps_tr_b, hT_ap, hT_b, hTf_ap=None, hTf_b=None):
        def _tr():
            i = None
            for k in range(8):
                i = pe.transpose(ps_tr[:, k * 128:(k + 1) * 128], xt[:, k * 128:(k + 1) * 128], identf[:])
            return i
        T.op("pe", _tr, reads=[xb, identf_b], writes=[ps_tr_b])

        def _ev():
            i = None
            for k in range(8):
                i = dve.tensor_scalar(out=hT_ap(k), in0=ps_tr[:, k * 128:(k + 1) * 128],
                                      scalar1=modcol[l][:, vsc, k, stream:stream + 1],
                                      scalar2=modcol[l][:, vsh, k, stream:stream + 1], op0=ALU.mult, op1=ALU.add)
            return i
        T.op("dve", _ev, reads=[ps_tr_b, modcol_b[l]], writes=[hT_b])
        if hTf_ap is not None:
            def _ev2():
                i = None
                for k in range(8):
                    i = dve.tensor_scalar(out=hTf_ap(k), in0=ps_tr[:, k * 128:(k + 1) * 128],
                                          scalar1=modcol[l][:, vsc, k, stream:stream + 1],
                                          scalar2=modcol[l][:, vsh, k, stream:stream + 1], op0=ALU.mult, op1=ALU.add)
                return i
            T.op("dve", _ev2, reads=[ps_tr_b, modcol_b[l]], writes=[hTf_b])

    lnw = {}
    lnw["st"] = sb("ln_st", [128, 2, 6], F32); lnw["mv"] = sb("ln_mv", [128, 2], F32)
    lnw["sd"] = sb("ln_sd", [128, 1], F32); lnw["rs"] = sb("ln_rs", [128, 1], F32)
    lnw["b0"] = Buf(); lnw["b1"] = Buf(); lnw["b2"] = Buf(); lnw["b3"] = Buf()

    def resid_ln(ps_halves, ps_bufs, zt, zb, gate_t, gate_bf, lg, lgb, lb, lbb, tmp, tmp_b):
        stt, mv, sd, rs = lnw["st"], lnw["mv"], lnw["sd"], lnw["rs"]

        def _a():
            for h in range(2):
                dve.tensor_tensor(out=tmp[:, h * 512:(h + 1) * 512], in0=ps_halves[h],
                                  in1=gate_t[:, h * 512:(h + 1) * 512], op=ALU.mult)
            dve.scalar_tensor_tensor(out=zt[:], in0=zt[:], scalar=ALPHA, in1=tmp[:], op0=ALU.mult, op1=ALU.add)
            i = None
            for h in range(2):
                i = dve.bn_stats(out=stt[:, h, :], in_=zt[:, h * 512:(h + 1) * 512])
            return i
        T.op("dve", _a, reads=list(ps_bufs) + [gate_bf], writes=[zb, tmp_b, lnw["b0"]])
        T.op("dve", lambda: dve.bn_aggr(out=mv[:], in_=stt[:].rearrange("p a b -> p (a b)")),
             reads=[lnw["b0"]], writes=[lnw["b1"]], hard=True)
        T.op("act", lambda: act.activation(out=sd[:], in_=mv[:, 1:2], func=AF.Sqrt, bias=epsc[:, 0:1], scale=1.0),
             reads=[lnw["b1"], eps_b], writes=[lnw["b2"]])
        T.op("dve", lambda: dve.reciprocal(out=rs[:], in_=sd[:]), reads=[lnw["b2"]], writes=[lnw["b3"]])

        def _b():
            dve.tensor_scalar(out=zt[:], in0=zt[:], scalar1=mv[:, 0:1], scalar2=rs[:, 0:1],
                              op0=ALU.subtract, op1=ALU.mult)
            dve.tensor_tensor(out=zt[:], in0=zt[:], in1=lg[:], op=ALU.mult)
            return dve.tensor_tensor(out=zt[:], in0=zt[:], in1=lb[:], op=ALU.add)
        T.op("dve", _b, reads=[lnw["b3"], lnw["b1"], lgb, lbb], writes=[zb], hard=True)

    def rms_heads(src_ap, nh, sq, ms, rt, dst_ap, wbuf, reads, gain_t=None, gain_b=None):
        T.op("act", lambda: act.activation(out=sq[:, 0:nh * 64], in_=src_ap, func=AF.Square),
             reads=reads, writes=[wbuf["sq"]])
        T.op("dve", lambda: dve.tensor_reduce(out=ms[:, 0:nh], in_=sq[:, 0:nh * 64].rearrange("p (h d) -> p h d", d=64),
                                              axis=AX.X, op=ALU.add),
             reads=[wbuf["sq"]], writes=[wbuf["ms"]])
        T.op("act", lambda: act.activation(out=rt[:, 0:nh], in_=ms[:, 0:nh], func=AF.Sqrt, bias=epsc[:, 1:2],
                                           scale=1.0 / 64.0),
             reads=[wbuf["ms"], eps_b], writes=[wbuf["rt"]])

        T.op("dve", lambda: dve.reciprocal(out=rt[:, 0:nh], in_=rt[:, 0:nh]), reads=[wbuf["rt"]], writes=[wbuf["rt"]])

        def _n():
            i = dve.tensor_tensor(out=dst_ap, in0=src_ap.rearrange("p (h d) -> p h d", d=64),
                                  in1=rt[:, 0:nh].unsqueeze(2).to_broadcast([128, nh, 64]), op=ALU.mult)
            if gain_t is not None:
                i = dve.tensor_tensor(out=dst_ap, in0=dst_ap, in1=gain_t, op=ALU.mult)
            return i
        T.op("dve", _n, reads=list(reads) + [wbuf["rt"]] + ([gain_b] if gain_b else []), writes=[wbuf["dst"]], hard=True)

    acnt = [0]

    asteps = []
    apre = []

    def attn_head(qT_ap, kT_fn, v_fn, key_list, ps_s, ps_s_b, pT_ring, ps_o, ps_o_b, nq, reads_q, bias_fn=None,
                  pre=None, post=None):
        nk = len(key_list)
        hseq = len(apre)
        apre.append(pre)
        for i, kt in enumerate(key_list):
            sidx = acnt[0] % 2
            acnt[0] += 1

            def s_fn(kt=kt, sidx=sidx):
                kap, kb = kT_fn(kt)
                bi = bias_fn(kt) if bias_fn is not None else None

                def _s():
                    ins = pe.matmul(ps_s[sidx][:, 0:nq], lhsT=kap, rhs=qT_ap, start=True, stop=(bi is None))
                    if bi is not None:
                        ins = pe.matmul(ps_s[sidx][:, 0:nq], lhsT=identb[:], rhs=bi[0], start=False, stop=True)
                    return ins
                T.op("pe", _s, reads=[kb] + list(reads_q) + ([bi[1], identb_b] if bi else []), writes=[ps_s_b[sidx]])

            def epv_fn(kt=kt, sidx=sidx, i=i):
                pt, pb, _ = pT_ring.next()
                T.op("act", lambda: act.activation(out=pt[:, 0:nq], in_=ps_s[sidx][:, 0:nq], func=AF.Exp),
                     reads=[ps_s_b[sidx]], writes=[pb])
                vap, vb = v_fn(kt)
                T.op("pe", lambda: pe.matmul(ps_o[0:65, 0:nq], lhsT=vap, rhs=pt[:, 0:nq],
                                             start=(i == 0), stop=(i == nk - 1)),
                     reads=[vb, pb], writes=[ps_o_b])
            asteps.append((s_fn, epv_fn, post if i == nk - 1 else None, hseq))

    def attn_flush(lookahead=2, fillers=()):
        n = len(asteps)
        done_pre = [0]
        nk0 = next((j for j in range(n) if asteps[j][2] is not None), n - 1) + 1
        fill_at = {}
        for i, f in enumerate(fillers):
            fill_at.setdefault((i * (nk0 + 1)) // max(1, len(fillers)), []).append(f)

        def run_pre(upto):
            while done_pre[0] <= min(upto, len(apre) - 1):
                f = apre[done_pre[0]]
                if f is not None:
                    f()
                done_pre[0] += 1
        pend = []
        for j in range(n):
            for f in fill_at.pop(j, []):
                f()
            run_pre(asteps[j][3] + lookahead)
            if j == 0:
                asteps[0][0]()
            if j + 1 < n:
                run_pre(asteps[j + 1][3])
                asteps[j + 1][0]()
            asteps[j][1]()
            while pend and pend[0][0] <= j:
                pend.pop(0)[1]()
            if asteps[j][2] is not None:
                pend.append((j + 3, asteps[j][2]))
        for _, f in pend:
            f()
        for j in sorted(fill_at):
            for f in fill_at[j]:
                f()
        asteps.clear()
        apre.clear()

    def attn_norm(ps_o, ps_o_b, osb, osb_b, ps_bc, ps_bc_b, dst_ap, dst_b, nq):
        T.op("dve", lambda: dve.tensor_copy(out=osb[0:65, 0:nq], in_=ps_o[0:65, 0:nq]), reads=[ps_o_b], writes=[osb_b])
        T.op("pe", lambda: pe.matmul(ps_bc[:, 0:nq], lhsT=sel64[:, :], rhs=osb[:, 0:nq], start=True, stop=True),
             reads=[osb_b, sel_b], writes=[ps_bc_b])
        def _n():
            dve.reciprocal(out=rcp_t[0:64, 0:nq], in_=ps_bc[0:64, 0:nq])
            return dve.tensor_tensor(out=dst_ap, in0=osb[0:64, 0:nq], in1=rcp_t[0:64, 0:nq], op=ALU.mult)
        T.op("dve", _n, reads=[osb_b, ps_bc_b], writes=[dst_b, rcp_b])

    def x_src(t):
        return x_in[t * 128:(t + 1) * 128, :] if t < NTL else ctx_in[(t - NTL) * 128:(t - NTL + 1) * 128, :]

    def qhead_pos(h):
        return (h % 3) + 3 * (h // 6), (h // 3) % 2

    L = 0
    with contextlib.ExitStack() as stL0:
        stR = contextlib.ExitStack()
        gate, gate_b = compute_mod(0, stL0, stR)
        QT = sb("QT", [128, 6, SEQ + CTXL], BF16, stR); QT_b = [Buf() for _ in range(NT)]
        KT = sb("KT", [128, 2, SEQ + CTXL], BF16, stR); KT_b = [Buf() for _ in range(NT)]
        VA = sb("VA", [128, NT, 4, 66], BF16, stR); VA_b = [Buf() for _ in range(NT)]
        bandt = sb("bandt", [128, 4, 5, 128], BF16, stR); band_b = Buf()
        poolw = sb("poolw", [128, 4, 128], BF16, stR); poolw_b = Buf()
        T.op("pool", lambda: pool.memset(poolw[:], 0.0), writes=[poolw_b])
        w0_s = T.semc("w0")
        T.op("dve", lambda: dve.memset(VA[:].rearrange("p t k d -> p (t k) d")[:, :, 64:65], 1.0), writes=VA_b)
        T.dma("sp", w0_s, bandt[:], band[:, :, :, :], writes=[band_b])

        with contextlib.ExitStack() as stA:
            win = sb("win0", [128, 8, 1536], BF16, stA); win_b = Buf()
            T.dma("pool", w0_s, win[:], ev_w_in.rearrange("(k p) n -> p k n", p=128), writes=[win_b])
            if n_layers == 2:
                W1s = dscr("W1s", [NEXP, 11, 128, 8, 256], BF16); W3s = dscr("W3s", [NEXP, 11, 128, 8, 256], BF16)
                W2s = dscr("W2s", [NEXP, 2, 128, NF, 512], BF16)
                W1s_b = [Buf() for _ in range(NEXP)]; W3s_b = [Buf() for _ in range(NEXP)]; W2s_b = [Buf() for _ in range(NEXP)]
                wc_s = T.semc("wcast")
                for e in range(NEXP):
                    for j in range(11):
                        T.dma("pool", wc_s, W1s[e, j], od_w1[e][:, j * 256:(j + 1) * 256].rearrange("(k p) n -> p k n", p=128),
                              writes=[W1s_b[e]])
                        T.dma("pool", wc_s, W3s[e, j], od_w3[e][:, j * 256:(j + 1) * 256].rearrange("(k p) n -> p k n", p=128),
                              writes=[W3s_b[e]])
                    for hh in range(2):
                        T.dma("pool", wc_s, W2s[e, hh], od_w2[e][:, hh * 512:(hh + 1) * 512].rearrange("(f p) n -> p f n", p=128),
                              writes=[W2s_b[e]])
            rring = Ring(T, stA, nc, "rope", 2, [128, 2, 64], F32)
            gaint = sb("gaint", [128, 16, 64], F32, stA); gain_b = Buf()
            T.dma("sp", w0_s, gaint[:].rearrange("p h d -> p (h d)"),
                  ev_qk_gain.rearrange("(o n) -> o n", o=1).to_broadcast([128, 1024]), writes=[gain_b])
            T.op("dve", lambda: dve.tensor_scalar(out=gaint[:, 0:12, :], in0=gaint[:, 0:12, :], scalar1=0.125,
                                                  scalar2=None, op0=ALU.mult), reads=[gain_b], writes=[gain_b])
            pwf = sb("pwf", [64, 4, 64], F32, stA); pws = sb("pws", [64, 4, 64], F32, stA); pw_b = Buf()
            T.dma("sp", w0_s, pwf[:], ev_pool_w.rearrange("g c d -> c g d"), writes=[pw_b])
            T.dma("sp", w0_s, pws[:].rearrange("p g d -> p (g d)"),
                  ev_pool_scale.rearrange("(o n) -> o n", o=1).to_broadcast([64, 256]), writes=[pw_b])
            T.op("dve", lambda: dve.tensor_tensor(out=poolw[0:64, :, 0:64], in0=pwf[:], in1=pws[:], op=ALU.mult),
                 reads=[pw_b], writes=[poolw_b])

            xring = Ring(T, stA, nc, "xa", 2, [128, D], F32)
            aring = Ring(T, stA, nc, "aa", 2, [128, 256], BF16)
            hT = sb("hTa", [128, 8, 128], BF16, stA); hT_b = Buf()
            ms = sb("msa", [128, 16], F32, stA); rt = sb("rta", [128, 16], F32, stA)
            qn = sb("qna", [128, 16, 64], F32, stA)
            t1 = sb("t1a", [128, 16, 64], F32, stA); t2 = sb("t2a", [128, 16, 64], F32, stA)
            sq = t2[:].rearrange("p h d -> p (h d)")
            stg = sb("stga", [128, 1024], BF16, stA)
            t12_b = Buf(); stg_b = Buf()
            wb_ = {"sq": t12_b, "ms": Buf(), "rt": Buf(), "dst": Buf()}
            ps_tr = ps("ps_trA", [128, 1024], F32, stA); ps_tr_b = Buf()
            ps_pj = ps("ps_pjA", [128, 1536], F32, stA); ps_pj_b = Buf()
            ps_t2 = ps("ps_t2A", [128, 1024], BF16, stA); ps_t2_b = Buf()
            for t in range(NT):
                stream = 0 if t < NTL else 1
                xt, xb, xs = xring.next()
                T.dma("sp", xs, xt[:], x_src(t), writes=[xb])
                make_hT(xt, xb, L, 0, 1, stream, ps_tr, ps_tr_b, lambda k: hT[:, k, :], hT_b)

                def _pj():
                    i = None
                    for (c0, n, w0) in ((0, 512, 256), (512, 512, 768), (1024, 256, 0), (1280, 256, 1280)):
                        for k in range(8):
                            i = pe.matmul(ps_pj[:, c0:c0 + n], lhsT=hT[:, k, :],
                                          rhs=win[:, k, w0:w0 + n], start=(k == 0), stop=(k == 7))
                    return i
                T.op("pe", _pj, reads=[hT_b, win_b], writes=[ps_pj_b])
                if t == 0:
                    dump("hT", hT[:].rearrange("p a b -> p (a b)"), [128, 1024], BF16, [hT_b])
                at_, ab_, as_ = aring.next()

                def _av(t=t, at_=at_):
                    act.copy(out=at_[:], in_=ps_pj[:, 1024:1280])
                    return act.copy(out=VA[:, t, :, 0:64], in_=ps_pj[:, 1280:1536].rearrange("p (k d) -> p k d", d=64))
                T.op("act", _av, reads=[ps_pj_b], writes=[ab_, VA_b[t]])
                T.dma("sp", as_, AD[t * 128:(t + 1) * 128, :], at_[:], reads=[ab_], writes=[AD_b[t]])
                rms_heads(ps_pj[:, 0:1024], 16, sq, ms, rt, qn[:], wb_, [ps_pj_b], gaint[:], gain_b)
                if t == 0:
                    dump("ms", ms[:], [128, 16], F32, [wb_["ms"]])
                    dump("qn", qn[:].rearrange("p a b -> p (a b)"), [128, 1024], F32, [wb_["dst"]])
                stq = stg[:, 0:768].rearrange("p (jj r hf d) -> p jj r hf d", jj=2, r=3, hf=2)
                stk = stg[:, 768:1024].rearrange("p (k d) -> p k d", d=64)

                def qperm(tt):
                    return tt[:, 0:12, :].rearrange("p (jj hf r) d -> p jj r hf d", jj=2, hf=2, r=3)
                if t < NTL:
                    rt_, rb_, rs_ = rring.next()
                    T.dma("sp", rs_, rt_[:, 0, :], ropeC[:, t, :], writes=[rb_])
                    T.dma("sp", rs_, rt_[:, 1, :], ropeS[:, t, :], writes=[rb_])

                    def _rope(rt_=rt_):
                        cb = rt_[:, 0, :].unsqueeze(1).to_broadcast([128, 16, 64])
                        dve.tensor_tensor(out=t1[:], in0=qn[:], in1=cb, op=ALU.mult)
                        qv = qn[:].rearrange("p h (a f) -> p h a f", a=4)
                        tv = t2[:].rearrange("p h (a f) -> p h a f", a=4)
                        sv = rt_[:, 1, :].rearrange("p (a f) -> p a f", a=4)
                        i = None
                        for ax in range(2):
                            for hf in range(2):
                                a_dst = ax * 2 + hf
                                a_src = ax * 2 + (1 - hf)
                                i = dve.tensor_tensor(out=tv[:, :, a_dst, :], in0=qv[:, :, a_src, :],
                                                      in1=sv[:, a_dst, :].unsqueeze(1).to_broadcast([128, 16, 16]),
                                                      op=ALU.mult)
                        return i
                    T.op("dve", _rope, reads=[wb_["dst"], rb_], writes=[t12_b])

                    def _st():
                        for jj in range(2):
                            dve.tensor_tensor(out=stq[:, jj], in0=qperm(t1)[:, jj], in1=qperm(t2)[:, jj], op=ALU.add)
                        return dve.tensor_tensor(out=stk, in0=t1[:, 12:16, :], in1=t2[:, 12:16, :], op=ALU.add)
                    T.op("dve", _st, reads=[t12_b], writes=[stg_b])
                else:
                    def _st():
                        for jj in range(2):
                            dve.tensor_copy(out=stq[:, jj], in_=qperm(qn)[:, jj])
                        return dve.tensor_copy(out=stk, in_=qn[:, 12:16, :])
                    T.op("dve", _st, reads=[wb_["dst"]], writes=[stg_b])

                def _t2():
                    i = None
                    for j in range(8):
                        i = pe.transpose(ps_t2[:, j * 128:(j + 1) * 128], stg[:, j * 128:(j + 1) * 128], identb[:])
                    return i
                T.op("pe", _t2, reads=[stg_b, identb_b], writes=[ps_t2_b])

                def _e2(t=t):
                    act.copy(out=QT[:, :, t * 128:(t + 1) * 128],
                             in_=ps_t2[:, 0:768].rearrange("p (j n) -> p j n", n=128))
                    return act.copy(out=KT[:, :, t * 128:(t + 1) * 128],
                                    in_=ps_t2[:, 768:1024].rearrange("p (j n) -> p j n", n=128))
                T.op("act", _e2, reads=[ps_t2_b], writes=[QT_b[t], KT_b[t]])
                if t == 0:
                    dump("stg", stg[:], [128, 1024], BF16, [stg_b])
                    dump("QT0", QT[:, :, 0:128], [128, 6, 128], BF16, [QT_b[0]])
                    dump("KT0", KT[:, :, 0:128], [128, 2, 128], BF16, [KT_b[0]])
                    dump("VA0", VA[:, 0, :, :], [128, 4, 66], BF16, [VA_b[0]])

        T.barrier()
        with contextlib.ExitStack() as stB:
            wout = sb("wout0", [128, 16, D], BF16, stB); wout_b = Buf()
            T.op("pool", lambda: pool.memset(wout[64:128, :, :], 0.0), writes=[wout_b])
            T.dma("pool", w0_s, wout[0:64], w_out[0].rearrange("(c p) n -> p c n", p=64), writes=[wout_b])
            lg, lgb = load_lnp(0, 0, stB, w0_s)
            lb, lbb = load_lnp(0, 1, stB, w0_s)
            ps_s = [ps(f"ps_s{i}", [128, 512], F32, stB) for i in range(2)]; ps_s_b = [Buf(), Buf()]
            ps_o = [ps(f"ps_o{i}", [128, 512], F32, stB) for i in range(2)]; ps_o_b = [Buf(), Buf()]
            ps_bc = ps("ps_bc", [128, 512], F32, stB); ps_bc_b = Buf()
            ps_y = ps("ps_y", [128, 1024], F32, stB); ps_y_b = Buf()
            ps_pl = ps("ps_pl", [128, 512], F32, stB); ps_pl_b = Buf()
            pT_ring = Ring(T, stB, nc, "pT", 3, [128, 512], BF16)
            osb = [sb(f"osb{i}", [128, 512], F32, stB) for i in range(2)]; osb_b = [Buf(), Buf()]
            for i in range(2):
                T.op("pool", lambda i=i: pool.memset(osb[i][:], 0.0), writes=[osb_b[i]])
            OT = sb("OT0", [128, 12, 512], BF16, stB); OT_b = [Buf() for _ in range(12)]
            T.op("pool", lambda: pool.memset(OT[64:128, :, :], 0.0), writes=OT_b)
            mT = sb("mT", [128, 4, 128], BF16, stB); mT_b = Buf()
            T.op("pool", lambda: pool.memset(mT[64:128, :, :], 0.0), writes=[mT_b])
            PTt = sb("PTt", [128, 4, 128], BF16, stB); PT_b = Buf()
            T.op("pool", lambda: pool.memset(PTt[64:128, :, :], 0.0), writes=[PT_b])
            a3ring = Ring(T, stB, nc, "a3", 2, [128, 3 * 256 + 64], BF16)
            for a3t_, a3b_ in zip(a3ring.t, a3ring.b):
                T.op("pool", lambda a3t_=a3t_: pool.memset(a3t_[:], 0.0), writes=[a3b_])
            zring = Ring(T, stB, nc, "zb", 2, [128, D], F32)
            tmp = sb("tmpb", [128, D], F32, stB); tmp_b = Buf()
            hcount = 0
            chunks = [(qc * 4, 4, list(range(NT))) for qc in range(8)] + [(NTL, 2, [NTL, NTL + 1])]
            qz = sb("Qz0", [128, 12, 512], BF16, stB); Qz_b = [Buf() for _ in range(12)]
            T.op("pool", lambda: pool.memset(qz[:], 0.0), writes=Qz_b)
            tails = []
            for ci, (t0, ntl, keys) in enumerate(chunks):
                nq = ntl * 128
                stream = 0 if t0 < NTL else 1
                for h in range(12):
                    ch, half = qhead_pos(h)
                    rows = slice(half * 64, (half + 1) * 64)
                    T.op("pool", lambda h=h, ch=ch, rows=rows, t0=t0, nq=nq: pool.tensor_copy(
                        out=qz[rows, h, 0:nq], in_=QT[rows, ch, t0 * 128:t0 * 128 + nq]),
                        reads=[QT_b[t0 + i] for i in range(ntl)], writes=[Qz_b[h]])
                for h in range(12):
                    ch, half = qhead_pos(h)
                    kv = h // 3
                    rows = slice(half * 64, (half + 1) * 64)
                    oi = hcount % 2
                    hcount += 1
                    attn_head(qz[:, h, 0:nq],
                              lambda kt, kv=kv: (KT[:, kv // 2, kt * 128:(kt + 1) * 128], KT_b[kt]),
                              lambda kt, kv=kv: (VA[:, kt, kv, 0:65], VA_b[kt]),
                              keys, ps_s, ps_s_b, pT_ring, ps_o[oi], ps_o_b[oi], nq,
                              [Qz_b[h]],
                              post=lambda oi=oi, h=h, nq=nq: attn_norm(ps_o[oi], ps_o_b[oi], osb[oi], osb_b[oi], ps_bc, ps_bc_b,
                                                                       OT[0:64, h, 0:nq], OT_b[h], nq))
                attn_flush(fillers=tails)
                tails = []
                for il in range(ntl):
                    def _tail(il=il, t0=t0, stream=stream):
                        t = t0 + il
                        first = (t == 0) or (t == NTL)
                        last = (t == NTL - 1) or (t == NT - 1)
                        tlo = t if first else t - 1
                        thi = t if last else t + 1
                        a3, a3b, a3s = a3ring.next()
                        nsrc = thi - tlo + 1
                        T.dma("sp", a3s, a3[:, 0:nsrc * 256].rearrange("p (j c) -> p j c", c=256),
                              AD[tlo * 128:(thi + 1) * 128, :].rearrange("(j p) c -> p j c", p=128),
                              reads=[AD_b[i] for i in range(tlo, thi + 1)], writes=[a3b])
                        srcs = []
                        if not first:
                            srcs.append((t - 1 - tlo, 0))
                        srcs.append((t - tlo, 3 if first else (4 if last else 1)))
                        if not last:
                            srcs.append((t + 1 - tlo, 2))

                        def _pm(srcs=srcs, a3=a3):
                            i = None
                            for g in range(4):
                                for j, (sl, var) in enumerate(srcs):
                                    i = pe.matmul(ps_pl[:, g * 128:(g + 1) * 128], lhsT=a3[:, sl * 256 + g * 64:sl * 256 + g * 64 + 128],
                                                  rhs=bandt[:, g, var, :], start=(j == 0), stop=(j == len(srcs) - 1))
                            return i
                        T.op("pe", _pm, reads=[a3b, band_b], writes=[ps_pl_b])
                        T.op("act", lambda: act.copy(out=mT[0:64].rearrange("p g n -> p (g n)"), in_=ps_pl[0:64, :]),
                             reads=[ps_pl_b], writes=[mT_b])

                        def _pp():
                            i = None
                            for g in range(4):
                                i = pe.matmul(ps_pl[:, g * 128:(g + 1) * 128], lhsT=poolw[:, g, :], rhs=mT[:, g, :],
                                              start=True, stop=True)
                            return i
                        T.op("pe", _pp, reads=[mT_b, poolw_b], writes=[ps_pl_b])
                        T.op("act", lambda: act.copy(out=PTt[0:64].rearrange("p g n -> p (g n)"), in_=ps_pl[0:64, :]),
                             reads=[ps_pl_b], writes=[PT_b])

                        def _y(il=il):
                            i = None
                            for n in range(2):
                                for c in range(16):
                                    lhs = PTt[:, c, :] if c < 4 else OT[:, c - 4, il * 128:(il + 1) * 128]
                                    i = pe.matmul(ps_y[:, n * 512:(n + 1) * 512], lhsT=lhs, rhs=wout[:, c, n * 512:(n + 1) * 512],
                                                  start=(c == 0), stop=(c == 15))
                            return i
                        T.op("pe", _y, reads=[PT_b, wout_b] + OT_b, writes=[ps_y_b])
                        if t == 0:
                            dump("OT", OT[0:64], [64, 12, 512], BF16, OT_b)
                            dump("PT", PTt[0:64], [64, 4, 128], BF16, [PT_b])
                        zt, zb, zs = zring.next()
                        T.dma("sp", zs, zt[:], x_src(t), writes=[zb])
                        resid_ln([ps_y[:, 0:512], ps_y[:, 512:1024]], [ps_y_b], zt, zb,
                                 gate[(stream, 0)], gate_b[(stream, 0)], lg, lgb, lb, lbb, tmp, tmp_b)
                        T.dma("sp", zs, X1[t * 128:(t + 1) * 128, :], zt[:], reads=[zb], writes=[X1b[t]], is_output=debug)
                    tails.append(_tail)
                if not DEFER_L0:
                    for f in tails:
                        f()
                    tails = []
            for f in tails:
                f()

        T.barrier()
        stR.close()
        with contextlib.ExitStack() as stF:
            w1 = sb("w1f", [128, 8, FFN], BF16, stF); w3 = sb("w3f", [128, 8, FFN], BF16, stF)
            w2 = sb("w2f", [128, NF, D], BF16, stF); wf_b = Buf(); wf_s = T.semc("wf")
            for k in range(8):
                T.dma("pool", wf_s, w1[:, k, :], ev_ffn_w1[k * 128:(k + 1) * 128, :], writes=[wf_b], max_dma_last_dim=4096)
                T.dma("pool", wf_s, w3[:, k, :], ev_ffn_w3[k * 128:(k + 1) * 128, :], writes=[wf_b], max_dma_last_dim=4096)
            for f in range(NF):
                T.dma("pool", wf_s, w2[:, f, :], ev_ffn_w2[f * 128:(f + 1) * 128, :], writes=[wf_b], max_dma_last_dim=4096)
            lg, lgb = load_lnp(0, 2, stF, wf_s)
            lb, lbb = load_lnp(0, 3, stF, wf_s)
            x1c = [sb(f"x1c{i}", [128, D], F32, stF) for i in range(4)]; x1c_b = [Buf() for _ in range(4)]
            x1c_s = [T.semc("x1c") for _ in range(4)]
            h2T = sb("h2T", [128, 8, 512], BF16, stF); h2T_b = [Buf() for _ in range(4)]
            AT = sb("ATf", [128, NF, 512], BF16, stF); AT_b = [Buf() for _ in range(NF)]
            sg = [sb(f"sgf{i}", [128, 512], BF16, stF) for i in range(2)]; sg_b = [Buf(), Buf()]
            tmp = sb("tmpf", [128, D], F32, stF); tmp_b = Buf()
            ps_tr = ps("ps_trF", [128, 1024], F32, stF); ps_tr_b = Buf()
            ps_g = [ps(f"ps_gF{i}", [128, 512], F32, stF) for i in range(2)]; ps_g_b = [Buf(), Buf()]
            ps_u = [ps(f"ps_uF{i}", [128, 512], F32, stF) for i in range(2)]; ps_u_b = [Buf(), Buf()]
            ps_o2 = [ps(f"ps_oF{i}", [128, 512], F32, stF) for i in range(2)]; ps_o2_b = [Buf(), Buf()]
            oc = 0
            chunks = [(qc * 4, 4) for qc in range(8)] + [(NTL, 2)]
            for (t0, ntl) in chunks:
                nq = ntl * 128
                stream = 0 if t0 < NTL else 1
                for il in range(ntl):
                    t = t0 + il
                    T.dma("sp", x1c_s[il], x1c[il][:], X1[t * 128:(t + 1) * 128, :], reads=[X1b[t]], writes=[x1c_b[il]])
                    make_hT(x1c[il], x1c_b[il], L, 2, 3, stream, ps_tr, ps_tr_b,
                            lambda k, il=il: h2T[:, k, il * 128:(il + 1) * 128], h2T_b[il])
                for f in range(NF):
                    gi = f % 2

                    def _gu(f=f, gi=gi, nq=nq):
                        i = None
                        for k in range(8):
                            i = pe.matmul(ps_g[gi][:, 0:nq], lhsT=w1[:, k, f * 128:(f + 1) * 128], rhs=h2T[:, k, 0:nq],
                                          start=(k == 0), stop=(k == 7))
                        for k in range(8):
                            i = pe.matmul(ps_u[gi][:, 0:nq], lhsT=w3[:, k, f * 128:(f + 1) * 128], rhs=h2T[:, k, 0:nq],
                                          start=(k == 0), stop=(k == 7))
                        return i
                    T.op("pe", _gu, reads=[wf_b] + h2T_b[0:ntl], writes=[ps_g_b[gi], ps_u_b[gi]])
                    T.op("act", lambda gi=gi, nq=nq: act.activation(out=sg[gi][:, 0:nq], in_=ps_g[gi][:, 0:nq], func=AF.Silu),
                         reads=[ps_g_b[gi]], writes=[sg_b[gi]])
                    T.op("dve", lambda gi=gi, f=f, nq=nq: dve.tensor_tensor(out=AT[:, f, 0:nq], in0=sg[gi][:, 0:nq],
                                                                           in1=ps_u[gi][:, 0:nq], op=ALU.mult),
                         reads=[sg_b[gi], ps_u_b[gi]], writes=[AT_b[f]])
                for il in range(ntl):
                    t = t0 + il
                    ois = []
                    for n in range(2):
                        oi = oc % 2
                        oc += 1
                        ois.append(oi)

                        def _o(il=il, n=n, oi=oi):
                            i = None
                            for f in range(NF):
                                i = pe.matmul(ps_o2[oi][:, :], lhsT=AT[:, f, il * 128:(il + 1) * 128],
                                              rhs=w2[:, f, n * 512:(n + 1) * 512], start=(f == 0), stop=(f == NF - 1))
                            return i
                        T.op("pe", _o, reads=AT_b + [wf_b], writes=[ps_o2_b[oi]])
                    resid_ln([ps_o2[ois[0]][:, :], ps_o2[ois[1]][:, :]], [ps_o2_b[ois[0]], ps_o2_b[ois[1]]],
                             x1c[il], x1c_b[il], gate[(stream, 1)], gate_b[(stream, 1)], lg, lgb, lb, lbb, tmp, tmp_b)
                    if n_layers == 1 and t < NTL:
                        T.dma("sp", x1c_s[il], out_ap[t * 128:(t + 1) * 128, :], x1c[il][:], reads=[x1c_b[il]], is_output=True)
                    T.dma("sp", x1c_s[il], X2[t * 128:(t + 1) * 128, :], x1c[il][:], reads=[x1c_b[il]], writes=[X2b[t]],
                          is_output=debug)

    T.barrier()
    if n_layers == 1:
        T.finish()
        es.close()
        return nc

    L = 1
    QD = dscr("QD", [128, 6, SEQ], BF16); QD_b = [Buf() for _ in range(NTL)]
    KD = dscr("KD", [128, 6, SEQ + CTXL], BF16); KD_b = [Buf() for _ in range(NT)]
    VD = dscr("VD", [NT, 128, 12 * 66], BF16); VD_b = [Buf() for _ in range(NT)]
    with contextlib.ExitStack() as stL1:
        stR = contextlib.ExitStack()
        gate, gate_b = compute_mod(1, stL1, stR)
        YT = sb("YT", [128, 2, SEQ], BF16, stR); YT_b = [Buf() for _ in range(8)]
        w1_s = T.semc("w1")
        stU = contextlib.ExitStack()
        UCS = sb("UCS", [128, NTL, 512], BF16, stU); UCS_b = [Buf() for _ in range(NTL)]
        with contextlib.ExitStack() as stA:
            win = sb("win1", [128, 8, 2560], BF16, stA); win_b = Buf()
            for k in range(8):
                T.dma("pool", w1_s, win[:, k, :], od_w_in[k * 128:(k + 1) * 128, :], writes=[win_b], max_dma_last_dim=4096)
            fg = sb("fg1", [128, 4, 64], F32, stA); fg_b = Buf()
            T.dma("sp", w1_s, fg[:].rearrange("p g d -> p (g d)"),
                  od_fgain.rearrange("(o n) -> o n", o=1).to_broadcast([128, 256]), writes=[fg_b])
            c64 = sb("c64", [128, 2, 128], BF16, stA); c64_b = Buf()
            T.dma("sp", w1_s, c64[:, 0, :], C64[:, :], writes=[c64_b])
            T.dma("sp", w1_s, c64[:, 1, :], S64[:, :], writes=[c64_b])
            xring = Ring(T, stA, nc, "xa1", 2, [128, D], F32)
            hT = sb("hTa1", [128, 8, 128], BF16, stA); hT_b = Buf()
            sq = sb("sqa1", [128, 256], F32, stA); ms = sb("msa1", [128, 4], F32, stA); rt = sb("rta1", [128, 4], F32, stA)
            un = sb("una1", [128, 4, 64], F32, stA)
            wb_ = {"sq": Buf(), "ms": Buf(), "rt": Buf(), "dst": Buf()}
            stg = sb("stga1", [128, 14 * 128], BF16, stA); stg_b = Buf()
            qkT = Ring(T, stA, nc, "qkT1", 2, [128, 12, 128], BF16)
            uT = sb("uT1", [128, 2, 128], BF16, stA); uT_b = Buf()
            vring = Ring(T, stA, nc, "va1", 2, [128, 12, 66], BF16)
            for i_ in range(2):
                T.op("dve", lambda i_=i_: dve.memset(vring.t[i_][:, :, 64:66], 1.0), writes=[vring.b[i_]])
            ps_tr = ps("ps_trA1", [128, 1024], F32, stA); ps_tr_b = Buf()
            ps_pj = ps("ps_pjA1", [128, 2048], F32, stA); ps_pj_b = Buf()
            ps_t2 = ps("ps_t2A1", [128, 2048], BF16, stA); ps_t2_b = Buf()
            for t in range(NT):
                lat = t < NTL
                stream = 0 if lat else 1
                xt, xb, xs = xring.next()
                T.dma("sp", xs, xt[:], X2[t * 128:(t + 1) * 128, :], reads=[X2b[t]], writes=[xb])
                make_hT(xt, xb, L, 0, 1, stream, ps_tr, ps_tr_b, lambda k: hT[:, k, :], hT_b)

                def _pj(lat=lat):
                    i = None
                    groups = [(1024, 512, 1024), (1536, 256, 1536)]
                    if lat:
                        groups = [(0, 512, 256), (512, 256, 768), (768, 256, 0)] + groups
                    for (c0, n, w0) in groups:
                        for k in range(8):
                            i = pe.matmul(ps_pj[:, c0:c0 + n], lhsT=hT[:, k, :], rhs=win[:, k, w0:w0 + n],
                                          start=(k == 0), stop=(k == 7))
                    return i
                T.op("pe", _pj, reads=[hT_b, win_b], writes=[ps_pj_b])
                if lat:
                    rms_heads(ps_pj[:, 768:1024], 4, sq, ms, rt, un[:], wb_, [ps_pj_b], fg[:], fg_b)

                def _stq(lat=lat):
                    i = act.copy(out=stg[:, 768:1536], in_=ps_pj[:, 1024:1792])
                    if lat:
                        i = act.mul(out=stg[:, 0:768], in_=ps_pj[:, 0:768], mul=0.125)
                    return i
                T.op("act", _stq, reads=[ps_pj_b] + ([wb_["dst"]] if lat else []), writes=[stg_b])
                if lat:
                    T.op("dve", lambda: dve.tensor_copy(out=stg[:, 1536:1792], in_=un[:].rearrange("p g d -> p (g d)")),
                         reads=[wb_["dst"]], writes=[stg_b])

                def _t2(lat=lat):
                    i = None
                    for j in (range(14) if lat else range(6, 12)):
                        i = pe.transpose(ps_t2[:, j * 128:(j + 1) * 128], stg[:, j * 128:(j + 1) * 128], identb[:])
                    return i
                T.op("pe", _t2, reads=[stg_b, identb_b], writes=[ps_t2_b])
                qk, qkb, qks = qkT.next()

                def _e2(lat=lat, qk=qk):
                    i = act.copy(out=qk[:, 6:12, :], in_=ps_t2[:, 768:1536].rearrange("p (j n) -> p j n", n=128))
                    if lat:
                        i = act.copy(out=qk[:, 0:6, :], in_=ps_t2[:, 0:768].rearrange("p (j n) -> p j n", n=128))
                    return i
                T.op("act", _e2, reads=[ps_t2_b], writes=[qkb])
                T.dma("sp", qks, KD[:, :, t * 128:(t + 1) * 128], qk[:, 6:12, :], reads=[qkb], writes=[KD_b[t]])
                if lat:
                    T.dma("sp", qks, QD[:, :, t * 128:(t + 1) * 128], qk[:, 0:6, :], reads=[qkb], writes=[QD_b[t]])
                    T.op("dve", lambda: dve.tensor_copy(out=uT[:].rearrange("p a n -> p (a n)"), in_=ps_t2[:, 1536:1792]),
                         reads=[ps_t2_b], writes=[uT_b])

                    def _uc():
                        i = None
                        for cs_ in range(2):
                            for c2 in range(2):
                                i = pe.matmul(ps_tr[:, (cs_ * 2 + c2) * 128:(cs_ * 2 + c2 + 1) * 128], lhsT=uT[:, c2, :],
                                              rhs=c64[:, cs_, :], start=True, stop=True)
                        return i
                    T.op("pe", _uc, reads=[uT_b, c64_b], writes=[ps_tr_b])
                    T.op("dve", lambda t=t: dve.tensor_copy(out=UCS[:, t, :], in_=ps_tr[:, 0:512]),
                         reads=[ps_tr_b], writes=[UCS_b[t]])
                def _pv():
                    i = None
                    for (c0, n, w0) in ((0, 512, 1792), (512, 256, 2304)):
                        for k in range(8):
                            i = pe.matmul(ps_pj[:, c0:c0 + n], lhsT=hT[:, k, :], rhs=win[:, k, w0:w0 + n],
                                          start=(k == 0), stop=(k == 7))
                    return i
                T.op("pe", _pv, reads=[hT_b, win_b], writes=[ps_pj_b])
                vt, vb, vs = vring.next()
                T.op("act", lambda vt=vt: act.copy(out=vt[:, :, 0:64], in_=ps_pj[:, 0:768].rearrange("p (h d) -> p h d", d=64)),
                     reads=[ps_pj_b], writes=[vb])
                T.dma("sp", vs, VD[t], vt[:].rearrange("p h d -> p (h d)"), reads=[vb], writes=[VD_b[t]])
        T.barrier()
        if stop_after == "A1":
            T.finish(); stU.close(); stR.close(); return nc
        with contextlib.ExitStack() as stFo:
            tring = Ring(T, stFo, nc, "dft", 3, [128, 2, 8, 512], BF16)
            ps_f = [ps(f"ps_f{i}", [128, 512], F32, stFo) for i in range(4)]; ps_f_b = [Buf() for _ in range(4)]
            for tc in range(8):
                pb_ = (tc % 2) * 2
                for sp_ in range(4):
                    tt, tb, ts_ = tring.next()
                    for ci, src in enumerate((CN, SN)):
                        T.dma("sp", ts_, tt[:, ci, :, :],
                              src[sp_ * 1024:(sp_ + 1) * 1024, tc * 512:(tc + 1) * 512].rearrange("(j p) n -> p j n", p=128),
                              writes=[tb])

                    def _f(tt=tt, sp_=sp_, pb_=pb_):
                        i = None
                        for c2 in range(2):
                            for j in range(8):
                                s_ = sp_ * 8 + j
                                for ci in range(2):
                                    i = pe.matmul(ps_f[pb_ + c2][:, :], lhsT=UCS[:, s_, (ci * 2 + c2) * 128:(ci * 2 + c2 + 1) * 128],
                                                  rhs=tt[:, ci, j, :], start=(s_ == 0 and ci == 0), stop=(s_ == 31 and ci == 1))
                        return i
                    T.op("pe", _f, reads=[tb] + UCS_b[sp_ * 8:(sp_ + 1) * 8], writes=[ps_f_b[pb_], ps_f_b[pb_ + 1]])

                def _fe(tc=tc, pb_=pb_):
                    i = None
                    for c2 in range(2):
                        i = act.copy(out=YT[:, c2, tc * 512:(tc + 1) * 512], in_=ps_f[pb_ + c2][:, :])
                    return i
                T.op("act", _fe, reads=[ps_f_b[pb_], ps_f_b[pb_ + 1]], writes=[YT_b[tc]])
        T.barrier()
        if stop_after == "FO":
            T.finish(); stU.close(); stR.close(); return nc
        stU.close()

        with contextlib.ExitStack() as stB:
            wout = sb("wout1", [128, 12, D], BF16, stB); woutF = sb("woutF1", [128, 2, D], BF16, stB); wout_b = Buf()
            T.op("pool", lambda: pool.memset(wout[64:128, :, :], 0.0), writes=[wout_b])
            T.dma("pool", w1_s, wout[0:64], w_out[1, 256:1024, :].rearrange("(c p) n -> p c n", p=64), writes=[wout_b])
            T.dma("pool", w1_s, woutF[:], w_out[1, 0:256, :].rearrange("(c p) n -> p c n", p=128), writes=[wout_b])
            lg, lgb = load_lnp(1, 0, stB, w1_s)
            lb, lbb = load_lnp(1, 1, stB, w1_s)
            KTc = sb("KTc", [128, 6, 256], BF16, stB); Vc = sb("Vc", [128, 2, 12 * 66], BF16, stB); kvc_b = Buf()
            T.dma("sp", w1_s, KTc[:], KD[:, :, SEQ:SEQ + CTXL], reads=[KD_b[32], KD_b[33]], writes=[kvc_b])
            T.dma("sp", w1_s, Vc[:], VD[NTL:NT].rearrange("j p n -> p j n"), reads=[VD_b[32], VD_b[33]], writes=[kvc_b])
            qz1 = sb("Qz1", [128, 12, 512], BF16, stB); Qz1_b = [Buf() for _ in range(12)]; qz1_s = T.semc("qz1")
            T.op("pool", lambda: pool.memset(qz1[:], 0.0), writes=Qz1_b)
            kring = Ring(T, stB, nc, "kb1", 2, [128, 6, 1024], BF16)
            vwring = Ring(T, stB, nc, "vb1", 2, [128, 8, 12 * 66], BF16)
            bring = Ring(T, stB, nc, "bias1", 3, [128, 8, 512], BF16)
            ps_s = [ps(f"ps_s1{i}", [128, 512], F32, stB) for i in range(2)]; ps_s_b = [Buf(), Buf()]
            ps_o = [ps(f"ps_o1{i}", [128, 512], F32, stB) for i in range(2)]; ps_o_b = [Buf(), Buf()]
            ps_bc = ps("ps_bc1", [128, 512], F32, stB); ps_bc_b = Buf()
            ps_y = ps("ps_y1", [128, 1024], F32, stB); ps_y_b = Buf()
            pT_ring = Ring(T, stB, nc, "pT1", 3, [128, 512], BF16)
            osb = [sb(f"osb1{i}", [128, 512], F32, stB) for i in range(2)]; osb_b = [Buf(), Buf()]
            for i in range(2):
                T.op("pool", lambda i=i: pool.memset(osb[i][:], 0.0), writes=[osb_b[i]])
            OT = sb("OT1", [128, 12, 512], BF16, stB); OT_b = [Buf() for _ in range(12)]
            T.op("pool", lambda: pool.memset(OT[64:128, :, :], 0.0), writes=OT_b)
            zring = Ring(T, stB, nc, "zb1", 2, [128, D], F32)
            tmp = sb("tmpb1", [128, D], F32, stB); tmp_b = Buf()
            hcount = 0
            tails = []
            for qb in range(8):
                t0 = 4 * qb
                bt = 0 if qb == 0 else (2 if qb == 7 else 1)
                klo = max(0, t0 - 2)
                khi = min(NTL, t0 + 6)
                nk = khi - klo
                slot0 = klo - (t0 - 2)
                for h in range(12):
                    ch, half = h // 2, h % 2
                    rows = slice(half * 64, (half + 1) * 64)
                    T.dma("sp", qz1_s, qz1[rows, h, :], QD[rows, ch, t0 * 128:(t0 + 4) * 128], reads=QD_b[t0:t0 + 4],
                          writes=[Qz1_b[h]])
                kt_, kb_, ks_ = kring.next()
                T.dma("sp", ks_, kt_[:, :, 0:nk * 128], KD[:, :, klo * 128:khi * 128], reads=KD_b[klo:khi], writes=[kb_])
                vt_, vb_, vs_ = vwring.next()
                T.dma("sp", vs_, vt_[:, 0:nk, :], VD[klo:khi].rearrange("j p n -> p j n"), reads=VD_b[klo:khi], writes=[vb_])
                keys = list(range(klo, khi)) + [NTL, NTL + 1]
                for h in range(12):
                    ch, half = h // 2, h % 2
                    rows = slice(half * 64, (half + 1) * 64)
                    oi = hcount % 2
                    hcount += 1
                    bt_, bb_, bs_ = bring.next()

                    def bpre(bt_=bt_, bb_=bb_, bs_=bs_, h=h, bt=bt, slot0=slot0, nk=nk):
                        T.dma("pool", bs_, bt_[:, 0:nk, :], od_bias[h, bt, slot0:slot0 + nk].rearrange("j p n -> p j n"),
                              writes=[bb_])

                    def kfn(kt, rows=rows, ch=ch, kt_=kt_, kb_=kb_, klo=klo):
                        if kt >= NTL:
                            return KTc[:, ch, (kt - NTL) * 128:(kt - NTL + 1) * 128], kvc_b
                        return kt_[:, ch, (kt - klo) * 128:(kt - klo + 1) * 128], kb_

                    def vfn(kt, h=h, vt_=vt_, vb_=vb_, klo=klo):
                        if kt >= NTL:
                            return Vc[:, kt - NTL, h * 66:h * 66 + 65], kvc_b
                        return vt_[:, kt - klo, h * 66:h * 66 + 65], vb_

                    def bfn(kt, bt_=bt_, bb_=bb_, klo=klo):
                        if kt >= NTL:
                            return None
                        return bt_[:, kt - klo, :], bb_
                    attn_head(qz1[:, h, :], kfn, vfn, keys, ps_s, ps_s_b, pT_ring, ps_o[oi], ps_o_b[oi], 512,
                              [Qz1_b[h]], bias_fn=bfn, pre=bpre,
                              post=lambda oi=oi, h=h: attn_norm(ps_o[oi], ps_o_b[oi], osb[oi], osb_b[oi], ps_bc, ps_bc_b,
                                                                OT[0:64, h, :], OT_b[h], 512))
                attn_flush(fillers=tails)
                tails = []
                for il in range(4):
                    def _tail(il=il, t0=t0):
                        t = t0 + il

                        def _y(il=il, t=t):
                            i = None
                            for n in range(2):
                                for c in range(14):
                                    if c < 2:
                                        lhs, rhs = YT[:, c, t * 128:(t + 1) * 128], woutF[:, c, n * 512:(n + 1) * 512]
                                    else:
                                        lhs, rhs = OT[:, c - 2, il * 128:(il + 1) * 128], wout[:, c - 2, n * 512:(n + 1) * 512]
                                    i = pe.matmul(ps_y[:, n * 512:(n + 1) * 512], lhsT=lhs, rhs=rhs, start=(c == 0), stop=(c == 13))
                            return i
                        T.op("pe", _y, reads=[YT_b[t // 4], wout_b] + OT_b, writes=[ps_y_b])
                        zt, zb, zs = zring.next()
                        T.dma("sp", zs, zt[:], X2[t * 128:(t + 1) * 128, :], reads=[X2b[t]], writes=[zb])
                        resid_ln([ps_y[:, 0:512], ps_y[:, 512:1024]], [ps_y_b], zt, zb,
                                 gate[(0, 0)], gate_b[(0, 0)], lg, lgb, lb, lbb, tmp, tmp_b)
                        T.dma("sp", zs, X3[t * 128:(t + 1) * 128, :], zt[:], reads=[zb], writes=[X3b[t]], is_output=debug)
                    tails.append(_tail)
            for f in tails:
                f()

        T.barrier()
        if stop_after == "NA":
            T.finish(); stR.close(); return nc
        stR.close()

        with contextlib.ExitStack() as stM:
            NG = 23
            NROW = NG * 512
            Xs = dscr("Xs", [NROW, D]); Ys = dscr("Ys", [NROW, D])
            Xs_b = Buf(); Ys_b = Buf()
            xs_s = T.semc("xs"); ys_s = T.semc("ys"); wg_s = T.semc("wg"); yg_s = T.semc("yg")
            W1r = W1s.rearrange("e j p k n -> (e j p) (k n)")
            W3r = W3s.rearrange("e j p k n -> (e j p) (k n)")
            W2r = W2s.rearrange("e h p f n -> (e h p) (f n)")
            lg, lgb = load_lnp(1, 2, stM, w1_s)
            lb, lbb = load_lnp(1, 3, stM, w1_s)
            wr = sb("wr", [128, 8, 8], F32, stM); wr_b = Buf()
            T.dma("sp", w1_s, wr[:], od_router.rearrange("(k p) e -> p k e", p=128), writes=[wr_b])
            pidx = sb("pidx", [128, 1], F32, stM); pidx_b = Buf()
            T.dma("sp", w1_s, pidx[:], pidx_in[:, :], writes=[pidx_b])
            utri = sb("utri", [128, 128], BF16, stM); utri_b = Buf()
            T.dma("pool", w1_s, utri[:], utri_in[:, :], writes=[utri_b])
            onesb = sb("onesb", [128, 128], BF16, stM); onesb_b = Buf()
            T.op("dve", lambda: dve.memset(onesb[:], 1.0), writes=[onesb_b])
            x3c = [sb(f"x3c{i}", [128, D], F32, stM) for i in range(4)]; x3c_b = [Buf() for _ in range(4)]
            x3c_s = [T.semc("x3c") for _ in range(4)]
            acc = [sb(f"acc{i}", [128, D], F32, stM) for i in range(4)]; acc_b = [Buf() for _ in range(4)]
            h2T = sb("h2Tm", [128, 8, 512], BF16, stM); h2T_b = [Buf() for _ in range(4)]
            h2T2 = sb("h2Tm2", [128, 8, 512], BF16, stM); h2T2_b = [Buf() for _ in range(4)]
            h2Tf = sb("h2Tf", [128, 8, 128], F32, stM); h2Tf_b = Buf()
            AT = sb("ATm", [128, NF, 512], BF16, stM); AT_b = [Buf() for _ in range(NF)]
            sg = [sb(f"sgm{i}", [128, 512], BF16, stM) for i in range(2)]; sg_b = [Buf(), Buf()]
            tmp = sb("tmpm", [128, D], F32, stM); tmp_b = Buf()
            lgt = sb("lgt", [128, 8], F32, stM); mx = sb("mxm", [128, 8], F32, stM)
            indb = sb("indb", [128, 8], BF16, stM)
            eqs = sb("eqs", [128, NTL, 2, 8], F32, stM)
            wts = sb("wts", [128, NTL, 2], F32, stM)
            posl = sb("posl", [128, NTL, 8], F32, stM)
            off = sb("offm", [128, 8], F32, stM)
            gb = [Buf() for _ in range(6)]
            route_b = Buf()
            w13 = Ring(T, stM, nc, "w13", 3, [128, 2, 8 * 256], BF16)
            w2r = Ring(T, stM, nc, "w2r", 3, [128, NF * 512], BF16)
            ps_tr = ps("ps_trM", [128, 1024], F32, stM); ps_tr_b = Buf()
            ps_g = [ps(f"ps_gM{i}", [128, 512], F32, stM) for i in range(2)]; ps_g_b = [Buf(), Buf()]
            ps_u = [ps(f"ps_uM{i}", [128, 512], F32, stM) for i in range(2)]; ps_u_b = [Buf(), Buf()]
            ps_o2 = [ps(f"ps_oM{i}", [128, 512], F32, stM) for i in range(2)]; ps_o2_b = [Buf(), Buf()]
            oc = 0
            T.op("dve", lambda: dve.memset(off[:], 0.0), writes=[gb[5]])

            for t in range(NTL):
                il = t % 4
                T.dma("sp", x3c_s[il], x3c[il][:], X3[t * 128:(t + 1) * 128, :], reads=[X3b[t]], writes=[x3c_b[il]])
                make_hT(x3c[il], x3c_b[il], L, 2, 3, 0, ps_tr, ps_tr_b,
                        lambda k: h2T[:, k, 0:128], h2T_b[0], lambda k: h2Tf[:, k, :], h2Tf_b)
                oi = oc % 2
                oc += 1

                def _r(oi=oi):
                    i = None
                    for k in range(8):
                        i = pe.matmul(ps_o2[oi][:, 0:8], lhsT=h2Tf[:, k, :], rhs=wr[:, k, :], start=(k == 0), stop=(k == 7))
                    return i
                T.op("pe", _r, reads=[h2Tf_b, wr_b], writes=[ps_o2_b[oi]])
                T.op("dve", lambda oi=oi: dve.tensor_copy(out=lgt[:], in_=ps_o2[oi][:, 0:8]), reads=[ps_o2_b[oi]], writes=[gb[0]])
                T.op("dve", lambda: dve.max(out=mx[:], in_=lgt[:]), reads=[gb[0]], writes=[gb[1]], hard=True)
                T.op("dve", lambda t=t: dve.tensor_tensor(out=wts[:, t, 0:1], in0=mx[:, 0:1], in1=mx[:, 1:2], op=ALU.subtract),
                     reads=[gb[1]], writes=[gb[2]], hard=True)
                T.op("act", lambda t=t: act.activation(out=wts[:, t, 0:1], in_=wts[:, t, 0:1], func=AF.Sigmoid),
                     reads=[gb[2]], writes=[gb[2]])

                def _g1(t=t):
                    dve.tensor_scalar(out=wts[:, t, 1:2], in0=wts[:, t, 0:1], scalar1=-1.0, scalar2=1.0, op0=ALU.mult, op1=ALU.add)
                    dve.tensor_scalar(out=eqs[:, t, 0, :], in0=lgt[:], scalar1=mx[:, 0:1], scalar2=None, op0=ALU.is_equal)
                    return dve.tensor_scalar(out=eqs[:, t, 1, :], in0=lgt[:], scalar1=mx[:, 1:2], scalar2=None, op0=ALU.is_equal)
                T.op("dve", _g1, reads=[gb[2], gb[1], gb[0]], writes=[gb[3]], hard=True)
                T.op("dve", lambda t=t: dve.tensor_tensor(out=indb[:], in0=eqs[:, t, 0, :], in1=eqs[:, t, 1, :], op=ALU.add),
                     reads=[gb[3]], writes=[gb[4]], hard=True)

                def _pf(oi=oi):
                    pe.matmul(ps_o2[oi][:, 8:16], lhsT=utri[:, :], rhs=indb[:, :], start=True, stop=True)
                    return pe.matmul(ps_o2[oi][:, 16:24], lhsT=onesb[:, :], rhs=indb[:, :], start=True, stop=True)
                T.op("pe", _pf, reads=[gb[4], utri_b, onesb_b], writes=[ps_o2_b[oi]])

                def _po(oi=oi, t=t):
                    dve.tensor_tensor(out=posl[:, t, :], in0=ps_o2[oi][:, 8:16], in1=off[:], op=ALU.add)
                    return dve.tensor_tensor(out=off[:], in0=off[:], in1=ps_o2[oi][:, 16:24], op=ALU.add)
                T.op("dve", _po, reads=[ps_o2_b[oi], gb[5]], writes=[gb[5], route_b], hard=True)

            thr = sb("thr", [128, 8, 8], F32, stM); gidx = sb("gidx", [128, NG, 8], F32, stM)
            cmp8 = sb("cmp8", [128, 8, 8], F32, stM); cmpg = sb("cmpg", [128, NG, 8], F32, stM)
            ngr = sb("ngr", [128, 8], F32, stM); gend = sb("gend", [128, 8], F32, stM); base = sb("basem", [128, 8], F32, stM)
            eg = sb("egm", [128, NG], F32, stM)
            jp1 = sb("jp1", [128, 11], F32, stM); jp2 = sb("jp2", [128, 2], F32, stM)
            w1if = sb("w1if", [128, NG, 11], F32, stM); w2if = sb("w2if", [128, NG, 2], F32, stM)
            w1idx = sb("w1idx", [128, NG, 11], I32, stM); w2idx = sb("w2idx", [128, NG, 2], I32, stM)
            posf = sb("posf", [128, NTL, 2, 8], F32, stM); pab = sb("pab", [128, NTL, 2], F32, stM)
            idxab = sb("idxab", [128, NTL, 2], I32, stM)
            idx_b = Buf()

            def dv(fn, extra=()):
                T.op("dve", fn, reads=[idx_b] + list(extra), writes=[idx_b], hard=True)
            for k in range(8):
                dv(lambda k=k: dve.memset(thr[:, :, k:k + 1], 512.0 * k))
            for g in range(NG):
                dv(lambda g=g: dve.memset(gidx[:, g, :], float(g)))
            for j in range(11):
                dv(lambda j=j: dve.tensor_scalar(out=jp1[:, j:j + 1], in0=pidx[:, 0:1], scalar1=128.0 * j, scalar2=None,
                                                 op0=ALU.add), [pidx_b])
            for n in range(2):
                dv(lambda n=n: dve.tensor_scalar(out=jp2[:, n:n + 1], in0=pidx[:, 0:1], scalar1=128.0 * n, scalar2=None,
                                                 op0=ALU.add), [pidx_b])
            dv(lambda: dve.tensor_tensor(out=cmp8[:], in0=off[:].unsqueeze(2).to_broadcast([128, 8, 8]), in1=thr[:], op=ALU.is_gt),
               [route_b, gb[5]])
            dv(lambda: dve.tensor_reduce(out=ngr[:], in_=cmp8[:], axis=AX.X, op=ALU.add))
            dv(lambda: dve.tensor_copy(out=gend[:, 0:1], in_=ngr[:, 0:1]))
            for e in range(1, 8):
                dv(lambda e=e: dve.tensor_tensor(out=gend[:, e:e + 1], in0=gend[:, e - 1:e], in1=ngr[:, e:e + 1], op=ALU.add))
            dv(lambda: dve.tensor_tensor(out=base[:], in0=gend[:], in1=ngr[:], op=ALU.subtract))
            dv(lambda: dve.tensor_scalar(out=base[:], in0=base[:], scalar1=512.0, scalar2=None, op0=ALU.mult))
            dv(lambda: dve.tensor_tensor(out=cmpg[:], in0=gend[:].unsqueeze(1).to_broadcast([128, NG, 8]), in1=gidx[:], op=ALU.is_le))
            dv(lambda: dve.tensor_reduce(out=eg[:], in_=cmpg[:], axis=AX.X, op=ALU.add))
            dv(lambda: dve.tensor_scalar(out=eg[:], in0=eg[:], scalar1=7.0, scalar2=None, op0=ALU.min))
            dv(lambda: dve.tensor_scalar(out=w1if[:], in0=eg[:].unsqueeze(2).to_broadcast([128, NG, 11]), scalar1=1408.0,
                                         scalar2=None, op0=ALU.mult))
            dv(lambda: dve.tensor_tensor(out=w1if[:], in0=w1if[:], in1=jp1[:].unsqueeze(1).to_broadcast([128, NG, 11]), op=ALU.add))
            dv(lambda: dve.tensor_scalar(out=w2if[:], in0=eg[:].unsqueeze(2).to_broadcast([128, NG, 2]), scalar1=256.0,
                                         scalar2=None, op0=ALU.mult))
            dv(lambda: dve.tensor_tensor(out=w2if[:], in0=w2if[:], in1=jp2[:].unsqueeze(1).to_broadcast([128, NG, 2]), op=ALU.add))
            dv(lambda: dve.tensor_copy(out=w1idx[:], in_=w1if[:]))
            dv(lambda: dve.tensor_copy(out=w2idx[:], in_=w2if[:]))
            dv(lambda: dve.tensor_tensor(out=posl[:], in0=posl[:], in1=base[:].unsqueeze(1).to_broadcast([128, NTL, 8]), op=ALU.add),
               [route_b])
            dv(lambda: dve.tensor_tensor(out=posf[:], in0=eqs[:], in1=posl[:].unsqueeze(2).to_broadcast([128, NTL, 2, 8]),
                                         op=ALU.mult), [gb[3]])
            dv(lambda: dve.tensor_reduce(out=pab[:], in_=posf[:], axis=AX.X, op=ALU.add))
            dv(lambda: dve.tensor_copy(out=idxab[:], in_=pab[:]))
            if debug:
                dump("idxab", idxab[:].rearrange("p a b -> p (a b)"), [128, NTL * 2], I32, [idx_b])
                dump("w1idx", w1idx[:].rearrange("p a b -> p (a b)"), [128, NG * 11], I32, [idx_b])
                dump("offm", off[:], [128, 8], F32, [idx_b])
                dump("egm", eg[:], [128, NG], F32, [idx_b])
                dump("wts", wts[:].rearrange("p a b -> p (a b)"), [128, NTL * 2], F32, [idx_b])

            for t in range(NTL):
                il = t % 4
                T.dma("sp", x3c_s[il], x3c[il][:], X3[t * 128:(t + 1) * 128, :], reads=[X3b[t]], writes=[x3c_b[il]])
                for a in range(2):
                    T.idma(xs_s, reads=[x3c_b[il], idx_b], writes=[Xs_b], out=Xs[:, :],
                           out_offset=bass.IndirectOffsetOnAxis(ap=idxab[:, t, a:a + 1], axis=0), in_=x3c[il][:, :], in_offset=None)

            hbuf = [(h2T, h2T_b), (h2T2, h2T2_b)]

            def load_group(g):
                hT_, hTb_ = hbuf[g % 2]
                for il in range(4):
                    r0 = (g * 4 + il) * 128
                    T.dma("sp", x3c_s[il], x3c[il][:], Xs[r0:r0 + 128, :], reads=[Xs_b], writes=[x3c_b[il]])
                    make_hT(x3c[il], x3c_b[il], L, 2, 3, 0, ps_tr, ps_tr_b,
                            lambda k, il=il: hT_[:, k, il * 128:(il + 1) * 128], hTb_[il])
            load_group(0)
            for g in range(NG):
                h2T, h2T_b = hbuf[g % 2]
                for fp in range(11):
                    wt_, wb2_, ws2_ = w13.next()
                    T.idma(ws2_, reads=[W1s_b[0], idx_b], writes=[wb2_], out=wt_[:, 0, :], out_offset=None, in_=W1r[:, :],
                           in_offset=bass.IndirectOffsetOnAxis(ap=w1idx[:, g, fp:fp + 1], axis=0))
                    T.idma(ws2_, reads=[W3s_b[0], idx_b], writes=[wb2_], out=wt_[:, 1, :], out_offset=None, in_=W3r[:, :],
                           in_offset=bass.IndirectOffsetOnAxis(ap=w1idx[:, g, fp:fp + 1], axis=0))
                    for f2 in range(2):
                        f = fp * 2 + f2
                        gi = f % 2

                        def _gu(f2=f2, gi=gi, wt_=wt_):
                            i = None
                            for k in range(8):
                                c0 = k * 256 + f2 * 128
                                i = pe.matmul(ps_g[gi][:, :], lhsT=wt_[:, 0, c0:c0 + 128], rhs=h2T[:, k, :],
                                              start=(k == 0), stop=(k == 7))
                            for k in range(8):
                                c0 = k * 256 + f2 * 128
                                i = pe.matmul(ps_u[gi][:, :], lhsT=wt_[:, 1, c0:c0 + 128], rhs=h2T[:, k, :],
                                              start=(k == 0), stop=(k == 7))
                            return i
                        T.op("pe", _gu, reads=[wb2_] + h2T_b, writes=[ps_g_b[gi], ps_u_b[gi]])
                        T.op("act", lambda gi=gi: act.activation(out=sg[gi][:, :], in_=ps_g[gi][:, :], func=AF.Silu),
                             reads=[ps_g_b[gi]], writes=[sg_b[gi]])
                        T.op("dve", lambda gi=gi, f=f: dve.tensor_tensor(out=AT[:, f, :], in0=sg[gi][:, :], in1=ps_u[gi][:, :],
                                                                         op=ALU.mult),
                             reads=[sg_b[gi], ps_u_b[gi]], writes=[AT_b[f]])
                if g + 1 < NG:
                    load_group(g + 1)
                for n in range(2):
                    w2t, w2b, w2s = w2r.next()
                    T.idma(w2s, reads=[W2s_b[0], idx_b], writes=[w2b], out=w2t[:, :], out_offset=None, in_=W2r[:, :],
                           in_offset=bass.IndirectOffsetOnAxis(ap=w2idx[:, g, n:n + 1], axis=0))
                    for il in range(4):
                        oi = oc % 2
                        oc += 1

                        def _o(il=il, oi=oi, w2t=w2t):
                            i = None
                            for f in range(NF):
                                i = pe.matmul(ps_o2[oi][:, :], lhsT=AT[:, f, il * 128:(il + 1) * 128],
                                              rhs=w2t[:, f * 512:(f + 1) * 512], start=(f == 0), stop=(f == NF - 1))
                            return i
                        T.op("pe", _o, reads=AT_b + [w2b], writes=[ps_o2_b[oi]])
                        T.op("act", lambda oi=oi, il=il, n=n: act.copy(out=acc[il][:, n * 512:(n + 1) * 512], in_=ps_o2[oi][:, :]),
                             reads=[ps_o2_b[oi]], writes=[acc_b[il]])
                for il in range(4):
                    r0 = (g * 4 + il) * 128
                    T.dma("sp", ys_s, Ys[r0:r0 + 128, :], acc[il][:], reads=[acc_b[il]], writes=[Ys_b])

            for t in range(NTL):
                il = t % 4
                ya, yb = acc[(t % 2) * 2], acc[(t % 2) * 2 + 1]
                ya_b, yb_b = acc_b[(t % 2) * 2], acc_b[(t % 2) * 2 + 1]
                T.dma("sp", x3c_s[il], x3c[il][:], X3[t * 128:(t + 1) * 128, :], reads=[X3b[t]], writes=[x3c_b[il]])
                T.idma(yg_s, reads=[Ys_b, idx_b], writes=[ya_b], out=ya[:, :], out_offset=None, in_=Ys[:, :],
                       in_offset=bass.IndirectOffsetOnAxis(ap=idxab[:, t, 0:1], axis=0))
                T.idma(yg_s, reads=[Ys_b, idx_b], writes=[yb_b], out=yb[:, :], out_offset=None, in_=Ys[:, :],
                       in_offset=bass.IndirectOffsetOnAxis(ap=idxab[:, t, 1:2], axis=0))

                def _cmb(t=t, ya=ya, yb=yb):
                    dve.tensor_scalar(out=ya[:], in0=ya[:], scalar1=wts[:, t, 0:1], scalar2=None, op0=ALU.mult)
                    return dve.scalar_tensor_tensor(out=ya[:], in0=yb[:], scalar=wts[:, t, 1:2], in1=ya[:], op0=ALU.mult, op1=ALU.add)
                T.op("dve", _cmb, reads=[ya_b, yb_b, route_b, gb[3]], writes=[ya_b])
                resid_ln([ya[:, 0:512], ya[:, 512:1024]], [ya_b], x3c[il], x3c_b[il],
                         gate[(0, 1)], gate_b[(0, 1)], lg, lgb, lb, lbb, tmp, tmp_b)
                T.dma("sp", x3c_s[il], out_ap[t * 128:(t + 1) * 128, :], x3c[il][:], reads=[x3c_b[il]], is_output=True)
    T.barrier()
    T.finish()
    es.close()
    return nc


_PROG = {}


def _get_prog(n_layers=2, debug=False):
    key = (n_layers, debug)
    if key not in _PROG:
        _PROG[key] = build_program(n_layers, debug)
    return _PROG[key]


L1_KEYS = ("pidx", "utri", "od_w_in", "od_fgain", "od_bias", "od_router", "od_w1", "od_w3", "od_w2", "C64", "S64", "CN", "SN")


def make_in_maps(inp, n_cores=N_CORES, n_layers=2):
    cs = _consts()
    f32 = lambda a: np.ascontiguousarray(np.asarray(a, dtype=np.float32))
    x = f32(inp["x"]); c = f32(inp["c"]); ctx = f32(inp["ctx"]); c_ctx = f32(inp["c_ctx"])
    ada_b = f32(inp["ada_b"])
    adab_col = np.ascontiguousarray(ada_b.reshape(2, 48, 128).transpose(0, 2, 1))
    qk_gain = np.concatenate([np.tile(f32(inp["ev_q_gain"])[0], 12), np.tile(f32(inp["ev_k_gain"])[0], 4)])
    rpb = f32(inp["od_rpb"])[0]
    rpb_ext = np.concatenate([rpb.reshape(12, -1), np.full((12, 1), NEG, np.float32)], axis=1)
    od_bias = np.ascontiguousarray(rpb_ext[:, cs["naidx"]])
    shared = dict(
        ada_w=f32(inp["ada_w"]), ada_b=ada_b, adab_col=adab_col,
        ln_mix_g=f32(inp["ln_mix_g"]), ln_mix_b=f32(inp["ln_mix_b"]),
        ln_ffn_g=f32(inp["ln_ffn_g"]), ln_ffn_b=f32(inp["ln_ffn_b"]),
        w_out=f32(inp["w_out"]), ev_w_in=f32(inp["ev_w_in"])[0], ev_pool_w=f32(inp["ev_pool_w"])[0],
        ev_pool_scale=f32(inp["ev_pool_scale"])[0], ev_qk_gain=f32(qk_gain),
        ev_ffn_w1=f32(inp["ev_ffn_w1"])[0], ev_ffn_w3=f32(inp["ev_ffn_w3"])[0], ev_ffn_w2=f32(inp["ev_ffn_w2"])[0],
        od_w_in=f32(inp["od_w_in"])[0], od_fgain=f32(inp["od_fourier_gain"])[0].reshape(256),
        od_bias=od_bias, od_router=f32(inp["od_router"])[0],
        od_w1=f32(inp["od_exp_w1"])[0], od_w3=f32(inp["od_exp_w3"])[0], od_w2=f32(inp["od_exp_w2"])[0],
        ropeC=cs["ropeC"], ropeS=cs["ropeS"], band=cs["band"], C64=cs["C64"], S64=cs["S64"],
        CN=cs["CN"], SN=cs["SN"], identf=cs["identf"], pidx=cs["pidx"], utri=cs["utri"],
    )
    if n_layers < 2:
        for k in L1_KEYS:
            shared.pop(k)
    maps = []
    for b in range(n_cores):
        cv = np.stack([c[b].reshape(8, 128).T, c_ctx.reshape(8, 128).T], axis=-1)
        m = dict(shared)
        m.update(x=x[b], ctx=ctx[b], cvec=np.ascontiguousarray(cv.astype(np.float32)))
        maps.append(m)
    return maps


def kernel(**inputs):
    nc = _get_prog(2, False)
    maps = make_in_maps(inputs)
    res = run_bass_kernel_spmd(nc, maps, core_ids=list(range(N_CORES)))
    return np.stack([np.asarray(r["out"], dtype=np.float32) for r in res.results], axis=0)
```
